# Optimizing a Trainium2 kernel written in Bass

```python
import jax
import jax.numpy as jnp
from jax import lax

D_MODEL = 1024
BATCH = 16
SEQ = 2048
DEPTH = 1

GRID_W = 64
CTX_LEN = 256
EPS = 1e-6
NEG_INF = -1e30

NA_HEADS = 8
NA_HEAD_DIM = 64
NA_WIDTH = NA_HEADS * NA_HEAD_DIM
NA_WIN_ROWS = 8
NA_WIN_COLS = 16
ROPE_THETA = 10000.0

LRU_WIDTH = D_MODEL
LRU_BLOCKS = 8
LRU_BLOCK = LRU_WIDTH // LRU_BLOCKS
LRU_CONV = 4
LRU_C = 8.0

N_GROUPS = 4
EXPERTS_PER_GROUP = 8
N_EXPERTS = N_GROUPS * EXPERTS_PER_GROUP
TOP_K = 2
D_EXPERT = 512
MOE_BLOCK = 256

K_OFF = 0
V_OFF = K_OFF + NA_WIDTH
LX_OFF = V_OFF + NA_WIDTH
CTX_COLS = LX_OFF + LRU_WIDTH
Q_OFF = CTX_COLS
LG_OFF = Q_OFF + NA_WIDTH
GA_OFF = LG_OFF + LRU_WIDTH
GB_OFF = GA_OFF + D_MODEL
PROJ_COLS = GB_OFF + D_MODEL

kernel_name = 'hybrid_natten_rglru_hmoe_dit_block'


def _rms(x):
    x32 = x.astype(jnp.float32)
    return x32 * lax.rsqrt(jnp.mean(x32 * x32, axis=-1, keepdims=True) + EPS)


def ada_rmsnorm(x, gain, shift, scale):
    n = _rms(x) * gain.astype(jnp.float32)
    return (n * (1.0 + scale.astype(jnp.float32)) + shift.astype(jnp.float32)).astype(x.dtype)


def axial_rope(x, row_pos, col_pos):
    hd = x.shape[-1]
    half = hd // 2
    nf = half // 2
    inv_freq = ROPE_THETA ** (-jnp.arange(nf, dtype=jnp.float32) / nf)

    def rot(xp, pos):
        ang = pos[:, None] * inv_freq
        cos = jnp.cos(ang)[:, None, :]
        sin = jnp.sin(ang)[:, None, :]
        x1, x2 = xp[..., :nf], xp[..., nf:]
        return jnp.concatenate([x1 * cos - x2 * sin, x1 * sin + x2 * cos], axis=-1)

    x32 = x.astype(jnp.float32)
    out = jnp.concatenate([rot(x32[..., :half], row_pos), rot(x32[..., half:], col_pos)], axis=-1)
    return out.astype(x.dtype)


def neighbourhood_attention(q, k, v, k_ctx, v_ctx, rpb):
    bsz, length, n_heads, hd = q.shape
    rows = length // GRID_W
    kr = min(NA_WIN_ROWS, rows)
    t = jnp.arange(length)
    row_pos = (t // GRID_W).astype(jnp.float32)
    col_pos = (t % GRID_W).astype(jnp.float32)
    scale = hd ** -0.5
    grid = (bsz, rows, GRID_W, n_heads, hd)
    q_plain = q.reshape(grid)
    q_rot = axial_rope(q, row_pos, col_pos).reshape(grid)
    k_rot = axial_rope(k, row_pos, col_pos).reshape(grid)
    v_g = v.reshape(grid)
    r = jnp.arange(rows)
    r_start = jnp.clip(r - kr // 2, 0, rows - kr)
    r_win = r_start[:, None] + jnp.arange(kr)[None, :]
    n_win = kr * GRID_W
    k_win = k_rot[:, r_win].reshape(bsz, rows, n_win, n_heads, hd)
    v_win = v_g[:, r_win].reshape(bsz, rows, n_win, n_heads, hd)
    cq = jnp.arange(GRID_W)
    c_start = jnp.clip(cq - NA_WIN_COLS // 2, 0, GRID_W - NA_WIN_COLS)
    band = (cq[None, :] >= c_start[:, None]) & (cq[None, :] < c_start[:, None] + NA_WIN_COLS)
    mask = jnp.broadcast_to(band[:, None, :], (GRID_W, kr, GRID_W)).reshape(GRID_W, n_win)
    dr = r_win - r[:, None] + (NA_WIN_ROWS - 1)
    dc = jnp.clip(cq[None, :] - cq[:, None], 1 - NA_WIN_COLS, NA_WIN_COLS - 1) + (NA_WIN_COLS - 1)
    bias = rpb.astype(jnp.float32)[:, dr][..., dc]
    bias = bias.transpose(0, 1, 3, 2, 4).reshape(n_heads, rows, GRID_W, n_win)
    s_lat = jnp.einsum('brqhd,brjhd->bhrqj', q_rot, k_win).astype(jnp.float32) * scale + bias[None]
    s_lat = jnp.where(mask, s_lat, NEG_INF)
    s_ctx = jnp.einsum('brqhd,bchd->bhrqc', q_plain, k_ctx).astype(jnp.float32) * scale
    p = jax.nn.softmax(jnp.concatenate([s_lat, s_ctx], axis=-1), axis=-1).astype(v.dtype)
    o = (jnp.einsum('bhrqj,brjhd->brqhd', p[..., :n_win], v_win)
         + jnp.einsum('bhrqc,bchd->brqhd', p[..., n_win:], v_ctx))
    return o.reshape(bsz, length, n_heads * hd)


def context_attention(q, k, v):
    bsz, n_ctx, n_heads, hd = q.shape
    s = jnp.einsum('bqhd,bkhd->bhqk', q, k).astype(jnp.float32) * (hd ** -0.5)
    p = jax.nn.softmax(s, axis=-1).astype(v.dtype)
    return jnp.einsum('bhqk,bkhd->bqhd', p, v).reshape(bsz, n_ctx, n_heads * hd)


def short_conv(x, w, b):
    n_ch = x.shape[-1]
    pad_l = LRU_CONV // 2
    y = lax.conv_general_dilated(x, w[:, None, :], window_strides=(1,),
                                 padding=[(pad_l, LRU_CONV - 1 - pad_l)],
                                 dimension_numbers=('NWC', 'WIO', 'NWC'),
                                 feature_group_count=n_ch)
    return y + b


def _scan_combine(left, right):
    a1, b1 = left
    a2, b2 = right
    return a1 * a2, a2 * b1 + b2


def lru_direction(xc, h0, wa, ba, wx, bx, lam, reverse):
    bsz, length, n_ch = xc.shape
    xb = xc.reshape(bsz, length, LRU_BLOCKS, LRU_BLOCK)
    r = jax.nn.sigmoid(jnp.einsum('blnc,ncd->blnd', xb, wa).reshape(bsz, length, n_ch) + ba)
    i = jax.nn.sigmoid(jnp.einsum('blnc,ncd->blnd', xb, wx).reshape(bsz, length, n_ch) + bx)
    log_a = -LRU_C * r.astype(jnp.float32) * jax.nn.softplus(-lam.astype(jnp.float32))
    a = jnp.exp(log_a)
    u = jnp.sqrt(-jnp.expm1(2.0 * log_a)) * (i * xc).astype(jnp.float32)
    a_cum, h = lax.associative_scan(_scan_combine, (a, u), axis=1, reverse=reverse)
    if h0 is not None:
        h = h + a_cum * h0[:, None, :]
    return h


def branch_merge(proj, o_att, o_lru, w_up_attn, w_up_lru, w_out):
    g_a = jax.nn.sigmoid(proj[..., GA_OFF:GA_OFF + D_MODEL])
    g_b = jax.nn.sigmoid(proj[..., GB_OFF:GB_OFF + D_MODEL])
    y = g_a * (o_att @ w_up_attn) + g_b * (o_lru @ w_up_lru)
    return y @ w_out


def hier_moe(h, wg, bg, we, be, w1, w3, w2):
    n_tok, d = h.shape
    h32 = h.astype(jnp.float32)
    p_grp = jax.nn.softmax(h32 @ wg.astype(jnp.float32) + bg.astype(jnp.float32), axis=-1)
    p_top, g_idx = lax.top_k(p_grp, 1)
    le = (h32 @ we.astype(jnp.float32) + be.astype(jnp.float32)).reshape(n_tok, N_GROUPS, EXPERTS_PER_GROUP)
    le_sel = le[jnp.arange(n_tok), g_idx[:, 0]]
    ev, e_idx = lax.top_k(le_sel, TOP_K)
    gate = p_top * jax.nn.softmax(ev, axis=-1)
    eid = g_idx * EXPERTS_PER_GROUP + e_idx
    n_asg = n_tok * TOP_K
    flat_e = eid.reshape(n_asg)
    flat_tok = jnp.repeat(jnp.arange(n_tok), TOP_K)
    flat_w = gate.reshape(n_asg)
    order = jnp.argsort(flat_e)
    e_s, tok_s, w_s = flat_e[order], flat_tok[order], flat_w[order]
    counts = jnp.bincount(flat_e, length=N_EXPERTS)
    starts = jnp.cumsum(counts) - counts
    padded = (counts + MOE_BLOCK - 1) // MOE_BLOCK * MOE_BLOCK
    p_ends = jnp.cumsum(padded)
    p_starts = p_ends - padded
    dest = p_starts[e_s] + (jnp.arange(n_asg) - starts[e_s])
    n_blk = -(-(n_asg + N_EXPERTS * (MOE_BLOCK - 1)) // MOE_BLOCK)
    buf_tok = jnp.full((n_blk * MOE_BLOCK,), n_tok, dtype=jnp.int32).at[dest].set(tok_s)
    buf_w = jnp.zeros((n_blk * MOE_BLOCK,), jnp.float32).at[dest].set(w_s)
    blk_e = jnp.minimum(jnp.searchsorted(p_ends, jnp.arange(n_blk) * MOE_BLOCK, side='right'), N_EXPERTS - 1)
    h_pad = jnp.concatenate([h, jnp.zeros((1, d), h.dtype)], axis=0)
    xb = h_pad[buf_tok].reshape(n_blk, MOE_BLOCK, d)

    def expert_block(args):
        xblk, e = args
        return (jax.nn.silu(xblk @ w1[e]) * (xblk @ w3[e])) @ w2[e]

    yb = lax.map(expert_block, (xb, blk_e)).reshape(n_blk * MOE_BLOCK, d)
    out = jnp.zeros((n_tok + 1, d), jnp.float32).at[buf_tok].add(yb.astype(jnp.float32) * buf_w[:, None])
    return out[:n_tok].astype(h.dtype)


def hybrid_layer(x, ctx, c, c_ctx, w_mod, b_mod, g_mix, g_ffn, w_in, rpb, conv_w, conv_b,
                 lru_wa, lru_ba, lru_wx, lru_bx, lru_lambda, w_up_attn, w_up_lru, w_out,
                 wg, bg, we, be, w1, w3, w2, last):
    bsz, length, d = x.shape
    n_ctx = ctx.shape[1]
    mod = (jax.nn.silu(c) @ w_mod + b_mod)[:, None, :]
    sh1, sc1, ga1, sh2, sc2, ga2 = jnp.split(mod, 6, axis=-1)
    n_cm = 2 if last else 6
    mod_c = jnp.split(jax.nn.silu(c_ctx) @ w_mod[:, :n_cm * d] + b_mod[:n_cm * d], n_cm)

    h = ada_rmsnorm(x, g_mix, sh1, sc1)
    hc = ada_rmsnorm(ctx, g_mix, mod_c[0], mod_c[1])
    proj = h @ w_in
    proj_c = hc @ (w_in[:, :CTX_COLS] if last else w_in)

    def heads(t):
        return t.reshape(t.shape[0], t.shape[1], NA_HEADS, NA_HEAD_DIM)

    q = heads(proj[..., Q_OFF:Q_OFF + NA_WIDTH])
    k = heads(proj[..., K_OFF:K_OFF + NA_WIDTH])
    v = heads(proj[..., V_OFF:V_OFF + NA_WIDTH])
    k_c = heads(proj_c[..., K_OFF:K_OFF + NA_WIDTH])
    v_c = heads(proj_c[..., V_OFF:V_OFF + NA_WIDTH])
    o_att = neighbourhood_attention(q, k, v, k_c, v_c, rpb)

    x_l = short_conv(proj[..., LX_OFF:LX_OFF + LRU_WIDTH], conv_w, conv_b)
    x_c = short_conv(proj_c[..., LX_OFF:LX_OFF + LRU_WIDTH], conv_w, conv_b)
    fwd = (lru_wa[0], lru_ba[0], lru_wx[0], lru_bx[0], lru_lambda[0])
    bwd = (lru_wa[1], lru_ba[1], lru_wx[1], lru_bx[1], lru_lambda[1])
    hc_f = lru_direction(x_c, None, *fwd, reverse=False)
    hc_b = lru_direction(x_c, None, *bwd, reverse=True)
    h_f = lru_direction(x_l, hc_f[:, -1], *fwd, reverse=False)
    h_b = lru_direction(x_l, hc_b[:, 0], *bwd, reverse=True)
    o_lru = ((h_f + h_b) * jax.nn.gelu(proj[..., LG_OFF:LG_OFF + LRU_WIDTH].astype(jnp.float32))).astype(x.dtype)
    x = x + ga1 * branch_merge(proj, o_att, o_lru, w_up_attn, w_up_lru, w_out)
    if not last:
        q_c = heads(proj_c[..., Q_OFF:Q_OFF + NA_WIDTH])
        oc_att = context_attention(q_c, k_c, v_c)
        oc_lru = ((hc_f + hc_b) * jax.nn.gelu(proj_c[..., LG_OFF:LG_OFF + LRU_WIDTH].astype(jnp.float32))).astype(ctx.dtype)
        ctx = ctx + mod_c[2] * branch_merge(proj_c, oc_att, oc_lru, w_up_attn, w_up_lru, w_out)

    h2 = ada_rmsnorm(x, g_ffn, sh2, sc2)
    x = x + ga2 * hier_moe(h2.reshape(bsz * length, d), wg, bg, we, be, w1, w3, w2).reshape(bsz, length, d)
    if not last:
        hc2 = ada_rmsnorm(ctx, g_ffn, mod_c[3], mod_c[4])
        ctx = ctx + mod_c[5] * hier_moe(hc2.reshape(bsz * n_ctx, d), wg, bg, we, be, w1, w3, w2).reshape(bsz, n_ctx, d)
    return x, ctx


def setup_inputs(seed: int = 0) -> dict:
    key = jax.random.key(seed)
    ks = jax.random.split(key, 32)
    f32 = jnp.float32
    d = D_MODEL

    def nrm(k, shape, scale):
        return jax.random.normal(k, shape, f32) * scale

    u = jax.random.uniform(ks[16], (DEPTH, 2, LRU_WIDTH), f32, 0.9, 0.999)
    a0 = u ** (1.0 / LRU_C)
    return {
        'x': nrm(ks[0], (BATCH, SEQ, d), 1.0),
        'c': nrm(ks[1], (BATCH, d), 1.0),
        'ctx': nrm(ks[2], (BATCH, CTX_LEN, d), 1.0),
        'c_ctx': nrm(ks[3], (d,), 1.0),
        'w_mod': nrm(ks[4], (DEPTH, d, 6 * d), 0.5 * d ** -0.5),
        'b_mod': nrm(ks[5], (DEPTH, 6 * d), 0.02),
        'g_mix': 1.0 + nrm(ks[6], (DEPTH, d), 0.02),
        'g_ffn': 1.0 + nrm(ks[7], (DEPTH, d), 0.02),
        'w_in': nrm(ks[8], (DEPTH, d, PROJ_COLS), d ** -0.5),
        'rpb': nrm(ks[9], (DEPTH, NA_HEADS, 2 * NA_WIN_ROWS - 1, 2 * NA_WIN_COLS - 1), 0.1),
        'conv_w': nrm(ks[10], (DEPTH, LRU_CONV, LRU_WIDTH), LRU_CONV ** -0.5),
        'conv_b': nrm(ks[11], (DEPTH, LRU_WIDTH), 0.02),
        'lru_wa': nrm(ks[12], (DEPTH, 2, LRU_BLOCKS, LRU_BLOCK, LRU_BLOCK), LRU_BLOCK ** -0.5),
        'lru_ba': nrm(ks[13], (DEPTH, 2, LRU_WIDTH), 0.02),
        'lru_wx': nrm(ks[14], (DEPTH, 2, LRU_BLOCKS, LRU_BLOCK, LRU_BLOCK), LRU_BLOCK ** -0.5),
        'lru_bx': nrm(ks[15], (DEPTH, 2, LRU_WIDTH), 0.02),
        'lru_lambda': jnp.log(a0) - jnp.log1p(-a0),
        'w_up_attn': nrm(ks[17], (DEPTH, NA_WIDTH, d), NA_WIDTH ** -0.5),
        'w_up_lru': nrm(ks[18], (DEPTH, LRU_WIDTH, d), LRU_WIDTH ** -0.5),
        'w_out': nrm(ks[19], (DEPTH, d, d), d ** -0.5),
        'router_group_w': nrm(ks[20], (DEPTH, d, N_GROUPS), d ** -0.5),
        'router_group_b': nrm(ks[21], (DEPTH, N_GROUPS), 0.01),
        'router_expert_w': nrm(ks[22], (DEPTH, d, N_EXPERTS), d ** -0.5),
        'router_expert_b': nrm(ks[23], (DEPTH, N_EXPERTS), 0.01),
        'expert_w_gate': nrm(ks[24], (DEPTH, N_EXPERTS, d, D_EXPERT), d ** -0.5),
        'expert_w_up': nrm(ks[25], (DEPTH, N_EXPERTS, d, D_EXPERT), d ** -0.5),
        'expert_w_down': nrm(ks[26], (DEPTH, N_EXPERTS, D_EXPERT, d), D_EXPERT ** -0.5),
        'g_final': 1.0 + nrm(ks[27], (d,), 0.02),
    }


def reference(x, c, ctx, c_ctx, w_mod, b_mod, g_mix, g_ffn, w_in, rpb, conv_w, conv_b,
              lru_wa, lru_ba, lru_wx, lru_bx, lru_lambda, w_up_attn, w_up_lru, w_out,
              router_group_w, router_group_b, router_expert_w, router_expert_b,
              expert_w_gate, expert_w_up, expert_w_down, g_final):
    for l in range(DEPTH):
        x, ctx = hybrid_layer(x, ctx, c, c_ctx, w_mod[l], b_mod[l], g_mix[l], g_ffn[l], w_in[l], rpb[l],
                              conv_w[l], conv_b[l], lru_wa[l], lru_ba[l], lru_wx[l], lru_bx[l], lru_lambda[l],
                              w_up_attn[l], w_up_lru[l], w_out[l],
                              router_group_w[l], router_group_b[l], router_expert_w[l], router_expert_b[l],
                              expert_w_gate[l], expert_w_up[l], expert_w_down[l], last=(l == DEPTH - 1))
    return (_rms(x) * g_final.astype(jnp.float32)).astype(x.dtype)
```

```python
import numpy as np
import concourse.bass as bass
import concourse.mybir as mybir
from concourse.bass_utils import run_bass_kernel_spmd
from contextlib import ExitStack

F32 = mybir.dt.float32
BF16 = mybir.dt.bfloat16
I32 = mybir.dt.int32
AF = mybir.ActivationFunctionType
ALU = mybir.AluOpType
AX = mybir.AxisListType

D = 1024
L = 2048
C = 256
NB = 2
NCORES = 8
LC = L + C
NTOK = NB * L
NBLK = 64
NROWS = NBLK * 256
EPS = 1e-6

V_GMIX, V_GFFN, V_CONVW, V_CONVB, V_BA, V_BX, V_LAM, V_BMOD, NV = 0, 8, 16, 48, 56, 72, 88, 104, 152
NTB = 21

DEBUG = {}
import os as _os
ATT_NHP = int(_os.environ.get("ATT_NHP", "4"))
ATT_NUNITS = int(_os.environ.get("ATT_NUNITS", "8"))
ATT_NROWS = int(_os.environ.get("ATT_NROWS", "8"))
ATT_NORM = int(_os.environ.get("ATT_NORM", "1"))
ATT_PARTS = int(_os.environ.get("ATT_PARTS", "31"))
STOP_AFTER = None


class Buf:
    __slots__ = ("name", "w", "wx", "r", "dsem", "dcnt", "grp")

    def __init__(self, name=""):
        self.name = name
        self.w = None
        self.wx = {}
        self.r = {}
        self.dsem = {}
        self.dcnt = {}
        self.grp = False


class Sync:
    ROLL = 30000

    def __init__(self, nc, es):
        self.nc = nc
        self.es = es
        self.eng = {"pe": nc.tensor, "act": nc.scalar, "dve": nc.vector, "pool": nc.gpsimd, "sp": nc.sync}
        self.sem = {}
        self.cnt = {}
        self.waited = {k: {} for k in self.eng}
        self.nsem = 0
        self.pe_sems = set()
        for k in self.eng:
            self._newsem(k)
        self.pend = []
        self.dbufs = []
        self.nops = {k: 0 for k in self.eng}
        self.nwaits = 0

    def _alloc(self, name):
        self.nsem += 1
        return self.es.enter_context(self.nc.semaphore(f"{name}{self.nsem}"))

    def _newsem(self, k):
        self.sem[k] = self._alloc("e" + k)
        self.cnt[k] = 0
        if k == "pe":
            self.pe_sems.add(id(self.sem[k]))

    def _wait(self, e, ev):
        semh, val = ev
        assert val is not None, "dependency on an unsignalled PE op"
        key = id(semh)
        if self.waited[e].get(key, 0) < val:
            self.eng[e].wait_ge(semh, val)
            self.waited[e][key] = val
            self.nwaits += 1

    def _dep1(self, e, ev, acc):
        if e == "pe" and (ev[1] is None or id(ev[0]) in self.pe_sems):
            return
        assert ev[1] is not None, "dependency on an unsignalled PE op"
        k = id(ev[0])
        if k not in acc or acc[k][1] < ev[1]:
            acc[k] = ev

    def _deps(self, e, reads, writes, group=False):
        acc = {}
        for b in reads:
            if b.w is not None:
                self._dep1(e, b.w, acc)
            for ev in b.wx.values():
                self._dep1(e, ev, acc)
        for b in writes:
            if not (group and b.grp):
                if b.w is not None:
                    self._dep1(e, b.w, acc)
                for ev in b.wx.values():
                    self._dep1(e, ev, acc)
            for ev in b.r.values():
                self._dep1(e, ev, acc)
        for ev in acc.values():
            self._wait(e, ev)

    def _mark(self, ev, reads, writes, key, group=False):
        for b in reads:
            b.r[key] = ev
        for b in writes:
            if group and b.grp:
                b.wx[key] = ev
            elif group:
                b.w = None
                b.wx = {key: ev}
            else:
                b.w = ev
                b.wx = {}
            b.grp = group
            b.r = {}

    def op(self, e, fn, reads=(), writes=(), sig=True):
        self._deps(e, reads, writes)
        ins = fn()
        self.nops[e] += 1
        if sig:
            if self.cnt[e] >= self.ROLL:
                self._newsem(e)
            self.cnt[e] += 1
            ins.then_inc(self.sem[e], 1)
            ev = [self.sem[e], self.cnt[e]]
            if e == "pe":
                for p in self.pend:
                    p[0] = self.sem[e]
                    p[1] = self.cnt[e]
                self.pend = []
        else:
            assert e == "pe"
            ev = [self.sem[e], None]
            self.pend.append(ev)
        self._mark(ev, reads, writes, e)
        return ins

    def _dma_common(self, q, issue, reads, writes, group, semof):
        d = semof if semof is not None else writes[0]
        c = "sw" if q == "pool" else "hw"
        if c not in d.dsem:
            d.dsem[c] = self._alloc("d")
            d.dcnt[c] = 0
            self.dbufs.append((d, c))
        self._deps(q, reads, writes, group=group)
        ins = issue()
        self.nops[q] += 1
        d.dcnt[c] += 16
        ins.then_inc(d.dsem[c], 16)
        ev = [d.dsem[c], d.dcnt[c]]
        self._mark(ev, reads, writes, id(d.dsem[c]), group=group)
        return ins

    def dma(self, q, out, in_, reads=(), writes=(), group=False, semof=None):
        return self._dma_common(q, lambda: self.eng[q].dma_start(out=out, in_=in_), reads, writes, group, semof)

    def dma_ind(self, out, out_off, in_, in_off, bound, reads=(), writes=(), group=False, semof=None):
        def issue():
            return self.nc.gpsimd.indirect_dma_start(out=out, out_offset=out_off, in_=in_, in_offset=in_off)
        return self._dma_common("pool", issue, reads, writes, group, semof)

    def barrier(self):
        assert not self.pend
        evs = [[self.sem[k], self.cnt[k]] for k in self.eng if k != "sp" and self.cnt[k] > 0]
        evs += [[b.dsem[c], b.dcnt[c]] for (b, c) in self.dbufs if b.dcnt[c] > 0]
        for ev in evs:
            self._wait("sp", ev)
        if self.cnt["sp"] >= self.ROLL:
            self._newsem("sp")
        self.cnt["sp"] += 1
        self.nc.sync.nop().then_inc(self.sem["sp"], 1)
        ev = [self.sem["sp"], self.cnt["sp"]]
        for k in self.eng:
            if k != "sp":
                self._wait(k, ev)


def build_program():
    nc = bass.Bass("TRN2", target_bir_lowering=False)

    def din(name, shape, dt=F32):
        return nc.dram_tensor(name, list(shape), dt, kind="ExternalInput").ap()

    xin = din("xin", [NB, L, D])
    ctxin = din("ctxin", [NB, C, D])
    cs_d = din("cs", [128, 24])
    wmod_d = din("wmod", [128, 8, 6 * D])
    vecs_d = din("vecs", [128, NV])
    wqkv_d = din("wqkv", [4, 128, 8 * 640])
    wlxlg_d = din("wlxlg", [8, 128, 8 * 256])
    wgagb_d = din("wgagb", [8, 128, 8 * 256])
    wup_d = din("wup", [8, 128, 12 * 128])
    wout_d = din("wout", [128, 8 * D])
    lruw_d = din("lruw", [128, 4 * 8 * 128])
    cossin_d = din("cossin", [128, 2 * L])
    btab_d = din("btab", [64, 8 * NTB * 64])
    wr_d = din("wr", [128, 8 * 36])
    brb_d = din("brb", [128, 36])
    gfb_d = din("gfb", [128, D])
    gffnb_d = din("gffnb", [128, D])
    w1_d = din("w1h", [32 * 128, 8 * 512])
    w3_d = din("w3h", [32 * 128, 8 * 512])
    w2_d = din("w2h", [32 * 128, 4 * 1024])
    ident_d = din("ident", [128, 128])
    iota_d = din("iota", [128, 1])
    utri_d = din("utri", [128, 128])
    out_d = nc.dram_tensor("out", [NB, L, D], F32, kind="ExternalOutput").ap()
    x1_d = nc.dram_tensor("x1s", [NTOK, D], F32, kind="Internal").ap()
    h2_d = nc.dram_tensor("h2s", [NTOK, D], BF16, kind="Internal").ap()
    xs_d = nc.dram_tensor("xss", [NROWS, D], BF16, kind="Internal").ap()
    ys_d = nc.dram_tensor("yss", [NROWS, D], BF16, kind="Internal").ap()
    dbg_d = {}
    for name, (shape, dt) in DEBUG.items():
        dbg_d[name] = nc.dram_tensor("dbg_" + name, list(shape), dt, kind="ExternalOutput").ap()

    with ExitStack() as es:
        S = Sync(nc, es)
        uid = [0]

        def sb(es_, shape, dt, name="t"):
            uid[0] += 1
            return es_.enter_context(nc.sbuf_tensor(f"{name}{uid[0]}", list(shape), dt))

        def ps(es_, shape, dt, name="p"):
            uid[0] += 1
            return es_.enter_context(nc.psum_tensor(f"{name}{uid[0]}", list(shape), dt))

        def pe_mm(out, lhsT, rhs, start, stop, reads, writes, sig=None):
            if sig is None:
                sig = stop
            return S.op("pe", lambda: nc.tensor.matmul(out, lhsT, rhs, start=start, stop=stop),
                        reads, writes, sig)

        def pe_tr(out, in_, ident, reads, writes, sig):
            return S.op("pe", lambda: nc.tensor.transpose(out, in_, ident), reads, writes, sig)

        def act(out, in_, func, reads, writes, **kw):
            return S.op("act", lambda: nc.scalar.activation(out=out, in_=in_, func=func, **kw), reads, writes)

        def V(e, fn, reads, writes):
            return S.op(e, fn, reads, writes)

        dbg_buf = Buf("dbg")

        def dump(name, ap, reads):
            if name in dbg_d:
                S.dma("sp", dbg_d[name], ap, reads=reads, writes=[dbg_buf], group=True, semof=Buf("dump_" + name))

        identf = sb(es, [128, 128], F32, "identf"); b_identf = Buf()
        identb = sb(es, [128, 128], BF16, "identb"); b_identb = Buf()
        onesf = sb(es, [128, 128], F32, "onesf"); b_onesf = Buf()
        onesb = sb(es, [128, 128], BF16, "onesb"); b_onesb = Buf()
        utri = sb(es, [128, 128], BF16, "utri"); b_utri = Buf()
        iota = sb(es, [128, 1], F32, "iota"); b_iota = Buf()
        epst = sb(es, [128, 1], F32, "eps"); b_eps = Buf()
        vecs = sb(es, [128, NV], F32, "vecs"); b_vecs = Buf()
        modfm = sb(es, [128, 48, 3], F32, "modfm"); b_modfm = Buf()
        A1 = sb(es, [128, 8, 3], F32, "A1"); b_A1 = Buf()
        lrup = sb(es, [128, 4, 16], F32, "lrup"); b_lrup = Buf()
        S.dma("sp", identf[:], ident_d, writes=[b_identf])
        S.dma("pool", identb[:], ident_d, writes=[b_identb])
        S.dma("pool", utri[:], utri_d, writes=[b_utri])
        S.dma("sp", iota[:], iota_d, writes=[b_iota])
        S.dma("sp", vecs[:], vecs_d, writes=[b_vecs])
        V("dve", lambda: nc.vector.memset(onesf[:], 1.0), [], [b_onesf])
        V("dve", lambda: nc.vector.memset(onesb[:], 1.0), [], [b_onesb])
        V("dve", lambda: nc.vector.memset(epst[:], EPS), [], [b_eps])

        with ExitStack() as es0:
            csb = sb(es0, [128, 24], F32, "cs"); b_cs = Buf()
            scs = sb(es0, [128, 24], F32, "scs"); b_scs = Buf()
            wm = [sb(es0, [128, 8, 512], F32, "wm") for _ in range(2)]
            b_wm = [Buf(), Buf()]
            psmod = ps(es0, [128, 144], F32, "psmod"); b_psmod = Buf()
            S.dma("sp", csb[:], cs_d, writes=[b_cs])
            act(scs[:], csb[:], AF.Silu, [b_cs], [b_scs])
            for cb in range(12):
                w = wm[cb % 2]; bw = b_wm[cb % 2]
                S.dma("sp", w[:], wmod_d[:, :, cb * 512:(cb + 1) * 512], writes=[bw])
                for cc in range(4):
                    col = cb * 4 + cc
                    for k in range(8):
                        pe_mm(psmod[:, col * 3:(col + 1) * 3], w[:, k, cc * 128:(cc + 1) * 128],
                              scs[:, k * 3:(k + 1) * 3], k == 0, k == 7, [bw, b_scs], [b_psmod],
                              sig=(k == 7 and cc == 3))
            V("dve", lambda: nc.vector.tensor_tensor(
                out=modfm[:], in0=psmod[:, :].rearrange("p (a b) -> p a b", b=3),
                in1=vecs[:, V_BMOD:V_BMOD + 48].unsqueeze(2).to_broadcast([128, 48, 3]), op=ALU.add),
              [b_psmod, b_vecs], [b_modfm])
            V("dve", lambda: nc.vector.scalar_tensor_tensor(
                out=A1[:], in0=modfm[:, 8:16, :], scalar=1.0,
                in1=vecs[:, V_GMIX:V_GMIX + 8].unsqueeze(2).to_broadcast([128, 8, 3]),
                op0=ALU.add, op1=ALU.mult), [b_modfm, b_vecs], [b_A1])
            act(lrup[:, 2, :], vecs[:, V_LAM:V_LAM + 16], AF.Exp, [b_vecs], [b_lrup], scale=-1.0)
            act(lrup[:, 3, :], lrup[:, 2, :], AF.Ln, [b_lrup], [b_lrup], bias=1.0)
            V("dve", lambda: nc.vector.tensor_scalar(out=lrup[:, 0, :], in0=lrup[:, 3, :], scalar1=-8.0, scalar2=None,
                                                     op0=ALU.mult), [b_lrup], [b_lrup])
            V("dve", lambda: nc.vector.tensor_scalar(out=lrup[:, 1, :], in0=lrup[:, 3, :], scalar1=-16.0, scalar2=None,
                                                     op0=ALU.mult), [b_lrup], [b_lrup])
            dump("modfm", modfm[:, :, :].rearrange("p a b -> p (a b)"), [b_modfm])
            S.barrier()

        def bcast_row(es_, v, j, psb, b_psb, diag, b_diag):
            for k in range(8):
                V("dve", lambda: nc.vector.tensor_scalar(out=diag[k % 2][:], in0=identf[:],
                                                         scalar1=modfm[:, v * 8 + k, j:j + 1], scalar2=None,
                                                         op0=ALU.mult), [b_identf, b_modfm], [b_diag[k % 2]])
                pe_mm(psb[:, k * 128:(k + 1) * 128], onesf[:], diag[k % 2][:], True, True,
                      [b_onesf, b_diag[k % 2]], [b_psb], sig=True)

        Mall = sb(es, [128, 32, 32], BF16, "Mall"); b_Mall = Buf()
        oh1all = sb(es, [128, 32, 32], BF16, "oh1"); b_oh1 = Buf()
        gates = sb(es, [128, 32, 2], F32, "gates"); b_gates = Buf()
        dest_f = sb(es, [128, 32, 2], F32, "destf"); b_destf = Buf()
        dest_i = sb(es, [128, 64], I32, "desti"); b_desti = Buf()
        b_x1d = Buf("x1d"); b_h2d = Buf("h2d"); b_xsd = Buf("xsd"); b_ysd = Buf("ysd"); b_outd = Buf("outd")
        zt = sb(es, [128, 2, D], BF16, "zt"); b_zt = Buf()
        V("dve", lambda: nc.vector.memset(zt[:], 0.0), [], [b_zt])
        for blk in range(NBLK):
            S.dma("sp", xs_d[blk * 256:(blk + 1) * 256, :].rearrange("(a p) d -> p a d", p=128), zt[:],
                  reads=[b_zt], writes=[b_xsd], group=True, semof=b_zt)

        for b in range(NB):
            with ExitStack() as esb:
                hT = sb(esb, [128, 8, LC], BF16, "hT")
                b_hT = [[Buf(f"hT{i}a"), Buf(f"hT{i}b")] for i in range(5)]
                oatt = sb(esb, [128, 4, L], BF16, "oatt")
                b_oatt = [[Buf() for _ in range(4)] for _ in range(4)]

                with ExitStack() as es1:
                    xt = [sb(es1, [128, D], F32, "xt") for _ in range(3)]
                    b_xt = [Buf() for _ in range(3)]
                    junk = sb(es1, [128, D], BF16, "junk"); b_junk = Buf()
                    xn = [sb(es1, [128, D], BF16, "xn") for _ in range(8)]
                    b_xn = [Buf() for _ in range(8)]
                    st = sb(es1, [128, 18, 3], F32, "st"); b_st = [Buf() for _ in range(18)]
                    pst = [ps(es1, [128, 512], BF16, "pst") for _ in range(4)]
                    b_pst = [Buf() for _ in range(4)]
                    ti = 0
                    for grp in range(5):
                        ntile = 4 if grp < 4 else 2
                        jmod = b if grp < 4 else 2
                        for i in range(ntile):
                            t = grp * 4 + i
                            xb_, bx_ = xt[ti % 3], b_xt[ti % 3]
                            src = xin[b, t * 128:(t + 1) * 128, :] if grp < 4 else ctxin[b, i * 128:(i + 1) * 128, :]
                            S.dma("sp", xb_[:], src, writes=[bx_])
                            act(junk[:], xb_[:], AF.Square, [bx_], [b_junk, b_st[t]], accum_out=st[:, t, 0:1])
                            act(st[:, t, 1:2], st[:, t, 0:1], AF.Sqrt, [b_st[t], b_eps], [b_st[t]],
                                scale=1.0 / D, bias=epst[:, 0:1])
                            V("dve", lambda: nc.vector.reciprocal(out=st[:, t, 2:3], in_=st[:, t, 1:2]),
                              [b_st[t]], [b_st[t]])
                            xi = (grp % 2) * 4 + i
                            act(xn[xi][:], xb_[:], AF.Copy, [bx_, b_st[t]], [b_xn[xi]], scale=st[:, t, 2:3])
                            ti += 1
                        for k in range(8):
                            pp, bp = pst[k % 4], b_pst[k % 4]
                            for i in range(ntile):
                                xi = (grp % 2) * 4 + i
                                pe_tr(pp[:, i * 128:(i + 1) * 128], xn[xi][:, k * 128:(k + 1) * 128], identb[:],
                                      [b_xn[xi], b_identb], [bp], sig=(i == ntile - 1))
                            n = ntile * 128
                            dst = hT[:, k, grp * 512:grp * 512 + n]
                            if k % 2 == 0:
                                V("dve", lambda: nc.vector.tensor_scalar(
                                    out=dst, in0=pp[:, 0:n], scalar1=A1[:, k, jmod:jmod + 1],
                                    scalar2=modfm[:, k, jmod:jmod + 1], op0=ALU.mult, op1=ALU.add),
                                  [bp, b_A1, b_modfm], [b_hT[grp][0]])
                            else:
                                act(dst, pp[:, 0:n], AF.Identity, [bp, b_A1, b_modfm], [b_hT[grp][1]],
                                    scale=A1[:, k, jmod:jmod + 1], bias=modfm[:, k, jmod:jmod + 1])
                    if b == 0:
                        dump("hT", hT[:, :, :].rearrange("p a b -> p (a b)"), [x for l_ in b_hT for x in l_])
                    S.barrier()
                if STOP_AFTER == "s1":
                    break

                with ExitStack() as es2:
                    cs_t = sb(es2, [128, 2 * L], F32, "cossin"); b_cst = Buf()
                    btab = sb(es2, [128, 8, NTB * 64], BF16, "btab"); b_btab = Buf()
                    S.dma("sp", cs_t[:], cossin_d, writes=[b_cst])
                    S.dma("pool", btab[0:64, :, :].rearrange("p a b -> p (a b)"), btab_d, writes=[b_btab], group=True)
                    S.dma("pool", btab[64:128, :, :].rearrange("p a b -> p (a b)"), btab_d, writes=[b_btab], group=True)
                    wq = [sb(es2, [128, 8, 640], BF16, "wq") for _ in range(1)]; b_wq = [Buf()]
                    Qr = [sb(es2, [128, L], BF16, "Qr") for _ in range(1)]
                    Qp = [sb(es2, [128, L], BF16, "Qp") for _ in range(1)]
                    Kr = [sb(es2, [128, L], BF16, "Kr") for _ in range(1)]
                    Kc = [sb(es2, [128, C], BF16, "Kc") for _ in range(1)]
                    Vx = [sb(es2, [128, 18, 192], BF16, "Vx") for _ in range(1)]
                    b_Q = [[Buf() for _ in range(4)] for _ in range(1)]
                    b_Qp = [[Buf() for _ in range(4)] for _ in range(1)]
                    b_K = [Buf()]
                    b_Kc = [Buf()]
                    b_V = [[Buf() for _ in range(5)]]
                    t1 = [sb(es2, [128, 512], F32, "t1") for _ in range(2)]; b_t1 = [Buf(), Buf()]
                    t2 = [sb(es2, [128, 512], F32, "t2") for _ in range(2)]; b_t2 = [Buf(), Buf()]
                    PTc = [sb(es2, [128, 512], BF16, "PTc") for _ in range(2)]; b_PTc = [Buf(), Buf()]
                    PT = [sb(es2, [128, 320], BF16, "PT") for _ in range(3)]; b_PT = [Buf() for _ in range(3)]
                    rc = [sb(es2, [128, 512], F32, "rc") for _ in range(2)]; b_rc = [Buf(), Buf()]
                    psP = [ps(es2, [128, 512], F32, "psP") for _ in range(2)]; b_psP = [Buf(), Buf()]
                    psSc = [ps(es2, [128, 512], F32, "psSc") for _ in range(2)]; b_psSc = [Buf(), Buf()]
                    psS = [ps(es2, [128, 512], F32, "psS") for _ in range(2)]; b_psS = [Buf(), Buf()]
                    psO = [ps(es2, [128, 512], F32, "psO") for _ in range(2)]; b_psO = [Buf(), Buf()]
                    V("dve", lambda: nc.vector.memset(Vx[0][:, :, 64:128], 1.0), [], b_V[0])
                    pcnt = [0]

                    def nextP():
                        i = pcnt[0] % 2
                        pcnt[0] += 1
                        return psP[i], b_psP[i]

                    cnt_t = [0]
                    for hp in range(ATT_NHP):
                        par = 0
                        w = wq[par]; bw = b_wq[par]
                        S.dma("pool", w[:, :, :].rearrange("p a b -> p (a b)"), wqkv_d[hp], writes=[bw])
                        for blk in range(4 if ATT_PARTS & 1 else 0):
                            tok = slice(blk * 512, (blk + 1) * 512)
                            for which in range(2):
                                c0 = which * 256
                                pa, bpa = nextP()
                                for k in range(8):
                                    pe_mm(pa[:], w[:, k, c0:c0 + 128], hT[:, k, tok], k == 0, k == 7,
                                          [bw, *b_hT[blk]], [bpa])
                                pb, bpb = nextP()
                                for k in range(8):
                                    pe_mm(pb[:], w[:, k, c0 + 128:c0 + 256], hT[:, k, tok], k == 0, k == 7,
                                          [bw, *b_hT[blk]], [bpb])
                                ii = cnt_t[0] % 2
                                cnt_t[0] += 1
                                sc_ = 0.125 if which == 0 else 1.0
                                dstb = b_Q[par][blk] if which == 0 else b_K[par]
                                dst = (Qr if which == 0 else Kr)[par][:, tok]
                                V("dve", lambda: nc.vector.scalar_tensor_tensor(
                                    out=t1[ii][:], in0=pa[:], scalar=sc_, in1=cs_t[:, tok], op0=ALU.mult, op1=ALU.mult),
                                  [bpa, b_cst], [b_t1[ii]])
                                if which == 0 and (ATT_PARTS & 16):
                                    V("dve", lambda: nc.vector.tensor_scalar(out=Qp[par][:, tok], in0=pa[:], scalar1=0.125, scalar2=None,
                                                                             op0=ALU.mult), [bpa], [b_Qp[par][blk]])
                                V("dve", lambda: nc.vector.scalar_tensor_tensor(
                                    out=t2[ii][:], in0=pb[:], scalar=sc_, in1=cs_t[:, L + blk * 512:L + (blk + 1) * 512],
                                    op0=ALU.mult, op1=ALU.mult), [bpb, b_cst], [b_t2[ii]])
                                V("pool" if ATT_PARTS & 8 else "dve", lambda: (nc.gpsimd if ATT_PARTS & 8 else nc.vector).tensor_tensor(out=dst, in0=t1[ii][:], in1=t2[ii][:], op=ALU.add),
                                  [b_t1[ii], b_t2[ii]], [dstb])
                        if ATT_PARTS & 2:
                            pa, bpa = nextP()
                            for k in range(8):
                                pe_mm(pa[:, 0:C], w[:, k, 256:384], hT[:, k, L:LC], k == 0, k == 7, [bw, *b_hT[4]], [bpa])
                            act(Kc[par][:], pa[:, 0:C], AF.Copy, [bpa], [b_Kc[par]])
                        for g4 in range(5 if ATT_PARTS & 4 else 0):
                            nch = 4 if g4 < 4 else 2
                            pa, bpa = nextP()
                            for i in range(nch):
                                ch = g4 * 4 + i
                                for k in range(8):
                                    pe_mm(pa[:, i * 128:(i + 1) * 128], hT[:, k, ch * 128:(ch + 1) * 128],
                                          w[:, k, 512:640], k == 0, k == 7, [bw, *b_hT[g4]], [bpa],
                                          sig=(k == 7 and i == nch - 1))
                            src = pa[:, 0:nch * 128].rearrange("p (c a d) -> p c a d", a=2, d=64)
                            dstv = Vx[par][:, g4 * 4:g4 * 4 + nch, :].rearrange("p c (a d) -> p c a d", d=64)[:, :, 0::2, :]
                            if g4 % 2 == 0:
                                act(dstv, src, AF.Copy, [bpa], [b_V[par][g4]])
                            else:
                                V("dve", lambda: nc.vector.tensor_copy(out=dstv, in_=src), [bpa], [b_V[par][g4]])

                        if b == 0 and hp == 0:
                            dump("Qp", Qp[0][:], b_Qp[0])
                            dump("Qr", Qr[0][:], b_Q[0])
                            dump("Kr", Kr[0][:], b_K)
                            dump("Kc", Kc[0][:], b_Kc)
                            dump("Vx", Vx[0][:, :, :].rearrange("p a b -> p (a b)"), b_V[0])
                        units = []
                        for e in range(2):
                            for qb in range(4):
                                units.append((e, qb))
                        ucnt = [0]
                        for (e, qb) in units[int(_os.environ.get("ATT_USTART", "0")):][:ATT_NUNITS]:
                            h = hp * 2 + e
                            pr = slice(64 * e, 64 * e + 64)
                            qs = slice(qb * 512, (qb + 1) * 512)
                            vcols = slice(0, 128) if e == 0 else slice(64, 192)
                            oi = ucnt[0] % 2
                            ucnt[0] += 1
                            pO, bO = psO[oi], b_psO[oi]
                            for c in range(2):
                                pS, bS = psSc[c], b_psSc[c]
                                pe_mm(pS[:], Kc[par][pr, c * 128:(c + 1) * 128], Qp[par][pr, qs], True, True,
                                      [b_Kc[par], b_Qp[par][qb]], [bS])
                                act(PTc[c][:], pS[:], AF.Exp, [bS], [b_PTc[c]])
                            if b == 0 and hp == 0 and e == 0 and qb == 0:
                                dump("PTc", PTc[0][:], [b_PTc[0]])
                            for c in range(2):
                                pe_mm(pO[:], Vx[par][:, 16 + c, vcols], PTc[c][:], c == 0, False,
                                      [b_V[par][4], b_PTc[c]], [bO], sig=(c == 1))
                            rows = list(range(qb * 8, qb * 8 + ATT_NROWS))
                            plan = []
                            for r in rows:
                                rs = min(max(r - 4, 0), 24)
                                dr0 = rs - r + 7
                                chunks = []
                                if rs % 2 == 0:
                                    for j in range(4):
                                        chunks.append(((rs + 2 * j) // 2, 1 + dr0 + 2 * j))
                                else:
                                    assert dr0 == 3
                                    c0 = (rs - 1) // 2
                                    chunks.append((c0, 17))
                                    for j in range(1, 4):
                                        chunks.append((c0 + j, dr0 + 2 * j))
                                    chunks.append((c0 + 4, 19))
                                plan.append((r, chunks))

                            def emit_qk(idx):
                                r, chunks = plan[idx]
                                si = idx % 2
                                pS, bS = psS[si], b_psS[si]
                                qcol = slice(r * 64, (r + 1) * 64)
                                for j, (kc, blk0) in enumerate(chunks):
                                    o = pS[:, j * 64:(j + 1) * 64]
                                    pe_mm(o, Kr[par][pr, kc * 128:(kc + 1) * 128], Qr[par][pr, qcol], True, False,
                                          [b_K[par], b_Q[par][qb]], [bS], sig=False)
                                    lt = btab[pr, h, blk0 * 64:(blk0 + 2) * 64]
                                    pe_mm(o, lt, identb[pr, pr], False, True, [b_btab, b_identb], [bS],
                                          sig=(j == len(chunks) - 1))
                                n = len(chunks) * 64
                                pi = idx % 3
                                act(PT[pi][:, 0:n], pS[:, 0:n], AF.Exp, [bS], [b_PT[pi]])

                            def emit_pv(idx):
                                r, chunks = plan[idx]
                                pi = idx % 3
                                rr = r - qb * 8
                                for j, (kc, _) in enumerate(chunks):
                                    last = (j == len(chunks) - 1)
                                    pe_mm(pO[:, rr * 64:(rr + 1) * 64], Vx[par][:, kc, vcols], PT[pi][:, j * 64:(j + 1) * 64],
                                          False, last and idx == len(plan) - 1, [b_V[par][kc // 4], b_PT[pi]], [bO], sig=last)

                            if plan:
                                emit_qk(0)
                            for idx in range(len(plan)):
                                if idx + 1 < len(plan):
                                    emit_qk(idx + 1)
                                emit_pv(idx)
                            dn = slice(64, 128) if e == 0 else slice(0, 64)
                            if not ATT_NORM:
                                continue
                            V("dve", lambda: nc.vector.reciprocal(out=rc[oi][pr, :], in_=pO[dn, :]), [bO], [b_rc[oi]])
                            V("dve", lambda: nc.vector.tensor_tensor(out=oatt[pr, hp, qs], in0=pO[pr, :], in1=rc[oi][pr, :],
                                                                     op=ALU.mult), [bO, b_rc[oi]], [b_oatt[hp][qb]])
                    if b == 0:
                        dump("oatt", oatt[:, :, :].rearrange("p a b -> p (a b)"), [x for l_ in b_oatt for x in l_])
                    S.barrier()
                if STOP_AFTER == "s2":
                    break

                olru = sb(esb, [128, 8, L], BF16, "olru")
                b_olru = [Buf() for _ in range(8)]
                with ExitStack() as es3:
                    TL = LC + 3
                    wl = [sb(es3, [128, 8, 256], BF16, "wl") for _ in range(2)]; b_wl = [Buf(), Buf()]
                    lw = sb(es3, [128, 4, 8, 128], BF16, "lw"); b_lw = Buf()
                    S.dma("pool", lw[:, :, :, :].rearrange("p a b c -> p (a b c)"), lruw_d, writes=[b_lw])
                    LXp = sb(es3, [128, TL + 3], F32, "LXp"); b_LXp = Buf()
                    xc = sb(es3, [128, TL], F32, "xc"); b_xc = Buf()
                    xcb = sb(es3, [128, TL], BF16, "xcb"); b_xcb = Buf()
                    av = sb(es3, [128, TL], F32, "av"); b_av = Buf()
                    wv = sb(es3, [128, TL], F32, "wv"); b_wv = Buf()
                    iv = sb(es3, [128, TL], F32, "iv"); b_iv = Buf()
                    hv = [sb(es3, [128, TL], F32, "hv") for _ in range(2)]; b_hv = [Buf(), Buf()]
                    gl = sb(es3, [128, L], BF16, "gl"); b_gl = Buf()
                    psA = [ps(es3, [128, 512], F32, "psA") for _ in range(4)]; b_psA = [Buf() for _ in range(4)]
                    pacnt = [0]

                    def nextA():
                        i = pacnt[0] % 4
                        pacnt[0] += 1
                        return psA[i], b_psA[i]

                    V("dve", lambda: nc.vector.memset(LXp[:], 0.0), [], [b_LXp])
                    for n in range(8):
                        w = wl[n % 2]; bw = b_wl[n % 2]
                        S.dma("pool", w[:, :, :].rearrange("p a b -> p (a b)"), wlxlg_d[n], writes=[bw])
                        for blk in range(5):
                            nt = 512 if blk < 4 else C
                            tok = slice(blk * 512, blk * 512 + nt)
                            pa, bpa = nextA()
                            for k in range(8):
                                pe_mm(pa[:, 0:nt], w[:, k, 0:128], hT[:, k, tok], k == 0, k == 7, [bw, *b_hT[blk]], [bpa])
                            d0 = 261 + blk * 512 if blk < 4 else 2
                            act(LXp[:, d0:d0 + nt], pa[:, 0:nt], AF.Copy, [bpa], [b_LXp])
                        cw = lambda j: vecs[:, V_CONVW + j * 8 + n:V_CONVW + j * 8 + n + 1]
                        V("dve", lambda: nc.vector.tensor_scalar(out=xc[:], in0=LXp[:, 0:TL], scalar1=cw(0),
                                                                 scalar2=vecs[:, V_CONVB + n:V_CONVB + n + 1],
                                                                 op0=ALU.mult, op1=ALU.add), [b_LXp, b_vecs], [b_xc])
                        for j in range(1, 4):
                            V("dve", lambda: nc.vector.scalar_tensor_tensor(out=xc[:], in0=LXp[:, j:j + TL], scalar=cw(j),
                                                                            in1=xc[:], op0=ALU.mult, op1=ALU.add),
                              [b_LXp, b_vecs, b_xc], [b_xc])
                        V("pool", lambda: nc.gpsimd.tensor_copy(out=xcb[:], in_=xc[:]), [b_xc], [b_xcb])
                        if b == 0 and n == 0:
                            dump("xc0", xc[:], [b_xc])
                        for dr in range(2):
                            di = dr * 8 + n
                            for blk in range(5):
                                nt = 512 if blk < 4 else TL - 2048
                                tok = slice(blk * 512, blk * 512 + nt)
                                pr_, bpr = nextA()
                                pe_mm(pr_[:, 0:nt], lw[:, dr, n, :], xcb[:, tok], True, True, [b_lw, b_xcb], [bpr])
                                pi_, bpi = nextA()
                                pe_mm(pi_[:, 0:nt], lw[:, 2 + dr, n, :], xcb[:, tok], True, True, [b_lw, b_xcb], [bpi])
                                act(av[:, tok], pr_[:, 0:nt], AF.Sigmoid, [bpr, b_vecs], [b_av],
                                    bias=vecs[:, V_BA + di:V_BA + di + 1])
                                act(iv[:, tok], pi_[:, 0:nt], AF.Sigmoid, [bpi, b_vecs], [b_iv],
                                    bias=vecs[:, V_BX + di:V_BX + di + 1])
                            act(wv[:], av[:], AF.Exp, [b_av, b_lrup], [b_wv], scale=lrup[:, 1, di:di + 1])
                            act(av[:], av[:], AF.Exp, [b_av, b_lrup], [b_av], scale=lrup[:, 0, di:di + 1])
                            act(wv[:], wv[:], AF.Sqrt, [b_wv], [b_wv], scale=-1.0, bias=1.0)
                            V("pool", lambda: nc.gpsimd.tensor_tensor(out=iv[:], in0=iv[:], in1=xc[:], op=ALU.mult),
                              [b_iv, b_xc], [b_iv])
                            V("dve", lambda: nc.vector.tensor_tensor(out=wv[:], in0=iv[:], in1=wv[:], op=ALU.mult),
                              [b_iv, b_wv], [b_wv])
                            hh = hv[dr]; bh = b_hv[dr]
                            if dr == 0:
                                V("dve", lambda: nc.vector.tensor_tensor_scan(
                                    out=hh[:, 0:C], data0=av[:, 0:C], data1=wv[:, 0:C], initial=0.0,
                                    op0=ALU.mult, op1=ALU.add), [b_av, b_wv], [bh])
                                V("dve", lambda: nc.vector.tensor_tensor_scan(
                                    out=hh[:, C + 3:TL], data0=av[:, C + 3:TL], data1=wv[:, C + 3:TL],
                                    initial=hh[:, C - 1:C], op0=ALU.mult, op1=ALU.add), [b_av, b_wv, bh], [bh])
                            else:
                                V("dve", lambda: nc.vector.tensor_tensor_scan(
                                    out=hh[:, C - 1::-1], data0=av[:, C - 1::-1], data1=wv[:, C - 1::-1], initial=0.0,
                                    op0=ALU.mult, op1=ALU.add), [b_av, b_wv], [bh])
                                V("dve", lambda: nc.vector.tensor_tensor_scan(
                                    out=hh[:, TL - 1:C + 2:-1], data0=av[:, TL - 1:C + 2:-1], data1=wv[:, TL - 1:C + 2:-1],
                                    initial=hh[:, 0:1], op0=ALU.mult, op1=ALU.add), [b_av, b_wv, bh], [bh])
                        for blk in range(4):
                            tok = slice(blk * 512, (blk + 1) * 512)
                            pa, bpa = nextA()
                            for k in range(8):
                                pe_mm(pa[:], w[:, k, 128:256], hT[:, k, tok], k == 0, k == 7, [bw, *b_hT[blk]], [bpa])
                            act(gl[:, tok], pa[:], AF.Gelu_apprx_tanh, [bpa], [b_gl])
                        V("pool", lambda: nc.gpsimd.tensor_tensor(out=hv[0][:, C + 3:TL], in0=hv[0][:, C + 3:TL],
                                                                  in1=hv[1][:, C + 3:TL], op=ALU.add),
                          [b_hv[0], b_hv[1]], [b_hv[0]])
                        if b == 0 and n == 0:
                            dump("hsum0", hv[0][:, C + 3:TL], [b_hv[0]])
                        V("dve", lambda: nc.vector.tensor_tensor(out=olru[:, n, :], in0=hv[0][:, C + 3:TL], in1=gl[:],
                                                                 op=ALU.mult), [b_hv[0], b_gl], [b_olru[n]])
                    if b == 0:
                        dump("olru", olru[:, :, :].rearrange("p a b -> p (a b)"), b_olru)
                    S.barrier()
                if STOP_AFTER == "s3":
                    break

                yT = sb(esb, [128, 8, L], BF16, "yT")
                b_yT = [Buf() for _ in range(4)]
                with ExitStack() as es4:
                    wg_ = [sb(es4, [128, 8, 256], BF16, "wg") for _ in range(2)]; b_wg = [Buf(), Buf()]
                    wu_ = [sb(es4, [128, 12, 128], BF16, "wu") for _ in range(2)]; b_wu = [Buf(), Buf()]
                    ga_ = [sb(es4, [128, 512], F32, "ga") for _ in range(2)]; b_ga = [Buf(), Buf()]
                    gb_ = [sb(es4, [128, 512], F32, "gb") for _ in range(2)]; b_gb = [Buf(), Buf()]
                    ya_ = [sb(es4, [128, 512], F32, "ya") for _ in range(2)]; b_ya = [Buf(), Buf()]
                    yb_ = [sb(es4, [128, 512], F32, "yb") for _ in range(2)]; b_yb = [Buf(), Buf()]
                    psM = [ps(es4, [128, 512], F32, "psM") for _ in range(8)]; b_psM = [Buf() for _ in range(8)]
                    it = 0
                    for f in range(8):
                        wgt, bwg = wg_[f % 2], b_wg[f % 2]
                        wut, bwu = wu_[f % 2], b_wu[f % 2]
                        S.dma("pool", wgt[:, :, :].rearrange("p a b -> p (a b)"), wgagb_d[f], writes=[bwg])
                        S.dma("pool", wut[:, :, :].rearrange("p a b -> p (a b)"), wup_d[f], writes=[bwu])
                        for blk in range(4):
                            tok = slice(blk * 512, (blk + 1) * 512)
                            i2 = it % 2
                            p0, p1, p2, p3 = [psM[(it % 2) * 4 + q] for q in range(4)]
                            q0, q1, q2, q3 = [b_psM[(it % 2) * 4 + q] for q in range(4)]
                            it += 1
                            for k in range(8):
                                pe_mm(p0[:], wgt[:, k, 0:128], hT[:, k, tok], k == 0, k == 7, [bwg, *b_hT[blk]], [q0])
                            act(ga_[i2][:], p0[:], AF.Sigmoid, [q0], [b_ga[i2]])
                            for k in range(4):
                                pe_mm(p1[:], wut[:, k, :], oatt[:, k, tok], k == 0, k == 3, [bwu, b_oatt[k][blk]], [q1])
                            V("dve", lambda: nc.vector.tensor_tensor(out=ya_[i2][:], in0=p1[:], in1=ga_[i2][:], op=ALU.mult),
                              [q1, b_ga[i2]], [b_ya[i2]])
                            for k in range(8):
                                pe_mm(p2[:], wgt[:, k, 128:256], hT[:, k, tok], k == 0, k == 7, [bwg, *b_hT[blk]], [q2])
                            act(gb_[i2][:], p2[:], AF.Sigmoid, [q2], [b_gb[i2]])
                            for k in range(8):
                                pe_mm(p3[:], wut[:, 4 + k, :], olru[:, k, tok], k == 0, k == 7, [bwu, b_olru[k]], [q3])
                            V("dve", lambda: nc.vector.tensor_tensor(out=yb_[i2][:], in0=p3[:], in1=gb_[i2][:], op=ALU.mult),
                              [q3, b_gb[i2]], [b_yb[i2]])
                            V("pool", lambda: nc.gpsimd.tensor_tensor(out=yT[:, f, tok], in0=ya_[i2][:], in1=yb_[i2][:],
                                                                      op=ALU.add), [b_ya[i2], b_yb[i2]], [b_yT[blk]])
                    if b == 0:
                        dump("yT", yT[:, :, :].rearrange("p a b -> p (a b)"), b_yT)
                    S.barrier()
                if STOP_AFTER == "s4":
                    break

                with ExitStack() as es5:
                    wo32 = [sb(es5, [128, D], F32, "wo32") for _ in range(2)]; b_wo32 = [Buf(), Buf()]
                    wob = sb(es5, [128, 8, D], BF16, "wob"); b_wob = Buf()
                    GA1 = sb(es5, [128, D], F32, "GA1"); b_GA1 = Buf()
                    G2 = sb(es5, [128, D], F32, "G2"); b_G2 = Buf()
                    S2 = sb(es5, [128, D], F32, "S2"); b_S2 = Buf()
                    gffnb = sb(es5, [128, D], F32, "gffnb"); b_gffnb = Buf()
                    diag = [sb(es5, [128, 128], F32, "diag") for _ in range(2)]; b_diag = [Buf(), Buf()]
                    wr = sb(es5, [128, 8, 36], F32, "wr"); b_wr = Buf()
                    brb = sb(es5, [128, 36], F32, "brb"); b_brb = Buf()
                    psB = ps(es5, [128, D], F32, "psB"); b_psB = Buf()
                    psO5 = [ps(es5, [128, D], F32, "psO5") for _ in range(2)]; b_psO5 = [Buf(), Buf()]
                    psT = ps(es5, [128, D], F32, "psT"); b_psT = Buf()
                    S.dma("sp", gffnb[:], gffnb_d, writes=[b_gffnb])
                    S.dma("sp", wr[:, :, :].rearrange("p a b -> p (a b)"), wr_d, writes=[b_wr])
                    S.dma("sp", brb[:], brb_d, writes=[b_brb])
                    bcast_row(es5, 2, b, psB, b_psB, diag, b_diag)
                    V("dve", lambda: nc.vector.tensor_copy(out=GA1[:], in_=psB[:]), [b_psB], [b_GA1])
                    bcast_row(es5, 4, b, psB, b_psB, diag, b_diag)
                    V("dve", lambda: nc.vector.scalar_tensor_tensor(out=G2[:], in0=psB[:], scalar=1.0, in1=gffnb[:],
                                                                    op0=ALU.add, op1=ALU.mult), [b_psB, b_gffnb], [b_G2])
                    bcast_row(es5, 3, b, psB, b_psB, diag, b_diag)
                    V("dve", lambda: nc.vector.tensor_copy(out=S2[:], in_=psB[:]), [b_psB], [b_S2])
                    for kk in range(8):
                        S.dma("sp", wo32[kk % 2][:], wout_d[:, kk * D:(kk + 1) * D], writes=[b_wo32[kk % 2]])
                        V("dve", lambda: nc.vector.tensor_tensor(out=wob[:, kk, :], in0=wo32[kk % 2][:], in1=GA1[:],
                                                                 op=ALU.mult), [b_wo32[kk % 2], b_GA1], [b_wob])
                    xt5 = [sb(es5, [128, D], F32, "xt5") for _ in range(2)]; b_xt5 = [Buf(), Buf()]
                    x1t = xt5; b_x1t = b_xt5
                    h2t = [sb(es5, [128, D], F32, "h2t") for _ in range(2)]; b_h2t = [Buf(), Buf()]
                    h2b = [sb(es5, [128, D], BF16, "h2b") for _ in range(2)]; b_h2b = [Buf(), Buf()]
                    h2T = sb(es5, [128, 8, 128], F32, "h2T"); b_h2T = Buf()
                    junk5 = sb(es5, [128, D], BF16, "junk5"); b_junk5 = Buf()
                    st5 = sb(es5, [128, 16, 3], F32, "st5"); b_st5 = [Buf() for _ in range(16)]
                    rt = sb(es5, [128, 2, 96], F32, "rt"); b_rt = [Buf() for _ in range(2)]
                    for j in range(16):
                        tg = b * 16 + j
                        i2 = j % 2
                        tsl = slice(j * 128, (j + 1) * 128)
                        S.dma("sp", xt5[i2][:], xin[b, tsl, :], writes=[b_xt5[i2]])
                        pO, bO = psO5[i2], b_psO5[i2]
                        for hf in range(2):
                            for k in range(8):
                                pe_mm(pO[:, hf * 512:(hf + 1) * 512], yT[:, k, tsl], wob[:, k, hf * 512:(hf + 1) * 512],
                                      k == 0, k == 7, [b_yT[j // 4], b_wob], [bO], sig=(k == 7 and hf == 1))
                        V("dve", lambda: nc.vector.tensor_tensor(out=x1t[i2][:], in0=pO[:], in1=xt5[i2][:], op=ALU.add),
                          [bO, b_xt5[i2]], [b_xt5[i2]])
                        S.dma("sp", x1_d[tg * 128:(tg + 1) * 128, :], x1t[i2][:], reads=[b_x1t[i2]], writes=[b_x1d], group=True, semof=b_x1t[i2])
                        act(junk5[:], x1t[i2][:], AF.Square, [b_x1t[i2]], [b_junk5, b_st5[j]], accum_out=st5[:, j, 0:1])
                        act(st5[:, j, 1:2], st5[:, j, 0:1], AF.Sqrt, [b_st5[j], b_eps], [b_st5[j]], scale=1.0 / D,
                            bias=epst[:, 0:1])
                        V("dve", lambda: nc.vector.reciprocal(out=st5[:, j, 2:3], in_=st5[:, j, 1:2]), [b_st5[j]], [b_st5[j]])
                        V("dve", lambda: nc.vector.scalar_tensor_tensor(out=h2t[i2][:], in0=x1t[i2][:], scalar=st5[:, j, 2:3],
                                                                        in1=G2[:], op0=ALU.mult, op1=ALU.mult),
                          [b_x1t[i2], b_st5[j], b_G2], [b_h2t[i2]])
                        V("pool", lambda: nc.gpsimd.tensor_tensor(out=h2t[i2][:], in0=h2t[i2][:], in1=S2[:], op=ALU.add),
                          [b_h2t[i2], b_S2], [b_h2t[i2]])
                        act(h2b[i2][:], h2t[i2][:], AF.Copy, [b_h2t[i2]], [b_h2b[i2]])
                        S.dma("sp", h2_d[tg * 128:(tg + 1) * 128, :], h2b[i2][:], reads=[b_h2b[i2]], writes=[b_h2d], group=True, semof=b_h2b[i2])
                        for k in range(8):
                            pe_tr(psT[:, k * 128:(k + 1) * 128], h2t[i2][:, k * 128:(k + 1) * 128], identf[:],
                                  [b_h2t[i2], b_identf], [b_psT], sig=(k == 7))
                        act(h2T[:, :, :].rearrange("p a b -> p (a b)"), psT[:], AF.Copy, [b_psT], [b_h2T])
                        for k in range(8):
                            pe_mm(psB[:, 0:36], h2T[:, k, :], wr[:, k, :], k == 0, k == 7, [b_h2T, b_wr], [b_psB])
                        R_ = rt[:, j % 2, :]
                        br_ = b_rt[j % 2]
                        lg = R_[:, 0:4]; le = R_[:, 4:36]
                        V("dve", lambda: nc.vector.tensor_tensor(out=R_[:, 0:36], in0=psB[:, 0:36], in1=brb[:], op=ALU.add),
                          [b_psB, b_brb], [br_])
                        if tg == 0:
                            dump("logit0", R_[:, 0:36], [br_])
                        mg = R_[:, 36:37]; nmg = R_[:, 37:38]; sg = R_[:, 38:39]; ptop = R_[:, 39:40]
                        V("dve", lambda: nc.vector.tensor_reduce(out=mg, in_=lg, axis=AX.X, op=ALU.max), [br_], [br_])
                        V("dve", lambda: nc.vector.tensor_scalar(out=nmg, in0=mg, scalar1=-1.0, scalar2=None, op0=ALU.mult),
                          [br_], [br_])
                        act(R_[:, 40:44], lg, AF.Exp, [br_], [br_], bias=nmg, accum_out=sg)
                        V("dve", lambda: nc.vector.reciprocal(out=ptop, in_=sg), [br_], [br_])
                        V("dve", lambda: nc.vector.tensor_scalar(out=R_[:, 44:48], in0=lg, scalar1=mg, scalar2=None,
                                                                 op0=ALU.is_equal), [br_], [br_])
                        V("dve", lambda: nc.vector.tensor_scalar(out=R_[:, 44:48], in0=R_[:, 44:48], scalar1=-1.0, scalar2=1e30,
                                                                 op0=ALU.add, op1=ALU.mult), [br_], [br_])
                        lem = R_[:, 48:80]
                        V("dve", lambda: nc.vector.tensor_tensor(
                            out=lem.rearrange("p (g e) -> p g e", e=8), in0=le.rearrange("p (g e) -> p g e", e=8),
                            in1=R_[:, 44:48].unsqueeze(2).to_broadcast([128, 4, 8]), op=ALU.add), [br_], [br_])
                        top8 = R_[:, 80:88]
                        V("dve", lambda: nc.vector.max(out=top8, in_=lem), [br_], [br_])
                        V("dve", lambda: nc.vector.tensor_scalar(out=oh1all[:, tg, :], in0=lem, scalar1=top8[:, 0:1], scalar2=None,
                                                                 op0=ALU.is_equal), [br_], [b_oh1])
                        V("dve", lambda: nc.vector.tensor_scalar(out=Mall[:, tg, :], in0=lem, scalar1=top8[:, 1:2], scalar2=None,
                                                                 op0=ALU.is_ge), [br_], [b_Mall])
                        dlt = R_[:, 88:89]; ew = R_[:, 89:90]; w1_ = R_[:, 90:91]
                        V("dve", lambda: nc.vector.tensor_tensor(out=dlt, in0=top8[:, 1:2], in1=top8[:, 0:1], op=ALU.subtract),
                          [br_], [br_])
                        act(ew, dlt, AF.Exp, [br_], [br_])
                        V("dve", lambda: nc.vector.tensor_scalar(out=ew, in0=ew, scalar1=1.0, scalar2=None, op0=ALU.add),
                          [br_], [br_])
                        V("dve", lambda: nc.vector.reciprocal(out=w1_, in_=ew), [br_], [br_])
                        V("dve", lambda: nc.vector.tensor_tensor(out=gates[:, tg, 0:1], in0=w1_, in1=ptop, op=ALU.mult),
                          [br_], [b_gates])
                        V("dve", lambda: nc.vector.tensor_tensor(out=gates[:, tg, 1:2], in0=ptop, in1=gates[:, tg, 0:1],
                                                                 op=ALU.subtract), [br_, b_gates], [b_gates])
                    S.barrier()
            if STOP_AFTER in ("s1", "s2", "s3", "s4"):
                break

        if STOP_AFTER is None or STOP_AFTER in ("route", "moe"):
            blk_i = sb(es, [128, NBLK], I32, "blki"); b_blki = Buf()
            with ExitStack() as esr:
                psC = ps(esr, [128, 32], F32, "psC"); b_psC = Buf()
                psR = [ps(esr, [128, 32], F32, "psR") for _ in range(2)]; b_psR = [Buf(), Buf()]
                cn = sb(esr, [128, 8, 32], F32, "cn"); b_cn = Buf()
                cni = sb(esr, [128, 32], I32, "cni"); b_cni = Buf()
                cmp_ = sb(esr, [128, NBLK, 32], F32, "cmp"); b_cmp = Buf()
                bst = sb(esr, [128, NBLK], F32, "bst"); b_bst = Buf()
                bs0 = sb(esr, [128, NBLK], F32, "bs0"); b_bs0 = Buf()
                blk_f = sb(esr, [128, NBLK], F32, "blkf"); b_blkf = Buf()
                tmp = sb(esr, [128, 2, 32], F32, "tmpr"); b_tmp = [Buf(), Buf()]
                for t in range(32):
                    pe_mm(psC[:], onesb[:], Mall[:, t, :], t == 0, t == 31, [b_onesb, b_Mall], [b_psC])
                V("dve", lambda: nc.vector.tensor_copy(out=cn[:, 0, :], in_=psC[:]), [b_psC], [b_cn])
                V("dve", lambda: nc.vector.tensor_scalar(out=cn[:, 1, :], in0=cn[:, 0, :], scalar1=255.0, scalar2=None, op0=ALU.add),
                  [b_cn], [b_cn])
                V("dve", lambda: nc.vector.tensor_copy(out=cni[:], in_=cn[:, 1, :]), [b_cn], [b_cni])
                V("dve", lambda: nc.vector.tensor_scalar(out=cni[:], in0=cni[:], scalar1=8, scalar2=8,
                                                         op0=ALU.arith_shift_right, op1=ALU.logical_shift_left), [b_cni], [b_cni])
                V("dve", lambda: nc.vector.tensor_copy(out=cn[:, 2, :], in_=cni[:]), [b_cni], [b_cn])
                V("dve", lambda: nc.vector.tensor_tensor_scan(out=cn[:, 3, :], data0=onesf[:, 0:32], data1=cn[:, 2, :],
                                                              initial=0.0, op0=ALU.mult, op1=ALU.add),
                  [b_cn, b_onesf], [b_cn])
                V("dve", lambda: nc.vector.tensor_tensor(out=cn[:, 4, :], in0=cn[:, 3, :], in1=cn[:, 2, :], op=ALU.subtract),
                  [b_cn], [b_cn])
                V("dve", lambda: nc.vector.tensor_scalar(out=bs0[:], in0=onesf[:, 0:NBLK], scalar1=256.0, scalar2=None, op0=ALU.mult),
                  [b_onesf], [b_bs0])
                V("dve", lambda: nc.vector.tensor_tensor_scan(out=bst[:], data0=onesf[:, 0:NBLK], data1=bs0[:], initial=-256.0,
                                                              op0=ALU.mult, op1=ALU.add), [b_bs0, b_onesf], [b_bst])
                V("dve", lambda: nc.vector.tensor_tensor(
                    out=cmp_[:], in0=cn[:, 3, :].unsqueeze(1).to_broadcast([128, NBLK, 32]),
                    in1=bst[:].unsqueeze(2).to_broadcast([128, NBLK, 32]), op=ALU.is_le), [b_cn, b_bst], [b_cmp])
                V("dve", lambda: nc.vector.tensor_reduce(out=blk_f[:], in_=cmp_[:], axis=AX.X, op=ALU.add), [b_cmp], [b_blkf])
                V("dve", lambda: nc.vector.tensor_scalar(out=blk_f[:], in0=blk_f[:], scalar1=31.0, scalar2=128.0,
                                                         op0=ALU.min, op1=ALU.mult), [b_blkf], [b_blkf])
                V("dve", lambda: nc.vector.tensor_scalar(out=blk_f[:], in0=blk_f[:], scalar1=iota[:, 0:1], scalar2=None,
                                                         op0=ALU.add), [b_blkf, b_iota], [b_blkf])
                V("dve", lambda: nc.vector.tensor_copy(out=blk_i[:], in_=blk_f[:]), [b_blkf], [b_blki])
                dump("blkf", blk_f[:], [b_blkf])
                dump("cn", cn[:, 0:5, :].rearrange("p a b -> p (a b)"), [b_cn])
                for t in range(32):
                    pR, bR = psR[t % 2], b_psR[t % 2]
                    for t2 in range(t):
                        pe_mm(pR[:], onesb[:], Mall[:, t2, :], t2 == 0, False, [b_onesb, b_Mall], [bR], sig=False)
                    pe_mm(pR[:], utri[:], Mall[:, t, :], t == 0, True, [b_utri, b_Mall], [bR])
                    tt = tmp[:, t % 2, :]; bt_ = b_tmp[t % 2]
                    V("dve", lambda: nc.vector.tensor_tensor(out=tt, in0=pR[:], in1=cn[:, 4, :], op=ALU.add), [bR, b_cn], [bt_])
                    V("dve", lambda: nc.vector.tensor_tensor(out=cmp_[:, 0, :], in0=tt, in1=oh1all[:, t, :], op=ALU.mult),
                      [bt_, b_oh1], [b_cmp])
                    V("dve", lambda: nc.vector.tensor_reduce(out=dest_f[:, t, 0:1], in_=cmp_[:, 0, :], axis=AX.X, op=ALU.add),
                      [b_cmp], [b_destf])
                    V("dve", lambda: nc.vector.tensor_tensor(out=cmp_[:, 1, :], in0=Mall[:, t, :], in1=oh1all[:, t, :], op=ALU.subtract),
                      [b_Mall, b_oh1], [b_cmp])
                    V("dve", lambda: nc.vector.tensor_tensor(out=cmp_[:, 1, :], in0=cmp_[:, 1, :], in1=tt, op=ALU.mult),
                      [bt_, b_cmp], [b_cmp])
                    V("dve", lambda: nc.vector.tensor_reduce(out=dest_f[:, t, 1:2], in_=cmp_[:, 1, :], axis=AX.X, op=ALU.add),
                      [b_cmp], [b_destf])
                V("dve", lambda: nc.vector.tensor_copy(out=dest_i[:], in_=dest_f[:, :, :].rearrange("p a b -> p (a b)")), [b_destf], [b_desti])
                dump("destf", dest_f[:, :, :].rearrange("p a b -> p (a b)"), [b_destf])
                dump("gates", gates[:, :, :].rearrange("p a b -> p (a b)"), [b_gates])
                hl = [sb(esr, [128, D], BF16, "hl") for _ in range(2)]; b_hl = [Buf(), Buf()]
                for t in range(32):
                    S.dma("sp", hl[t % 2][:], h2_d[t * 128:(t + 1) * 128, :], reads=[b_h2d], writes=[b_hl[t % 2]])
                    for kk in range(2):
                        S.dma_ind(xs_d[:, :], bass.IndirectOffsetOnAxis(ap=dest_i[:, 2 * t + kk:2 * t + kk + 1], axis=0), hl[t % 2][:, :], None,
                                  NROWS - 1, reads=[b_hl[t % 2], b_desti], writes=[b_xsd], group=True, semof=b_hl[t % 2])
                S.barrier()

            if STOP_AFTER != "route":
                with ExitStack() as esm:
                    w1b = [sb(esm, [128, 8, 512], BF16, "w1b") for _ in range(2)]; b_w1 = [Buf(), Buf()]
                    w3b = [sb(esm, [128, 8, 512], BF16, "w3b") for _ in range(2)]; b_w3 = [Buf(), Buf()]
                    w2b = [sb(esm, [128, 4, 1024], BF16, "w2b") for _ in range(2)]; b_w2 = [Buf(), Buf()]
                    xr = [sb(esm, [128, 2, D], BF16, "xr") for _ in range(2)]; b_xr = [Buf(), Buf()]
                    xT = [sb(esm, [128, 8, 256], BF16, "xT") for _ in range(2)]; b_xT = [[Buf(), Buf()], [Buf(), Buf()]]
                    sl = [sb(esm, [128, 256], F32, "sl") for _ in range(2)]; b_sl = [Buf(), Buf()]
                    hm = [sb(esm, [128, 4, 256], BF16, "hm") for _ in range(2)]; b_hm = [Buf(), Buf()]
                    yo = [sb(esm, [128, 2, D], BF16, "yo") for _ in range(2)]; b_yo = [[Buf(), Buf()], [Buf(), Buf()]]
                    psX = [ps(esm, [128, 1024], BF16, "psX") for _ in range(2)]; b_psX = [Buf(), Buf()]
                    psH = [ps(esm, [128, 512], F32, "psH") for _ in range(2)]; b_psH = [Buf(), Buf()]
                    psY = [ps(esm, [128, 512], F32, "psY") for _ in range(4)]; b_psY = [Buf() for _ in range(4)]
                    xcnt = [0]; hcnt = [0]; ycnt = [0]
                    for blk in range(NBLK):
                        i2 = blk % 2
                        off = bass.IndirectOffsetOnAxis(ap=blk_i[:, blk:blk + 1], axis=0)
                        S.dma_ind(w1b[i2][:, :, :].rearrange("p a b -> p (a b)"), None, w1_d[:, :], off, 32 * 128 - 1,
                                  reads=[b_blki], writes=[b_w1[i2]])
                        S.dma_ind(w3b[i2][:, :, :].rearrange("p a b -> p (a b)"), None, w3_d[:, :], off, 32 * 128 - 1,
                                  reads=[b_blki], writes=[b_w3[i2]])
                        S.dma_ind(w2b[i2][:, :, :].rearrange("p a b -> p (a b)"), None, w2_d[:, :], off, 32 * 128 - 1,
                                  reads=[b_blki], writes=[b_w2[i2]])
                        S.dma("sp", xr[i2][:], xs_d[blk * 256:(blk + 1) * 256, :].rearrange("(a p) d -> p a d", p=128),
                              reads=[b_xsd], writes=[b_xr[i2]])
                        for k in range(8):
                            xi = xcnt[0] % 2
                            if k % 4 == 0:
                                pX, bX = psX[(xcnt[0] // 4) % 2], b_psX[(xcnt[0] // 4) % 2]
                            for a in range(2):
                                pe_tr(pX[:, (k % 4) * 256 + a * 128:(k % 4) * 256 + (a + 1) * 128],
                                      xr[i2][:, a, k * 128:(k + 1) * 128], identb[:], [b_xr[i2], b_identb], [bX],
                                      sig=(k % 4 == 3 and a == 1))
                            xcnt[0] += 1
                            if k % 4 == 3:
                                dstx = xT[i2][:, k - 3:k + 1, :].rearrange("p a b -> p (a b)")
                                if (k // 4) % 2 == 0:
                                    act(dstx, pX[:], AF.Copy, [bX], [b_xT[i2][0]])
                                else:
                                    V("dve", lambda: nc.vector.tensor_copy(out=dstx, in_=pX[:]), [bX], [b_xT[i2][1]])
                        for c4 in range(4):
                            pH, bH = psH[hcnt[0] % 2], b_psH[hcnt[0] % 2]
                            si = hcnt[0] % 2
                            hcnt[0] += 1
                            for k in range(8):
                                pe_mm(pH[:, 0:256], w1b[i2][:, k, c4 * 128:(c4 + 1) * 128], xT[i2][:, k, :], k == 0, k == 7,
                                      [b_w1[i2], *b_xT[i2]], [bH], sig=False)
                            for k in range(8):
                                pe_mm(pH[:, 256:512], w3b[i2][:, k, c4 * 128:(c4 + 1) * 128], xT[i2][:, k, :], k == 0, k == 7,
                                      [b_w3[i2], *b_xT[i2]], [bH])
                            act(sl[si][:], pH[:, 0:256], AF.Silu, [bH], [b_sl[si]])
                            V("dve", lambda: nc.vector.tensor_tensor(out=hm[i2][:, c4, :], in0=pH[:, 256:512], in1=sl[si][:],
                                                                     op=ALU.mult), [bH, b_sl[si]], [b_hm[i2]])
                        for a in range(2):
                            for hf in range(2):
                                pY, bY = psY[ycnt[0] % 4], b_psY[ycnt[0] % 4]
                                ycnt[0] += 1
                                for k in range(4):
                                    pe_mm(pY[:], hm[i2][:, k, a * 128:(a + 1) * 128], w2b[i2][:, k, hf * 512:(hf + 1) * 512],
                                          k == 0, k == 3, [b_hm[i2], b_w2[i2]], [bY])
                                if hf == 0:
                                    act(yo[i2][:, a, 0:512], pY[:], AF.Copy, [bY], [b_yo[i2][0]])
                                else:
                                    V("dve", lambda: nc.vector.tensor_copy(out=yo[i2][:, a, 512:1024], in_=pY[:]), [bY], [b_yo[i2][1]])
                        S.dma("sp", ys_d[blk * 256:(blk + 1) * 256, :].rearrange("(a p) d -> p a d", p=128), yo[i2][:],
                              reads=b_yo[i2], writes=[b_ysd], group=True, semof=b_yo[i2][0])
                    S.barrier()

                with ExitStack() as esf:
                    GA2 = [sb(esf, [128, D], F32, "GA2") for _ in range(2)]; b_GA2 = [Buf(), Buf()]
                    gfb = sb(esf, [128, D], F32, "gfb"); b_gfb = Buf()
                    diag = [sb(esf, [128, 128], F32, "diagf") for _ in range(2)]; b_diag = [Buf(), Buf()]
                    psB = ps(esf, [128, D], F32, "psBf"); b_psB = Buf()
                    S.dma("sp", gfb[:], gfb_d, writes=[b_gfb])
                    for b in range(NB):
                        bcast_row(esf, 5, b, psB, b_psB, diag, b_diag)
                        V("dve", lambda: nc.vector.tensor_copy(out=GA2[b][:], in_=psB[:]), [b_psB], [b_GA2[b]])
                    y0 = [sb(esf, [128, D], BF16, "y0") for _ in range(2)]; b_y0 = [Buf(), Buf()]
                    y1 = [sb(esf, [128, D], BF16, "y1") for _ in range(2)]; b_y1 = [Buf(), Buf()]
                    x1l = [sb(esf, [128, D], F32, "x1l") for _ in range(2)]; b_x1l = [Buf(), Buf()]
                    mo = [sb(esf, [128, D], F32, "mo") for _ in range(2)]; b_mo = [Buf(), Buf()]
                    ot = [sb(esf, [128, D], F32, "ot") for _ in range(2)]; b_ot = [Buf(), Buf()]
                    junkf = sb(esf, [128, D], BF16, "junkf"); b_junkf = Buf()
                    stf = sb(esf, [128, 32, 3], F32, "stf"); b_stf = [Buf() for _ in range(32)]
                    for t in range(32):
                        i2 = t % 2
                        bb = t // 16
                        S.dma_ind(y0[i2][:, :], None, ys_d[:, :], bass.IndirectOffsetOnAxis(ap=dest_i[:, 2 * t:2 * t + 1], axis=0),
                                  NROWS - 1, reads=[b_ysd, b_desti], writes=[b_y0[i2]])
                        S.dma_ind(y1[i2][:, :], None, ys_d[:, :], bass.IndirectOffsetOnAxis(ap=dest_i[:, 2 * t + 1:2 * t + 2], axis=0),
                                  NROWS - 1, reads=[b_ysd, b_desti], writes=[b_y1[i2]])
                        S.dma("sp", x1l[i2][:], x1_d[t * 128:(t + 1) * 128, :], reads=[b_x1d], writes=[b_x1l[i2]])
                        V("dve", lambda: nc.vector.tensor_scalar(out=mo[i2][:], in0=y0[i2][:], scalar1=gates[:, t, 0:1], scalar2=None,
                                                                 op0=ALU.mult), [b_y0[i2], b_gates], [b_mo[i2]])
                        V("dve", lambda: nc.vector.scalar_tensor_tensor(out=mo[i2][:], in0=y1[i2][:], scalar=gates[:, t, 1:2],
                                                                        in1=mo[i2][:], op0=ALU.mult, op1=ALU.add),
                          [b_y1[i2], b_gates, b_mo[i2]], [b_mo[i2]])
                        if t == 0:
                            dump("moe0", mo[i2][:], [b_mo[i2]])
                        V("pool", lambda: nc.gpsimd.tensor_tensor(out=mo[i2][:], in0=mo[i2][:], in1=GA2[bb][:], op=ALU.mult),
                          [b_mo[i2], b_GA2[bb]], [b_mo[i2]])
                        V("dve", lambda: nc.vector.tensor_tensor(out=mo[i2][:], in0=mo[i2][:], in1=x1l[i2][:], op=ALU.add),
                          [b_mo[i2], b_x1l[i2]], [b_mo[i2]])
                        act(junkf[:], mo[i2][:], AF.Square, [b_mo[i2]], [b_junkf, b_stf[t]], accum_out=stf[:, t, 0:1])
                        act(stf[:, t, 1:2], stf[:, t, 0:1], AF.Sqrt, [b_stf[t], b_eps], [b_stf[t]], scale=1.0 / D, bias=epst[:, 0:1])
                        V("dve", lambda: nc.vector.reciprocal(out=stf[:, t, 2:3], in_=stf[:, t, 1:2]), [b_stf[t]], [b_stf[t]])
                        V("dve", lambda: nc.vector.scalar_tensor_tensor(out=ot[i2][:], in0=mo[i2][:], scalar=stf[:, t, 2:3],
                                                                        in1=gfb[:], op0=ALU.mult, op1=ALU.mult),
                          [b_mo[i2], b_stf[t], b_gfb], [b_ot[i2]])
                        S.dma("sp", out_d[bb, (t % 16) * 128:(t % 16 + 1) * 128, :], ot[i2][:], reads=[b_ot[i2]], writes=[b_outd],
                              group=True, semof=b_ot[i2])
        S.barrier()
        build_program.stats = dict(nops=dict(S.nops), nwaits=S.nwaits, nsem=S.nsem)
    return nc


def _fm(v):
    return np.ascontiguousarray(np.asarray(v, np.float32).reshape(-1, 128).T)


def _kp(w):
    K, N = w.shape
    return np.ascontiguousarray(w.reshape(K // 128, 128, N).transpose(1, 0, 2))


def _swap_cols():
    idx = np.arange(64)
    half = idx // 32
    within = idx % 32
    sw = np.where(within < 16, within + 16, within - 16)
    return half * 32 + sw


def _host_consts():
    nf = 16
    inv_freq = (10000.0 ** (-np.arange(nf, dtype=np.float32) / nf)).astype(np.float32)
    t = np.arange(L)
    row = (t // 64).astype(np.float32)
    col = (t % 64).astype(np.float32)
    cos = np.zeros((128, L), np.float32)
    sin = np.zeros((128, L), np.float32)
    for p in range(128):
        d = p % 64
        pos = row if d < 32 else col
        ang = (pos * inv_freq[(d % 32) % 16]).astype(np.float32)
        sign = -1.0 if (d % 32) < 16 else 1.0
        cos[p] = np.cos(ang)
        sin[p] = sign * np.sin(ang)
    cossin = np.concatenate([cos, sin], axis=1)
    ident = np.eye(128, dtype=np.float32)
    iota = np.arange(128, dtype=np.float32).reshape(128, 1)
    utri = np.triu(np.ones((128, 128), np.float32), k=1)
    return cossin, ident, iota, utri


def _bias_table(rpb):
    cq = np.arange(64)
    c_start = np.clip(cq - 8, 0, 48)
    band = (cq[None, :] >= c_start[:, None]) & (cq[None, :] < c_start[:, None] + 16)
    dc = np.clip(cq[None, :] - cq[:, None], -15, 15) + 15
    tab = np.full((64, 8, NTB, 64), -1e30, np.float32)
    for h in range(8):
        for dr in range(15):
            vals = rpb[h, dr][dc]
            tab[:, h, 1 + dr, :] = np.where(band, vals, np.float32(-1e30))
        tab[:, h, 18, :] = tab[:, h, 1 + 3, :]
        tab[:, h, 19, :] = tab[:, h, 1 + 10, :]
    return tab.reshape(64, 8 * NTB * 64)


def _prepare(inputs):
    f = lambda k: np.asarray(inputs[k], np.float32)
    w_in = f("w_in")[0]
    K_OFF, V_OFF, LX_OFF, Q_OFF, LG_OFF, GA_OFF, GB_OFF = 0, 512, 1024, 2048, 2560, 3584, 4608
    sw = _swap_cols()
    wqkv = []
    for hp in range(4):
        cols = []
        for base in (Q_OFF, K_OFF):
            plain = np.concatenate([base + (2 * hp + e) * 64 + np.arange(64) for e in range(2)])
            swp = np.concatenate([base + (2 * hp + e) * 64 + sw for e in range(2)])
            cols += [plain, swp]
        cols.append(V_OFF + hp * 128 + np.arange(128))
        wqkv.append(_kp(w_in[:, np.concatenate(cols)]).reshape(128, 8 * 640))
    wqkv = np.stack(wqkv)
    wlxlg = np.stack([_kp(w_in[:, np.concatenate([LX_OFF + n * 128 + np.arange(128), LG_OFF + n * 128 + np.arange(128)])]
                          ).reshape(128, 8 * 256) for n in range(8)])
    wgagb = np.stack([_kp(w_in[:, np.concatenate([GA_OFF + n * 128 + np.arange(128), GB_OFF + n * 128 + np.arange(128)])]
                          ).reshape(128, 8 * 256) for n in range(8)])
    wua = _kp(f("w_up_attn")[0])
    wul = _kp(f("w_up_lru")[0])
    wup = np.stack([np.concatenate([wua[:, :, n * 128:(n + 1) * 128], wul[:, :, n * 128:(n + 1) * 128]], axis=1
                                   ).reshape(128, 12 * 128) for n in range(8)])
    wout = _kp(f("w_out")[0]).reshape(128, 8 * D)
    wa = f("lru_wa")[0]
    wx = f("lru_wx")[0]
    lruw = np.stack([wa[0], wa[1], wx[0], wx[1]])
    lruw = np.ascontiguousarray(lruw.transpose(2, 0, 1, 3)).reshape(128, 4 * 8 * 128)
    vecs = np.concatenate([
        _fm(f("g_mix")[0]), _fm(f("g_ffn")[0]),
        np.concatenate([_fm(f("conv_w")[0][j]) for j in range(4)], axis=1),
        _fm(f("conv_b")[0]),
        np.concatenate([_fm(f("lru_ba")[0][d_]) for d_ in range(2)], axis=1),
        np.concatenate([_fm(f("lru_bx")[0][d_]) for d_ in range(2)], axis=1),
        np.concatenate([_fm(f("lru_lambda")[0][d_]) for d_ in range(2)], axis=1),
        _fm(f("b_mod")[0]),
    ], axis=1)
    assert vecs.shape == (128, NV)
    wmod = _kp(f("w_mod")[0])
    wr = _kp(np.concatenate([f("router_group_w")[0], f("router_expert_w")[0]], axis=1)).reshape(128, 8 * 36)
    brb = np.ascontiguousarray(np.broadcast_to(
        np.concatenate([f("router_group_b")[0], f("router_expert_b")[0]])[None, :], (128, 36)))
    gfb = np.ascontiguousarray(np.broadcast_to(f("g_final")[None, :], (128, D)))
    gffnb = np.ascontiguousarray(np.broadcast_to(f("g_ffn")[0][None, :], (128, D)))
    w1 = f("expert_w_gate")[0]
    w3 = f("expert_w_up")[0]
    w2 = f("expert_w_down")[0]
    w1h = np.ascontiguousarray(w1.reshape(32, 8, 128, 512).transpose(0, 2, 1, 3)).reshape(32 * 128, 8 * 512)
    w3h = np.ascontiguousarray(w3.reshape(32, 8, 128, 512).transpose(0, 2, 1, 3)).reshape(32 * 128, 8 * 512)
    w2h = np.ascontiguousarray(w2.reshape(32, 4, 128, 1024).transpose(0, 2, 1, 3)).reshape(32 * 128, 4 * 1024)
    cossin, ident, iota, utri = _host_consts()
    btab = _bias_table(f("rpb")[0])
    shared = dict(wmod=wmod, vecs=vecs, wqkv=wqkv, wlxlg=wlxlg, wgagb=wgagb, wup=wup, wout=wout, lruw=lruw,
                  cossin=cossin, btab=btab, wr=wr, brb=brb, gfb=gfb, gffnb=gffnb, w1h=w1h, w3h=w3h, w2h=w2h,
                  ident=ident, iota=iota, utri=utri)
    x = f("x")
    ctx = f("ctx")
    c = f("c")
    c_ctx = f("c_ctx")
    in_maps = []
    for core in range(NCORES):
        b0 = core * NB
        cs = np.stack([c[b0], c[b0 + 1], c_ctx], axis=-1)
        cs = np.ascontiguousarray(cs.reshape(8, 128, 3).transpose(1, 0, 2)).reshape(128, 24)
        m = dict(shared)
        m["xin"] = np.ascontiguousarray(x[b0:b0 + NB])
        m["ctxin"] = np.ascontiguousarray(ctx[b0:b0 + NB])
        m["cs"] = cs
        in_maps.append(m)
    return in_maps


def kernel(**inputs):
    in_maps = _prepare(inputs)
    nc = build_program()
    res = run_bass_kernel_spmd(nc, in_maps, core_ids=list(range(NCORES)))
    out = np.concatenate([np.asarray(r["out"], np.float32) for r in res.results], axis=0)
    return out
```

```python
import numpy as np
import concourse.bass as bass
import concourse.mybir as mybir
from concourse.bass_utils import run_bass_kernel_spmd
from contextlib import ExitStack

F32 = mybir.dt.float32
BF16 = mybir.dt.bfloat16
I32 = mybir.dt.int32
AF = mybir.ActivationFunctionType
ALU = mybir.AluOpType
AX = mybir.AxisListType

D = 1024
L = 2048
C = 256
NB = 2
NCORES = 8
LC = L + C
NTOK = NB * L
NBLK = 64
NROWS = NBLK * 256
EPS = 1e-6

V_GMIX, V_GFFN, V_CONVW, V_CONVB, V_BA, V_BX, V_LAM, V_BMOD, NV = 0, 8, 16, 48, 56, 72, 88, 104, 152
NTB = 21

DEBUG = {}
import os as _os
ATT_NHP = int(_os.environ.get("ATT_NHP", "4"))
ATT_NUNITS = int(_os.environ.get("ATT_NUNITS", "8"))
ATT_NROWS = int(_os.environ.get("ATT_NROWS", "8"))
ATT_NORM = int(_os.environ.get("ATT_NORM", "1"))
ATT_PARTS = int(_os.environ.get("ATT_PARTS", "31"))
STOP_AFTER = None


class Buf:
    __slots__ = ("name", "w", "wx", "r", "dsem", "dcnt", "grp")

    def __init__(self, name=""):
        self.name = name
        self.w = None
        self.wx = {}
        self.r = {}
        self.dsem = {}
        self.dcnt = {}
        self.grp = False


class Sync:
    ROLL = 30000

    def __init__(self, nc, es):
        self.nc = nc
        self.es = es
        self.eng = {"pe": nc.tensor, "act": nc.scalar, "dve": nc.vector, "pool": nc.gpsimd, "sp": nc.sync}
        self.sem = {}
        self.cnt = {}
        self.waited = {k: {} for k in self.eng}
        self.nsem = 0
        self.pe_sems = set()
        for k in self.eng:
            self._newsem(k)
        self.pend = []
        self.dbufs = []
        self.nops = {k: 0 for k in self.eng}
        self.nwaits = 0

    def _alloc(self, name):
        self.nsem += 1
        return self.es.enter_context(self.nc.semaphore(f"{name}{self.nsem}"))

    def _newsem(self, k):
        self.sem[k] = self._alloc("e" + k)
        self.cnt[k] = 0
        if k == "pe":
            self.pe_sems.add(id(self.sem[k]))

    def _wait(self, e, ev):
        semh, val = ev
        assert val is not None, "dependency on an unsignalled PE op"
        key = id(semh)
        if self.waited[e].get(key, 0) < val:
            self.eng[e].wait_ge(semh, val)
            self.waited[e][key] = val
            self.nwaits += 1

    def _dep1(self, e, ev, acc):
        if e == "pe" and (ev[1] is None or id(ev[0]) in self.pe_sems):
            return
        assert ev[1] is not None, "dependency on an unsignalled PE op"
        k = id(ev[0])
        if k not in acc or acc[k][1] < ev[1]:
            acc[k] = ev

    def _deps(self, e, reads, writes, group=False):
        acc = {}
        for b in reads:
            if b.w is not None:
                self._dep1(e, b.w, acc)
            for ev in b.wx.values():
                self._dep1(e, ev, acc)
        for b in writes:
            if not (group and b.grp):
                if b.w is not None:
                    self._dep1(e, b.w, acc)
                for ev in b.wx.values():
                    self._dep1(e, ev, acc)
            for ev in b.r.values():
                self._dep1(e, ev, acc)
        for ev in acc.values():
            self._wait(e, ev)

    def _mark(self, ev, reads, writes, key, group=False):
        for b in reads:
            b.r[key] = ev
        for b in writes:
            if group and b.grp:
                b.wx[key] = ev
            elif group:
                b.w = None
                b.wx = {key: ev}
            else:
                b.w = ev
                b.wx = {}
            b.grp = group
            b.r = {}

    def op(self, e, fn, reads=(), writes=(), sig=True):
        self._deps(e, reads, writes)
        ins = fn()
        self.nops[e] += 1
        if sig:
            if self.cnt[e] >= self.ROLL:
                self._newsem(e)
            self.cnt[e] += 1
            ins.then_inc(self.sem[e], 1)
            ev = [self.sem[e], self.cnt[e]]
            if e == "pe":
                for p in self.pend:
                    p[0] = self.sem[e]
                    p[1] = self.cnt[e]
                self.pend = []
        else:
            assert e == "pe"
            ev = [self.sem[e], None]
            self.pend.append(ev)
        self._mark(ev, reads, writes, e)
        return ins

    def _dma_common(self, q, issue, reads, writes, group, semof):
        d = semof if semof is not None else writes[0]
        c = "sw" if q == "pool" else "hw"
        if c not in d.dsem:
            d.dsem[c] = self._alloc("d")
            d.dcnt[c] = 0
            self.dbufs.append((d, c))
        self._deps(q, reads, writes, group=group)
        ins = issue()
        self.nops[q] += 1
        d.dcnt[c] += 16
        ins.then_inc(d.dsem[c], 16)
        ev = [d.dsem[c], d.dcnt[c]]
        self._mark(ev, reads, writes, id(d.dsem[c]), group=group)
        return ins

    def dma(self, q, out, in_, reads=(), writes=(), group=False, semof=None):
        return self._dma_common(q, lambda: self.eng[q].dma_start(out=out, in_=in_), reads, writes, group, semof)

    def dma_ind(self, out, out_off, in_, in_off, bound, reads=(), writes=(), group=False, semof=None):
        def issue():
            return self.nc.gpsimd.indirect_dma_start(out=out, out_offset=out_off, in_=in_, in_offset=in_off)
        return self._dma_common("pool", issue, reads, writes, group, semof)

    def barrier(self):
        assert not self.pend
        evs = [[self.sem[k], self.cnt[k]] for k in self.eng if k != "sp" and self.cnt[k] > 0]
        evs += [[b.dsem[c], b.dcnt[c]] for (b, c) in self.dbufs if b.dcnt[c] > 0]
        for ev in evs:
            self._wait("sp", ev)
        if self.cnt["sp"] >= self.ROLL:
            self._newsem("sp")
        self.cnt["sp"] += 1
        self.nc.sync.nop().then_inc(self.sem["sp"], 1)
        ev = [self.sem["sp"], self.cnt["sp"]]
        for k in self.eng:
            if k != "sp":
                self._wait(k, ev)


def build_program():
    nc = bass.Bass("TRN2", target_bir_lowering=False)

    def din(name, shape, dt=F32):
        return nc.dram_tensor(name, list(shape), dt, kind="ExternalInput").ap()

    xin = din("xin", [NB, L, D])
    ctxin = din("ctxin", [NB, C, D])
    cs_d = din("cs", [128, 24])
    wmod_d = din("wmod", [128, 8, 6 * D])
    vecs_d = din("vecs", [128, NV])
    wqkv_d = din("wqkv", [4, 128, 8 * 640])
    wlxlg_d = din("wlxlg", [8, 128, 8 * 256])
    wgagb_d = din("wgagb", [8, 128, 8 * 256])
    wup_d = din("wup", [8, 128, 12 * 128])
    wout_d = din("wout", [128, 8 * D])
    lruw_d = din("lruw", [128, 4 * 8 * 128])
    cossin_d = din("cossin", [128, 2 * L])
    btab_d = din("btab", [64, 8 * NTB * 64])
    wr_d = din("wr", [128, 8 * 36])
    brb_d = din("brb", [128, 36])
    gfb_d = din("gfb", [128, D])
    gffnb_d = din("gffnb", [128, D])
    w1_d = din("w1h", [32 * 128, 8 * 512])
    w3_d = din("w3h", [32 * 128, 8 * 512])
    w2_d = din("w2h", [32 * 128, 4 * 1024])
    ident_d = din("ident", [128, 128])
    iota_d = din("iota", [128, 1])
    utri_d = din("utri", [128, 128])
    out_d = nc.dram_tensor("out", [NB, L, D], F32, kind="ExternalOutput").ap()
    x1_d = nc.dram_tensor("x1s", [NTOK, D], F32, kind="Internal").ap()
    h2_d = nc.dram_tensor("h2s", [NTOK, D], BF16, kind="Internal").ap()
    xs_d = nc.dram_tensor("xss", [NROWS, D], BF16, kind="Internal").ap()
    ys_d = nc.dram_tensor("yss", [NROWS, D], BF16, kind="Internal").ap()
    dbg_d = {}
    for name, (shape, dt) in DEBUG.items():
        dbg_d[name] = nc.dram_tensor("dbg_" + name, list(shape), dt, kind="ExternalOutput").ap()

    with ExitStack() as es:
        S = Sync(nc, es)
        uid = [0]

        def sb(es_, shape, dt, name="t"):
            uid[0] += 1
            return es_.enter_context(nc.sbuf_tensor(f"{name}{uid[0]}", list(shape), dt))

        def ps(es_, shape, dt, name="p"):
            uid[0] += 1
            return es_.enter_context(nc.psum_tensor(f"{name}{uid[0]}", list(shape), dt))

        def pe_mm(out, lhsT, rhs, start, stop, reads, writes, sig=None):
            if sig is None:
                sig = stop
            return S.op("pe", lambda: nc.tensor.matmul(out, lhsT, rhs, start=start, stop=stop),
                        reads, writes, sig)

        def pe_tr(out, in_, ident, reads, writes, sig):
            return S.op("pe", lambda: nc.tensor.transpose(out, in_, ident), reads, writes, sig)

        def act(out, in_, func, reads, writes, **kw):
            return S.op("act", lambda: nc.scalar.activation(out=out, in_=in_, func=func, **kw), reads, writes)

        def V(e, fn, reads, writes):
            return S.op(e, fn, reads, writes)

        dbg_buf = Buf("dbg")

        def dump(name, ap, reads):
            if name in dbg_d:
                S.dma("sp", dbg_d[name], ap, reads=reads, writes=[dbg_buf], group=True, semof=Buf("dump_" + name))

        identf = sb(es, [128, 128], F32, "identf"); b_identf = Buf()
        identb = sb(es, [128, 128], BF16, "identb"); b_identb = Buf()
        onesf = sb(es, [128, 128], F32, "onesf"); b_onesf = Buf()
        onesb = sb(es, [128, 128], BF16, "onesb"); b_onesb = Buf()
        utri = sb(es, [128, 128], BF16, "utri"); b_utri = Buf()
        iota = sb(es, [128, 1], F32, "iota"); b_iota = Buf()
        epst = sb(es, [128, 1], F32, "eps"); b_eps = Buf()
        vecs = sb(es, [128, NV], F32, "vecs"); b_vecs = Buf()
        modfm = sb(es, [128, 48, 3], F32, "modfm"); b_modfm = Buf()
        A1 = sb(es, [128, 8, 3], F32, "A1"); b_A1 = Buf()
        lrup = sb(es, [128, 4, 16], F32, "lrup"); b_lrup = Buf()
        S.dma("sp", identf[:], ident_d, writes=[b_identf])
        S.dma("pool", identb[:], ident_d, writes=[b_identb])
        S.dma("pool", utri[:], utri_d, writes=[b_utri])
        S.dma("sp", iota[:], iota_d, writes=[b_iota])
        S.dma("sp", vecs[:], vecs_d, writes=[b_vecs])
        V("dve", lambda: nc.vector.memset(onesf[:], 1.0), [], [b_onesf])
        V("dve", lambda: nc.vector.memset(onesb[:], 1.0), [], [b_onesb])
        V("dve", lambda: nc.vector.memset(epst[:], EPS), [], [b_eps])

        with ExitStack() as es0:
            csb = sb(es0, [128, 24], F32, "cs"); b_cs = Buf()
            scs = sb(es0, [128, 24], F32, "scs"); b_scs = Buf()
            wm = [sb(es0, [128, 8, 512], F32, "wm") for _ in range(2)]
            b_wm = [Buf(), Buf()]
            psmod = ps(es0, [128, 144], F32, "psmod"); b_psmod = Buf()
            S.dma("sp", csb[:], cs_d, writes=[b_cs])
            act(scs[:], csb[:], AF.Silu, [b_cs], [b_scs])
            for cb in range(12):
                w = wm[cb % 2]; bw = b_wm[cb % 2]
                S.dma("sp", w[:], wmod_d[:, :, cb * 512:(cb + 1) * 512], writes=[bw])
                for cc in range(4):
                    col = cb * 4 + cc
                    for k in range(8):
                        pe_mm(psmod[:, col * 3:(col + 1) * 3], w[:, k, cc * 128:(cc + 1) * 128],
                              scs[:, k * 3:(k + 1) * 3], k == 0, k == 7, [bw, b_scs], [b_psmod],
                              sig=(k == 7 and cc == 3))
            V("dve", lambda: nc.vector.tensor_tensor(
                out=modfm[:], in0=psmod[:, :].rearrange("p (a b) -> p a b", b=3),
                in1=vecs[:, V_BMOD:V_BMOD + 48].unsqueeze(2).to_broadcast([128, 48, 3]), op=ALU.add),
              [b_psmod, b_vecs], [b_modfm])
            V("dve", lambda: nc.vector.scalar_tensor_tensor(
                out=A1[:], in0=modfm[:, 8:16, :], scalar=1.0,
                in1=vecs[:, V_GMIX:V_GMIX + 8].unsqueeze(2).to_broadcast([128, 8, 3]),
                op0=ALU.add, op1=ALU.mult), [b_modfm, b_vecs], [b_A1])
            act(lrup[:, 2, :], vecs[:, V_LAM:V_LAM + 16], AF.Exp, [b_vecs], [b_lrup], scale=-1.0)
            act(lrup[:, 3, :], lrup[:, 2, :], AF.Ln, [b_lrup], [b_lrup], bias=1.0)
            V("dve", lambda: nc.vector.tensor_scalar(out=lrup[:, 0, :], in0=lrup[:, 3, :], scalar1=-8.0, scalar2=None,
                                                     op0=ALU.mult), [b_lrup], [b_lrup])
            V("dve", lambda: nc.vector.tensor_scalar(out=lrup[:, 1, :], in0=lrup[:, 3, :], scalar1=-16.0, scalar2=None,
                                                     op0=ALU.mult), [b_lrup], [b_lrup])
            dump("modfm", modfm[:, :, :].rearrange("p a b -> p (a b)"), [b_modfm])
            S.barrier()

        def bcast_row(es_, v, j, psb, b_psb, diag, b_diag):
            for k in range(8):
                V("dve", lambda: nc.vector.tensor_scalar(out=diag[k % 2][:], in0=identf[:],
                                                         scalar1=modfm[:, v * 8 + k, j:j + 1], scalar2=None,
                                                         op0=ALU.mult), [b_identf, b_modfm], [b_diag[k % 2]])
                pe_mm(psb[:, k * 128:(k + 1) * 128], onesf[:], diag[k % 2][:], True, True,
                      [b_onesf, b_diag[k % 2]], [b_psb], sig=True)

        Mall = sb(es, [128, 32, 32], BF16, "Mall"); b_Mall = Buf()
        oh1all = sb(es, [128, 32, 32], BF16, "oh1"); b_oh1 = Buf()
        gates = sb(es, [128, 32, 2], F32, "gates"); b_gates = Buf()
        dest_f = sb(es, [128, 32, 2], F32, "destf"); b_destf = Buf()
        dest_i = sb(es, [128, 64], I32, "desti"); b_desti = Buf()
        b_x1d = Buf("x1d"); b_h2d = Buf("h2d"); b_xsd = Buf("xsd"); b_ysd = Buf("ysd"); b_outd = Buf("outd")
        zt = sb(es, [128, 2, D], BF16, "zt"); b_zt = Buf()
        V("dve", lambda: nc.vector.memset(zt[:], 0.0), [], [b_zt])
        for blk in range(NBLK):
            S.dma("sp", xs_d[blk * 256:(blk + 1) * 256, :].rearrange("(a p) d -> p a d", p=128), zt[:],
                  reads=[b_zt], writes=[b_xsd], group=True, semof=b_zt)

        for b in range(NB):
            with ExitStack() as esb:
                hT = sb(esb, [128, 8, LC], BF16, "hT")
                b_hT = [[Buf(f"hT{i}a"), Buf(f"hT{i}b")] for i in range(5)]
                oatt = sb(esb, [128, 4, L], BF16, "oatt")
                b_oatt = [[Buf() for _ in range(4)] for _ in range(4)]

                with ExitStack() as es1:
                    xt = [sb(es1, [128, D], F32, "xt") for _ in range(3)]
                    b_xt = [Buf() for _ in range(3)]
                    junk = sb(es1, [128, D], BF16, "junk"); b_junk = Buf()
                    xn = [sb(es1, [128, D], BF16, "xn") for _ in range(8)]
                    b_xn = [Buf() for _ in range(8)]
                    st = sb(es1, [128, 18, 3], F32, "st"); b_st = [Buf() for _ in range(18)]
                    pst = [ps(es1, [128, 512], BF16, "pst") for _ in range(4)]
                    b_pst = [Buf() for _ in range(4)]
                    ti = 0
                    for grp in range(5):
                        ntile = 4 if grp < 4 else 2
                        jmod = b if grp < 4 else 2
                        for i in range(ntile):
                            t = grp * 4 + i
                            xb_, bx_ = xt[ti % 3], b_xt[ti % 3]
                            src = xin[b, t * 128:(t + 1) * 128, :] if grp < 4 else ctxin[b, i * 128:(i + 1) * 128, :]
                            S.dma("sp", xb_[:], src, writes=[bx_])
                            act(junk[:], xb_[:], AF.Square, [bx_], [b_junk, b_st[t]], accum_out=st[:, t, 0:1])
                            act(st[:, t, 1:2], st[:, t, 0:1], AF.Sqrt, [b_st[t], b_eps], [b_st[t]],
                                scale=1.0 / D, bias=epst[:, 0:1])
                            V("dve", lambda: nc.vector.reciprocal(out=st[:, t, 2:3], in_=st[:, t, 1:2]),
                              [b_st[t]], [b_st[t]])
                            xi = (grp % 2) * 4 + i
                            act(xn[xi][:], xb_[:], AF.Copy, [bx_, b_st[t]], [b_xn[xi]], scale=st[:, t, 2:3])
                            ti += 1
                        for k in range(8):
                            pp, bp = pst[k % 4], b_pst[k % 4]
                            for i in range(ntile):
                                xi = (grp % 2) * 4 + i
                                pe_tr(pp[:, i * 128:(i + 1) * 128], xn[xi][:, k * 128:(k + 1) * 128], identb[:],
                                      [b_xn[xi], b_identb], [bp], sig=(i == ntile - 1))
                            n = ntile * 128
                            dst = hT[:, k, grp * 512:grp * 512 + n]
                            if k % 2 == 0:
                                V("dve", lambda: nc.vector.tensor_scalar(
                                    out=dst, in0=pp[:, 0:n], scalar1=A1[:, k, jmod:jmod + 1],
                                    scalar2=modfm[:, k, jmod:jmod + 1], op0=ALU.mult, op1=ALU.add),
                                  [bp, b_A1, b_modfm], [b_hT[grp][0]])
                            else:
                                act(dst, pp[:, 0:n], AF.Identity, [bp, b_A1, b_modfm], [b_hT[grp][1]],
                                    scale=A1[:, k, jmod:jmod + 1], bias=modfm[:, k, jmod:jmod + 1])
                    if b == 0:
                        dump("hT", hT[:, :, :].rearrange("p a b -> p (a b)"), [x for l_ in b_hT for x in l_])
                    S.barrier()
                if STOP_AFTER == "s1":
                    break

                with ExitStack() as es2:
                    cs_t = sb(es2, [128, 2 * L], F32, "cossin"); b_cst = Buf()
                    btab = sb(es2, [128, 8, NTB * 64], BF16, "btab"); b_btab = Buf()
                    S.dma("sp", cs_t[:], cossin_d, writes=[b_cst])
                    S.dma("pool", btab[0:64, :, :].rearrange("p a b -> p (a b)"), btab_d, writes=[b_btab], group=True)
                    S.dma("pool", btab[64:128, :, :].rearrange("p a b -> p (a b)"), btab_d, writes=[b_btab], group=True)
                    wq = [sb(es2, [128, 8, 640], BF16, "wq") for _ in range(1)]; b_wq = [Buf()]
                    Qr = [sb(es2, [128, L], BF16, "Qr") for _ in range(1)]
                    Qp = [sb(es2, [128, L], BF16, "Qp") for _ in range(1)]
                    Kr = [sb(es2, [128, L], BF16, "Kr") for _ in range(1)]
                    Kc = [sb(es2, [128, C], BF16, "Kc") for _ in range(1)]
                    Vx = [sb(es2, [128, 18, 192], BF16, "Vx") for _ in range(1)]
                    b_Q = [[Buf() for _ in range(4)] for _ in range(1)]
                    b_Qp = [[Buf() for _ in range(4)] for _ in range(1)]
                    b_K = [Buf()]
                    b_Kc = [Buf()]
                    b_V = [[Buf() for _ in range(5)]]
                    t1 = [sb(es2, [128, 512], F32, "t1") for _ in range(2)]; b_t1 = [Buf(), Buf()]
                    t2 = [sb(es2, [128, 512], F32, "t2") for _ in range(2)]; b_t2 = [Buf(), Buf()]
                    PTc = [sb(es2, [128, 512], BF16, "PTc") for _ in range(2)]; b_PTc = [Buf(), Buf()]
                    PT = [sb(es2, [128, 320], BF16, "PT") for _ in range(3)]; b_PT = [Buf() for _ in range(3)]
                    rc = [sb(es2, [128, 512], F32, "rc") for _ in range(2)]; b_rc = [Buf(), Buf()]
                    psP = [ps(es2, [128, 512], F32, "psP") for _ in range(2)]; b_psP = [Buf(), Buf()]
                    psSc = [ps(es2, [128, 512], F32, "psSc") for _ in range(2)]; b_psSc = [Buf(), Buf()]
                    psS = [ps(es2, [128, 512], F32, "psS") for _ in range(2)]; b_psS = [Buf(), Buf()]
                    psO = [ps(es2, [128, 512], F32, "psO") for _ in range(2)]; b_psO = [Buf(), Buf()]
                    V("dve", lambda: nc.vector.memset(Vx[0][:, :, 64:128], 1.0), [], b_V[0])
                    pcnt = [0]

                    def nextP():
                        i = pcnt[0] % 2
                        pcnt[0] += 1
                        return psP[i], b_psP[i]

                    cnt_t = [0]
                    for hp in range(ATT_NHP):
                        par = 0
                        w = wq[par]; bw = b_wq[par]
                        S.dma("pool", w[:, :, :].rearrange("p a b -> p (a b)"), wqkv_d[hp], writes=[bw])
                        for blk in range(4 if ATT_PARTS & 1 else 0):
                            tok = slice(blk * 512, (blk + 1) * 512)
                            for which in range(2):
                                c0 = which * 256
                                pa, bpa = nextP()
                                for k in range(8):
                                    pe_mm(pa[:], w[:, k, c0:c0 + 128], hT[:, k, tok], k == 0, k == 7,
                                          [bw, *b_hT[blk]], [bpa])
                                pb, bpb = nextP()
                                for k in range(8):
                                    pe_mm(pb[:], w[:, k, c0 + 128:c0 + 256], hT[:, k, tok], k == 0, k == 7,
                                          [bw, *b_hT[blk]], [bpb])
                                ii = cnt_t[0] % 2
                                cnt_t[0] += 1
                                sc_ = 0.125 if which == 0 else 1.0
                                dstb = b_Q[par][blk] if which == 0 else b_K[par]
                                dst = (Qr if which == 0 else Kr)[par][:, tok]
                                V("dve", lambda: nc.vector.scalar_tensor_tensor(
                                    out=t1[ii][:], in0=pa[:], scalar=sc_, in1=cs_t[:, tok], op0=ALU.mult, op1=ALU.mult),
                                  [bpa, b_cst], [b_t1[ii]])
                                if which == 0 and (ATT_PARTS & 16):
                                    V("dve", lambda: nc.vector.tensor_scalar(out=Qp[par][:, tok], in0=pa[:], scalar1=0.125, scalar2=None,
                                                                             op0=ALU.mult), [bpa], [b_Qp[par][blk]])
                                V("dve", lambda: nc.vector.scalar_tensor_tensor(
                                    out=t2[ii][:], in0=pb[:], scalar=sc_, in1=cs_t[:, L + blk * 512:L + (blk + 1) * 512],
                                    op0=ALU.mult, op1=ALU.mult), [bpb, b_cst], [b_t2[ii]])
                                V("pool" if ATT_PARTS & 8 else "dve", lambda: (nc.gpsimd if ATT_PARTS & 8 else nc.vector).tensor_tensor(out=dst, in0=t1[ii][:], in1=t2[ii][:], op=ALU.add),
                                  [b_t1[ii], b_t2[ii]], [dstb])
                        if ATT_PARTS & 2:
                            pa, bpa = nextP()
                            for k in range(8):
                                pe_mm(pa[:, 0:C], w[:, k, 256:384], hT[:, k, L:LC], k == 0, k == 7, [bw, *b_hT[4]], [bpa])
                            act(Kc[par][:], pa[:, 0:C], AF.Copy, [bpa], [b_Kc[par]])
                        for g4 in range(5 if ATT_PARTS & 4 else 0):
                            nch = 4 if g4 < 4 else 2
                            pa, bpa = nextP()
                            for i in range(nch):
                                ch = g4 * 4 + i
                                for k in range(8):
                                    pe_mm(pa[:, i * 128:(i + 1) * 128], hT[:, k, ch * 128:(ch + 1) * 128],
                                          w[:, k, 512:640], k == 0, k == 7, [bw, *b_hT[g4]], [bpa],
                                          sig=(k == 7 and i == nch - 1))
                            src = pa[:, 0:nch * 128].rearrange("p (c a d) -> p c a d", a=2, d=64)
                            dstv = Vx[par][:, g4 * 4:g4 * 4 + nch, :].rearrange("p c (a d) -> p c a d", d=64)[:, :, 0::2, :]
                            if g4 % 2 == 0:
                                act(dstv, src, AF.Copy, [bpa], [b_V[par][g4]])
                            else:
                                V("dve", lambda: nc.vector.tensor_copy(out=dstv, in_=src), [bpa], [b_V[par][g4]])

                        if b == 0 and hp == 0:
                            dump("Qp", Qp[0][:], b_Qp[0])
                            dump("Qr", Qr[0][:], b_Q[0])
                            dump("Kr", Kr[0][:], b_K)
                            dump("Kc", Kc[0][:], b_Kc)
                            dump("Vx", Vx[0][:, :, :].rearrange("p a b -> p (a b)"), b_V[0])
                        units = []
                        for e in range(2):
                            for qb in range(4):
                                units.append((e, qb))
                        ucnt = [0]
                        for (e, qb) in units[int(_os.environ.get("ATT_USTART", "0")):][:ATT_NUNITS]:
                            h = hp * 2 + e
                            pr = slice(64 * e, 64 * e + 64)
                            qs = slice(qb * 512, (qb + 1) * 512)
                            vcols = slice(0, 128) if e == 0 else slice(64, 192)
                            oi = ucnt[0] % 2
                            ucnt[0] += 1
                            pO, bO = psO[oi], b_psO[oi]
                            for c in range(2):
                                pS, bS = psSc[c], b_psSc[c]
                                pe_mm(pS[:], Kc[par][pr, c * 128:(c + 1) * 128], Qp[par][pr, qs], True, True,
                                      [b_Kc[par], b_Qp[par][qb]], [bS])
                                act(PTc[c][:], pS[:], AF.Exp, [bS], [b_PTc[c]])
                            if b == 0 and hp == 0 and e == 0 and qb == 0:
                                dump("PTc", PTc[0][:], [b_PTc[0]])
                            for c in range(2):
                                pe_mm(pO[:], Vx[par][:, 16 + c, vcols], PTc[c][:], c == 0, False,
                                      [b_V[par][4], b_PTc[c]], [bO], sig=(c == 1))
                            rows = list(range(qb * 8, qb * 8 + ATT_NROWS))
                            plan = []
                            for r in rows:
                                rs = min(max(r - 4, 0), 24)
                                dr0 = rs - r + 7
                                chunks = []
                                if rs % 2 == 0:
                                    for j in range(4):
                                        chunks.append(((rs + 2 * j) // 2, 1 + dr0 + 2 * j))
                                else:
                                    assert dr0 == 3
                                    c0 = (rs - 1) // 2
                                    chunks.append((c0, 17))
                                    for j in range(1, 4):
                                        chunks.append((c0 + j, dr0 + 2 * j))
                                    chunks.append((c0 + 4, 19))
                                plan.append((r, chunks))

                            def emit_qk(idx):
                                r, chunks = plan[idx]
                                si = idx % 2
                                pS, bS = psS[si], b_psS[si]
                                qcol = slice(r * 64, (r + 1) * 64)
                                for j, (kc, blk0) in enumerate(chunks):
                                    o = pS[:, j * 64:(j + 1) * 64]
                                    pe_mm(o, Kr[par][pr, kc * 128:(kc + 1) * 128], Qr[par][pr, qcol], True, False,
                                          [b_K[par], b_Q[par][qb]], [bS], sig=False)
                                    lt = btab[pr, h, blk0 * 64:(blk0 + 2) * 64]
                                    pe_mm(o, lt, identb[pr, pr], False, True, [b_btab, b_identb], [bS],
                                          sig=(j == len(chunks) - 1))
                                n = len(chunks) * 64
                                pi = idx % 3
                                act(PT[pi][:, 0:n], pS[:, 0:n], AF.Exp, [bS], [b_PT[pi]])

                            def emit_pv(idx):
                                r, chunks = plan[idx]
                                pi = idx % 3
                                rr = r - qb * 8
                                for j, (kc, _) in enumerate(chunks):
                                    last = (j == len(chunks) - 1)
                                    pe_mm(pO[:, rr * 64:(rr + 1) * 64], Vx[par][:, kc, vcols], PT[pi][:, j * 64:(j + 1) * 64],
                                          False, last and idx == len(plan) - 1, [b_V[par][kc // 4], b_PT[pi]], [bO], sig=last)

                            if plan:
                                emit_qk(0)
                            for idx in range(len(plan)):
                                if idx + 1 < len(plan):
                                    emit_qk(idx + 1)
                                emit_pv(idx)
                            dn = slice(64, 128) if e == 0 else slice(0, 64)
                            if not ATT_NORM:
                                continue
                            V("dve", lambda: nc.vector.reciprocal(out=rc[oi][pr, :], in_=pO[dn, :]), [bO], [b_rc[oi]])
                            V("dve", lambda: nc.vector.tensor_tensor(out=oatt[pr, hp, qs], in0=pO[pr, :], in1=rc[oi][pr, :],
                                                                     op=ALU.mult), [bO, b_rc[oi]], [b_oatt[hp][qb]])
                    if b == 0:
                        dump("oatt", oatt[:, :, :].rearrange("p a b -> p (a b)"), [x for l_ in b_oatt for x in l_])
                    S.barrier()
                if STOP_AFTER == "s2":
                    break

                olru = sb(esb, [128, 8, L], BF16, "olru")
                b_olru = [Buf() for _ in range(8)]
                with ExitStack() as es3:
                    TL = LC + 3
                    wl = [sb(es3, [128, 8, 256], BF16, "wl") for _ in range(2)]; b_wl = [Buf(), Buf()]
                    lw = sb(es3, [128, 4, 8, 128], BF16, "lw"); b_lw = Buf()
                    S.dma("pool", lw[:, :, :, :].rearrange("p a b c -> p (a b c)"), lruw_d, writes=[b_lw])
                    LXp = sb(es3, [128, TL + 3], F32, "LXp"); b_LXp = Buf()
                    xc = sb(es3, [128, TL], F32, "xc"); b_xc = Buf()
                    xcb = sb(es3, [128, TL], BF16, "xcb"); b_xcb = Buf()
                    av = sb(es3, [128, TL], F32, "av"); b_av = Buf()
                    wv = sb(es3, [128, TL], F32, "wv"); b_wv = Buf()
                    iv = sb(es3, [128, TL], F32, "iv"); b_iv = Buf()
                    hv = [sb(es3, [128, TL], F32, "hv") for _ in range(2)]; b_hv = [Buf(), Buf()]
                    gl = sb(es3, [128, L], BF16, "gl"); b_gl = Buf()
                    psA = [ps(es3, [128, 512], F32, "psA") for _ in range(4)]; b_psA = [Buf() for _ in range(4)]
                    pacnt = [0]

                    def nextA():
                        i = pacnt[0] % 4
                        pacnt[0] += 1
                        return psA[i], b_psA[i]

                    V("dve", lambda: nc.vector.memset(LXp[:], 0.0), [], [b_LXp])
                    for n in range(8):
                        w = wl[n % 2]; bw = b_wl[n % 2]
                        S.dma("pool", w[:, :, :].rearrange("p a b -> p (a b)"), wlxlg_d[n], writes=[bw])
                        for blk in range(5):
                            nt = 512 if blk < 4 else C
                            tok = slice(blk * 512, blk * 512 + nt)
                            pa, bpa = nextA()
                            for k in range(8):
                                pe_mm(pa[:, 0:nt], w[:, k, 0:128], hT[:, k, tok], k == 0, k == 7, [bw, *b_hT[blk]], [bpa])
                            d0 = 261 + blk * 512 if blk < 4 else 2
                            act(LXp[:, d0:d0 + nt], pa[:, 0:nt], AF.Copy, [bpa], [b_LXp])
                        cw = lambda j: vecs[:, V_CONVW + j * 8 + n:V_CONVW + j * 8 + n + 1]
                        V("dve", lambda: nc.vector.tensor_scalar(out=xc[:], in0=LXp[:, 0:TL], scalar1=cw(0),
                                                                 scalar2=vecs[:, V_CONVB + n:V_CONVB + n + 1],
                                                                 op0=ALU.mult, op1=ALU.add), [b_LXp, b_vecs], [b_xc])
                        for j in range(1, 4):
                            V("dve", lambda: nc.vector.scalar_tensor_tensor(out=xc[:], in0=LXp[:, j:j + TL], scalar=cw(j),
                                                                            in1=xc[:], op0=ALU.mult, op1=ALU.add),
                              [b_LXp, b_vecs, b_xc], [b_xc])
                        V("pool", lambda: nc.gpsimd.tensor_copy(out=xcb[:], in_=xc[:]), [b_xc], [b_xcb])
                        if b == 0 and n == 0:
                            dump("xc0", xc[:], [b_xc])
                        for dr in range(2):
                            di = dr * 8 + n
                            for blk in range(5):
                                nt = 512 if blk < 4 else TL - 2048
                                tok = slice(blk * 512, blk * 512 + nt)
                                pr_, bpr = nextA()
                                pe_mm(pr_[:, 0:nt], lw[:, dr, n, :], xcb[:, tok], True, True, [b_lw, b_xcb], [bpr])
                                pi_, bpi = nextA()
                                pe_mm(pi_[:, 0:nt], lw[:, 2 + dr, n, :], xcb[:, tok], True, True, [b_lw, b_xcb], [bpi])
                                act(av[:, tok], pr_[:, 0:nt], AF.Sigmoid, [bpr, b_vecs], [b_av],
                                    bias=vecs[:, V_BA + di:V_BA + di + 1])
                                act(iv[:, tok], pi_[:, 0:nt], AF.Sigmoid, [bpi, b_vecs], [b_iv],
                                    bias=vecs[:, V_BX + di:V_BX + di + 1])
                            act(wv[:], av[:], AF.Exp, [b_av, b_lrup], [b_wv], scale=lrup[:, 1, di:di + 1])
                            act(av[:], av[:], AF.Exp, [b_av, b_lrup], [b_av], scale=lrup[:, 0, di:di + 1])
                            act(wv[:], wv[:], AF.Sqrt, [b_wv], [b_wv], scale=-1.0, bias=1.0)
                            V("pool", lambda: nc.gpsimd.tensor_tensor(out=iv[:], in0=iv[:], in1=xc[:], op=ALU.mult),
                              [b_iv, b_xc], [b_iv])
                            V("dve", lambda: nc.vector.tensor_tensor(out=wv[:], in0=iv[:], in1=wv[:], op=ALU.mult),
                              [b_iv, b_wv], [b_wv])
                            hh = hv[dr]; bh = b_hv[dr]
                            if dr == 0:
                                V("dve", lambda: nc.vector.tensor_tensor_scan(
                                    out=hh[:, 0:C], data0=av[:, 0:C], data1=wv[:, 0:C], initial=0.0,
                                    op0=ALU.mult, op1=ALU.add), [b_av, b_wv], [bh])
                                V("dve", lambda: nc.vector.tensor_tensor_scan(
                                    out=hh[:, C + 3:TL], data0=av[:, C + 3:TL], data1=wv[:, C + 3:TL],
                                    initial=hh[:, C - 1:C], op0=ALU.mult, op1=ALU.add), [b_av, b_wv, bh], [bh])
                            else:
                                V("dve", lambda: nc.vector.tensor_tensor_scan(
                                    out=hh[:, C - 1::-1], data0=av[:, C - 1::-1], data1=wv[:, C - 1::-1], initial=0.0,
                                    op0=ALU.mult, op1=ALU.add), [b_av, b_wv], [bh])
                                V("dve", lambda: nc.vector.tensor_tensor_scan(
                                    out=hh[:, TL - 1:C + 2:-1], data0=av[:, TL - 1:C + 2:-1], data1=wv[:, TL - 1:C + 2:-1],
                                    initial=hh[:, 0:1], op0=ALU.mult, op1=ALU.add), [b_av, b_wv, bh], [bh])
                        for blk in range(4):
                            tok = slice(blk * 512, (blk + 1) * 512)
                            pa, bpa = nextA()
                            for k in range(8):
                                pe_mm(pa[:], w[:, k, 128:256], hT[:, k, tok], k == 0, k == 7, [bw, *b_hT[blk]], [bpa])
                            act(gl[:, tok], pa[:], AF.Gelu_apprx_tanh, [bpa], [b_gl])
                        V("pool", lambda: nc.gpsimd.tensor_tensor(out=hv[0][:, C + 3:TL], in0=hv[0][:, C + 3:TL],
                                                                  in1=hv[1][:, C + 3:TL], op=ALU.add),
                          [b_hv[0], b_hv[1]], [b_hv[0]])
                        if b == 0 and n == 0:
                            dump("hsum0", hv[0][:, C + 3:TL], [b_hv[0]])
                        V("dve", lambda: nc.vector.tensor_tensor(out=olru[:, n, :], in0=hv[0][:, C + 3:TL], in1=gl[:],
                                                                 op=ALU.mult), [b_hv[0], b_gl], [b_olru[n]])
                    if b == 0:
                        dump("olru", olru[:, :, :].rearrange("p a b -> p (a b)"), b_olru)
                    S.barrier()
                if STOP_AFTER == "s3":
                    break

                yT = sb(esb, [128, 8, L], BF16, "yT")
                b_yT = [Buf() for _ in range(4)]
                with ExitStack() as es4:
                    wg_ = [sb(es4, [128, 8, 256], BF16, "wg") for _ in range(2)]; b_wg = [Buf(), Buf()]
                    wu_ = [sb(es4, [128, 12, 128], BF16, "wu") for _ in range(2)]; b_wu = [Buf(), Buf()]
                    ga_ = [sb(es4, [128, 512], F32, "ga") for _ in range(2)]; b_ga = [Buf(), Buf()]
                    gb_ = [sb(es4, [128, 512], F32, "gb") for _ in range(2)]; b_gb = [Buf(), Buf()]
                    ya_ = [sb(es4, [128, 512], F32, "ya") for _ in range(2)]; b_ya = [Buf(), Buf()]
                    yb_ = [sb(es4, [128, 512], F32, "yb") for _ in range(2)]; b_yb = [Buf(), Buf()]
                    psM = [ps(es4, [128, 512], F32, "psM") for _ in range(8)]; b_psM = [Buf() for _ in range(8)]
                    it = 0
                    for f in range(8):
                        wgt, bwg = wg_[f % 2], b_wg[f % 2]
                        wut, bwu = wu_[f % 2], b_wu[f % 2]
                        S.dma("pool", wgt[:, :, :].rearrange("p a b -> p (a b)"), wgagb_d[f], writes=[bwg])
                        S.dma("pool", wut[:, :, :].rearrange("p a b -> p (a b)"), wup_d[f], writes=[bwu])
                        for blk in range(4):
                            tok = slice(blk * 512, (blk + 1) * 512)
                            i2 = it % 2
                            p0, p1, p2, p3 = [psM[(it % 2) * 4 + q] for q in range(4)]
                            q0, q1, q2, q3 = [b_psM[(it % 2) * 4 + q] for q in range(4)]
                            it += 1
                            for k in range(8):
                                pe_mm(p0[:], wgt[:, k, 0:128], hT[:, k, tok], k == 0, k == 7, [bwg, *b_hT[blk]], [q0])
                            act(ga_[i2][:], p0[:], AF.Sigmoid, [q0], [b_ga[i2]])
                            for k in range(4):
                                pe_mm(p1[:], wut[:, k, :], oatt[:, k, tok], k == 0, k == 3, [bwu, b_oatt[k][blk]], [q1])
                            V("dve", lambda: nc.vector.tensor_tensor(out=ya_[i2][:], in0=p1[:], in1=ga_[i2][:], op=ALU.mult),
                              [q1, b_ga[i2]], [b_ya[i2]])
                            for k in range(8):
                                pe_mm(p2[:], wgt[:, k, 128:256], hT[:, k, tok], k == 0, k == 7, [bwg, *b_hT[blk]], [q2])
                            act(gb_[i2][:], p2[:], AF.Sigmoid, [q2], [b_gb[i2]])
                            for k in range(8):
                                pe_mm(p3[:], wut[:, 4 + k, :], olru[:, k, tok], k == 0, k == 7, [bwu, b_olru[k]], [q3])
                            V("dve", lambda: nc.vector.tensor_tensor(out=yb_[i2][:], in0=p3[:], in1=gb_[i2][:], op=ALU.mult),
                              [q3, b_gb[i2]], [b_yb[i2]])
                            V("pool", lambda: nc.gpsimd.tensor_tensor(out=yT[:, f, tok], in0=ya_[i2][:], in1=yb_[i2][:],
                                                                      op=ALU.add), [b_ya[i2], b_yb[i2]], [b_yT[blk]])
                    if b == 0:
                        dump("yT", yT[:, :, :].rearrange("p a b -> p (a b)"), b_yT)
                    S.barrier()
                if STOP_AFTER == "s4":
                    break

                with ExitStack() as es5:
                    wo32 = [sb(es5, [128, D], F32, "wo32") for _ in range(2)]; b_wo32 = [Buf(), Buf()]
                    wob = sb(es5, [128, 8, D], BF16, "wob"); b_wob = Buf()
                    GA1 = sb(es5, [128, D], F32, "GA1"); b_GA1 = Buf()
                    G2 = sb(es5, [128, D], F32, "G2"); b_G2 = Buf()
                    S2 = sb(es5, [128, D], F32, "S2"); b_S2 = Buf()
                    gffnb = sb(es5, [128, D], F32, "gffnb"); b_gffnb = Buf()
                    diag = [sb(es5, [128, 128], F32, "diag") for _ in range(2)]; b_diag = [Buf(), Buf()]
                    wr = sb(es5, [128, 8, 36], F32, "wr"); b_wr = Buf()
                    brb = sb(es5, [128, 36], F32, "brb"); b_brb = Buf()
                    psB = ps(es5, [128, D], F32, "psB"); b_psB = Buf()
                    psO5 = [ps(es5, [128, D], F32, "psO5") for _ in range(2)]; b_psO5 = [Buf(), Buf()]
                    psT = ps(es5, [128, D], F32, "psT"); b_psT = Buf()
                    S.dma("sp", gffnb[:], gffnb_d, writes=[b_gffnb])
                    S.dma("sp", wr[:, :, :].rearrange("p a b -> p (a b)"), wr_d, writes=[b_wr])
                    S.dma("sp", brb[:], brb_d, writes=[b_brb])
                    bcast_row(es5, 2, b, psB, b_psB, diag, b_diag)
                    V("dve", lambda: nc.vector.tensor_copy(out=GA1[:], in_=psB[:]), [b_psB], [b_GA1])
                    bcast_row(es5, 4, b, psB, b_psB, diag, b_diag)
                    V("dve", lambda: nc.vector.scalar_tensor_tensor(out=G2[:], in0=psB[:], scalar=1.0, in1=gffnb[:],
                                                                    op0=ALU.add, op1=ALU.mult), [b_psB, b_gffnb], [b_G2])
                    bcast_row(es5, 3, b, psB, b_psB, diag, b_diag)
                    V("dve", lambda: nc.vector.tensor_copy(out=S2[:], in_=psB[:]), [b_psB], [b_S2])
                    for kk in range(8):
                        S.dma("sp", wo32[kk % 2][:], wout_d[:, kk * D:(kk + 1) * D], writes=[b_wo32[kk % 2]])
                        V("dve", lambda: nc.vector.tensor_tensor(out=wob[:, kk, :], in0=wo32[kk % 2][:], in1=GA1[:],
                                                                 op=ALU.mult), [b_wo32[kk % 2], b_GA1], [b_wob])
                    xt5 = [sb(es5, [128, D], F32, "xt5") for _ in range(2)]; b_xt5 = [Buf(), Buf()]
                    x1t = xt5; b_x1t = b_xt5
                    h2t = [sb(es5, [128, D], F32, "h2t") for _ in range(2)]; b_h2t = [Buf(), Buf()]
                    h2b = [sb(es5, [128, D], BF16, "h2b") for _ in range(2)]; b_h2b = [Buf(), Buf()]
                    h2T = sb(es5, [128, 8, 128], F32, "h2T"); b_h2T = Buf()
                    junk5 = sb(es5, [128, D], BF16, "junk5"); b_junk5 = Buf()
                    st5 = sb(es5, [128, 16, 3], F32, "st5"); b_st5 = [Buf() for _ in range(16)]
                    rt = sb(es5, [128, 2, 96], F32, "rt"); b_rt = [Buf() for _ in range(2)]
                    for j in range(16):
                        tg = b * 16 + j
                        i2 = j % 2
                        tsl = slice(j * 128, (j + 1) * 128)
                        S.dma("sp", xt5[i2][:], xin[b, tsl, :], writes=[b_xt5[i2]])
                        pO, bO = psO5[i2], b_psO5[i2]
                        for hf in range(2):
                            for k in range(8):
                                pe_mm(pO[:, hf * 512:(hf + 1) * 512], yT[:, k, tsl], wob[:, k, hf * 512:(hf + 1) * 512],
                                      k == 0, k == 7, [b_yT[j // 4], b_wob], [bO], sig=(k == 7 and hf == 1))
                        V("dve", lambda: nc.vector.tensor_tensor(out=x1t[i2][:], in0=pO[:], in1=xt5[i2][:], op=ALU.add),
                          [bO, b_xt5[i2]], [b_xt5[i2]])
                        S.dma("pool", x1_d[tg * 128:(tg + 1) * 128, :], x1t[i2][:], reads=[b_x1t[i2]], writes=[b_x1d], group=True, semof=b_x1t[i2])
                        act(junk5[:], x1t[i2][:], AF.Square, [b_x1t[i2]], [b_junk5, b_st5[j]], accum_out=st5[:, j, 0:1])
                        act(st5[:, j, 1:2], st5[:, j, 0:1], AF.Sqrt, [b_st5[j], b_eps], [b_st5[j]], scale=1.0 / D,
                            bias=epst[:, 0:1])
                        V("dve", lambda: nc.vector.reciprocal(out=st5[:, j, 2:3], in_=st5[:, j, 1:2]), [b_st5[j]], [b_st5[j]])
                        V("dve", lambda: nc.vector.scalar_tensor_tensor(out=h2t[i2][:], in0=x1t[i2][:], scalar=st5[:, j, 2:3],
                                                                        in1=G2[:], op0=ALU.mult, op1=ALU.mult),
                          [b_x1t[i2], b_st5[j], b_G2], [b_h2t[i2]])
                        V("pool", lambda: nc.gpsimd.tensor_tensor(out=h2t[i2][:], in0=h2t[i2][:], in1=S2[:], op=ALU.add),
                          [b_h2t[i2], b_S2], [b_h2t[i2]])
                        act(h2b[i2][:], h2t[i2][:], AF.Copy, [b_h2t[i2]], [b_h2b[i2]])
                        S.dma("pool", h2_d[tg * 128:(tg + 1) * 128, :], h2b[i2][:], reads=[b_h2b[i2]], writes=[b_h2d], group=True, semof=b_h2b[i2])
                        for k in range(8):
                            pe_tr(psT[:, k * 128:(k + 1) * 128], h2t[i2][:, k * 128:(k + 1) * 128], identf[:],
                                  [b_h2t[i2], b_identf], [b_psT], sig=(k == 7))
                        act(h2T[:, :, :].rearrange("p a b -> p (a b)"), psT[:], AF.Copy, [b_psT], [b_h2T])
                        for k in range(8):
                            pe_mm(psB[:, 0:36], h2T[:, k, :], wr[:, k, :], k == 0, k == 7, [b_h2T, b_wr], [b_psB])
                        R_ = rt[:, j % 2, :]
                        br_ = b_rt[j % 2]
                        lg = R_[:, 0:4]; le = R_[:, 4:36]
                        V("dve", lambda: nc.vector.tensor_tensor(out=R_[:, 0:36], in0=psB[:, 0:36], in1=brb[:], op=ALU.add),
                          [b_psB, b_brb], [br_])
                        if tg == 0:
                            dump("logit0", R_[:, 0:36], [br_])
                        mg = R_[:, 36:37]; nmg = R_[:, 37:38]; sg = R_[:, 38:39]; ptop = R_[:, 39:40]
                        V("dve", lambda: nc.vector.tensor_reduce(out=mg, in_=lg, axis=AX.X, op=ALU.max), [br_], [br_])
                        V("dve", lambda: nc.vector.tensor_scalar(out=nmg, in0=mg, scalar1=-1.0, scalar2=None, op0=ALU.mult),
                          [br_], [br_])
                        act(R_[:, 40:44], lg, AF.Exp, [br_], [br_], bias=nmg, accum_out=sg)
                        V("dve", lambda: nc.vector.reciprocal(out=ptop, in_=sg), [br_], [br_])
                        V("dve", lambda: nc.vector.tensor_scalar(out=R_[:, 44:48], in0=lg, scalar1=mg, scalar2=None,
                                                                 op0=ALU.is_equal), [br_], [br_])
                        V("dve", lambda: nc.vector.tensor_scalar(out=R_[:, 44:48], in0=R_[:, 44:48], scalar1=-1.0, scalar2=1e30,
                                                                 op0=ALU.add, op1=ALU.mult), [br_], [br_])
                        lem = R_[:, 48:80]
                        V("dve", lambda: nc.vector.tensor_tensor(
                            out=lem.rearrange("p (g e) -> p g e", e=8), in0=le.rearrange("p (g e) -> p g e", e=8),
                            in1=R_[:, 44:48].unsqueeze(2).to_broadcast([128, 4, 8]), op=ALU.add), [br_], [br_])
                        top8 = R_[:, 80:88]
                        V("dve", lambda: nc.vector.max(out=top8, in_=lem), [br_], [br_])
                        V("dve", lambda: nc.vector.tensor_scalar(out=oh1all[:, tg, :], in0=lem, scalar1=top8[:, 0:1], scalar2=None,
                                                                 op0=ALU.is_equal), [br_], [b_oh1])
                        V("dve", lambda: nc.vector.tensor_scalar(out=Mall[:, tg, :], in0=lem, scalar1=top8[:, 1:2], scalar2=None,
                                                                 op0=ALU.is_ge), [br_], [b_Mall])
                        dlt = R_[:, 88:89]; ew = R_[:, 89:90]; w1_ = R_[:, 90:91]
                        V("dve", lambda: nc.vector.tensor_tensor(out=dlt, in0=top8[:, 1:2], in1=top8[:, 0:1], op=ALU.subtract),
                          [br_], [br_])
                        act(ew, dlt, AF.Exp, [br_], [br_])
                        V("dve", lambda: nc.vector.tensor_scalar(out=ew, in0=ew, scalar1=1.0, scalar2=None, op0=ALU.add),
                          [br_], [br_])
                        V("dve", lambda: nc.vector.reciprocal(out=w1_, in_=ew), [br_], [br_])
                        V("dve", lambda: nc.vector.tensor_tensor(out=gates[:, tg, 0:1], in0=w1_, in1=ptop, op=ALU.mult),
                          [br_], [b_gates])
                        V("dve", lambda: nc.vector.tensor_tensor(out=gates[:, tg, 1:2], in0=ptop, in1=gates[:, tg, 0:1],
                                                                 op=ALU.subtract), [br_, b_gates], [b_gates])
                    S.barrier()
            if STOP_AFTER in ("s1", "s2", "s3", "s4"):
                break

        if STOP_AFTER is None or STOP_AFTER in ("route", "moe"):
            blk_i = sb(es, [128, NBLK], I32, "blki"); b_blki = Buf()
            with ExitStack() as esr:
                psC = ps(esr, [128, 32], F32, "psC"); b_psC = Buf()
                psR = [ps(esr, [128, 32], F32, "psR") for _ in range(2)]; b_psR = [Buf(), Buf()]
                cn = sb(esr, [128, 8, 32], F32, "cn"); b_cn = Buf()
                cni = sb(esr, [128, 32], I32, "cni"); b_cni = Buf()
                cmp_ = sb(esr, [128, NBLK, 32], F32, "cmp"); b_cmp = Buf()
                bst = sb(esr, [128, NBLK], F32, "bst"); b_bst = Buf()
                bs0 = sb(esr, [128, NBLK], F32, "bs0"); b_bs0 = Buf()
                blk_f = sb(esr, [128, NBLK], F32, "blkf"); b_blkf = Buf()
                tmp = sb(esr, [128, 2, 32], F32, "tmpr"); b_tmp = [Buf(), Buf()]
                for t in range(32):
                    pe_mm(psC[:], onesb[:], Mall[:, t, :], t == 0, t == 31, [b_onesb, b_Mall], [b_psC])
                V("dve", lambda: nc.vector.tensor_copy(out=cn[:, 0, :], in_=psC[:]), [b_psC], [b_cn])
                V("dve", lambda: nc.vector.tensor_scalar(out=cn[:, 1, :], in0=cn[:, 0, :], scalar1=255.0, scalar2=None, op0=ALU.add),
                  [b_cn], [b_cn])
                V("dve", lambda: nc.vector.tensor_copy(out=cni[:], in_=cn[:, 1, :]), [b_cn], [b_cni])
                V("dve", lambda: nc.vector.tensor_scalar(out=cni[:], in0=cni[:], scalar1=8, scalar2=8,
                                                         op0=ALU.arith_shift_right, op1=ALU.logical_shift_left), [b_cni], [b_cni])
                V("dve", lambda: nc.vector.tensor_copy(out=cn[:, 2, :], in_=cni[:]), [b_cni], [b_cn])
                V("dve", lambda: nc.vector.tensor_tensor_scan(out=cn[:, 3, :], data0=onesf[:, 0:32], data1=cn[:, 2, :],
                                                              initial=0.0, op0=ALU.mult, op1=ALU.add),
                  [b_cn, b_onesf], [b_cn])
                V("dve", lambda: nc.vector.tensor_tensor(out=cn[:, 4, :], in0=cn[:, 3, :], in1=cn[:, 2, :], op=ALU.subtract),
                  [b_cn], [b_cn])
                V("dve", lambda: nc.vector.tensor_scalar(out=bs0[:], in0=onesf[:, 0:NBLK], scalar1=256.0, scalar2=None, op0=ALU.mult),
                  [b_onesf], [b_bs0])
                V("dve", lambda: nc.vector.tensor_tensor_scan(out=bst[:], data0=onesf[:, 0:NBLK], data1=bs0[:], initial=-256.0,
                                                              op0=ALU.mult, op1=ALU.add), [b_bs0, b_onesf], [b_bst])
                V("dve", lambda: nc.vector.tensor_tensor(
                    out=cmp_[:], in0=cn[:, 3, :].unsqueeze(1).to_broadcast([128, NBLK, 32]),
                    in1=bst[:].unsqueeze(2).to_broadcast([128, NBLK, 32]), op=ALU.is_le), [b_cn, b_bst], [b_cmp])
                V("dve", lambda: nc.vector.tensor_reduce(out=blk_f[:], in_=cmp_[:], axis=AX.X, op=ALU.add), [b_cmp], [b_blkf])
                V("dve", lambda: nc.vector.tensor_scalar(out=blk_f[:], in0=blk_f[:], scalar1=31.0, scalar2=128.0,
                                                         op0=ALU.min, op1=ALU.mult), [b_blkf], [b_blkf])
                V("dve", lambda: nc.vector.tensor_scalar(out=blk_f[:], in0=blk_f[:], scalar1=iota[:, 0:1], scalar2=None,
                                                         op0=ALU.add), [b_blkf, b_iota], [b_blkf])
                V("dve", lambda: nc.vector.tensor_copy(out=blk_i[:], in_=blk_f[:]), [b_blkf], [b_blki])
                dump("blkf", blk_f[:], [b_blkf])
                dump("cn", cn[:, 0:5, :].rearrange("p a b -> p (a b)"), [b_cn])
                for t in range(32):
                    pR, bR = psR[t % 2], b_psR[t % 2]
                    for t2 in range(t):
                        pe_mm(pR[:], onesb[:], Mall[:, t2, :], t2 == 0, False, [b_onesb, b_Mall], [bR], sig=False)
                    pe_mm(pR[:], utri[:], Mall[:, t, :], t == 0, True, [b_utri, b_Mall], [bR])
                    tt = tmp[:, t % 2, :]; bt_ = b_tmp[t % 2]
                    V("dve", lambda: nc.vector.tensor_tensor(out=tt, in0=pR[:], in1=cn[:, 4, :], op=ALU.add), [bR, b_cn], [bt_])
                    V("dve", lambda: nc.vector.tensor_tensor(out=cmp_[:, 0, :], in0=tt, in1=oh1all[:, t, :], op=ALU.mult),
                      [bt_, b_oh1], [b_cmp])
                    V("dve", lambda: nc.vector.tensor_reduce(out=dest_f[:, t, 0:1], in_=cmp_[:, 0, :], axis=AX.X, op=ALU.add),
                      [b_cmp], [b_destf])
                    V("dve", lambda: nc.vector.tensor_tensor(out=cmp_[:, 1, :], in0=Mall[:, t, :], in1=oh1all[:, t, :], op=ALU.subtract),
                      [b_Mall, b_oh1], [b_cmp])
                    V("dve", lambda: nc.vector.tensor_tensor(out=cmp_[:, 1, :], in0=cmp_[:, 1, :], in1=tt, op=ALU.mult),
                      [bt_, b_cmp], [b_cmp])
                    V("dve", lambda: nc.vector.tensor_reduce(out=dest_f[:, t, 1:2], in_=cmp_[:, 1, :], axis=AX.X, op=ALU.add),
                      [b_cmp], [b_destf])
                V("dve", lambda: nc.vector.tensor_copy(out=dest_i[:], in_=dest_f[:, :, :].rearrange("p a b -> p (a b)")), [b_destf], [b_desti])
                dump("destf", dest_f[:, :, :].rearrange("p a b -> p (a b)"), [b_destf])
                dump("gates", gates[:, :, :].rearrange("p a b -> p (a b)"), [b_gates])
                hl = [sb(esr, [128, D], BF16, "hl") for _ in range(2)]; b_hl = [Buf(), Buf()]
                for t in range(32):
                    S.dma("sp", hl[t % 2][:], h2_d[t * 128:(t + 1) * 128, :], reads=[b_h2d], writes=[b_hl[t % 2]])
                    for kk in range(2):
                        S.dma_ind(xs_d[:, :], bass.IndirectOffsetOnAxis(ap=dest_i[:, 2 * t + kk:2 * t + kk + 1], axis=0), hl[t % 2][:, :], None,
                                  NROWS - 1, reads=[b_hl[t % 2], b_desti], writes=[b_xsd], group=True, semof=b_hl[t % 2])
                S.barrier()

            if STOP_AFTER != "route":
                with ExitStack() as esm:
                    w1b = [sb(esm, [128, 8, 512], BF16, "w1b") for _ in range(3)]; b_w1 = [Buf() for _ in range(3)]
                    w3b = [sb(esm, [128, 8, 512], BF16, "w3b") for _ in range(3)]; b_w3 = [Buf() for _ in range(3)]
                    w2b = [sb(esm, [128, 4, 1024], BF16, "w2b") for _ in range(3)]; b_w2 = [Buf() for _ in range(3)]
                    xr = [sb(esm, [128, 2, D], BF16, "xr") for _ in range(3)]; b_xr = [Buf() for _ in range(3)]
                    xT = [sb(esm, [128, 8, 256], BF16, "xT") for _ in range(3)]; b_xT = [[Buf(), Buf()] for _ in range(3)]
                    sl = [sb(esm, [128, 256], F32, "sl") for _ in range(2)]; b_sl = [Buf(), Buf()]
                    hm = [sb(esm, [128, 4, 256], BF16, "hm") for _ in range(2)]; b_hm = [Buf(), Buf()]
                    yo = [sb(esm, [128, 2, D], BF16, "yo") for _ in range(2)]; b_yo = [[Buf(), Buf()], [Buf(), Buf()]]
                    psX = [ps(esm, [128, 1024], BF16, "psX") for _ in range(2)]; b_psX = [Buf(), Buf()]
                    psH = [ps(esm, [128, 512], F32, "psH") for _ in range(2)]; b_psH = [Buf(), Buf()]
                    psY = [ps(esm, [128, 512], F32, "psY") for _ in range(4)]; b_psY = [Buf() for _ in range(4)]
                    xcnt = [0]; hcnt = [0]; ycnt = [0]

                    def moe_w(blk):
                        i3 = blk % 3
                        off = bass.IndirectOffsetOnAxis(ap=blk_i[:, blk:blk + 1], axis=0)
                        S.dma_ind(w1b[i3][:, :, :].rearrange("p a b -> p (a b)"), None, w1_d[:, :], off, 0,
                                  reads=[b_blki], writes=[b_w1[i3]])
                        S.dma_ind(w3b[i3][:, :, :].rearrange("p a b -> p (a b)"), None, w3_d[:, :], off, 0,
                                  reads=[b_blki], writes=[b_w3[i3]])
                        S.dma_ind(w2b[i3][:, :, :].rearrange("p a b -> p (a b)"), None, w2_d[:, :], off, 0,
                                  reads=[b_blki], writes=[b_w2[i3]])

                    def moe_x(blk):
                        i2 = blk % 3
                        S.dma("sp", xr[i2][:], xs_d[blk * 256:(blk + 1) * 256, :].rearrange("(a p) d -> p a d", p=128),
                              reads=[b_xsd], writes=[b_xr[i2]])
                        for k in range(8):
                            if k % 4 == 0:
                                pX, bX = psX[(xcnt[0] // 4) % 2], b_psX[(xcnt[0] // 4) % 2]
                            for a in range(2):
                                pe_tr(pX[:, (k % 4) * 256 + a * 128:(k % 4) * 256 + (a + 1) * 128],
                                      xr[i2][:, a, k * 128:(k + 1) * 128], identb[:], [b_xr[i2], b_identb], [bX],
                                      sig=(k % 4 == 3 and a == 1))
                            xcnt[0] += 1
                            if k % 4 == 3:
                                dstx = xT[i2][:, k - 3:k + 1, :].rearrange("p a b -> p (a b)")
                                V("dve", lambda: nc.vector.tensor_copy(out=dstx, in_=pX[:]), [bX], [b_xT[i2][(k // 4) % 2]])

                    def moe_c(blk):
                        i2 = blk % 2
                        i3 = blk % 3
                        ix = blk % 3
                        for c4 in range(4):
                            pH, bH = psH[hcnt[0] % 2], b_psH[hcnt[0] % 2]
                            si = hcnt[0] % 2
                            hcnt[0] += 1
                            for k in range(8):
                                pe_mm(pH[:, 0:256], w1b[i3][:, k, c4 * 128:(c4 + 1) * 128], xT[ix][:, k, :], k == 0, k == 7,
                                      [b_w1[i3], *b_xT[ix]], [bH], sig=False)
                            for k in range(8):
                                pe_mm(pH[:, 256:512], w3b[i3][:, k, c4 * 128:(c4 + 1) * 128], xT[ix][:, k, :], k == 0, k == 7,
                                      [b_w3[i3], *b_xT[ix]], [bH])
                            act(sl[si][:], pH[:, 0:256], AF.Silu, [bH], [b_sl[si]])
                            V("dve", lambda: nc.vector.tensor_tensor(out=hm[i2][:, c4, :], in0=pH[:, 256:512], in1=sl[si][:],
                                                                     op=ALU.mult), [bH, b_sl[si]], [b_hm[i2]])
                        for a in range(2):
                            for hf in range(2):
                                pY, bY = psY[ycnt[0] % 4], b_psY[ycnt[0] % 4]
                                ycnt[0] += 1
                                for k in range(4):
                                    pe_mm(pY[:], hm[i2][:, k, a * 128:(a + 1) * 128], w2b[i3][:, k, hf * 512:(hf + 1) * 512],
                                          k == 0, k == 3, [b_hm[i2], b_w2[i3]], [bY])
                                V("dve", lambda: nc.vector.tensor_copy(out=yo[i2][:, a, hf * 512:(hf + 1) * 512], in_=pY[:]), [bY],
                                  [b_yo[i2][hf]])
                        S.dma("act", ys_d[blk * 256:(blk + 1) * 256, :].rearrange("(a p) d -> p a d", p=128), yo[i2][:],
                              reads=b_yo[i2], writes=[b_ysd], group=True, semof=b_yo[i2][0])

                    moe_w(0)
                    moe_w(1)
                    moe_x(0)
                    moe_x(1)
                    for blk in range(NBLK):
                        if blk + 2 < NBLK:
                            moe_w(blk + 2)
                            moe_x(blk + 2)
                        moe_c(blk)
                    S.barrier()

                with ExitStack() as esf:
                    GA2 = [sb(esf, [128, D], F32, "GA2") for _ in range(2)]; b_GA2 = [Buf(), Buf()]
                    gfb = sb(esf, [128, D], F32, "gfb"); b_gfb = Buf()
                    diag = [sb(esf, [128, 128], F32, "diagf") for _ in range(2)]; b_diag = [Buf(), Buf()]
                    psB = ps(esf, [128, D], F32, "psBf"); b_psB = Buf()
                    S.dma("sp", gfb[:], gfb_d, writes=[b_gfb])
                    for b in range(NB):
                        bcast_row(esf, 5, b, psB, b_psB, diag, b_diag)
                        V("dve", lambda: nc.vector.tensor_copy(out=GA2[b][:], in_=psB[:]), [b_psB], [b_GA2[b]])
                    y0 = [sb(esf, [128, D], BF16, "y0") for _ in range(2)]; b_y0 = [Buf(), Buf()]
                    y1 = [sb(esf, [128, D], BF16, "y1") for _ in range(2)]; b_y1 = [Buf(), Buf()]
                    x1l = [sb(esf, [128, D], F32, "x1l") for _ in range(2)]; b_x1l = [Buf(), Buf()]
                    mo = [sb(esf, [128, D], F32, "mo") for _ in range(2)]; b_mo = [Buf(), Buf()]
                    ot = [sb(esf, [128, D], F32, "ot") for _ in range(2)]; b_ot = [Buf(), Buf()]
                    junkf = sb(esf, [128, D], BF16, "junkf"); b_junkf = Buf()
                    stf = sb(esf, [128, 32, 3], F32, "stf"); b_stf = [Buf() for _ in range(32)]
                    for t in range(32):
                        i2 = t % 2
                        bb = t // 16
                        S.dma_ind(y0[i2][:, :], None, ys_d[:, :], bass.IndirectOffsetOnAxis(ap=dest_i[:, 2 * t:2 * t + 1], axis=0),
                                  NROWS - 1, reads=[b_ysd, b_desti], writes=[b_y0[i2]])
                        S.dma_ind(y1[i2][:, :], None, ys_d[:, :], bass.IndirectOffsetOnAxis(ap=dest_i[:, 2 * t + 1:2 * t + 2], axis=0),
                                  NROWS - 1, reads=[b_ysd, b_desti], writes=[b_y1[i2]])
                        S.dma("sp", x1l[i2][:], x1_d[t * 128:(t + 1) * 128, :], reads=[b_x1d], writes=[b_x1l[i2]])
                        act(mo[i2][:], y0[i2][:], AF.Copy, [b_y0[i2], b_gates], [b_mo[i2]], scale=gates[:, t, 0:1])
                        V("dve", lambda: nc.vector.scalar_tensor_tensor(out=mo[i2][:], in0=y1[i2][:], scalar=gates[:, t, 1:2],
                                                                        in1=mo[i2][:], op0=ALU.mult, op1=ALU.add),
                          [b_y1[i2], b_gates, b_mo[i2]], [b_mo[i2]])
                        if t == 0:
                            dump("moe0", mo[i2][:], [b_mo[i2]])
                        V("pool", lambda: nc.gpsimd.tensor_tensor(out=mo[i2][:], in0=mo[i2][:], in1=GA2[bb][:], op=ALU.mult),
                          [b_mo[i2], b_GA2[bb]], [b_mo[i2]])
                        V("pool", lambda: nc.gpsimd.tensor_tensor(out=mo[i2][:], in0=mo[i2][:], in1=x1l[i2][:], op=ALU.add),
                          [b_mo[i2], b_x1l[i2]], [b_mo[i2]])
                        act(junkf[:], mo[i2][:], AF.Square, [b_mo[i2]], [b_junkf, b_stf[t]], accum_out=stf[:, t, 0:1])
                        act(stf[:, t, 1:2], stf[:, t, 0:1], AF.Sqrt, [b_stf[t], b_eps], [b_stf[t]], scale=1.0 / D, bias=epst[:, 0:1])
                        V("dve", lambda: nc.vector.reciprocal(out=stf[:, t, 2:3], in_=stf[:, t, 1:2]), [b_stf[t]], [b_stf[t]])
                        V("dve", lambda: nc.vector.scalar_tensor_tensor(out=ot[i2][:], in0=mo[i2][:], scalar=stf[:, t, 2:3],
                                                                        in1=gfb[:], op0=ALU.mult, op1=ALU.mult),
                          [b_mo[i2], b_stf[t], b_gfb], [b_ot[i2]])
                        S.dma("act", out_d[bb, (t % 16) * 128:(t % 16 + 1) * 128, :], ot[i2][:], reads=[b_ot[i2]], writes=[b_outd],
                              group=True, semof=b_ot[i2])
        S.barrier()
        build_program.stats = dict(nops=dict(S.nops), nwaits=S.nwaits, nsem=S.nsem)
    return nc


def _fm(v):
    return np.ascontiguousarray(np.asarray(v, np.float32).reshape(-1, 128).T)


def _kp(w):
    K, N = w.shape
    return np.ascontiguousarray(w.reshape(K // 128, 128, N).transpose(1, 0, 2))


def _swap_cols():
    idx = np.arange(64)
    half = idx // 32
    within = idx % 32
    sw = np.where(within < 16, within + 16, within - 16)
    return half * 32 + sw


def _host_consts():
    nf = 16
    inv_freq = (10000.0 ** (-np.arange(nf, dtype=np.float32) / nf)).astype(np.float32)
    t = np.arange(L)
    row = (t // 64).astype(np.float32)
    col = (t % 64).astype(np.float32)
    cos = np.zeros((128, L), np.float32)
    sin = np.zeros((128, L), np.float32)
    for p in range(128):
        d = p % 64
        pos = row if d < 32 else col
        ang = (pos * inv_freq[(d % 32) % 16]).astype(np.float32)
        sign = -1.0 if (d % 32) < 16 else 1.0
        cos[p] = np.cos(ang)
        sin[p] = sign * np.sin(ang)
    cossin = np.concatenate([cos, sin], axis=1)
    ident = np.eye(128, dtype=np.float32)
    iota = np.arange(128, dtype=np.float32).reshape(128, 1)
    utri = np.triu(np.ones((128, 128), np.float32), k=1)
    return cossin, ident, iota, utri


def _bias_table(rpb):
    cq = np.arange(64)
    c_start = np.clip(cq - 8, 0, 48)
    band = (cq[None, :] >= c_start[:, None]) & (cq[None, :] < c_start[:, None] + 16)
    dc = np.clip(cq[None, :] - cq[:, None], -15, 15) + 15
    tab = np.full((64, 8, NTB, 64), -1e30, np.float32)
    for h in range(8):
        for dr in range(15):
            vals = rpb[h, dr][dc]
            tab[:, h, 1 + dr, :] = np.where(band, vals, np.float32(-1e30))
        tab[:, h, 18, :] = tab[:, h, 1 + 3, :]
        tab[:, h, 19, :] = tab[:, h, 1 + 10, :]
    return tab.reshape(64, 8 * NTB * 64)


def _prepare(inputs):
    f = lambda k: np.asarray(inputs[k], np.float32)
    w_in = f("w_in")[0]
    K_OFF, V_OFF, LX_OFF, Q_OFF, LG_OFF, GA_OFF, GB_OFF = 0, 512, 1024, 2048, 2560, 3584, 4608
    sw = _swap_cols()
    wqkv = []
    for hp in range(4):
        cols = []
        for base in (Q_OFF, K_OFF):
            plain = np.concatenate([base + (2 * hp + e) * 64 + np.arange(64) for e in range(2)])
            swp = np.concatenate([base + (2 * hp + e) * 64 + sw for e in range(2)])
            cols += [plain, swp]
        cols.append(V_OFF + hp * 128 + np.arange(128))
        wqkv.append(_kp(w_in[:, np.concatenate(cols)]).reshape(128, 8 * 640))
    wqkv = np.stack(wqkv)
    wlxlg = np.stack([_kp(w_in[:, np.concatenate([LX_OFF + n * 128 + np.arange(128), LG_OFF + n * 128 + np.arange(128)])]
                          ).reshape(128, 8 * 256) for n in range(8)])
    wgagb = np.stack([_kp(w_in[:, np.concatenate([GA_OFF + n * 128 + np.arange(128), GB_OFF + n * 128 + np.arange(128)])]
                          ).reshape(128, 8 * 256) for n in range(8)])
    wua = _kp(f("w_up_attn")[0])
    wul = _kp(f("w_up_lru")[0])
    wup = np.stack([np.concatenate([wua[:, :, n * 128:(n + 1) * 128], wul[:, :, n * 128:(n + 1) * 128]], axis=1
                                   ).reshape(128, 12 * 128) for n in range(8)])
    wout = _kp(f("w_out")[0]).reshape(128, 8 * D)
    wa = f("lru_wa")[0]
    wx = f("lru_wx")[0]
    lruw = np.stack([wa[0], wa[1], wx[0], wx[1]])
    lruw = np.ascontiguousarray(lruw.transpose(2, 0, 1, 3)).reshape(128, 4 * 8 * 128)
    vecs = np.concatenate([
        _fm(f("g_mix")[0]), _fm(f("g_ffn")[0]),
        np.concatenate([_fm(f("conv_w")[0][j]) for j in range(4)], axis=1),
        _fm(f("conv_b")[0]),
        np.concatenate([_fm(f("lru_ba")[0][d_]) for d_ in range(2)], axis=1),
        np.concatenate([_fm(f("lru_bx")[0][d_]) for d_ in range(2)], axis=1),
        np.concatenate([_fm(f("lru_lambda")[0][d_]) for d_ in range(2)], axis=1),
        _fm(f("b_mod")[0]),
    ], axis=1)
    assert vecs.shape == (128, NV)
    wmod = _kp(f("w_mod")[0])
    wr = _kp(np.concatenate([f("router_group_w")[0], f("router_expert_w")[0]], axis=1)).reshape(128, 8 * 36)
    brb = np.ascontiguousarray(np.broadcast_to(
        np.concatenate([f("router_group_b")[0], f("router_expert_b")[0]])[None, :], (128, 36)))
    gfb = np.ascontiguousarray(np.broadcast_to(f("g_final")[None, :], (128, D)))
    gffnb = np.ascontiguousarray(np.broadcast_to(f("g_ffn")[0][None, :], (128, D)))
    w1 = f("expert_w_gate")[0]
    w3 = f("expert_w_up")[0]
    w2 = f("expert_w_down")[0]
    w1h = np.ascontiguousarray(w1.reshape(32, 8, 128, 512).transpose(0, 2, 1, 3)).reshape(32 * 128, 8 * 512)
    w3h = np.ascontiguousarray(w3.reshape(32, 8, 128, 512).transpose(0, 2, 1, 3)).reshape(32 * 128, 8 * 512)
    w2h = np.ascontiguousarray(w2.reshape(32, 4, 128, 1024).transpose(0, 2, 1, 3)).reshape(32 * 128, 4 * 1024)
    cossin, ident, iota, utri = _host_consts()
    btab = _bias_table(f("rpb")[0])
    shared = dict(wmod=wmod, vecs=vecs, wqkv=wqkv, wlxlg=wlxlg, wgagb=wgagb, wup=wup, wout=wout, lruw=lruw,
                  cossin=cossin, btab=btab, wr=wr, brb=brb, gfb=gfb, gffnb=gffnb, w1h=w1h, w3h=w3h, w2h=w2h,
                  ident=ident, iota=iota, utri=utri)
    x = f("x")
    ctx = f("ctx")
    c = f("c")
    c_ctx = f("c_ctx")
    in_maps = []
    for core in range(NCORES):
        b0 = core * NB
        cs = np.stack([c[b0], c[b0 + 1], c_ctx], axis=-1)
        cs = np.ascontiguousarray(cs.reshape(8, 128, 3).transpose(1, 0, 2)).reshape(128, 24)
        m = dict(shared)
        m["xin"] = np.ascontiguousarray(x[b0:b0 + NB])
        m["ctxin"] = np.ascontiguousarray(ctx[b0:b0 + NB])
        m["cs"] = cs
        in_maps.append(m)
    return in_maps


def kernel(**inputs):
    in_maps = _prepare(inputs)
    nc = build_program()
    res = run_bass_kernel_spmd(nc, in_maps, core_ids=list(range(NCORES)))
    out = np.concatenate([np.asarray(r["out"], np.float32) for r in res.results], axis=0)
    return out
```

```python
import numpy as np
import concourse.bass as bass
import concourse.mybir as mybir
from concourse.bass_utils import run_bass_kernel_spmd
from contextlib import ExitStack

F32 = mybir.dt.float32
BF16 = mybir.dt.bfloat16
I32 = mybir.dt.int32
AF = mybir.ActivationFunctionType
ALU = mybir.AluOpType
AX = mybir.AxisListType

D = 1024
L = 2048
C = 256
NB = 2
NCORES = 8
LC = L + C
NTOK = NB * L
NBLK = 64
NROWS = NBLK * 256
EPS = 1e-6

V_GMIX, V_GFFN, V_CONVW, V_CONVB, V_BA, V_BX, V_LAM, V_BMOD, NV = 0, 8, 16, 48, 56, 72, 88, 104, 152
NTB = 21

DEBUG = {}
import os as _os
ATT_NHP = int(_os.environ.get("ATT_NHP", "4"))
ATT_NUNITS = int(_os.environ.get("ATT_NUNITS", "8"))
ATT_NROWS = int(_os.environ.get("ATT_NROWS", "8"))
ATT_NORM = int(_os.environ.get("ATT_NORM", "1"))
ATT_PARTS = int(_os.environ.get("ATT_PARTS", "31"))
STOP_AFTER = None


class Buf:
    __slots__ = ("name", "w", "wx", "r", "dsem", "dcnt", "grp")

    def __init__(self, name=""):
        self.name = name
        self.w = None
        self.wx = {}
        self.r = {}
        self.dsem = {}
        self.dcnt = {}
        self.grp = False


class Sync:
    ROLL = 30000

    def __init__(self, nc, es):
        self.nc = nc
        self.es = es
        self.eng = {"pe": nc.tensor, "act": nc.scalar, "dve": nc.vector, "pool": nc.gpsimd, "sp": nc.sync}
        self.sem = {}
        self.cnt = {}
        self.waited = {k: {} for k in self.eng}
        self.nsem = 0
        self.pe_sems = set()
        for k in self.eng:
            self._newsem(k)
        self.pend = []
        self.dbufs = []
        self.nops = {k: 0 for k in self.eng}
        self.nwaits = 0

    def _alloc(self, name):
        self.nsem += 1
        return self.es.enter_context(self.nc.semaphore(f"{name}{self.nsem}"))

    def _newsem(self, k):
        self.sem[k] = self._alloc("e" + k)
        self.cnt[k] = 0
        if k == "pe":
            self.pe_sems.add(id(self.sem[k]))

    def _wait(self, e, ev):
        semh, val = ev
        assert val is not None, "dependency on an unsignalled PE op"
        key = id(semh)
        if self.waited[e].get(key, 0) < val:
            self.eng[e].wait_ge(semh, val)
            self.waited[e][key] = val
            self.nwaits += 1

    def _dep1(self, e, ev, acc):
        if e == "pe" and (ev[1] is None or id(ev[0]) in self.pe_sems):
            return
        assert ev[1] is not None, "dependency on an unsignalled PE op"
        k = id(ev[0])
        if k not in acc or acc[k][1] < ev[1]:
            acc[k] = ev

    def _deps(self, e, reads, writes, group=False):
        acc = {}
        for b in reads:
            if b.w is not None:
                self._dep1(e, b.w, acc)
            for ev in b.wx.values():
                self._dep1(e, ev, acc)
        for b in writes:
            if not (group and b.grp):
                if b.w is not None:
                    self._dep1(e, b.w, acc)
                for ev in b.wx.values():
                    self._dep1(e, ev, acc)
            for ev in b.r.values():
                self._dep1(e, ev, acc)
        for ev in acc.values():
            self._wait(e, ev)

    def _mark(self, ev, reads, writes, key, group=False):
        for b in reads:
            b.r[key] = ev
        for b in writes:
            if group and b.grp:
                b.wx[key] = ev
            elif group:
                b.w = None
                b.wx = {key: ev}
            else:
                b.w = ev
                b.wx = {}
            b.grp = group
            b.r = {}

    def op(self, e, fn, reads=(), writes=(), sig=True):
        self._deps(e, reads, writes)
        ins = fn()
        self.nops[e] += 1
        if sig:
            if self.cnt[e] >= self.ROLL:
                self._newsem(e)
            self.cnt[e] += 1
            ins.then_inc(self.sem[e], 1)
            ev = [self.sem[e], self.cnt[e]]
            if e == "pe":
                for p in self.pend:
                    p[0] = self.sem[e]
                    p[1] = self.cnt[e]
                self.pend = []
        else:
            assert e == "pe"
            ev = [self.sem[e], None]
            self.pend.append(ev)
        self._mark(ev, reads, writes, e)
        return ins

    def _dma_common(self, q, issue, reads, writes, group, semof):
        d = semof if semof is not None else writes[0]
        c = "sw" if q == "pool" else "hw"
        if c not in d.dsem:
            d.dsem[c] = self._alloc("d")
            d.dcnt[c] = 0
            self.dbufs.append((d, c))
        self._deps(q, reads, writes, group=group)
        ins = issue()
        self.nops[q] += 1
        d.dcnt[c] += 16
        ins.then_inc(d.dsem[c], 16)
        ev = [d.dsem[c], d.dcnt[c]]
        self._mark(ev, reads, writes, id(d.dsem[c]), group=group)
        return ins

    def dma(self, q, out, in_, reads=(), writes=(), group=False, semof=None):
        return self._dma_common(q, lambda: self.eng[q].dma_start(out=out, in_=in_), reads, writes, group, semof)

    def dma_ind(self, out, out_off, in_, in_off, bound, reads=(), writes=(), group=False, semof=None):
        def issue():
            return self.nc.gpsimd.indirect_dma_start(out=out, out_offset=out_off, in_=in_, in_offset=in_off)
        return self._dma_common("pool", issue, reads, writes, group, semof)

    def barrier(self):
        assert not self.pend
        evs = [[self.sem[k], self.cnt[k]] for k in self.eng if k != "sp" and self.cnt[k] > 0]
        evs += [[b.dsem[c], b.dcnt[c]] for (b, c) in self.dbufs if b.dcnt[c] > 0]
        for ev in evs:
            self._wait("sp", ev)
        if self.cnt["sp"] >= self.ROLL:
            self._newsem("sp")
        self.cnt["sp"] += 1
        self.nc.sync.nop().then_inc(self.sem["sp"], 1)
        ev = [self.sem["sp"], self.cnt["sp"]]
        for k in self.eng:
            if k != "sp":
                self._wait(k, ev)


def build_program():
    nc = bass.Bass("TRN2", target_bir_lowering=False)

    def din(name, shape, dt=F32):
        return nc.dram_tensor(name, list(shape), dt, kind="ExternalInput").ap()

    xin = din("xin", [NB, L, D])
    ctxin = din("ctxin", [NB, C, D])
    cs_d = din("cs", [128, 24])
    wmod_d = din("wmod", [128, 8, 6 * D])
    vecs_d = din("vecs", [128, NV])
    wqkv_d = din("wqkv", [4, 128, 8 * 640])
    wlxlg_d = din("wlxlg", [8, 128, 8 * 256])
    wgagb_d = din("wgagb", [8, 128, 8 * 256])
    wup_d = din("wup", [8, 128, 12 * 128])
    wout_d = din("wout", [128, 8 * D])
    lruw_d = din("lruw", [128, 4 * 8 * 128])
    cossin_d = din("cossin", [128, 2 * L])
    btab_d = din("btab", [64, 8 * NTB * 64])
    wr_d = din("wr", [128, 8 * 36])
    brb_d = din("brb", [128, 36])
    gfb_d = din("gfb", [128, D])
    gffnb_d = din("gffnb", [128, D])
    w1_d = din("w1h", [32 * 128, 8 * 512])
    w3_d = din("w3h", [32 * 128, 8 * 512])
    w2_d = din("w2h", [32 * 128, 4 * 1024])
    ident_d = din("ident", [128, 128])
    iota_d = din("iota", [128, 1])
    utri_d = din("utri", [128, 128])
    out_d = nc.dram_tensor("out", [NB, L, D], F32, kind="ExternalOutput").ap()
    x1_d = nc.dram_tensor("x1s", [NTOK, D], F32, kind="Internal").ap()
    h2_d = nc.dram_tensor("h2s", [NTOK, D], BF16, kind="Internal").ap()
    xs_d = nc.dram_tensor("xss", [NROWS, D], BF16, kind="Internal").ap()
    ys_d = nc.dram_tensor("yss", [NROWS, D], BF16, kind="Internal").ap()
    dbg_d = {}
    for name, (shape, dt) in DEBUG.items():
        dbg_d[name] = nc.dram_tensor("dbg_" + name, list(shape), dt, kind="ExternalOutput").ap()

    with ExitStack() as es:
        S = Sync(nc, es)
        uid = [0]

        def sb(es_, shape, dt, name="t"):
            uid[0] += 1
            return es_.enter_context(nc.sbuf_tensor(f"{name}{uid[0]}", list(shape), dt))

        def ps(es_, shape, dt, name="p"):
            uid[0] += 1
            return es_.enter_context(nc.psum_tensor(f"{name}{uid[0]}", list(shape), dt))

        def pe_mm(out, lhsT, rhs, start, stop, reads, writes, sig=None):
            if sig is None:
                sig = stop
            return S.op("pe", lambda: nc.tensor.matmul(out, lhsT, rhs, start=start, stop=stop),
                        reads, writes, sig)

        def pe_tr(out, in_, ident, reads, writes, sig):
            return S.op("pe", lambda: nc.tensor.transpose(out, in_, ident), reads, writes, sig)

        def act(out, in_, func, reads, writes, **kw):
            return S.op("act", lambda: nc.scalar.activation(out=out, in_=in_, func=func, **kw), reads, writes)

        def V(e, fn, reads, writes):
            return S.op(e, fn, reads, writes)

        dbg_buf = Buf("dbg")

        def dump(name, ap, reads):
            if name in dbg_d:
                S.dma("sp", dbg_d[name], ap, reads=reads, writes=[dbg_buf], group=True, semof=Buf("dump_" + name))

        identf = sb(es, [128, 128], F32, "identf"); b_identf = Buf()
        identb = sb(es, [128, 128], BF16, "identb"); b_identb = Buf()
        onesf = sb(es, [128, 128], F32, "onesf"); b_onesf = Buf()
        onesb = sb(es, [128, 128], BF16, "onesb"); b_onesb = Buf()
        utri = sb(es, [128, 128], BF16, "utri"); b_utri = Buf()
        iota = sb(es, [128, 1], F32, "iota"); b_iota = Buf()
        epst = sb(es, [128, 1], F32, "eps"); b_eps = Buf()
        vecs = sb(es, [128, NV], F32, "vecs"); b_vecs = Buf()
        modfm = sb(es, [128, 48, 3], F32, "modfm"); b_modfm = Buf()
        A1 = sb(es, [128, 8, 3], F32, "A1"); b_A1 = Buf()
        lrup = sb(es, [128, 4, 16], F32, "lrup"); b_lrup = Buf()
        S.dma("sp", identf[:], ident_d, writes=[b_identf])
        S.dma("pool", identb[:], ident_d, writes=[b_identb])
        S.dma("pool", utri[:], utri_d, writes=[b_utri])
        S.dma("sp", iota[:], iota_d, writes=[b_iota])
        S.dma("sp", vecs[:], vecs_d, writes=[b_vecs])
        V("dve", lambda: nc.vector.memset(onesf[:], 1.0), [], [b_onesf])
        V("dve", lambda: nc.vector.memset(onesb[:], 1.0), [], [b_onesb])
        V("dve", lambda: nc.vector.memset(epst[:], EPS), [], [b_eps])

        with ExitStack() as es0:
            csb = sb(es0, [128, 24], F32, "cs"); b_cs = Buf()
            scs = sb(es0, [128, 24], F32, "scs"); b_scs = Buf()
            wm = [sb(es0, [128, 8, 512], F32, "wm") for _ in range(2)]
            b_wm = [Buf(), Buf()]
            psmod = ps(es0, [128, 144], F32, "psmod"); b_psmod = Buf()
            S.dma("sp", csb[:], cs_d, writes=[b_cs])
            act(scs[:], csb[:], AF.Silu, [b_cs], [b_scs])
            for cb in range(12):
                w = wm[cb % 2]; bw = b_wm[cb % 2]
                S.dma("sp", w[:], wmod_d[:, :, cb * 512:(cb + 1) * 512], writes=[bw])
                for cc in range(4):
                    col = cb * 4 + cc
                    for k in range(8):
                        pe_mm(psmod[:, col * 3:(col + 1) * 3], w[:, k, cc * 128:(cc + 1) * 128],
                              scs[:, k * 3:(k + 1) * 3], k == 0, k == 7, [bw, b_scs], [b_psmod],
                              sig=(k == 7 and cc == 3))
            V("dve", lambda: nc.vector.tensor_tensor(
                out=modfm[:], in0=psmod[:, :].rearrange("p (a b) -> p a b", b=3),
                in1=vecs[:, V_BMOD:V_BMOD + 48].unsqueeze(2).to_broadcast([128, 48, 3]), op=ALU.add),
              [b_psmod, b_vecs], [b_modfm])
            V("dve", lambda: nc.vector.scalar_tensor_tensor(
                out=A1[:], in0=modfm[:, 8:16, :], scalar=1.0,
                in1=vecs[:, V_GMIX:V_GMIX + 8].unsqueeze(2).to_broadcast([128, 8, 3]),
                op0=ALU.add, op1=ALU.mult), [b_modfm, b_vecs], [b_A1])
            act(lrup[:, 2, :], vecs[:, V_LAM:V_LAM + 16], AF.Exp, [b_vecs], [b_lrup], scale=-1.0)
            act(lrup[:, 3, :], lrup[:, 2, :], AF.Ln, [b_lrup], [b_lrup], bias=1.0)
            V("dve", lambda: nc.vector.tensor_scalar(out=lrup[:, 0, :], in0=lrup[:, 3, :], scalar1=-8.0, scalar2=None,
                                                     op0=ALU.mult), [b_lrup], [b_lrup])
            V("dve", lambda: nc.vector.tensor_scalar(out=lrup[:, 1, :], in0=lrup[:, 3, :], scalar1=-16.0, scalar2=None,
                                                     op0=ALU.mult), [b_lrup], [b_lrup])
            dump("modfm", modfm[:, :, :].rearrange("p a b -> p (a b)"), [b_modfm])
            S.barrier()

        def bcast_row(es_, v, j, psb, b_psb, diag, b_diag):
            for k in range(8):
                V("dve", lambda: nc.vector.tensor_scalar(out=diag[k % 2][:], in0=identf[:],
                                                         scalar1=modfm[:, v * 8 + k, j:j + 1], scalar2=None,
                                                         op0=ALU.mult), [b_identf, b_modfm], [b_diag[k % 2]])
                pe_mm(psb[:, k * 128:(k + 1) * 128], onesf[:], diag[k % 2][:], True, True,
                      [b_onesf, b_diag[k % 2]], [b_psb], sig=True)

        Mall = sb(es, [128, 32, 32], BF16, "Mall"); b_Mall = Buf()
        oh1all = sb(es, [128, 32, 32], BF16, "oh1"); b_oh1 = Buf()
        gates = sb(es, [128, 32, 2], F32, "gates"); b_gates = Buf()
        dest_f = sb(es, [128, 32, 2], F32, "destf"); b_destf = Buf()
        dest_i = sb(es, [128, 64], I32, "desti"); b_desti = Buf()
        b_x1d = Buf("x1d"); b_h2d = Buf("h2d"); b_xsd = Buf("xsd"); b_ysd = Buf("ysd"); b_outd = Buf("outd")
        zt = sb(es, [128, 2, D], BF16, "zt"); b_zt = Buf()
        V("dve", lambda: nc.vector.memset(zt[:], 0.0), [], [b_zt])
        for blk in range(NBLK):
            S.dma("sp", xs_d[blk * 256:(blk + 1) * 256, :].rearrange("(a p) d -> p a d", p=128), zt[:],
                  reads=[b_zt], writes=[b_xsd], group=True, semof=b_zt)

        for b in range(NB):
            with ExitStack() as esb:
                hT = sb(esb, [128, 8, LC], BF16, "hT")
                b_hT = [[Buf(f"hT{i}a"), Buf(f"hT{i}b")] for i in range(5)]
                oatt = sb(esb, [128, 4, L], BF16, "oatt")
                b_oatt = [[Buf() for _ in range(4)] for _ in range(4)]

                with ExitStack() as es1:
                    xt = [sb(es1, [128, D], F32, "xt") for _ in range(3)]
                    b_xt = [Buf() for _ in range(3)]
                    junk = sb(es1, [128, D], BF16, "junk"); b_junk = Buf()
                    xn = [sb(es1, [128, D], BF16, "xn") for _ in range(8)]
                    b_xn = [Buf() for _ in range(8)]
                    st = sb(es1, [128, 18, 3], F32, "st"); b_st = [Buf() for _ in range(18)]
                    pst = [ps(es1, [128, 512], BF16, "pst") for _ in range(4)]
                    b_pst = [Buf() for _ in range(4)]
                    ti = 0
                    for grp in range(5):
                        ntile = 4 if grp < 4 else 2
                        jmod = b if grp < 4 else 2
                        for i in range(ntile):
                            t = grp * 4 + i
                            xb_, bx_ = xt[ti % 3], b_xt[ti % 3]
                            src = xin[b, t * 128:(t + 1) * 128, :] if grp < 4 else ctxin[b, i * 128:(i + 1) * 128, :]
                            S.dma("sp", xb_[:], src, writes=[bx_])
                            act(junk[:], xb_[:], AF.Square, [bx_], [b_junk, b_st[t]], accum_out=st[:, t, 0:1])
                            act(st[:, t, 1:2], st[:, t, 0:1], AF.Sqrt, [b_st[t], b_eps], [b_st[t]],
                                scale=1.0 / D, bias=epst[:, 0:1])
                            V("dve", lambda: nc.vector.reciprocal(out=st[:, t, 2:3], in_=st[:, t, 1:2]),
                              [b_st[t]], [b_st[t]])
                            xi = (grp % 2) * 4 + i
                            act(xn[xi][:], xb_[:], AF.Copy, [bx_, b_st[t]], [b_xn[xi]], scale=st[:, t, 2:3])
                            ti += 1
                        for k in range(8):
                            pp, bp = pst[k % 4], b_pst[k % 4]
                            for i in range(ntile):
                                xi = (grp % 2) * 4 + i
                                pe_tr(pp[:, i * 128:(i + 1) * 128], xn[xi][:, k * 128:(k + 1) * 128], identb[:],
                                      [b_xn[xi], b_identb], [bp], sig=(i == ntile - 1))
                            n = ntile * 128
                            dst = hT[:, k, grp * 512:grp * 512 + n]
                            if k % 2 == 0:
                                V("dve", lambda: nc.vector.tensor_scalar(
                                    out=dst, in0=pp[:, 0:n], scalar1=A1[:, k, jmod:jmod + 1],
                                    scalar2=modfm[:, k, jmod:jmod + 1], op0=ALU.mult, op1=ALU.add),
                                  [bp, b_A1, b_modfm], [b_hT[grp][0]])
                            else:
                                act(dst, pp[:, 0:n], AF.Identity, [bp, b_A1, b_modfm], [b_hT[grp][1]],
                                    scale=A1[:, k, jmod:jmod + 1], bias=modfm[:, k, jmod:jmod + 1])
                    if b == 0:
                        dump("hT", hT[:, :, :].rearrange("p a b -> p (a b)"), [x for l_ in b_hT for x in l_])
                    S.barrier()
                if STOP_AFTER == "s1":
                    break

                with ExitStack() as es2:
                    cs_t = sb(es2, [128, 2 * L], F32, "cossin"); b_cst = Buf()
                    btab = sb(es2, [128, 8, NTB * 64], BF16, "btab"); b_btab = Buf()
                    S.dma("sp", cs_t[:], cossin_d, writes=[b_cst])
                    S.dma("pool", btab[0:64, :, :].rearrange("p a b -> p (a b)"), btab_d, writes=[b_btab], group=True)
                    S.dma("pool", btab[64:128, :, :].rearrange("p a b -> p (a b)"), btab_d, writes=[b_btab], group=True)
                    wq = [sb(es2, [128, 8, 640], BF16, "wq") for _ in range(1)]; b_wq = [Buf()]
                    Qr = [sb(es2, [128, L], BF16, "Qr") for _ in range(1)]
                    Qp = [sb(es2, [128, L], BF16, "Qp") for _ in range(1)]
                    Kr = [sb(es2, [128, L], BF16, "Kr") for _ in range(1)]
                    Kc = [sb(es2, [128, C], BF16, "Kc") for _ in range(1)]
                    Vx = [sb(es2, [128, 18, 192], BF16, "Vx") for _ in range(1)]
                    b_Q = [[Buf() for _ in range(4)] for _ in range(1)]
                    b_Qp = [[Buf() for _ in range(4)] for _ in range(1)]
                    b_K = [Buf()]
                    b_Kc = [Buf()]
                    b_V = [[Buf() for _ in range(5)]]
                    t1 = [sb(es2, [128, 512], F32, "t1") for _ in range(2)]; b_t1 = [Buf(), Buf()]
                    t2 = [sb(es2, [128, 512], F32, "t2") for _ in range(2)]; b_t2 = [Buf(), Buf()]
                    PTc = [sb(es2, [128, 512], BF16, "PTc") for _ in range(2)]; b_PTc = [Buf(), Buf()]
                    PT = [sb(es2, [128, 320], BF16, "PT") for _ in range(3)]; b_PT = [Buf() for _ in range(3)]
                    rc = [sb(es2, [128, 512], F32, "rc") for _ in range(2)]; b_rc = [Buf(), Buf()]
                    psP = [ps(es2, [128, 512], F32, "psP") for _ in range(2)]; b_psP = [Buf(), Buf()]
                    psSc = [ps(es2, [128, 512], F32, "psSc") for _ in range(2)]; b_psSc = [Buf(), Buf()]
                    psS = [ps(es2, [128, 512], F32, "psS") for _ in range(2)]; b_psS = [Buf(), Buf()]
                    psO = [ps(es2, [128, 512], F32, "psO") for _ in range(2)]; b_psO = [Buf(), Buf()]
                    V("dve", lambda: nc.vector.memset(Vx[0][:, :, 64:128], 1.0), [], b_V[0])
                    pcnt = [0]

                    def nextP():
                        i = pcnt[0] % 2
                        pcnt[0] += 1
                        return psP[i], b_psP[i]

                    cnt_t = [0]
                    for hp in range(ATT_NHP):
                        par = 0
                        w = wq[par]; bw = b_wq[par]
                        S.dma("pool", w[:, :, :].rearrange("p a b -> p (a b)"), wqkv_d[hp], writes=[bw])
                        for blk in range(4 if ATT_PARTS & 1 else 0):
                            tok = slice(blk * 512, (blk + 1) * 512)
                            for which in range(2):
                                c0 = which * 256
                                pa, bpa = nextP()
                                for k in range(8):
                                    pe_mm(pa[:], w[:, k, c0:c0 + 128], hT[:, k, tok], k == 0, k == 7,
                                          [bw, *b_hT[blk]], [bpa])
                                pb, bpb = nextP()
                                for k in range(8):
                                    pe_mm(pb[:], w[:, k, c0 + 128:c0 + 256], hT[:, k, tok], k == 0, k == 7,
                                          [bw, *b_hT[blk]], [bpb])
                                ii = cnt_t[0] % 2
                                cnt_t[0] += 1
                                sc_ = 0.125 if which == 0 else 1.0
                                dstb = b_Q[par][blk] if which == 0 else b_K[par]
                                dst = (Qr if which == 0 else Kr)[par][:, tok]
                                V("dve", lambda: nc.vector.scalar_tensor_tensor(
                                    out=t1[ii][:], in0=pa[:], scalar=sc_, in1=cs_t[:, tok], op0=ALU.mult, op1=ALU.mult),
                                  [bpa, b_cst], [b_t1[ii]])
                                if which == 0 and (ATT_PARTS & 16):
                                    V("dve", lambda: nc.vector.tensor_scalar(out=Qp[par][:, tok], in0=pa[:], scalar1=0.125, scalar2=None,
                                                                             op0=ALU.mult), [bpa], [b_Qp[par][blk]])
                                V("dve", lambda: nc.vector.scalar_tensor_tensor(
                                    out=t2[ii][:], in0=pb[:], scalar=sc_, in1=cs_t[:, L + blk * 512:L + (blk + 1) * 512],
                                    op0=ALU.mult, op1=ALU.mult), [bpb, b_cst], [b_t2[ii]])
                                V("pool" if ATT_PARTS & 8 else "dve", lambda: (nc.gpsimd if ATT_PARTS & 8 else nc.vector).tensor_tensor(out=dst, in0=t1[ii][:], in1=t2[ii][:], op=ALU.add),
                                  [b_t1[ii], b_t2[ii]], [dstb])
                        if ATT_PARTS & 2:
                            pa, bpa = nextP()
                            for k in range(8):
                                pe_mm(pa[:, 0:C], w[:, k, 256:384], hT[:, k, L:LC], k == 0, k == 7, [bw, *b_hT[4]], [bpa])
                            act(Kc[par][:], pa[:, 0:C], AF.Copy, [bpa], [b_Kc[par]])
                        for g4 in range(5 if ATT_PARTS & 4 else 0):
                            nch = 4 if g4 < 4 else 2
                            pa, bpa = nextP()
                            for i in range(nch):
                                ch = g4 * 4 + i
                                for k in range(8):
                                    pe_mm(pa[:, i * 128:(i + 1) * 128], hT[:, k, ch * 128:(ch + 1) * 128],
                                          w[:, k, 512:640], k == 0, k == 7, [bw, *b_hT[g4]], [bpa],
                                          sig=(k == 7 and i == nch - 1))
                            src = pa[:, 0:nch * 128].rearrange("p (c a d) -> p c a d", a=2, d=64)
                            dstv = Vx[par][:, g4 * 4:g4 * 4 + nch, :].rearrange("p c (a d) -> p c a d", d=64)[:, :, 0::2, :]
                            if g4 % 2 == 0:
                                act(dstv, src, AF.Copy, [bpa], [b_V[par][g4]])
                            else:
                                V("dve", lambda: nc.vector.tensor_copy(out=dstv, in_=src), [bpa], [b_V[par][g4]])

                        if b == 0 and hp == 0:
                            dump("Qp", Qp[0][:], b_Qp[0])
                            dump("Qr", Qr[0][:], b_Q[0])
                            dump("Kr", Kr[0][:], b_K)
                            dump("Kc", Kc[0][:], b_Kc)
                            dump("Vx", Vx[0][:, :, :].rearrange("p a b -> p (a b)"), b_V[0])
                        units = []
                        for e in range(2):
                            for qb in range(4):
                                units.append((e, qb))
                        ucnt = [0]
                        for (e, qb) in units[int(_os.environ.get("ATT_USTART", "0")):][:ATT_NUNITS]:
                            h = hp * 2 + e
                            pr = slice(64 * e, 64 * e + 64)
                            qs = slice(qb * 512, (qb + 1) * 512)
                            vcols = slice(0, 128) if e == 0 else slice(64, 192)
                            oi = ucnt[0] % 2
                            ucnt[0] += 1
                            pO, bO = psO[oi], b_psO[oi]
                            for c in range(2):
                                pS, bS = psSc[c], b_psSc[c]
                                pe_mm(pS[:], Kc[par][pr, c * 128:(c + 1) * 128], Qp[par][pr, qs], True, True,
                                      [b_Kc[par], b_Qp[par][qb]], [bS])
                                act(PTc[c][:], pS[:], AF.Exp, [bS], [b_PTc[c]])
                            if b == 0 and hp == 0 and e == 0 and qb == 0:
                                dump("PTc", PTc[0][:], [b_PTc[0]])
                            for c in range(2):
                                pe_mm(pO[:], Vx[par][:, 16 + c, vcols], PTc[c][:], c == 0, False,
                                      [b_V[par][4], b_PTc[c]], [bO], sig=(c == 1))
                            rows = list(range(qb * 8, qb * 8 + ATT_NROWS))
                            plan = []
                            for r in rows:
                                rs = min(max(r - 4, 0), 24)
                                dr0 = rs - r + 7
                                chunks = []
                                if rs % 2 == 0:
                                    for j in range(4):
                                        chunks.append(((rs + 2 * j) // 2, 1 + dr0 + 2 * j))
                                else:
                                    assert dr0 == 3
                                    c0 = (rs - 1) // 2
                                    chunks.append((c0, 17))
                                    for j in range(1, 4):
                                        chunks.append((c0 + j, dr0 + 2 * j))
                                    chunks.append((c0 + 4, 19))
                                plan.append((r, chunks))

                            def emit_qk(idx):
                                r, chunks = plan[idx]
                                si = idx % 2
                                pS, bS = psS[si], b_psS[si]
                                qcol = slice(r * 64, (r + 1) * 64)
                                for j, (kc, blk0) in enumerate(chunks):
                                    o = pS[:, j * 64:(j + 1) * 64]
                                    pe_mm(o, Kr[par][pr, kc * 128:(kc + 1) * 128], Qr[par][pr, qcol], True, False,
                                          [b_K[par], b_Q[par][qb]], [bS], sig=False)
                                    lt = btab[pr, h, blk0 * 64:(blk0 + 2) * 64]
                                    pe_mm(o, lt, identb[pr, pr], False, True, [b_btab, b_identb], [bS],
                                          sig=(j == len(chunks) - 1))
                                n = len(chunks) * 64
                                pi = idx % 3
                                act(PT[pi][:, 0:n], pS[:, 0:n], AF.Exp, [bS], [b_PT[pi]])

                            def emit_pv(idx):
                                r, chunks = plan[idx]
                                pi = idx % 3
                                rr = r - qb * 8
                                for j, (kc, _) in enumerate(chunks):
                                    last = (j == len(chunks) - 1)
                                    pe_mm(pO[:, rr * 64:(rr + 1) * 64], Vx[par][:, kc, vcols], PT[pi][:, j * 64:(j + 1) * 64],
                                          False, last and idx == len(plan) - 1, [b_V[par][kc // 4], b_PT[pi]], [bO], sig=last)

                            if plan:
                                emit_qk(0)
                            for idx in range(len(plan)):
                                if idx + 1 < len(plan):
                                    emit_qk(idx + 1)
                                emit_pv(idx)
                            dn = slice(64, 128) if e == 0 else slice(0, 64)
                            if not ATT_NORM:
                                continue
                            V("dve", lambda: nc.vector.reciprocal(out=rc[oi][pr, :], in_=pO[dn, :]), [bO], [b_rc[oi]])
                            V("dve", lambda: nc.vector.tensor_tensor(out=oatt[pr, hp, qs], in0=pO[pr, :], in1=rc[oi][pr, :],
                                                                     op=ALU.mult), [bO, b_rc[oi]], [b_oatt[hp][qb]])
                    if b == 0:
                        dump("oatt", oatt[:, :, :].rearrange("p a b -> p (a b)"), [x for l_ in b_oatt for x in l_])
                    S.barrier()
                if STOP_AFTER == "s2":
                    break

                olru = sb(esb, [128, 8, L], BF16, "olru")
                b_olru = [Buf() for _ in range(8)]
                with ExitStack() as es3:
                    TL = LC + 3
                    wl = [sb(es3, [128, 8, 256], BF16, "wl") for _ in range(2)]; b_wl = [Buf(), Buf()]
                    lw = sb(es3, [128, 4, 8, 128], BF16, "lw"); b_lw = Buf()
                    S.dma("pool", lw[:, :, :, :].rearrange("p a b c -> p (a b c)"), lruw_d, writes=[b_lw])
                    LXp = sb(es3, [128, TL + 3], F32, "LXp"); b_LXp = Buf()
                    xc = sb(es3, [128, TL], F32, "xc"); b_xc = Buf()
                    xcb = [sb(es3, [128, TL], BF16, "xcb") for _ in range(2)]; b_xcb = [Buf(), Buf()]
                    av = sb(es3, [128, TL], F32, "av"); b_av = Buf()
                    wv = sb(es3, [128, TL], F32, "wv"); b_wv = Buf()
                    iv = sb(es3, [128, TL], F32, "iv"); b_iv = Buf()
                    hv = [sb(es3, [128, TL], F32, "hv") for _ in range(2)]; b_hv = [Buf(), Buf()]
                    gl = sb(es3, [128, L], BF16, "gl"); b_gl = Buf()
                    psA = [ps(es3, [128, 512], F32, "psA") for _ in range(4)]; b_psA = [Buf() for _ in range(4)]
                    pacnt = [0]

                    def nextA():
                        i = pacnt[0] % 4
                        pacnt[0] += 1
                        return psA[i], b_psA[i]

                    V("dve", lambda: nc.vector.memset(LXp[:], 0.0), [], [b_LXp])

                    def load_wl(n):
                        S.dma("pool", wl[n % 2][:, :, :].rearrange("p a b -> p (a b)"), wlxlg_d[n], writes=[b_wl[n % 2]])

                    def lx_conv(n):
                        w = wl[n % 2]; bw = b_wl[n % 2]
                        for blk in range(5):
                            nt = 512 if blk < 4 else C
                            tok = slice(blk * 512, blk * 512 + nt)
                            pa, bpa = nextA()
                            for k in range(8):
                                pe_mm(pa[:, 0:nt], w[:, k, 0:128], hT[:, k, tok], k == 0, k == 7, [bw, *b_hT[blk]], [bpa])
                            d0 = 261 + blk * 512 if blk < 4 else 2
                            act(LXp[:, d0:d0 + nt], pa[:, 0:nt], AF.Copy, [bpa], [b_LXp])
                        cw = lambda j: vecs[:, V_CONVW + j * 8 + n:V_CONVW + j * 8 + n + 1]
                        V("dve", lambda: nc.vector.tensor_scalar(out=xc[:], in0=LXp[:, 0:TL], scalar1=cw(0),
                                                                 scalar2=vecs[:, V_CONVB + n:V_CONVB + n + 1],
                                                                 op0=ALU.mult, op1=ALU.add), [b_LXp, b_vecs], [b_xc])
                        for j in range(1, 3):
                            V("dve", lambda: nc.vector.scalar_tensor_tensor(out=xc[:], in0=LXp[:, j:j + TL], scalar=cw(j),
                                                                            in1=xc[:], op0=ALU.mult, op1=ALU.add),
                              [b_LXp, b_vecs, b_xc], [b_xc])
                        V("dve", lambda: nc.vector.scalar_tensor_tensor(out=xcb[n % 2][:], in0=LXp[:, 3:3 + TL], scalar=cw(3),
                                                                        in1=xc[:], op0=ALU.mult, op1=ALU.add),
                          [b_LXp, b_vecs, b_xc], [b_xcb[n % 2]])

                    load_wl(0)
                    lx_conv(0)
                    for n in range(8):
                        w = wl[n % 2]; bw = b_wl[n % 2]
                        xb_ = xcb[n % 2]; bxb = b_xcb[n % 2]
                        if n + 1 < 8:
                            load_wl(n + 1)
                        if b == 0 and n == 0:
                            dump("xc0", xb_[:], [bxb])
                        for dr in range(2):
                            di = dr * 8 + n
                            for blk in range(5):
                                nt = 512 if blk < 4 else TL - 2048
                                tok = slice(blk * 512, blk * 512 + nt)
                                pr_, bpr = nextA()
                                pe_mm(pr_[:, 0:nt], lw[:, dr, n, :], xb_[:, tok], True, True, [b_lw, bxb], [bpr])
                                pi_, bpi = nextA()
                                pe_mm(pi_[:, 0:nt], lw[:, 2 + dr, n, :], xb_[:, tok], True, True, [b_lw, bxb], [bpi])
                                act(av[:, tok], pr_[:, 0:nt], AF.Sigmoid, [bpr, b_vecs], [b_av],
                                    bias=vecs[:, V_BA + di:V_BA + di + 1])
                                act(iv[:, tok], pi_[:, 0:nt], AF.Sigmoid, [bpi, b_vecs], [b_iv],
                                    bias=vecs[:, V_BX + di:V_BX + di + 1])
                            act(wv[:], av[:], AF.Exp, [b_av, b_lrup], [b_wv], scale=lrup[:, 1, di:di + 1])
                            act(av[:], av[:], AF.Exp, [b_av, b_lrup], [b_av], scale=lrup[:, 0, di:di + 1])
                            act(wv[:], wv[:], AF.Sqrt, [b_wv], [b_wv], scale=-1.0, bias=1.0)
                            V("pool", lambda: nc.gpsimd.tensor_tensor(out=iv[:], in0=iv[:], in1=xb_[:], op=ALU.mult),
                              [b_iv, bxb], [b_iv])
                            V("dve", lambda: nc.vector.tensor_tensor(out=wv[:], in0=iv[:], in1=wv[:], op=ALU.mult),
                              [b_iv, b_wv], [b_wv])
                            hh = hv[dr]; bh = b_hv[dr]
                            if dr == 0:
                                V("dve", lambda: nc.vector.tensor_tensor_scan(
                                    out=hh[:, 0:C], data0=av[:, 0:C], data1=wv[:, 0:C], initial=0.0,
                                    op0=ALU.mult, op1=ALU.add), [b_av, b_wv], [bh])
                                V("dve", lambda: nc.vector.tensor_tensor_scan(
                                    out=hh[:, C + 3:TL], data0=av[:, C + 3:TL], data1=wv[:, C + 3:TL],
                                    initial=hh[:, C - 1:C], op0=ALU.mult, op1=ALU.add), [b_av, b_wv, bh], [bh])
                                if n + 1 < 8:
                                    lx_conv(n + 1)
                            else:
                                V("dve", lambda: nc.vector.tensor_tensor_scan(
                                    out=hh[:, C - 1::-1], data0=av[:, C - 1::-1], data1=wv[:, C - 1::-1], initial=0.0,
                                    op0=ALU.mult, op1=ALU.add), [b_av, b_wv], [bh])
                                V("dve", lambda: nc.vector.tensor_tensor_scan(
                                    out=hh[:, TL - 1:C + 2:-1], data0=av[:, TL - 1:C + 2:-1], data1=wv[:, TL - 1:C + 2:-1],
                                    initial=hh[:, 0:1], op0=ALU.mult, op1=ALU.add), [b_av, b_wv, bh], [bh])
                        for blk in range(4):
                            tok = slice(blk * 512, (blk + 1) * 512)
                            pa, bpa = nextA()
                            for k in range(8):
                                pe_mm(pa[:], w[:, k, 128:256], hT[:, k, tok], k == 0, k == 7, [bw, *b_hT[blk]], [bpa])
                            act(gl[:, tok], pa[:], AF.Gelu_apprx_tanh, [bpa], [b_gl])
                        V("dve", lambda: nc.vector.tensor_tensor(out=hv[0][:, C + 3:TL], in0=hv[0][:, C + 3:TL],
                                                                 in1=hv[1][:, C + 3:TL], op=ALU.add),
                          [b_hv[0], b_hv[1]], [b_hv[0]])
                        if b == 0 and n == 0:
                            dump("hsum0", hv[0][:, C + 3:TL], [b_hv[0]])
                        V("dve", lambda: nc.vector.tensor_tensor(out=olru[:, n, :], in0=hv[0][:, C + 3:TL], in1=gl[:],
                                                                 op=ALU.mult), [b_hv[0], b_gl], [b_olru[n]])
                    if b == 0:
                        dump("olru", olru[:, :, :].rearrange("p a b -> p (a b)"), b_olru)
                    S.barrier()
                if STOP_AFTER == "s3":
                    break

                yT = sb(esb, [128, 8, L], BF16, "yT")
                b_yT = [Buf() for _ in range(4)]
                with ExitStack() as es4:
                    wg_ = [sb(es4, [128, 8, 256], BF16, "wg") for _ in range(2)]; b_wg = [Buf(), Buf()]
                    wu_ = [sb(es4, [128, 12, 128], BF16, "wu") for _ in range(2)]; b_wu = [Buf(), Buf()]
                    ga_ = [sb(es4, [128, 512], F32, "ga") for _ in range(2)]; b_ga = [Buf(), Buf()]
                    gb_ = [sb(es4, [128, 512], F32, "gb") for _ in range(2)]; b_gb = [Buf(), Buf()]
                    ya_ = [sb(es4, [128, 512], F32, "ya") for _ in range(2)]; b_ya = [Buf(), Buf()]
                    yb_ = [sb(es4, [128, 512], F32, "yb") for _ in range(2)]; b_yb = [Buf(), Buf()]
                    psM = [ps(es4, [128, 512], F32, "psM") for _ in range(8)]; b_psM = [Buf() for _ in range(8)]
                    it = 0
                    for f in range(8):
                        wgt, bwg = wg_[f % 2], b_wg[f % 2]
                        wut, bwu = wu_[f % 2], b_wu[f % 2]
                        S.dma("pool", wgt[:, :, :].rearrange("p a b -> p (a b)"), wgagb_d[f], writes=[bwg])
                        S.dma("pool", wut[:, :, :].rearrange("p a b -> p (a b)"), wup_d[f], writes=[bwu])
                        for blk in range(4):
                            tok = slice(blk * 512, (blk + 1) * 512)
                            i2 = it % 2
                            p0, p1, p2, p3 = [psM[(it % 2) * 4 + q] for q in range(4)]
                            q0, q1, q2, q3 = [b_psM[(it % 2) * 4 + q] for q in range(4)]
                            it += 1
                            for k in range(8):
                                pe_mm(p0[:], wgt[:, k, 0:128], hT[:, k, tok], k == 0, k == 7, [bwg, *b_hT[blk]], [q0])
                            act(ga_[i2][:], p0[:], AF.Sigmoid, [q0], [b_ga[i2]])
                            for k in range(4):
                                pe_mm(p1[:], wut[:, k, :], oatt[:, k, tok], k == 0, k == 3, [bwu, b_oatt[k][blk]], [q1])
                            V("dve", lambda: nc.vector.tensor_tensor(out=ya_[i2][:], in0=p1[:], in1=ga_[i2][:], op=ALU.mult),
                              [q1, b_ga[i2]], [b_ya[i2]])
                            for k in range(8):
                                pe_mm(p2[:], wgt[:, k, 128:256], hT[:, k, tok], k == 0, k == 7, [bwg, *b_hT[blk]], [q2])
                            act(gb_[i2][:], p2[:], AF.Sigmoid, [q2], [b_gb[i2]])
                            for k in range(8):
                                pe_mm(p3[:], wut[:, 4 + k, :], olru[:, k, tok], k == 0, k == 7, [bwu, b_olru[k]], [q3])
                            V("dve", lambda: nc.vector.tensor_tensor(out=yb_[i2][:], in0=p3[:], in1=gb_[i2][:], op=ALU.mult),
                              [q3, b_gb[i2]], [b_yb[i2]])
                            V("pool", lambda: nc.gpsimd.tensor_tensor(out=yT[:, f, tok], in0=ya_[i2][:], in1=yb_[i2][:],
                                                                      op=ALU.add), [b_ya[i2], b_yb[i2]], [b_yT[blk]])
                    if b == 0:
                        dump("yT", yT[:, :, :].rearrange("p a b -> p (a b)"), b_yT)
                    S.barrier()
                if STOP_AFTER == "s4":
                    break

                with ExitStack() as es5:
                    wo32 = [sb(es5, [128, D], F32, "wo32") for _ in range(2)]; b_wo32 = [Buf(), Buf()]
                    wob = sb(es5, [128, 8, D], BF16, "wob"); b_wob = Buf()
                    GA1 = sb(es5, [128, D], F32, "GA1"); b_GA1 = Buf()
                    G2 = sb(es5, [128, D], F32, "G2"); b_G2 = Buf()
                    S2 = sb(es5, [128, D], F32, "S2"); b_S2 = Buf()
                    gffnb = sb(es5, [128, D], F32, "gffnb"); b_gffnb = Buf()
                    diag = [sb(es5, [128, 128], F32, "diag") for _ in range(2)]; b_diag = [Buf(), Buf()]
                    wr = sb(es5, [128, 8, 36], F32, "wr"); b_wr = Buf()
                    brb = sb(es5, [128, 36], F32, "brb"); b_brb = Buf()
                    psB = ps(es5, [128, D], F32, "psB"); b_psB = Buf()
                    psO5 = [ps(es5, [128, D], F32, "psO5") for _ in range(2)]; b_psO5 = [Buf(), Buf()]
                    psT = ps(es5, [128, D], F32, "psT"); b_psT = Buf()
                    S.dma("sp", gffnb[:], gffnb_d, writes=[b_gffnb])
                    S.dma("sp", wr[:, :, :].rearrange("p a b -> p (a b)"), wr_d, writes=[b_wr])
                    S.dma("sp", brb[:], brb_d, writes=[b_brb])
                    bcast_row(es5, 2, b, psB, b_psB, diag, b_diag)
                    V("dve", lambda: nc.vector.tensor_copy(out=GA1[:], in_=psB[:]), [b_psB], [b_GA1])
                    bcast_row(es5, 4, b, psB, b_psB, diag, b_diag)
                    V("dve", lambda: nc.vector.scalar_tensor_tensor(out=G2[:], in0=psB[:], scalar=1.0, in1=gffnb[:],
                                                                    op0=ALU.add, op1=ALU.mult), [b_psB, b_gffnb], [b_G2])
                    bcast_row(es5, 3, b, psB, b_psB, diag, b_diag)
                    V("dve", lambda: nc.vector.tensor_copy(out=S2[:], in_=psB[:]), [b_psB], [b_S2])
                    for kk in range(8):
                        S.dma("sp", wo32[kk % 2][:], wout_d[:, kk * D:(kk + 1) * D], writes=[b_wo32[kk % 2]])
                        V("dve", lambda: nc.vector.tensor_tensor(out=wob[:, kk, :], in0=wo32[kk % 2][:], in1=GA1[:],
                                                                 op=ALU.mult), [b_wo32[kk % 2], b_GA1], [b_wob])
                    xt5 = [sb(es5, [128, D], F32, "xt5") for _ in range(2)]; b_xt5 = [Buf(), Buf()]
                    x1t = xt5; b_x1t = b_xt5
                    h2t = [sb(es5, [128, D], F32, "h2t") for _ in range(2)]; b_h2t = [Buf(), Buf()]
                    h2b = [sb(es5, [128, D], BF16, "h2b") for _ in range(2)]; b_h2b = [Buf(), Buf()]
                    h2T = sb(es5, [128, 8, 128], F32, "h2T"); b_h2T = Buf()
                    junk5 = sb(es5, [128, D], BF16, "junk5"); b_junk5 = Buf()
                    st5 = sb(es5, [128, 16, 3], F32, "st5"); b_st5 = [Buf() for _ in range(16)]
                    rt = sb(es5, [128, 2, 96], F32, "rt"); b_rt = [Buf() for _ in range(2)]
                    for j in range(16):
                        tg = b * 16 + j
                        i2 = j % 2
                        tsl = slice(j * 128, (j + 1) * 128)
                        S.dma("sp", xt5[i2][:], xin[b, tsl, :], writes=[b_xt5[i2]])
                        pO, bO = psO5[i2], b_psO5[i2]
                        for hf in range(2):
                            for k in range(8):
                                pe_mm(pO[:, hf * 512:(hf + 1) * 512], yT[:, k, tsl], wob[:, k, hf * 512:(hf + 1) * 512],
                                      k == 0, k == 7, [b_yT[j // 4], b_wob], [bO], sig=(k == 7 and hf == 1))
                        V("dve", lambda: nc.vector.tensor_tensor(out=x1t[i2][:], in0=pO[:], in1=xt5[i2][:], op=ALU.add),
                          [bO, b_xt5[i2]], [b_xt5[i2]])
                        S.dma("pool", x1_d[tg * 128:(tg + 1) * 128, :], x1t[i2][:], reads=[b_x1t[i2]], writes=[b_x1d], group=True, semof=b_x1t[i2])
                        act(junk5[:], x1t[i2][:], AF.Square, [b_x1t[i2]], [b_junk5, b_st5[j]], accum_out=st5[:, j, 0:1])
                        act(st5[:, j, 1:2], st5[:, j, 0:1], AF.Sqrt, [b_st5[j], b_eps], [b_st5[j]], scale=1.0 / D,
                            bias=epst[:, 0:1])
                        V("dve", lambda: nc.vector.reciprocal(out=st5[:, j, 2:3], in_=st5[:, j, 1:2]), [b_st5[j]], [b_st5[j]])
                        V("dve", lambda: nc.vector.scalar_tensor_tensor(out=h2t[i2][:], in0=x1t[i2][:], scalar=st5[:, j, 2:3],
                                                                        in1=G2[:], op0=ALU.mult, op1=ALU.mult),
                          [b_x1t[i2], b_st5[j], b_G2], [b_h2t[i2]])
                        V("pool", lambda: nc.gpsimd.tensor_tensor(out=h2t[i2][:], in0=h2t[i2][:], in1=S2[:], op=ALU.add),
                          [b_h2t[i2], b_S2], [b_h2t[i2]])
                        act(h2b[i2][:], h2t[i2][:], AF.Copy, [b_h2t[i2]], [b_h2b[i2]])
                        S.dma("pool", h2_d[tg * 128:(tg + 1) * 128, :], h2b[i2][:], reads=[b_h2b[i2]], writes=[b_h2d], group=True, semof=b_h2b[i2])
                        for k in range(8):
                            pe_tr(psT[:, k * 128:(k + 1) * 128], h2t[i2][:, k * 128:(k + 1) * 128], identf[:],
                                  [b_h2t[i2], b_identf], [b_psT], sig=(k == 7))
                        act(h2T[:, :, :].rearrange("p a b -> p (a b)"), psT[:], AF.Copy, [b_psT], [b_h2T])
                        for k in range(8):
                            pe_mm(psB[:, 0:36], h2T[:, k, :], wr[:, k, :], k == 0, k == 7, [b_h2T, b_wr], [b_psB])
                        R_ = rt[:, j % 2, :]
                        br_ = b_rt[j % 2]
                        lg = R_[:, 0:4]; le = R_[:, 4:36]
                        V("dve", lambda: nc.vector.tensor_tensor(out=R_[:, 0:36], in0=psB[:, 0:36], in1=brb[:], op=ALU.add),
                          [b_psB, b_brb], [br_])
                        if tg == 0:
                            dump("logit0", R_[:, 0:36], [br_])
                        mg = R_[:, 36:37]; nmg = R_[:, 37:38]; sg = R_[:, 38:39]; ptop = R_[:, 39:40]
                        V("dve", lambda: nc.vector.tensor_reduce(out=mg, in_=lg, axis=AX.X, op=ALU.max), [br_], [br_])
                        V("dve", lambda: nc.vector.tensor_scalar(out=nmg, in0=mg, scalar1=-1.0, scalar2=None, op0=ALU.mult),
                          [br_], [br_])
                        act(R_[:, 40:44], lg, AF.Exp, [br_], [br_], bias=nmg, accum_out=sg)
                        V("dve", lambda: nc.vector.reciprocal(out=ptop, in_=sg), [br_], [br_])
                        V("dve", lambda: nc.vector.tensor_scalar(out=R_[:, 44:48], in0=lg, scalar1=mg, scalar2=None,
                                                                 op0=ALU.is_equal), [br_], [br_])
                        V("dve", lambda: nc.vector.tensor_scalar(out=R_[:, 44:48], in0=R_[:, 44:48], scalar1=-1.0, scalar2=1e30,
                                                                 op0=ALU.add, op1=ALU.mult), [br_], [br_])
                        lem = R_[:, 48:80]
                        V("dve", lambda: nc.vector.tensor_tensor(
                            out=lem.rearrange("p (g e) -> p g e", e=8), in0=le.rearrange("p (g e) -> p g e", e=8),
                            in1=R_[:, 44:48].unsqueeze(2).to_broadcast([128, 4, 8]), op=ALU.add), [br_], [br_])
                        top8 = R_[:, 80:88]
                        V("dve", lambda: nc.vector.max(out=top8, in_=lem), [br_], [br_])
                        V("dve", lambda: nc.vector.tensor_scalar(out=oh1all[:, tg, :], in0=lem, scalar1=top8[:, 0:1], scalar2=None,
                                                                 op0=ALU.is_equal), [br_], [b_oh1])
                        V("dve", lambda: nc.vector.tensor_scalar(out=Mall[:, tg, :], in0=lem, scalar1=top8[:, 1:2], scalar2=None,
                                                                 op0=ALU.is_ge), [br_], [b_Mall])
                        dlt = R_[:, 88:89]; ew = R_[:, 89:90]; w1_ = R_[:, 90:91]
                        V("dve", lambda: nc.vector.tensor_tensor(out=dlt, in0=top8[:, 1:2], in1=top8[:, 0:1], op=ALU.subtract),
                          [br_], [br_])
                        act(ew, dlt, AF.Exp, [br_], [br_])
                        V("dve", lambda: nc.vector.tensor_scalar(out=ew, in0=ew, scalar1=1.0, scalar2=None, op0=ALU.add),
                          [br_], [br_])
                        V("dve", lambda: nc.vector.reciprocal(out=w1_, in_=ew), [br_], [br_])
                        V("dve", lambda: nc.vector.tensor_tensor(out=gates[:, tg, 0:1], in0=w1_, in1=ptop, op=ALU.mult),
                          [br_], [b_gates])
                        V("dve", lambda: nc.vector.tensor_tensor(out=gates[:, tg, 1:2], in0=ptop, in1=gates[:, tg, 0:1],
                                                                 op=ALU.subtract), [br_, b_gates], [b_gates])
                    S.barrier()
            if STOP_AFTER in ("s1", "s2", "s3", "s4"):
                break

        if STOP_AFTER is None or STOP_AFTER in ("route", "moe"):
            blk_i = sb(es, [128, NBLK], I32, "blki"); b_blki = Buf()
            with ExitStack() as esr:
                psC = ps(esr, [128, 32], F32, "psC"); b_psC = Buf()
                psR = [ps(esr, [128, 32], F32, "psR") for _ in range(2)]; b_psR = [Buf(), Buf()]
                cn = sb(esr, [128, 8, 32], F32, "cn"); b_cn = Buf()
                cni = sb(esr, [128, 32], I32, "cni"); b_cni = Buf()
                cmp_ = sb(esr, [128, NBLK, 32], F32, "cmp"); b_cmp = Buf()
                bst = sb(esr, [128, NBLK], F32, "bst"); b_bst = Buf()
                bs0 = sb(esr, [128, NBLK], F32, "bs0"); b_bs0 = Buf()
                blk_f = sb(esr, [128, NBLK], F32, "blkf"); b_blkf = Buf()
                tmp = sb(esr, [128, 2, 32], F32, "tmpr"); b_tmp = [Buf(), Buf()]
                for t in range(32):
                    pe_mm(psC[:], onesb[:], Mall[:, t, :], t == 0, t == 31, [b_onesb, b_Mall], [b_psC])
                V("dve", lambda: nc.vector.tensor_copy(out=cn[:, 0, :], in_=psC[:]), [b_psC], [b_cn])
                V("dve", lambda: nc.vector.tensor_scalar(out=cn[:, 1, :], in0=cn[:, 0, :], scalar1=255.0, scalar2=None, op0=ALU.add),
                  [b_cn], [b_cn])
                V("dve", lambda: nc.vector.tensor_copy(out=cni[:], in_=cn[:, 1, :]), [b_cn], [b_cni])
                V("dve", lambda: nc.vector.tensor_scalar(out=cni[:], in0=cni[:], scalar1=8, scalar2=8,
                                                         op0=ALU.arith_shift_right, op1=ALU.logical_shift_left), [b_cni], [b_cni])
                V("dve", lambda: nc.vector.tensor_copy(out=cn[:, 2, :], in_=cni[:]), [b_cni], [b_cn])
                V("dve", lambda: nc.vector.tensor_tensor_scan(out=cn[:, 3, :], data0=onesf[:, 0:32], data1=cn[:, 2, :],
                                                              initial=0.0, op0=ALU.mult, op1=ALU.add),
                  [b_cn, b_onesf], [b_cn])
                V("dve", lambda: nc.vector.tensor_tensor(out=cn[:, 4, :], in0=cn[:, 3, :], in1=cn[:, 2, :], op=ALU.subtract),
                  [b_cn], [b_cn])
                V("dve", lambda: nc.vector.tensor_scalar(out=bs0[:], in0=onesf[:, 0:NBLK], scalar1=256.0, scalar2=None, op0=ALU.mult),
                  [b_onesf], [b_bs0])
                V("dve", lambda: nc.vector.tensor_tensor_scan(out=bst[:], data0=onesf[:, 0:NBLK], data1=bs0[:], initial=-256.0,
                                                              op0=ALU.mult, op1=ALU.add), [b_bs0, b_onesf], [b_bst])
                V("dve", lambda: nc.vector.tensor_tensor(
                    out=cmp_[:], in0=cn[:, 3, :].unsqueeze(1).to_broadcast([128, NBLK, 32]),
                    in1=bst[:].unsqueeze(2).to_broadcast([128, NBLK, 32]), op=ALU.is_le), [b_cn, b_bst], [b_cmp])
                V("dve", lambda: nc.vector.tensor_reduce(out=blk_f[:], in_=cmp_[:], axis=AX.X, op=ALU.add), [b_cmp], [b_blkf])
                V("dve", lambda: nc.vector.tensor_scalar(out=blk_f[:], in0=blk_f[:], scalar1=31.0, scalar2=128.0,
                                                         op0=ALU.min, op1=ALU.mult), [b_blkf], [b_blkf])
                V("dve", lambda: nc.vector.tensor_scalar(out=blk_f[:], in0=blk_f[:], scalar1=iota[:, 0:1], scalar2=None,
                                                         op0=ALU.add), [b_blkf, b_iota], [b_blkf])
                V("dve", lambda: nc.vector.tensor_copy(out=blk_i[:], in_=blk_f[:]), [b_blkf], [b_blki])
                dump("blkf", blk_f[:], [b_blkf])
                dump("cn", cn[:, 0:5, :].rearrange("p a b -> p (a b)"), [b_cn])
                for t in range(32):
                    pR, bR = psR[t % 2], b_psR[t % 2]
                    for t2 in range(t):
                        pe_mm(pR[:], onesb[:], Mall[:, t2, :], t2 == 0, False, [b_onesb, b_Mall], [bR], sig=False)
                    pe_mm(pR[:], utri[:], Mall[:, t, :], t == 0, True, [b_utri, b_Mall], [bR])
                    tt = tmp[:, t % 2, :]; bt_ = b_tmp[t % 2]
                    V("dve", lambda: nc.vector.tensor_tensor(out=tt, in0=pR[:], in1=cn[:, 4, :], op=ALU.add), [bR, b_cn], [bt_])
                    V("dve", lambda: nc.vector.tensor_tensor(out=cmp_[:, 0, :], in0=tt, in1=oh1all[:, t, :], op=ALU.mult),
                      [bt_, b_oh1], [b_cmp])
                    V("dve", lambda: nc.vector.tensor_reduce(out=dest_f[:, t, 0:1], in_=cmp_[:, 0, :], axis=AX.X, op=ALU.add),
                      [b_cmp], [b_destf])
                    V("dve", lambda: nc.vector.tensor_tensor(out=cmp_[:, 1, :], in0=Mall[:, t, :], in1=oh1all[:, t, :], op=ALU.subtract),
                      [b_Mall, b_oh1], [b_cmp])
                    V("dve", lambda: nc.vector.tensor_tensor(out=cmp_[:, 1, :], in0=cmp_[:, 1, :], in1=tt, op=ALU.mult),
                      [bt_, b_cmp], [b_cmp])
                    V("dve", lambda: nc.vector.tensor_reduce(out=dest_f[:, t, 1:2], in_=cmp_[:, 1, :], axis=AX.X, op=ALU.add),
                      [b_cmp], [b_destf])
                V("dve", lambda: nc.vector.tensor_copy(out=dest_i[:], in_=dest_f[:, :, :].rearrange("p a b -> p (a b)")), [b_destf], [b_desti])
                dump("destf", dest_f[:, :, :].rearrange("p a b -> p (a b)"), [b_destf])
                dump("gates", gates[:, :, :].rearrange("p a b -> p (a b)"), [b_gates])
                hl = [sb(esr, [128, D], BF16, "hl") for _ in range(2)]; b_hl = [Buf(), Buf()]
                for t in range(32):
                    S.dma("sp", hl[t % 2][:], h2_d[t * 128:(t + 1) * 128, :], reads=[b_h2d], writes=[b_hl[t % 2]])
                    for kk in range(2):
                        S.dma_ind(xs_d[:, :], bass.IndirectOffsetOnAxis(ap=dest_i[:, 2 * t + kk:2 * t + kk + 1], axis=0), hl[t % 2][:, :], None,
                                  NROWS - 1, reads=[b_hl[t % 2], b_desti], writes=[b_xsd], group=True, semof=b_hl[t % 2])
                S.barrier()

            if STOP_AFTER != "route":
                with ExitStack() as esm:
                    w1b = [sb(esm, [128, 8, 512], BF16, "w1b") for _ in range(3)]; b_w1 = [Buf() for _ in range(3)]
                    w3b = [sb(esm, [128, 8, 512], BF16, "w3b") for _ in range(3)]; b_w3 = [Buf() for _ in range(3)]
                    w2b = [sb(esm, [128, 4, 1024], BF16, "w2b") for _ in range(3)]; b_w2 = [Buf() for _ in range(3)]
                    xr = [sb(esm, [128, 2, D], BF16, "xr") for _ in range(3)]; b_xr = [Buf() for _ in range(3)]
                    xT = [sb(esm, [128, 8, 256], BF16, "xT") for _ in range(3)]; b_xT = [[Buf(), Buf()] for _ in range(3)]
                    sl = [sb(esm, [128, 256], F32, "sl") for _ in range(2)]; b_sl = [Buf(), Buf()]
                    hm = [sb(esm, [128, 4, 256], BF16, "hm") for _ in range(2)]; b_hm = [Buf(), Buf()]
                    yo = [sb(esm, [128, 2, D], BF16, "yo") for _ in range(2)]; b_yo = [[Buf(), Buf()], [Buf(), Buf()]]
                    psX = [ps(esm, [128, 1024], BF16, "psX") for _ in range(2)]; b_psX = [Buf(), Buf()]
                    psH = [ps(esm, [128, 512], F32, "psH") for _ in range(2)]; b_psH = [Buf(), Buf()]
                    psY = [ps(esm, [128, 512], F32, "psY") for _ in range(4)]; b_psY = [Buf() for _ in range(4)]
                    xcnt = [0]; hcnt = [0]; ycnt = [0]

                    def moe_w(blk):
                        i3 = blk % 3
                        off = bass.IndirectOffsetOnAxis(ap=blk_i[:, blk:blk + 1], axis=0)
                        S.dma_ind(w1b[i3][:, :, :].rearrange("p a b -> p (a b)"), None, w1_d[:, :], off, 0,
                                  reads=[b_blki], writes=[b_w1[i3]])
                        S.dma_ind(w3b[i3][:, :, :].rearrange("p a b -> p (a b)"), None, w3_d[:, :], off, 0,
                                  reads=[b_blki], writes=[b_w3[i3]])
                        S.dma_ind(w2b[i3][:, :, :].rearrange("p a b -> p (a b)"), None, w2_d[:, :], off, 0,
                                  reads=[b_blki], writes=[b_w2[i3]])

                    def moe_x(blk):
                        i2 = blk % 3
                        S.dma("sp", xr[i2][:], xs_d[blk * 256:(blk + 1) * 256, :].rearrange("(a p) d -> p a d", p=128),
                              reads=[b_xsd], writes=[b_xr[i2]])
                        for k in range(8):
                            if k % 4 == 0:
                                pX, bX = psX[(xcnt[0] // 4) % 2], b_psX[(xcnt[0] // 4) % 2]
                            for a in range(2):
                                pe_tr(pX[:, (k % 4) * 256 + a * 128:(k % 4) * 256 + (a + 1) * 128],
                                      xr[i2][:, a, k * 128:(k + 1) * 128], identb[:], [b_xr[i2], b_identb], [bX],
                                      sig=(k % 4 == 3 and a == 1))
                            xcnt[0] += 1
                            if k % 4 == 3:
                                dstx = xT[i2][:, k - 3:k + 1, :].rearrange("p a b -> p (a b)")
                                V("dve", lambda: nc.vector.tensor_copy(out=dstx, in_=pX[:]), [bX], [b_xT[i2][(k // 4) % 2]])

                    def moe_c(blk):
                        i2 = blk % 2
                        i3 = blk % 3
                        ix = blk % 3
                        for c4 in range(4):
                            pH, bH = psH[hcnt[0] % 2], b_psH[hcnt[0] % 2]
                            si = hcnt[0] % 2
                            hcnt[0] += 1
                            for k in range(8):
                                pe_mm(pH[:, 0:256], w1b[i3][:, k, c4 * 128:(c4 + 1) * 128], xT[ix][:, k, :], k == 0, k == 7,
                                      [b_w1[i3], *b_xT[ix]], [bH], sig=False)
                            for k in range(8):
                                pe_mm(pH[:, 256:512], w3b[i3][:, k, c4 * 128:(c4 + 1) * 128], xT[ix][:, k, :], k == 0, k == 7,
                                      [b_w3[i3], *b_xT[ix]], [bH])
                            act(sl[si][:], pH[:, 0:256], AF.Silu, [bH], [b_sl[si]])
                            V("dve", lambda: nc.vector.tensor_tensor(out=hm[i2][:, c4, :], in0=pH[:, 256:512], in1=sl[si][:],
                                                                     op=ALU.mult), [bH, b_sl[si]], [b_hm[i2]])
                        for a in range(2):
                            for hf in range(2):
                                pY, bY = psY[ycnt[0] % 4], b_psY[ycnt[0] % 4]
                                ycnt[0] += 1
                                for k in range(4):
                                    pe_mm(pY[:], hm[i2][:, k, a * 128:(a + 1) * 128], w2b[i3][:, k, hf * 512:(hf + 1) * 512],
                                          k == 0, k == 3, [b_hm[i2], b_w2[i3]], [bY])
                                V("dve", lambda: nc.vector.tensor_copy(out=yo[i2][:, a, hf * 512:(hf + 1) * 512], in_=pY[:]), [bY],
                                  [b_yo[i2][hf]])
                        S.dma("act", ys_d[blk * 256:(blk + 1) * 256, :].rearrange("(a p) d -> p a d", p=128), yo[i2][:],
                              reads=b_yo[i2], writes=[b_ysd], group=True, semof=b_yo[i2][0])

                    moe_w(0)
                    moe_w(1)
                    moe_x(0)
                    moe_x(1)
                    for blk in range(NBLK):
                        if blk + 2 < NBLK:
                            moe_w(blk + 2)
                            moe_x(blk + 2)
                        moe_c(blk)
                    S.barrier()

                with ExitStack() as esf:
                    GA2 = [sb(esf, [128, D], F32, "GA2") for _ in range(2)]; b_GA2 = [Buf(), Buf()]
                    gfb = sb(esf, [128, D], F32, "gfb"); b_gfb = Buf()
                    diag = [sb(esf, [128, 128], F32, "diagf") for _ in range(2)]; b_diag = [Buf(), Buf()]
                    psB = ps(esf, [128, D], F32, "psBf"); b_psB = Buf()
                    S.dma("sp", gfb[:], gfb_d, writes=[b_gfb])
                    for b in range(NB):
                        bcast_row(esf, 5, b, psB, b_psB, diag, b_diag)
                        V("dve", lambda: nc.vector.tensor_copy(out=GA2[b][:], in_=psB[:]), [b_psB], [b_GA2[b]])
                    y0 = [sb(esf, [128, D], BF16, "y0") for _ in range(2)]; b_y0 = [Buf(), Buf()]
                    y1 = [sb(esf, [128, D], BF16, "y1") for _ in range(2)]; b_y1 = [Buf(), Buf()]
                    x1l = [sb(esf, [128, D], F32, "x1l") for _ in range(2)]; b_x1l = [Buf(), Buf()]
                    mo = [sb(esf, [128, D], F32, "mo") for _ in range(2)]; b_mo = [Buf(), Buf()]
                    ot = [sb(esf, [128, D], F32, "ot") for _ in range(2)]; b_ot = [Buf(), Buf()]
                    junkf = sb(esf, [128, D], BF16, "junkf"); b_junkf = Buf()
                    stf = sb(esf, [128, 32, 3], F32, "stf"); b_stf = [Buf() for _ in range(32)]
                    for t in range(32):
                        i2 = t % 2
                        bb = t // 16
                        S.dma_ind(y0[i2][:, :], None, ys_d[:, :], bass.IndirectOffsetOnAxis(ap=dest_i[:, 2 * t:2 * t + 1], axis=0),
                                  NROWS - 1, reads=[b_ysd, b_desti], writes=[b_y0[i2]])
                        S.dma_ind(y1[i2][:, :], None, ys_d[:, :], bass.IndirectOffsetOnAxis(ap=dest_i[:, 2 * t + 1:2 * t + 2], axis=0),
                                  NROWS - 1, reads=[b_ysd, b_desti], writes=[b_y1[i2]])
                        S.dma("sp", x1l[i2][:], x1_d[t * 128:(t + 1) * 128, :], reads=[b_x1d], writes=[b_x1l[i2]])
                        act(mo[i2][:], y0[i2][:], AF.Copy, [b_y0[i2], b_gates], [b_mo[i2]], scale=gates[:, t, 0:1])
                        V("dve", lambda: nc.vector.scalar_tensor_tensor(out=mo[i2][:], in0=y1[i2][:], scalar=gates[:, t, 1:2],
                                                                        in1=mo[i2][:], op0=ALU.mult, op1=ALU.add),
                          [b_y1[i2], b_gates, b_mo[i2]], [b_mo[i2]])
                        if t == 0:
                            dump("moe0", mo[i2][:], [b_mo[i2]])
                        V("pool", lambda: nc.gpsimd.tensor_tensor(out=mo[i2][:], in0=mo[i2][:], in1=GA2[bb][:], op=ALU.mult),
                          [b_mo[i2], b_GA2[bb]], [b_mo[i2]])
                        V("dve", lambda: nc.vector.tensor_tensor(out=mo[i2][:], in0=mo[i2][:], in1=x1l[i2][:], op=ALU.add),
                          [b_mo[i2], b_x1l[i2]], [b_mo[i2]])
                        act(junkf[:], mo[i2][:], AF.Square, [b_mo[i2]], [b_junkf, b_stf[t]], accum_out=stf[:, t, 0:1])
                        act(stf[:, t, 1:2], stf[:, t, 0:1], AF.Sqrt, [b_stf[t], b_eps], [b_stf[t]], scale=1.0 / D, bias=epst[:, 0:1])
                        V("dve", lambda: nc.vector.reciprocal(out=stf[:, t, 2:3], in_=stf[:, t, 1:2]), [b_stf[t]], [b_stf[t]])
                        V("dve", lambda: nc.vector.scalar_tensor_tensor(out=ot[i2][:], in0=mo[i2][:], scalar=stf[:, t, 2:3],
                                                                        in1=gfb[:], op0=ALU.mult, op1=ALU.mult),
                          [b_mo[i2], b_stf[t], b_gfb], [b_ot[i2]])
                        S.dma("act", out_d[bb, (t % 16) * 128:(t % 16 + 1) * 128, :], ot[i2][:], reads=[b_ot[i2]], writes=[b_outd],
                              group=True, semof=b_ot[i2])
        S.barrier()
        build_program.stats = dict(nops=dict(S.nops), nwaits=S.nwaits, nsem=S.nsem)
    return nc


def _fm(v):
    return np.ascontiguousarray(np.asarray(v, np.float32).reshape(-1, 128).T)


def _kp(w):
    K, N = w.shape
    return np.ascontiguousarray(w.reshape(K // 128, 128, N).transpose(1, 0, 2))


def _swap_cols():
    idx = np.arange(64)
    half = idx // 32
    within = idx % 32
    sw = np.where(within < 16, within + 16, within - 16)
    return half * 32 + sw


def _host_consts():
    nf = 16
    inv_freq = (10000.0 ** (-np.arange(nf, dtype=np.float32) / nf)).astype(np.float32)
    t = np.arange(L)
    row = (t // 64).astype(np.float32)
    col = (t % 64).astype(np.float32)
    cos = np.zeros((128, L), np.float32)
    sin = np.zeros((128, L), np.float32)
    for p in range(128):
        d = p % 64
        pos = row if d < 32 else col
        ang = (pos * inv_freq[(d % 32) % 16]).astype(np.float32)
        sign = -1.0 if (d % 32) < 16 else 1.0
        cos[p] = np.cos(ang)
        sin[p] = sign * np.sin(ang)
    cossin = np.concatenate([cos, sin], axis=1)
    ident = np.eye(128, dtype=np.float32)
    iota = np.arange(128, dtype=np.float32).reshape(128, 1)
    utri = np.triu(np.ones((128, 128), np.float32), k=1)
    return cossin, ident, iota, utri


def _bias_table(rpb):
    cq = np.arange(64)
    c_start = np.clip(cq - 8, 0, 48)
    band = (cq[None, :] >= c_start[:, None]) & (cq[None, :] < c_start[:, None] + 16)
    dc = np.clip(cq[None, :] - cq[:, None], -15, 15) + 15
    tab = np.full((64, 8, NTB, 64), -1e30, np.float32)
    for h in range(8):
        for dr in range(15):
            vals = rpb[h, dr][dc]
            tab[:, h, 1 + dr, :] = np.where(band, vals, np.float32(-1e30))
        tab[:, h, 18, :] = tab[:, h, 1 + 3, :]
        tab[:, h, 19, :] = tab[:, h, 1 + 10, :]
    return tab.reshape(64, 8 * NTB * 64)


def _prepare(inputs):
    f = lambda k: np.asarray(inputs[k], np.float32)
    w_in = f("w_in")[0]
    K_OFF, V_OFF, LX_OFF, Q_OFF, LG_OFF, GA_OFF, GB_OFF = 0, 512, 1024, 2048, 2560, 3584, 4608
    sw = _swap_cols()
    wqkv = []
    for hp in range(4):
        cols = []
        for base in (Q_OFF, K_OFF):
            plain = np.concatenate([base + (2 * hp + e) * 64 + np.arange(64) for e in range(2)])
            swp = np.concatenate([base + (2 * hp + e) * 64 + sw for e in range(2)])
            cols += [plain, swp]
        cols.append(V_OFF + hp * 128 + np.arange(128))
        wqkv.append(_kp(w_in[:, np.concatenate(cols)]).reshape(128, 8 * 640))
    wqkv = np.stack(wqkv)
    wlxlg = np.stack([_kp(w_in[:, np.concatenate([LX_OFF + n * 128 + np.arange(128), LG_OFF + n * 128 + np.arange(128)])]
                          ).reshape(128, 8 * 256) for n in range(8)])
    wgagb = np.stack([_kp(w_in[:, np.concatenate([GA_OFF + n * 128 + np.arange(128), GB_OFF + n * 128 + np.arange(128)])]
                          ).reshape(128, 8 * 256) for n in range(8)])
    wua = _kp(f("w_up_attn")[0])
    wul = _kp(f("w_up_lru")[0])
    wup = np.stack([np.concatenate([wua[:, :, n * 128:(n + 1) * 128], wul[:, :, n * 128:(n + 1) * 128]], axis=1
                                   ).reshape(128, 12 * 128) for n in range(8)])
    wout = _kp(f("w_out")[0]).reshape(128, 8 * D)
    wa = f("lru_wa")[0]
    wx = f("lru_wx")[0]
    lruw = np.stack([wa[0], wa[1], wx[0], wx[1]])
    lruw = np.ascontiguousarray(lruw.transpose(2, 0, 1, 3)).reshape(128, 4 * 8 * 128)
    vecs = np.concatenate([
        _fm(f("g_mix")[0]), _fm(f("g_ffn")[0]),
        np.concatenate([_fm(f("conv_w")[0][j]) for j in range(4)], axis=1),
        _fm(f("conv_b")[0]),
        np.concatenate([_fm(f("lru_ba")[0][d_]) for d_ in range(2)], axis=1),
        np.concatenate([_fm(f("lru_bx")[0][d_]) for d_ in range(2)], axis=1),
        np.concatenate([_fm(f("lru_lambda")[0][d_]) for d_ in range(2)], axis=1),
        _fm(f("b_mod")[0]),
    ], axis=1)
    assert vecs.shape == (128, NV)
    wmod = _kp(f("w_mod")[0])
    wr = _kp(np.concatenate([f("router_group_w")[0], f("router_expert_w")[0]], axis=1)).reshape(128, 8 * 36)
    brb = np.ascontiguousarray(np.broadcast_to(
        np.concatenate([f("router_group_b")[0], f("router_expert_b")[0]])[None, :], (128, 36)))
    gfb = np.ascontiguousarray(np.broadcast_to(f("g_final")[None, :], (128, D)))
    gffnb = np.ascontiguousarray(np.broadcast_to(f("g_ffn")[0][None, :], (128, D)))
    w1 = f("expert_w_gate")[0]
    w3 = f("expert_w_up")[0]
    w2 = f("expert_w_down")[0]
    w1h = np.ascontiguousarray(w1.reshape(32, 8, 128, 512).transpose(0, 2, 1, 3)).reshape(32 * 128, 8 * 512)
    w3h = np.ascontiguousarray(w3.reshape(32, 8, 128, 512).transpose(0, 2, 1, 3)).reshape(32 * 128, 8 * 512)
    w2h = np.ascontiguousarray(w2.reshape(32, 4, 128, 1024).transpose(0, 2, 1, 3)).reshape(32 * 128, 4 * 1024)
    cossin, ident, iota, utri = _host_consts()
    btab = _bias_table(f("rpb")[0])
    shared = dict(wmod=wmod, vecs=vecs, wqkv=wqkv, wlxlg=wlxlg, wgagb=wgagb, wup=wup, wout=wout, lruw=lruw,
                  cossin=cossin, btab=btab, wr=wr, brb=brb, gfb=gfb, gffnb=gffnb, w1h=w1h, w3h=w3h, w2h=w2h,
                  ident=ident, iota=iota, utri=utri)
    x = f("x")
    ctx = f("ctx")
    c = f("c")
    c_ctx = f("c_ctx")
    in_maps = []
    for core in range(NCORES):
        b0 = core * NB
        cs = np.stack([c[b0], c[b0 + 1], c_ctx], axis=-1)
        cs = np.ascontiguousarray(cs.reshape(8, 128, 3).transpose(1, 0, 2)).reshape(128, 24)
        m = dict(shared)
        m["xin"] = np.ascontiguousarray(x[b0:b0 + NB])
        m["ctxin"] = np.ascontiguousarray(ctx[b0:b0 + NB])
        m["cs"] = cs
        in_maps.append(m)
    return in_maps


def kernel(**inputs):
    in_maps = _prepare(inputs)
    nc = build_program()
    res = run_bass_kernel_spmd(nc, in_maps, core_ids=list(range(NCORES)))
    out = np.concatenate([np.asarray(r["out"], np.float32) for r in res.results], axis=0)
    return out
```

```python
import numpy as np
import concourse.bass as bass
import concourse.mybir as mybir
from concourse.bass_utils import run_bass_kernel_spmd
from contextlib import ExitStack

F32 = mybir.dt.float32
BF16 = mybir.dt.bfloat16
I32 = mybir.dt.int32
AF = mybir.ActivationFunctionType
ALU = mybir.AluOpType
AX = mybir.AxisListType

D = 1024
L = 2048
C = 256
NB = 2
NCORES = 8
LC = L + C
NTOK = NB * L
NBLK = 64
NROWS = NBLK * 256
EPS = 1e-6

V_GMIX, V_GFFN, V_CONVW, V_CONVB, V_BA, V_BX, V_LAM, V_BMOD, NV = 0, 8, 16, 48, 56, 72, 88, 104, 152
NTB = 21

DEBUG = {}
import os as _os
ATT_NHP = int(_os.environ.get("ATT_NHP", "4"))
ATT_NUNITS = int(_os.environ.get("ATT_NUNITS", "8"))
ATT_NROWS = int(_os.environ.get("ATT_NROWS", "8"))
ATT_NORM = int(_os.environ.get("ATT_NORM", "1"))
ATT_PARTS = int(_os.environ.get("ATT_PARTS", "31"))
STOP_AFTER = None


class Buf:
    __slots__ = ("name", "w", "wx", "r", "dsem", "dcnt", "grp")

    def __init__(self, name=""):
        self.name = name
        self.w = None
        self.wx = {}
        self.r = {}
        self.dsem = {}
        self.dcnt = {}
        self.grp = False


class Sync:
    ROLL = 30000

    def __init__(self, nc, es):
        self.nc = nc
        self.es = es
        self.eng = {"pe": nc.tensor, "act": nc.scalar, "dve": nc.vector, "pool": nc.gpsimd, "sp": nc.sync}
        self.sem = {}
        self.cnt = {}
        self.waited = {k: {} for k in self.eng}
        self.nsem = 0
        self.pe_sems = set()
        for k in self.eng:
            self._newsem(k)
        self.pend = []
        self.dbufs = []
        self.nops = {k: 0 for k in self.eng}
        self.nwaits = 0

    def _alloc(self, name):
        self.nsem += 1
        return self.es.enter_context(self.nc.semaphore(f"{name}{self.nsem}"))

    def _newsem(self, k):
        self.sem[k] = self._alloc("e" + k)
        self.cnt[k] = 0
        if k == "pe":
            self.pe_sems.add(id(self.sem[k]))

    def _wait(self, e, ev):
        semh, val = ev
        assert val is not None, "dependency on an unsignalled PE op"
        key = id(semh)
        if self.waited[e].get(key, 0) < val:
            self.eng[e].wait_ge(semh, val)
            self.waited[e][key] = val
            self.nwaits += 1

    def _dep1(self, e, ev, acc):
        if e == "pe" and (ev[1] is None or id(ev[0]) in self.pe_sems):
            return
        assert ev[1] is not None, "dependency on an unsignalled PE op"
        k = id(ev[0])
        if k not in acc or acc[k][1] < ev[1]:
            acc[k] = ev

    def _deps(self, e, reads, writes, group=False):
        acc = {}
        for b in reads:
            if b.w is not None:
                self._dep1(e, b.w, acc)
            for ev in b.wx.values():
                self._dep1(e, ev, acc)
        for b in writes:
            if not (group and b.grp):
                if b.w is not None:
                    self._dep1(e, b.w, acc)
                for ev in b.wx.values():
                    self._dep1(e, ev, acc)
            for ev in b.r.values():
                self._dep1(e, ev, acc)
        for ev in acc.values():
            self._wait(e, ev)

    def _mark(self, ev, reads, writes, key, group=False):
        for b in reads:
            b.r[key] = ev
        for b in writes:
            if group and b.grp:
                b.wx[key] = ev
            elif group:
                b.w = None
                b.wx = {key: ev}
            else:
                b.w = ev
                b.wx = {}
            b.grp = group
            b.r = {}

    def op(self, e, fn, reads=(), writes=(), sig=True):
        self._deps(e, reads, writes)
        ins = fn()
        self.nops[e] += 1
        if sig:
            if self.cnt[e] >= self.ROLL:
                self._newsem(e)
            self.cnt[e] += 1
            ins.then_inc(self.sem[e], 1)
            ev = [self.sem[e], self.cnt[e]]
            if e == "pe":
                for p in self.pend:
                    p[0] = self.sem[e]
                    p[1] = self.cnt[e]
                self.pend = []
        else:
            assert e == "pe"
            ev = [self.sem[e], None]
            self.pend.append(ev)
        self._mark(ev, reads, writes, e)
        return ins

    def _dma_common(self, q, issue, reads, writes, group, semof):
        d = semof if semof is not None else writes[0]
        c = "sw" if q == "pool" else "hw"
        if c not in d.dsem:
            d.dsem[c] = self._alloc("d")
            d.dcnt[c] = 0
            self.dbufs.append((d, c))
        self._deps(q, reads, writes, group=group)
        ins = issue()
        self.nops[q] += 1
        d.dcnt[c] += 16
        ins.then_inc(d.dsem[c], 16)
        ev = [d.dsem[c], d.dcnt[c]]
        self._mark(ev, reads, writes, id(d.dsem[c]), group=group)
        return ins

    def dma(self, q, out, in_, reads=(), writes=(), group=False, semof=None):
        return self._dma_common(q, lambda: self.eng[q].dma_start(out=out, in_=in_), reads, writes, group, semof)

    def dma_ind(self, out, out_off, in_, in_off, bound, reads=(), writes=(), group=False, semof=None):
        def issue():
            return self.nc.gpsimd.indirect_dma_start(out=out, out_offset=out_off, in_=in_, in_offset=in_off)
        return self._dma_common("pool", issue, reads, writes, group, semof)

    def barrier(self):
        assert not self.pend
        evs = [[self.sem[k], self.cnt[k]] for k in self.eng if k != "sp" and self.cnt[k] > 0]
        evs += [[b.dsem[c], b.dcnt[c]] for (b, c) in self.dbufs if b.dcnt[c] > 0]
        for ev in evs:
            self._wait("sp", ev)
        if self.cnt["sp"] >= self.ROLL:
            self._newsem("sp")
        self.cnt["sp"] += 1
        self.nc.sync.nop().then_inc(self.sem["sp"], 1)
        ev = [self.sem["sp"], self.cnt["sp"]]
        for k in self.eng:
            if k != "sp":
                self._wait(k, ev)


def build_program():
    nc = bass.Bass("TRN2", target_bir_lowering=False)

    def din(name, shape, dt=F32):
        return nc.dram_tensor(name, list(shape), dt, kind="ExternalInput").ap()

    xin = din("xin", [NB, L, D])
    ctxin = din("ctxin", [NB, C, D])
    cs_d = din("cs", [128, 24])
    wmod_d = din("wmod", [128, 8, 6 * D])
    vecs_d = din("vecs", [128, NV])
    wqkv_d = din("wqkv", [4, 128, 8 * 640])
    wlxlg_d = din("wlxlg", [8, 128, 8 * 256])
    wgagb_d = din("wgagb", [8, 128, 8 * 256])
    wup_d = din("wup", [8, 128, 12 * 128])
    wout_d = din("wout", [128, 8 * D])
    lruw_d = din("lruw", [128, 4 * 8 * 128])
    cossin_d = din("cossin", [128, 2 * L])
    btab_d = din("btab", [64, 8 * NTB * 64])
    wr_d = din("wr", [128, 8 * 36])
    brb_d = din("brb", [128, 36])
    gfb_d = din("gfb", [128, D])
    gffnb_d = din("gffnb", [128, D])
    w1_d = din("w1h", [32 * 128, 8 * 512])
    w3_d = din("w3h", [32 * 128, 8 * 512])
    w2_d = din("w2h", [32 * 128, 4 * 1024])
    ident_d = din("ident", [128, 128])
    iota_d = din("iota", [128, 1])
    utri_d = din("utri", [128, 128])
    out_d = nc.dram_tensor("out", [NB, L, D], F32, kind="ExternalOutput").ap()
    x1_d = nc.dram_tensor("x1s", [NTOK, D], F32, kind="Internal").ap()
    h2_d = nc.dram_tensor("h2s", [NTOK, D], BF16, kind="Internal").ap()
    xs_d = nc.dram_tensor("xss", [NROWS, D], BF16, kind="Internal").ap()
    ys_d = nc.dram_tensor("yss", [NROWS, D], BF16, kind="Internal").ap()
    dbg_d = {}
    for name, (shape, dt) in DEBUG.items():
        dbg_d[name] = nc.dram_tensor("dbg_" + name, list(shape), dt, kind="ExternalOutput").ap()

    with ExitStack() as es:
        S = Sync(nc, es)
        uid = [0]

        def sb(es_, shape, dt, name="t"):
            uid[0] += 1
            return es_.enter_context(nc.sbuf_tensor(f"{name}{uid[0]}", list(shape), dt))

        def ps(es_, shape, dt, name="p"):
            uid[0] += 1
            return es_.enter_context(nc.psum_tensor(f"{name}{uid[0]}", list(shape), dt))

        def pe_mm(out, lhsT, rhs, start, stop, reads, writes, sig=None):
            if sig is None:
                sig = stop
            return S.op("pe", lambda: nc.tensor.matmul(out, lhsT, rhs, start=start, stop=stop),
                        reads, writes, sig)

        def pe_tr(out, in_, ident, reads, writes, sig):
            return S.op("pe", lambda: nc.tensor.transpose(out, in_, ident), reads, writes, sig)

        def act(out, in_, func, reads, writes, **kw):
            return S.op("act", lambda: nc.scalar.activation(out=out, in_=in_, func=func, **kw), reads, writes)

        def V(e, fn, reads, writes):
            return S.op(e, fn, reads, writes)

        dbg_buf = Buf("dbg")

        def dump(name, ap, reads):
            if name in dbg_d:
                S.dma("sp", dbg_d[name], ap, reads=reads, writes=[dbg_buf], group=True, semof=Buf("dump_" + name))

        identf = sb(es, [128, 128], F32, "identf"); b_identf = Buf()
        identb = sb(es, [128, 128], BF16, "identb"); b_identb = Buf()
        onesf = sb(es, [128, 128], F32, "onesf"); b_onesf = Buf()
        onesb = sb(es, [128, 128], BF16, "onesb"); b_onesb = Buf()
        utri = sb(es, [128, 128], BF16, "utri"); b_utri = Buf()
        iota = sb(es, [128, 1], F32, "iota"); b_iota = Buf()
        epst = sb(es, [128, 1], F32, "eps"); b_eps = Buf()
        vecs = sb(es, [128, NV], F32, "vecs"); b_vecs = Buf()
        modfm = sb(es, [128, 48, 3], F32, "modfm"); b_modfm = Buf()
        A1 = sb(es, [128, 8, 3], F32, "A1"); b_A1 = Buf()
        lrup = sb(es, [128, 4, 16], F32, "lrup"); b_lrup = Buf()
        S.dma("sp", identf[:], ident_d, writes=[b_identf])
        S.dma("pool", identb[:], ident_d, writes=[b_identb])
        S.dma("pool", utri[:], utri_d, writes=[b_utri])
        S.dma("sp", iota[:], iota_d, writes=[b_iota])
        S.dma("sp", vecs[:], vecs_d, writes=[b_vecs])
        V("dve", lambda: nc.vector.memset(onesf[:], 1.0), [], [b_onesf])
        V("dve", lambda: nc.vector.memset(onesb[:], 1.0), [], [b_onesb])
        V("dve", lambda: nc.vector.memset(epst[:], EPS), [], [b_eps])

        with ExitStack() as es0:
            csb = sb(es0, [128, 24], F32, "cs"); b_cs = Buf()
            scs = sb(es0, [128, 24], F32, "scs"); b_scs = Buf()
            wm = [sb(es0, [128, 8, 512], F32, "wm") for _ in range(2)]
            b_wm = [Buf(), Buf()]
            psmod = ps(es0, [128, 144], F32, "psmod"); b_psmod = Buf()
            S.dma("sp", csb[:], cs_d, writes=[b_cs])
            act(scs[:], csb[:], AF.Silu, [b_cs], [b_scs])
            for cb in range(12):
                w = wm[cb % 2]; bw = b_wm[cb % 2]
                S.dma("sp", w[:], wmod_d[:, :, cb * 512:(cb + 1) * 512], writes=[bw])
                for cc in range(4):
                    col = cb * 4 + cc
                    for k in range(8):
                        pe_mm(psmod[:, col * 3:(col + 1) * 3], w[:, k, cc * 128:(cc + 1) * 128],
                              scs[:, k * 3:(k + 1) * 3], k == 0, k == 7, [bw, b_scs], [b_psmod],
                              sig=(k == 7 and cc == 3))
            V("dve", lambda: nc.vector.tensor_tensor(
                out=modfm[:], in0=psmod[:, :].rearrange("p (a b) -> p a b", b=3),
                in1=vecs[:, V_BMOD:V_BMOD + 48].unsqueeze(2).to_broadcast([128, 48, 3]), op=ALU.add),
              [b_psmod, b_vecs], [b_modfm])
            V("dve", lambda: nc.vector.scalar_tensor_tensor(
                out=A1[:], in0=modfm[:, 8:16, :], scalar=1.0,
                in1=vecs[:, V_GMIX:V_GMIX + 8].unsqueeze(2).to_broadcast([128, 8, 3]),
                op0=ALU.add, op1=ALU.mult), [b_modfm, b_vecs], [b_A1])
            act(lrup[:, 2, :], vecs[:, V_LAM:V_LAM + 16], AF.Exp, [b_vecs], [b_lrup], scale=-1.0)
            act(lrup[:, 3, :], lrup[:, 2, :], AF.Ln, [b_lrup], [b_lrup], bias=1.0)
            V("dve", lambda: nc.vector.tensor_scalar(out=lrup[:, 0, :], in0=lrup[:, 3, :], scalar1=-8.0, scalar2=None,
                                                     op0=ALU.mult), [b_lrup], [b_lrup])
            V("dve", lambda: nc.vector.tensor_scalar(out=lrup[:, 1, :], in0=lrup[:, 3, :], scalar1=-16.0, scalar2=None,
                                                     op0=ALU.mult), [b_lrup], [b_lrup])
            dump("modfm", modfm[:, :, :].rearrange("p a b -> p (a b)"), [b_modfm])
            S.barrier()

        def bcast_row(es_, v, j, psb, b_psb, diag, b_diag):
            for k in range(8):
                V("dve", lambda: nc.vector.tensor_scalar(out=diag[k % 2][:], in0=identf[:],
                                                         scalar1=modfm[:, v * 8 + k, j:j + 1], scalar2=None,
                                                         op0=ALU.mult), [b_identf, b_modfm], [b_diag[k % 2]])
                pe_mm(psb[:, k * 128:(k + 1) * 128], onesf[:], diag[k % 2][:], True, True,
                      [b_onesf, b_diag[k % 2]], [b_psb], sig=True)

        Mall = sb(es, [128, 32, 32], BF16, "Mall"); b_Mall = Buf()
        oh1all = sb(es, [128, 32, 32], BF16, "oh1"); b_oh1 = Buf()
        gates = sb(es, [128, 32, 2], F32, "gates"); b_gates = Buf()
        dest_f = sb(es, [128, 32, 2], F32, "destf"); b_destf = Buf()
        dest_i = sb(es, [128, 64], I32, "desti"); b_desti = Buf()
        b_x1d = Buf("x1d"); b_h2d = Buf("h2d"); b_xsd = Buf("xsd"); b_ysd = Buf("ysd"); b_outd = Buf("outd")
        zt = sb(es, [128, 2, D], BF16, "zt"); b_zt = Buf()
        V("dve", lambda: nc.vector.memset(zt[:], 0.0), [], [b_zt])
        for blk in range(NBLK):
            S.dma("sp", xs_d[blk * 256:(blk + 1) * 256, :].rearrange("(a p) d -> p a d", p=128), zt[:],
                  reads=[b_zt], writes=[b_xsd], group=True, semof=b_zt)

        for b in range(NB):
            with ExitStack() as esb:
                hT = sb(esb, [128, 8, LC], BF16, "hT")
                b_hT = [[Buf(f"hT{i}a"), Buf(f"hT{i}b")] for i in range(5)]
                oatt = sb(esb, [128, 4, L], BF16, "oatt")
                b_oatt = [[Buf() for _ in range(4)] for _ in range(4)]

                with ExitStack() as es1:
                    xt = [sb(es1, [128, D], F32, "xt") for _ in range(3)]
                    b_xt = [Buf() for _ in range(3)]
                    junk = sb(es1, [128, D], BF16, "junk"); b_junk = Buf()
                    xn = [sb(es1, [128, D], BF16, "xn") for _ in range(8)]
                    b_xn = [Buf() for _ in range(8)]
                    st = sb(es1, [128, 18, 3], F32, "st"); b_st = [Buf() for _ in range(18)]
                    pst = [ps(es1, [128, 512], BF16, "pst") for _ in range(4)]
                    b_pst = [Buf() for _ in range(4)]
                    ti = 0
                    for grp in range(5):
                        ntile = 4 if grp < 4 else 2
                        jmod = b if grp < 4 else 2
                        for i in range(ntile):
                            t = grp * 4 + i
                            xb_, bx_ = xt[ti % 3], b_xt[ti % 3]
                            src = xin[b, t * 128:(t + 1) * 128, :] if grp < 4 else ctxin[b, i * 128:(i + 1) * 128, :]
                            S.dma("sp", xb_[:], src, writes=[bx_])
                            act(junk[:], xb_[:], AF.Square, [bx_], [b_junk, b_st[t]], accum_out=st[:, t, 0:1])
                            act(st[:, t, 1:2], st[:, t, 0:1], AF.Sqrt, [b_st[t], b_eps], [b_st[t]],
                                scale=1.0 / D, bias=epst[:, 0:1])
                            V("dve", lambda: nc.vector.reciprocal(out=st[:, t, 2:3], in_=st[:, t, 1:2]),
                              [b_st[t]], [b_st[t]])
                            xi = (grp % 2) * 4 + i
                            act(xn[xi][:], xb_[:], AF.Copy, [bx_, b_st[t]], [b_xn[xi]], scale=st[:, t, 2:3])
                            ti += 1
                        for k in range(8):
                            pp, bp = pst[k % 4], b_pst[k % 4]
                            for i in range(ntile):
                                xi = (grp % 2) * 4 + i
                                pe_tr(pp[:, i * 128:(i + 1) * 128], xn[xi][:, k * 128:(k + 1) * 128], identb[:],
                                      [b_xn[xi], b_identb], [bp], sig=(i == ntile - 1))
                            n = ntile * 128
                            dst = hT[:, k, grp * 512:grp * 512 + n]
                            if k % 2 == 0:
                                V("dve", lambda: nc.vector.tensor_scalar(
                                    out=dst, in0=pp[:, 0:n], scalar1=A1[:, k, jmod:jmod + 1],
                                    scalar2=modfm[:, k, jmod:jmod + 1], op0=ALU.mult, op1=ALU.add),
                                  [bp, b_A1, b_modfm], [b_hT[grp][0]])
                            else:
                                act(dst, pp[:, 0:n], AF.Identity, [bp, b_A1, b_modfm], [b_hT[grp][1]],
                                    scale=A1[:, k, jmod:jmod + 1], bias=modfm[:, k, jmod:jmod + 1])
                    if b == 0:
                        dump("hT", hT[:, :, :].rearrange("p a b -> p (a b)"), [x for l_ in b_hT for x in l_])
                    S.barrier()
                if STOP_AFTER == "s1":
                    break

                with ExitStack() as es2:
                    cs_t = sb(es2, [128, 2 * L], F32, "cossin"); b_cst = Buf()
                    btab = sb(es2, [128, 8, NTB * 64], BF16, "btab"); b_btab = Buf()
                    S.dma("sp", cs_t[:], cossin_d, writes=[b_cst])
                    S.dma("pool", btab[0:64, :, :].rearrange("p a b -> p (a b)"), btab_d, writes=[b_btab], group=True)
                    S.dma("pool", btab[64:128, :, :].rearrange("p a b -> p (a b)"), btab_d, writes=[b_btab], group=True)
                    wq = [sb(es2, [128, 8, 640], BF16, "wq") for _ in range(1)]; b_wq = [Buf()]
                    Qr = [sb(es2, [128, L], BF16, "Qr") for _ in range(1)]
                    Qp = [sb(es2, [128, L], BF16, "Qp") for _ in range(1)]
                    Kr = [sb(es2, [128, L], BF16, "Kr") for _ in range(1)]
                    Kc = [sb(es2, [128, C], BF16, "Kc") for _ in range(1)]
                    Vx = [sb(es2, [128, 18, 192], BF16, "Vx") for _ in range(1)]
                    b_Q = [[Buf() for _ in range(4)] for _ in range(1)]
                    b_Qp = [[Buf() for _ in range(4)] for _ in range(1)]
                    b_K = [Buf()]
                    b_Kc = [Buf()]
                    b_V = [[Buf() for _ in range(5)]]
                    t1 = [sb(es2, [128, 512], F32, "t1") for _ in range(2)]; b_t1 = [Buf(), Buf()]
                    t2 = [sb(es2, [128, 512], F32, "t2") for _ in range(2)]; b_t2 = [Buf(), Buf()]
                    PTc = [sb(es2, [128, 512], BF16, "PTc") for _ in range(2)]; b_PTc = [Buf(), Buf()]
                    PT = [sb(es2, [128, 320], BF16, "PT") for _ in range(3)]; b_PT = [Buf() for _ in range(3)]
                    rc = [sb(es2, [128, 512], F32, "rc") for _ in range(2)]; b_rc = [Buf(), Buf()]
                    psP = [ps(es2, [128, 512], F32, "psP") for _ in range(2)]; b_psP = [Buf(), Buf()]
                    psSc = [ps(es2, [128, 512], F32, "psSc") for _ in range(2)]; b_psSc = [Buf(), Buf()]
                    psS = [ps(es2, [128, 512], F32, "psS") for _ in range(2)]; b_psS = [Buf(), Buf()]
                    psO = [ps(es2, [128, 512], F32, "psO") for _ in range(2)]; b_psO = [Buf(), Buf()]
                    V("dve", lambda: nc.vector.memset(Vx[0][:, :, 64:128], 1.0), [], b_V[0])
                    pcnt = [0]

                    def nextP():
                        i = pcnt[0] % 2
                        pcnt[0] += 1
                        return psP[i], b_psP[i]

                    cnt_t = [0]
                    for hp in range(ATT_NHP):
                        par = 0
                        w = wq[par]; bw = b_wq[par]
                        S.dma("pool", w[:, :, :].rearrange("p a b -> p (a b)"), wqkv_d[hp], writes=[bw])
                        for blk in range(4 if ATT_PARTS & 1 else 0):
                            tok = slice(blk * 512, (blk + 1) * 512)
                            for which in range(2):
                                c0 = which * 256
                                pa, bpa = nextP()
                                for k in range(8):
                                    pe_mm(pa[:], w[:, k, c0:c0 + 128], hT[:, k, tok], k == 0, k == 7,
                                          [bw, *b_hT[blk]], [bpa])
                                pb, bpb = nextP()
                                for k in range(8):
                                    pe_mm(pb[:], w[:, k, c0 + 128:c0 + 256], hT[:, k, tok], k == 0, k == 7,
                                          [bw, *b_hT[blk]], [bpb])
                                ii = cnt_t[0] % 2
                                cnt_t[0] += 1
                                sc_ = 0.125 if which == 0 else 1.0
                                dstb = b_Q[par][blk] if which == 0 else b_K[par]
                                dst = (Qr if which == 0 else Kr)[par][:, tok]
                                V("dve", lambda: nc.vector.scalar_tensor_tensor(
                                    out=t1[ii][:], in0=pa[:], scalar=sc_, in1=cs_t[:, tok], op0=ALU.mult, op1=ALU.mult),
                                  [bpa, b_cst], [b_t1[ii]])
                                if which == 0 and (ATT_PARTS & 16):
                                    V("dve", lambda: nc.vector.tensor_scalar(out=Qp[par][:, tok], in0=pa[:], scalar1=0.125, scalar2=None,
                                                                             op0=ALU.mult), [bpa], [b_Qp[par][blk]])
                                V("dve", lambda: nc.vector.scalar_tensor_tensor(
                                    out=t2[ii][:], in0=pb[:], scalar=sc_, in1=cs_t[:, L + blk * 512:L + (blk + 1) * 512],
                                    op0=ALU.mult, op1=ALU.mult), [bpb, b_cst], [b_t2[ii]])
                                V("pool" if ATT_PARTS & 8 else "dve", lambda: (nc.gpsimd if ATT_PARTS & 8 else nc.vector).tensor_tensor(out=dst, in0=t1[ii][:], in1=t2[ii][:], op=ALU.add),
                                  [b_t1[ii], b_t2[ii]], [dstb])
                        if ATT_PARTS & 2:
                            pa, bpa = nextP()
                            for k in range(8):
                                pe_mm(pa[:, 0:C], w[:, k, 256:384], hT[:, k, L:LC], k == 0, k == 7, [bw, *b_hT[4]], [bpa])
                            act(Kc[par][:], pa[:, 0:C], AF.Copy, [bpa], [b_Kc[par]])
                        for g4 in range(5 if ATT_PARTS & 4 else 0):
                            nch = 4 if g4 < 4 else 2
                            pa, bpa = nextP()
                            for i in range(nch):
                                ch = g4 * 4 + i
                                for k in range(8):
                                    pe_mm(pa[:, i * 128:(i + 1) * 128], hT[:, k, ch * 128:(ch + 1) * 128],
                                          w[:, k, 512:640], k == 0, k == 7, [bw, *b_hT[g4]], [bpa],
                                          sig=(k == 7 and i == nch - 1))
                            src = pa[:, 0:nch * 128].rearrange("p (c a d) -> p c a d", a=2, d=64)
                            dstv = Vx[par][:, g4 * 4:g4 * 4 + nch, :].rearrange("p c (a d) -> p c a d", d=64)[:, :, 0::2, :]
                            if g4 % 2 == 0:
                                act(dstv, src, AF.Copy, [bpa], [b_V[par][g4]])
                            else:
                                V("dve", lambda: nc.vector.tensor_copy(out=dstv, in_=src), [bpa], [b_V[par][g4]])

                        if b == 0 and hp == 0:
                            dump("Qp", Qp[0][:], b_Qp[0])
                            dump("Qr", Qr[0][:], b_Q[0])
                            dump("Kr", Kr[0][:], b_K)
                            dump("Kc", Kc[0][:], b_Kc)
                            dump("Vx", Vx[0][:, :, :].rearrange("p a b -> p (a b)"), b_V[0])
                        units = []
                        for e in range(2):
                            for qb in range(4):
                                units.append((e, qb))
                        ucnt = [0]
                        for (e, qb) in units[int(_os.environ.get("ATT_USTART", "0")):][:ATT_NUNITS]:
                            h = hp * 2 + e
                            pr = slice(64 * e, 64 * e + 64)
                            qs = slice(qb * 512, (qb + 1) * 512)
                            vcols = slice(0, 128) if e == 0 else slice(64, 192)
                            oi = ucnt[0] % 2
                            ucnt[0] += 1
                            pO, bO = psO[oi], b_psO[oi]
                            for c in range(2):
                                pS, bS = psSc[c], b_psSc[c]
                                pe_mm(pS[:], Kc[par][pr, c * 128:(c + 1) * 128], Qp[par][pr, qs], True, True,
                                      [b_Kc[par], b_Qp[par][qb]], [bS])
                                act(PTc[c][:], pS[:], AF.Exp, [bS], [b_PTc[c]])
                            if b == 0 and hp == 0 and e == 0 and qb == 0:
                                dump("PTc", PTc[0][:], [b_PTc[0]])
                            for c in range(2):
                                pe_mm(pO[:], Vx[par][:, 16 + c, vcols], PTc[c][:], c == 0, False,
                                      [b_V[par][4], b_PTc[c]], [bO], sig=(c == 1))
                            rows = list(range(qb * 8, qb * 8 + ATT_NROWS))
                            plan = []
                            for r in rows:
                                rs = min(max(r - 4, 0), 24)
                                dr0 = rs - r + 7
                                chunks = []
                                if rs % 2 == 0:
                                    for j in range(4):
                                        chunks.append(((rs + 2 * j) // 2, 1 + dr0 + 2 * j))
                                else:
                                    assert dr0 == 3
                                    c0 = (rs - 1) // 2
                                    chunks.append((c0, 17))
                                    for j in range(1, 4):
                                        chunks.append((c0 + j, dr0 + 2 * j))
                                    chunks.append((c0 + 4, 19))
                                plan.append((r, chunks))

                            def emit_qk(idx):
                                r, chunks = plan[idx]
                                si = idx % 2
                                pS, bS = psS[si], b_psS[si]
                                qcol = slice(r * 64, (r + 1) * 64)
                                for j, (kc, blk0) in enumerate(chunks):
                                    o = pS[:, j * 64:(j + 1) * 64]
                                    pe_mm(o, Kr[par][pr, kc * 128:(kc + 1) * 128], Qr[par][pr, qcol], True, False,
                                          [b_K[par], b_Q[par][qb]], [bS], sig=False)
                                    lt = btab[pr, h, blk0 * 64:(blk0 + 2) * 64]
                                    pe_mm(o, lt, identb[pr, pr], False, True, [b_btab, b_identb], [bS],
                                          sig=(j == len(chunks) - 1))
                                n = len(chunks) * 64
                                pi = idx % 3
                                act(PT[pi][:, 0:n], pS[:, 0:n], AF.Exp, [bS], [b_PT[pi]])

                            def emit_pv(idx):
                                r, chunks = plan[idx]
                                pi = idx % 3
                                rr = r - qb * 8
                                for j, (kc, _) in enumerate(chunks):
                                    last = (j == len(chunks) - 1)
                                    pe_mm(pO[:, rr * 64:(rr + 1) * 64], Vx[par][:, kc, vcols], PT[pi][:, j * 64:(j + 1) * 64],
                                          False, last and idx == len(plan) - 1, [b_V[par][kc // 4], b_PT[pi]], [bO], sig=last)

                            if plan:
                                emit_qk(0)
                            for idx in range(len(plan)):
                                if idx + 1 < len(plan):
                                    emit_qk(idx + 1)
                                emit_pv(idx)
                            dn = slice(64, 128) if e == 0 else slice(0, 64)
                            if not ATT_NORM:
                                continue
                            V("dve", lambda: nc.vector.reciprocal(out=rc[oi][pr, :], in_=pO[dn, :]), [bO], [b_rc[oi]])
                            V("dve", lambda: nc.vector.tensor_tensor(out=oatt[pr, hp, qs], in0=pO[pr, :], in1=rc[oi][pr, :],
                                                                     op=ALU.mult), [bO, b_rc[oi]], [b_oatt[hp][qb]])
                    if b == 0:
                        dump("oatt", oatt[:, :, :].rearrange("p a b -> p (a b)"), [x for l_ in b_oatt for x in l_])
                    S.barrier()
                if STOP_AFTER == "s2":
                    break

                olru = sb(esb, [128, 8, L], BF16, "olru")
                b_olru = [Buf() for _ in range(8)]
                with ExitStack() as es3:
                    TL = LC + 3
                    wl = [sb(es3, [128, 8, 256], BF16, "wl") for _ in range(2)]; b_wl = [Buf(), Buf()]
                    lw = sb(es3, [128, 4, 8, 128], BF16, "lw"); b_lw = Buf()
                    S.dma("pool", lw[:, :, :, :].rearrange("p a b c -> p (a b c)"), lruw_d, writes=[b_lw])
                    LXp = sb(es3, [128, TL + 3], F32, "LXp"); b_LXp = Buf()
                    xc = sb(es3, [128, TL], F32, "xc"); b_xc = Buf()
                    xcb = [sb(es3, [128, TL], BF16, "xcb") for _ in range(2)]; b_xcb = [Buf(), Buf()]
                    av = sb(es3, [128, TL], F32, "av"); b_av = Buf()
                    wv = sb(es3, [128, TL], F32, "wv"); b_wv = Buf()
                    iv = sb(es3, [128, TL], F32, "iv"); b_iv = Buf()
                    hv = [sb(es3, [128, TL], F32, "hv") for _ in range(2)]; b_hv = [Buf(), Buf()]
                    gl = sb(es3, [128, L], BF16, "gl"); b_gl = Buf()
                    psA = [ps(es3, [128, 512], F32, "psA") for _ in range(4)]; b_psA = [Buf() for _ in range(4)]
                    pacnt = [0]

                    def nextA():
                        i = pacnt[0] % 4
                        pacnt[0] += 1
                        return psA[i], b_psA[i]

                    V("dve", lambda: nc.vector.memset(LXp[:], 0.0), [], [b_LXp])

                    def load_wl(n):
                        S.dma("pool", wl[n % 2][:, :, :].rearrange("p a b -> p (a b)"), wlxlg_d[n], writes=[b_wl[n % 2]])

                    def lx_conv(n):
                        w = wl[n % 2]; bw = b_wl[n % 2]
                        for blk in range(5):
                            nt = 512 if blk < 4 else C
                            tok = slice(blk * 512, blk * 512 + nt)
                            pa, bpa = nextA()
                            for k in range(8):
                                pe_mm(pa[:, 0:nt], w[:, k, 0:128], hT[:, k, tok], k == 0, k == 7, [bw, *b_hT[blk]], [bpa])
                            d0 = 261 + blk * 512 if blk < 4 else 2
                            act(LXp[:, d0:d0 + nt], pa[:, 0:nt], AF.Copy, [bpa], [b_LXp])
                        cw = lambda j: vecs[:, V_CONVW + j * 8 + n:V_CONVW + j * 8 + n + 1]
                        V("dve", lambda: nc.vector.tensor_scalar(out=xc[:], in0=LXp[:, 0:TL], scalar1=cw(0),
                                                                 scalar2=vecs[:, V_CONVB + n:V_CONVB + n + 1],
                                                                 op0=ALU.mult, op1=ALU.add), [b_LXp, b_vecs], [b_xc])
                        for j in range(1, 3):
                            V("dve", lambda: nc.vector.scalar_tensor_tensor(out=xc[:], in0=LXp[:, j:j + TL], scalar=cw(j),
                                                                            in1=xc[:], op0=ALU.mult, op1=ALU.add),
                              [b_LXp, b_vecs, b_xc], [b_xc])
                        V("dve", lambda: nc.vector.scalar_tensor_tensor(out=xcb[n % 2][:], in0=LXp[:, 3:3 + TL], scalar=cw(3),
                                                                        in1=xc[:], op0=ALU.mult, op1=ALU.add),
                          [b_LXp, b_vecs, b_xc], [b_xcb[n % 2]])

                    load_wl(0)
                    lx_conv(0)
                    for n in range(8):
                        w = wl[n % 2]; bw = b_wl[n % 2]
                        xb_ = xcb[n % 2]; bxb = b_xcb[n % 2]
                        if n + 1 < 8:
                            load_wl(n + 1)
                        if b == 0 and n == 0:
                            dump("xc0", xb_[:], [bxb])
                        for dr in range(2):
                            di = dr * 8 + n
                            for blk in range(5):
                                nt = 512 if blk < 4 else TL - 2048
                                tok = slice(blk * 512, blk * 512 + nt)
                                pr_, bpr = nextA()
                                pe_mm(pr_[:, 0:nt], lw[:, dr, n, :], xb_[:, tok], True, True, [b_lw, bxb], [bpr])
                                pi_, bpi = nextA()
                                pe_mm(pi_[:, 0:nt], lw[:, 2 + dr, n, :], xb_[:, tok], True, True, [b_lw, bxb], [bpi])
                                act(av[:, tok], pr_[:, 0:nt], AF.Sigmoid, [bpr, b_vecs], [b_av],
                                    bias=vecs[:, V_BA + di:V_BA + di + 1])
                                act(iv[:, tok], pi_[:, 0:nt], AF.Sigmoid, [bpi, b_vecs], [b_iv],
                                    bias=vecs[:, V_BX + di:V_BX + di + 1])
                            act(wv[:], av[:], AF.Exp, [b_av, b_lrup], [b_wv], scale=lrup[:, 1, di:di + 1])
                            act(av[:], av[:], AF.Exp, [b_av, b_lrup], [b_av], scale=lrup[:, 0, di:di + 1])
                            act(wv[:], wv[:], AF.Sqrt, [b_wv], [b_wv], scale=-1.0, bias=1.0)
                            V("pool", lambda: nc.gpsimd.tensor_tensor(out=iv[:], in0=iv[:], in1=xb_[:], op=ALU.mult),
                              [b_iv, bxb], [b_iv])
                            V("dve", lambda: nc.vector.tensor_tensor(out=wv[:], in0=iv[:], in1=wv[:], op=ALU.mult),
                              [b_iv, b_wv], [b_wv])
                            hh = hv[dr]; bh = b_hv[dr]
                            if dr == 0:
                                V("dve", lambda: nc.vector.tensor_tensor_scan(
                                    out=hh[:, 0:C], data0=av[:, 0:C], data1=wv[:, 0:C], initial=0.0,
                                    op0=ALU.mult, op1=ALU.add), [b_av, b_wv], [bh])
                                V("dve", lambda: nc.vector.tensor_tensor_scan(
                                    out=hh[:, C + 3:TL], data0=av[:, C + 3:TL], data1=wv[:, C + 3:TL],
                                    initial=hh[:, C - 1:C], op0=ALU.mult, op1=ALU.add), [b_av, b_wv, bh], [bh])
                                if n + 1 < 8:
                                    lx_conv(n + 1)
                            else:
                                V("dve", lambda: nc.vector.tensor_tensor_scan(
                                    out=hh[:, C - 1::-1], data0=av[:, C - 1::-1], data1=wv[:, C - 1::-1], initial=0.0,
                                    op0=ALU.mult, op1=ALU.add), [b_av, b_wv], [bh])
                                V("dve", lambda: nc.vector.tensor_tensor_scan(
                                    out=hh[:, TL - 1:C + 2:-1], data0=av[:, TL - 1:C + 2:-1], data1=wv[:, TL - 1:C + 2:-1],
                                    initial=hh[:, 0:1], op0=ALU.mult, op1=ALU.add), [b_av, b_wv, bh], [bh])
                        for blk in range(4):
                            tok = slice(blk * 512, (blk + 1) * 512)
                            pa, bpa = nextA()
                            for k in range(8):
                                pe_mm(pa[:], w[:, k, 128:256], hT[:, k, tok], k == 0, k == 7, [bw, *b_hT[blk]], [bpa])
                            act(gl[:, tok], pa[:], AF.Gelu_apprx_tanh, [bpa], [b_gl])
                        V("dve", lambda: nc.vector.tensor_tensor(out=hv[0][:, C + 3:TL], in0=hv[0][:, C + 3:TL],
                                                                 in1=hv[1][:, C + 3:TL], op=ALU.add),
                          [b_hv[0], b_hv[1]], [b_hv[0]])
                        if b == 0 and n == 0:
                            dump("hsum0", hv[0][:, C + 3:TL], [b_hv[0]])
                        V("dve", lambda: nc.vector.tensor_tensor(out=olru[:, n, :], in0=hv[0][:, C + 3:TL], in1=gl[:],
                                                                 op=ALU.mult), [b_hv[0], b_gl], [b_olru[n]])
                    if b == 0:
                        dump("olru", olru[:, :, :].rearrange("p a b -> p (a b)"), b_olru)
                    S.barrier()
                if STOP_AFTER == "s3":
                    break

                yT = sb(esb, [128, 8, L], BF16, "yT")
                b_yT = [Buf() for _ in range(4)]
                with ExitStack() as es4:
                    wg_ = [sb(es4, [128, 8, 256], BF16, "wg") for _ in range(2)]; b_wg = [Buf(), Buf()]
                    wu_ = [sb(es4, [128, 12, 128], BF16, "wu") for _ in range(2)]; b_wu = [Buf(), Buf()]
                    ga_ = [sb(es4, [128, 512], F32, "ga") for _ in range(2)]; b_ga = [Buf(), Buf()]
                    gb_ = [sb(es4, [128, 512], F32, "gb") for _ in range(2)]; b_gb = [Buf(), Buf()]
                    ya_ = [sb(es4, [128, 512], F32, "ya") for _ in range(2)]; b_ya = [Buf(), Buf()]
                    yb_ = [sb(es4, [128, 512], F32, "yb") for _ in range(2)]; b_yb = [Buf(), Buf()]
                    psM = [ps(es4, [128, 512], F32, "psM") for _ in range(8)]; b_psM = [Buf() for _ in range(8)]
                    it = 0
                    for f in range(8):
                        wgt, bwg = wg_[f % 2], b_wg[f % 2]
                        wut, bwu = wu_[f % 2], b_wu[f % 2]
                        S.dma("pool", wgt[:, :, :].rearrange("p a b -> p (a b)"), wgagb_d[f], writes=[bwg])
                        S.dma("pool", wut[:, :, :].rearrange("p a b -> p (a b)"), wup_d[f], writes=[bwu])
                        for blk in range(4):
                            tok = slice(blk * 512, (blk + 1) * 512)
                            i2 = it % 2
                            p0, p1, p2, p3 = [psM[(it % 2) * 4 + q] for q in range(4)]
                            q0, q1, q2, q3 = [b_psM[(it % 2) * 4 + q] for q in range(4)]
                            it += 1
                            for k in range(8):
                                pe_mm(p0[:], wgt[:, k, 0:128], hT[:, k, tok], k == 0, k == 7, [bwg, *b_hT[blk]], [q0])
                            act(ga_[i2][:], p0[:], AF.Sigmoid, [q0], [b_ga[i2]])
                            for k in range(4):
                                pe_mm(p1[:], wut[:, k, :], oatt[:, k, tok], k == 0, k == 3, [bwu, b_oatt[k][blk]], [q1])
                            V("dve", lambda: nc.vector.tensor_tensor(out=ya_[i2][:], in0=p1[:], in1=ga_[i2][:], op=ALU.mult),
                              [q1, b_ga[i2]], [b_ya[i2]])
                            for k in range(8):
                                pe_mm(p2[:], wgt[:, k, 128:256], hT[:, k, tok], k == 0, k == 7, [bwg, *b_hT[blk]], [q2])
                            act(gb_[i2][:], p2[:], AF.Sigmoid, [q2], [b_gb[i2]])
                            for k in range(8):
                                pe_mm(p3[:], wut[:, 4 + k, :], olru[:, k, tok], k == 0, k == 7, [bwu, b_olru[k]], [q3])
                            V("dve", lambda: nc.vector.tensor_tensor(out=yb_[i2][:], in0=p3[:], in1=gb_[i2][:], op=ALU.mult),
                              [q3, b_gb[i2]], [b_yb[i2]])
                            V("pool", lambda: nc.gpsimd.tensor_tensor(out=yT[:, f, tok], in0=ya_[i2][:], in1=yb_[i2][:],
                                                                      op=ALU.add), [b_ya[i2], b_yb[i2]], [b_yT[blk]])
                    if b == 0:
                        dump("yT", yT[:, :, :].rearrange("p a b -> p (a b)"), b_yT)
                    S.barrier()
                if STOP_AFTER == "s4":
                    break

                with ExitStack() as es5:
                    wo32 = [sb(es5, [128, D], F32, "wo32") for _ in range(2)]; b_wo32 = [Buf(), Buf()]
                    wob = sb(es5, [128, 8, D], BF16, "wob"); b_wob = Buf()
                    GA1 = sb(es5, [128, D], F32, "GA1"); b_GA1 = Buf()
                    G2 = sb(es5, [128, D], F32, "G2"); b_G2 = Buf()
                    S2 = sb(es5, [128, D], F32, "S2"); b_S2 = Buf()
                    gffnb = sb(es5, [128, D], F32, "gffnb"); b_gffnb = Buf()
                    diag = [sb(es5, [128, 128], F32, "diag") for _ in range(2)]; b_diag = [Buf(), Buf()]
                    wr = sb(es5, [128, 8, 36], F32, "wr"); b_wr = Buf()
                    brb = sb(es5, [128, 36], F32, "brb"); b_brb = Buf()
                    psB = ps(es5, [128, D], F32, "psB"); b_psB = Buf()
                    psO5 = [ps(es5, [128, D], F32, "psO5") for _ in range(2)]; b_psO5 = [Buf(), Buf()]
                    psT = ps(es5, [128, D], F32, "psT"); b_psT = Buf()
                    S.dma("sp", gffnb[:], gffnb_d, writes=[b_gffnb])
                    S.dma("sp", wr[:, :, :].rearrange("p a b -> p (a b)"), wr_d, writes=[b_wr])
                    S.dma("sp", brb[:], brb_d, writes=[b_brb])
                    bcast_row(es5, 2, b, psB, b_psB, diag, b_diag)
                    V("dve", lambda: nc.vector.tensor_copy(out=GA1[:], in_=psB[:]), [b_psB], [b_GA1])
                    bcast_row(es5, 4, b, psB, b_psB, diag, b_diag)
                    V("dve", lambda: nc.vector.scalar_tensor_tensor(out=G2[:], in0=psB[:], scalar=1.0, in1=gffnb[:],
                                                                    op0=ALU.add, op1=ALU.mult), [b_psB, b_gffnb], [b_G2])
                    bcast_row(es5, 3, b, psB, b_psB, diag, b_diag)
                    V("dve", lambda: nc.vector.tensor_copy(out=S2[:], in_=psB[:]), [b_psB], [b_S2])
                    for kk in range(8):
                        S.dma("sp", wo32[kk % 2][:], wout_d[:, kk * D:(kk + 1) * D], writes=[b_wo32[kk % 2]])
                        V("dve", lambda: nc.vector.tensor_tensor(out=wob[:, kk, :], in0=wo32[kk % 2][:], in1=GA1[:],
                                                                 op=ALU.mult), [b_wo32[kk % 2], b_GA1], [b_wob])
                    xt5 = [sb(es5, [128, D], F32, "xt5") for _ in range(2)]; b_xt5 = [Buf(), Buf()]
                    x1t = xt5; b_x1t = b_xt5
                    h2t = [sb(es5, [128, D], F32, "h2t") for _ in range(2)]; b_h2t = [Buf(), Buf()]
                    h2b = [sb(es5, [128, D], BF16, "h2b") for _ in range(2)]; b_h2b = [Buf(), Buf()]
                    h2T = sb(es5, [128, 8, 128], F32, "h2T"); b_h2T = Buf()
                    junk5 = sb(es5, [128, D], BF16, "junk5"); b_junk5 = Buf()
                    st5 = sb(es5, [128, 16, 3], F32, "st5"); b_st5 = [Buf() for _ in range(16)]
                    LGa = sb(es5, [128, 16, 36], F32, "LGa"); b_LGa = Buf()
                    RW = sb(es5, [128, 8, 16], F32, "RW"); b_RW = Buf()
                    RG = sb(es5, [128, 16, 12], F32, "RG"); b_RG = Buf()
                    LEM = sb(es5, [128, 16, 32], F32, "LEM"); b_LEM = Buf()
                    LEM2 = sb(es5, [128, 16, 32], F32, "LEM2"); b_LEM2 = Buf()
                    def wout_mm(j):
                        i2 = j % 2
                        tsl = slice(j * 128, (j + 1) * 128)
                        S.dma("sp", xt5[i2][:], xin[b, tsl, :], writes=[b_xt5[i2]])
                        pO, bO = psO5[i2], b_psO5[i2]
                        for hf in range(2):
                            for k in range(8):
                                pe_mm(pO[:, hf * 512:(hf + 1) * 512], yT[:, k, tsl], wob[:, k, hf * 512:(hf + 1) * 512],
                                      k == 0, k == 7, [b_yT[j // 4], b_wob], [bO], sig=(k == 7 and hf == 1))

                    wout_mm(0)
                    for j in range(16):
                        tg = b * 16 + j
                        i2 = j % 2
                        tsl = slice(j * 128, (j + 1) * 128)
                        pO, bO = psO5[i2], b_psO5[i2]
                        if j + 1 < 16:
                            wout_mm(j + 1)
                        V("dve", lambda: nc.vector.tensor_tensor(out=x1t[i2][:], in0=pO[:], in1=xt5[i2][:], op=ALU.add),
                          [bO, b_xt5[i2]], [b_xt5[i2]])
                        S.dma("pool", x1_d[tg * 128:(tg + 1) * 128, :], x1t[i2][:], reads=[b_x1t[i2]], writes=[b_x1d], group=True, semof=b_x1t[i2])
                        act(junk5[:], x1t[i2][:], AF.Square, [b_x1t[i2]], [b_junk5, b_st5[j]], accum_out=st5[:, j, 0:1])
                        act(st5[:, j, 1:2], st5[:, j, 0:1], AF.Sqrt, [b_st5[j], b_eps], [b_st5[j]], scale=1.0 / D,
                            bias=epst[:, 0:1])
                        V("dve", lambda: nc.vector.reciprocal(out=st5[:, j, 2:3], in_=st5[:, j, 1:2]), [b_st5[j]], [b_st5[j]])
                        V("dve", lambda: nc.vector.scalar_tensor_tensor(out=h2t[i2][:], in0=x1t[i2][:], scalar=st5[:, j, 2:3],
                                                                        in1=G2[:], op0=ALU.mult, op1=ALU.mult),
                          [b_x1t[i2], b_st5[j], b_G2], [b_h2t[i2]])
                        V("dve", lambda: nc.vector.tensor_tensor(out=h2t[i2][:], in0=h2t[i2][:], in1=S2[:], op=ALU.add),
                          [b_h2t[i2], b_S2], [b_h2t[i2]])
                        act(h2b[i2][:], h2t[i2][:], AF.Copy, [b_h2t[i2]], [b_h2b[i2]])
                        S.dma("pool", h2_d[tg * 128:(tg + 1) * 128, :], h2b[i2][:], reads=[b_h2b[i2]], writes=[b_h2d], group=True, semof=b_h2b[i2])
                        for k in range(8):
                            pe_tr(psT[:, k * 128:(k + 1) * 128], h2t[i2][:, k * 128:(k + 1) * 128], identf[:],
                                  [b_h2t[i2], b_identf], [b_psT], sig=(k == 7))
                        act(h2T[:, :, :].rearrange("p a b -> p (a b)"), psT[:], AF.Copy, [b_psT], [b_h2T])
                        for k in range(8):
                            pe_mm(psB[:, 0:36], h2T[:, k, :], wr[:, k, :], k == 0, k == 7, [b_h2T, b_wr], [b_psB])
                        V("dve", lambda: nc.vector.tensor_tensor(out=LGa[:, j, :], in0=psB[:, 0:36], in1=brb[:], op=ALU.add),
                          [b_psB, b_brb], [b_LGa])
                        if tg == 0:
                            dump("logit0", LGa[:, 0, :], [b_LGa])
                    T16 = slice(b * 16, (b + 1) * 16)
                    lg = LGa[:, :, 0:4]
                    le = LGa[:, :, 4:36]
                    rb = [b_LGa, b_RW]
                    V("dve", lambda: nc.vector.tensor_reduce(out=RW[:, 0, 0:16], in_=lg, axis=AX.X, op=ALU.max), [b_LGa], [b_RW])
                    V("dve", lambda: nc.vector.tensor_tensor(out=RG[:, :, 0:4], in0=lg,
                                                             in1=RW[:, 0, 0:16].unsqueeze(2).to_broadcast([128, 16, 4]),
                                                             op=ALU.subtract), rb, [b_RG])
                    act(RG[:, :, 4:8], RG[:, :, 0:4], AF.Exp, [b_RG], [b_RG])
                    V("dve", lambda: nc.vector.tensor_reduce(out=RW[:, 1, 0:16], in_=RG[:, :, 4:8], axis=AX.X, op=ALU.add), [b_RG], [b_RW])
                    V("dve", lambda: nc.vector.reciprocal(out=RW[:, 2, 0:16], in_=RW[:, 1, 0:16]), [b_RW], [b_RW])
                    V("dve", lambda: nc.vector.tensor_scalar(out=RG[:, :, 8:12], in0=RG[:, :, 0:4], scalar1=0.0, scalar2=None,
                                                             op0=ALU.is_equal), [b_RG], [b_RG])
                    V("dve", lambda: nc.vector.tensor_scalar(out=RG[:, :, 8:12], in0=RG[:, :, 8:12], scalar1=-1.0, scalar2=1e30,
                                                             op0=ALU.add, op1=ALU.mult), [b_RG], [b_RG])
                    V("dve", lambda: nc.vector.tensor_tensor(
                        out=LEM[:, :, :].rearrange("p t (g e) -> p t g e", e=8), in0=le.rearrange("p t (g e) -> p t g e", e=8),
                        in1=RG[:, :, 8:12].unsqueeze(3).to_broadcast([128, 16, 4, 8]), op=ALU.add), [b_LGa, b_RG], [b_LEM])
                    V("dve", lambda: nc.vector.tensor_reduce(out=RW[:, 3, 0:16], in_=LEM[:], axis=AX.X, op=ALU.max), [b_LEM], [b_RW])
                    V("dve", lambda: nc.vector.tensor_tensor(out=oh1all[:, T16, :], in0=LEM[:],
                                                             in1=RW[:, 3, 0:16].unsqueeze(2).to_broadcast([128, 16, 32]),
                                                             op=ALU.is_equal), [b_LEM, b_RW], [b_oh1])
                    V("dve", lambda: nc.vector.scalar_tensor_tensor(out=LEM2[:], in0=oh1all[:, T16, :], scalar=-1e30, in1=LEM[:],
                                                                    op0=ALU.mult, op1=ALU.add), [b_oh1, b_LEM], [b_LEM2])
                    V("dve", lambda: nc.vector.tensor_reduce(out=RW[:, 4, 0:16], in_=LEM2[:], axis=AX.X, op=ALU.max), [b_LEM2], [b_RW])
                    V("dve", lambda: nc.vector.tensor_tensor(out=Mall[:, T16, :], in0=LEM[:],
                                                             in1=RW[:, 4, 0:16].unsqueeze(2).to_broadcast([128, 16, 32]),
                                                             op=ALU.is_ge), [b_LEM, b_RW], [b_Mall])
                    V("dve", lambda: nc.vector.tensor_tensor(out=RW[:, 5, 0:16], in0=RW[:, 4, 0:16], in1=RW[:, 3, 0:16], op=ALU.subtract),
                      [b_RW], [b_RW])
                    act(RW[:, 6, 0:16], RW[:, 5, 0:16], AF.Exp, [b_RW], [b_RW])
                    V("dve", lambda: nc.vector.tensor_scalar(out=RW[:, 6, 0:16], in0=RW[:, 6, 0:16], scalar1=1.0, scalar2=None, op0=ALU.add),
                      [b_RW], [b_RW])
                    V("dve", lambda: nc.vector.reciprocal(out=RW[:, 7, 0:16], in_=RW[:, 6, 0:16]), [b_RW], [b_RW])
                    V("dve", lambda: nc.vector.tensor_tensor(out=gates[:, T16, 0], in0=RW[:, 7, 0:16], in1=RW[:, 2, 0:16], op=ALU.mult),
                      [b_RW], [b_gates])
                    V("dve", lambda: nc.vector.tensor_tensor(out=gates[:, T16, 1], in0=RW[:, 2, 0:16], in1=gates[:, T16, 0], op=ALU.subtract),
                      [b_RW, b_gates], [b_gates])
                    S.barrier()
            if STOP_AFTER in ("s1", "s2", "s3", "s4"):
                break

        if STOP_AFTER is None or STOP_AFTER in ("route", "moe"):
            blk_i = sb(es, [128, NBLK], I32, "blki"); b_blki = Buf()
            with ExitStack() as esr:
                psC = ps(esr, [128, 32], F32, "psC"); b_psC = Buf()
                psR = [ps(esr, [128, 32], F32, "psR") for _ in range(2)]; b_psR = [Buf(), Buf()]
                cn = sb(esr, [128, 8, 32], F32, "cn"); b_cn = Buf()
                cni = sb(esr, [128, 32], I32, "cni"); b_cni = Buf()
                cmp_ = sb(esr, [128, NBLK, 32], F32, "cmp"); b_cmp = Buf()
                bst = sb(esr, [128, NBLK], F32, "bst"); b_bst = Buf()
                bs0 = sb(esr, [128, NBLK], F32, "bs0"); b_bs0 = Buf()
                blk_f = sb(esr, [128, NBLK], F32, "blkf"); b_blkf = Buf()
                tmp = sb(esr, [128, 2, 32], F32, "tmpr"); b_tmp = [Buf(), Buf()]
                for t in range(32):
                    pe_mm(psC[:], onesb[:], Mall[:, t, :], t == 0, t == 31, [b_onesb, b_Mall], [b_psC])
                V("dve", lambda: nc.vector.tensor_copy(out=cn[:, 0, :], in_=psC[:]), [b_psC], [b_cn])
                V("dve", lambda: nc.vector.tensor_scalar(out=cn[:, 1, :], in0=cn[:, 0, :], scalar1=255.0, scalar2=None, op0=ALU.add),
                  [b_cn], [b_cn])
                V("dve", lambda: nc.vector.tensor_copy(out=cni[:], in_=cn[:, 1, :]), [b_cn], [b_cni])
                V("dve", lambda: nc.vector.tensor_scalar(out=cni[:], in0=cni[:], scalar1=8, scalar2=8,
                                                         op0=ALU.arith_shift_right, op1=ALU.logical_shift_left), [b_cni], [b_cni])
                V("dve", lambda: nc.vector.tensor_copy(out=cn[:, 2, :], in_=cni[:]), [b_cni], [b_cn])
                V("dve", lambda: nc.vector.tensor_tensor_scan(out=cn[:, 3, :], data0=onesf[:, 0:32], data1=cn[:, 2, :],
                                                              initial=0.0, op0=ALU.mult, op1=ALU.add),
                  [b_cn, b_onesf], [b_cn])
                V("dve", lambda: nc.vector.tensor_tensor(out=cn[:, 4, :], in0=cn[:, 3, :], in1=cn[:, 2, :], op=ALU.subtract),
                  [b_cn], [b_cn])
                V("dve", lambda: nc.vector.tensor_scalar(out=bs0[:], in0=onesf[:, 0:NBLK], scalar1=256.0, scalar2=None, op0=ALU.mult),
                  [b_onesf], [b_bs0])
                V("dve", lambda: nc.vector.tensor_tensor_scan(out=bst[:], data0=onesf[:, 0:NBLK], data1=bs0[:], initial=-256.0,
                                                              op0=ALU.mult, op1=ALU.add), [b_bs0, b_onesf], [b_bst])
                V("dve", lambda: nc.vector.tensor_tensor(
                    out=cmp_[:], in0=cn[:, 3, :].unsqueeze(1).to_broadcast([128, NBLK, 32]),
                    in1=bst[:].unsqueeze(2).to_broadcast([128, NBLK, 32]), op=ALU.is_le), [b_cn, b_bst], [b_cmp])
                V("dve", lambda: nc.vector.tensor_reduce(out=blk_f[:], in_=cmp_[:], axis=AX.X, op=ALU.add), [b_cmp], [b_blkf])
                V("dve", lambda: nc.vector.tensor_scalar(out=blk_f[:], in0=blk_f[:], scalar1=31.0, scalar2=128.0,
                                                         op0=ALU.min, op1=ALU.mult), [b_blkf], [b_blkf])
                V("dve", lambda: nc.vector.tensor_scalar(out=blk_f[:], in0=blk_f[:], scalar1=iota[:, 0:1], scalar2=None,
                                                         op0=ALU.add), [b_blkf, b_iota], [b_blkf])
                V("dve", lambda: nc.vector.tensor_copy(out=blk_i[:], in_=blk_f[:]), [b_blkf], [b_blki])
                dump("blkf", blk_f[:], [b_blkf])
                dump("cn", cn[:, 0:5, :].rearrange("p a b -> p (a b)"), [b_cn])
                for t in range(32):
                    pR, bR = psR[t % 2], b_psR[t % 2]
                    for t2 in range(t):
                        pe_mm(pR[:], onesb[:], Mall[:, t2, :], t2 == 0, False, [b_onesb, b_Mall], [bR], sig=False)
                    pe_mm(pR[:], utri[:], Mall[:, t, :], t == 0, True, [b_utri, b_Mall], [bR])
                    tt = tmp[:, t % 2, :]; bt_ = b_tmp[t % 2]
                    V("dve", lambda: nc.vector.tensor_tensor(out=tt, in0=pR[:], in1=cn[:, 4, :], op=ALU.add), [bR, b_cn], [bt_])
                    V("dve", lambda: nc.vector.tensor_tensor(out=cmp_[:, 0, :], in0=tt, in1=oh1all[:, t, :], op=ALU.mult),
                      [bt_, b_oh1], [b_cmp])
                    V("dve", lambda: nc.vector.tensor_reduce(out=dest_f[:, t, 0:1], in_=cmp_[:, 0, :], axis=AX.X, op=ALU.add),
                      [b_cmp], [b_destf])
                    V("dve", lambda: nc.vector.tensor_tensor(out=cmp_[:, 1, :], in0=Mall[:, t, :], in1=oh1all[:, t, :], op=ALU.subtract),
                      [b_Mall, b_oh1], [b_cmp])
                    V("dve", lambda: nc.vector.tensor_tensor(out=cmp_[:, 1, :], in0=cmp_[:, 1, :], in1=tt, op=ALU.mult),
                      [bt_, b_cmp], [b_cmp])
                    V("dve", lambda: nc.vector.tensor_reduce(out=dest_f[:, t, 1:2], in_=cmp_[:, 1, :], axis=AX.X, op=ALU.add),
                      [b_cmp], [b_destf])
                V("dve", lambda: nc.vector.tensor_copy(out=dest_i[:], in_=dest_f[:, :, :].rearrange("p a b -> p (a b)")), [b_destf], [b_desti])
                dump("destf", dest_f[:, :, :].rearrange("p a b -> p (a b)"), [b_destf])
                dump("gates", gates[:, :, :].rearrange("p a b -> p (a b)"), [b_gates])
                hl = [sb(esr, [128, D], BF16, "hl") for _ in range(2)]; b_hl = [Buf(), Buf()]
                for t in range(32):
                    S.dma("sp", hl[t % 2][:], h2_d[t * 128:(t + 1) * 128, :], reads=[b_h2d], writes=[b_hl[t % 2]])
                    for kk in range(2):
                        S.dma_ind(xs_d[:, :], bass.IndirectOffsetOnAxis(ap=dest_i[:, 2 * t + kk:2 * t + kk + 1], axis=0), hl[t % 2][:, :], None,
                                  NROWS - 1, reads=[b_hl[t % 2], b_desti], writes=[b_xsd], group=True, semof=b_hl[t % 2])
                S.barrier()

            if STOP_AFTER != "route":
                with ExitStack() as esm:
                    w1b = [sb(esm, [128, 8, 512], BF16, "w1b") for _ in range(3)]; b_w1 = [Buf() for _ in range(3)]
                    w3b = [sb(esm, [128, 8, 512], BF16, "w3b") for _ in range(3)]; b_w3 = [Buf() for _ in range(3)]
                    w2b = [sb(esm, [128, 4, 1024], BF16, "w2b") for _ in range(3)]; b_w2 = [Buf() for _ in range(3)]
                    xr = [sb(esm, [128, 2, D], BF16, "xr") for _ in range(3)]; b_xr = [Buf() for _ in range(3)]
                    xT = [sb(esm, [128, 8, 256], BF16, "xT") for _ in range(3)]; b_xT = [[Buf(), Buf()] for _ in range(3)]
                    sl = [sb(esm, [128, 256], F32, "sl") for _ in range(2)]; b_sl = [Buf(), Buf()]
                    hm = [sb(esm, [128, 4, 256], BF16, "hm") for _ in range(2)]; b_hm = [Buf(), Buf()]
                    yo = [sb(esm, [128, 2, D], BF16, "yo") for _ in range(2)]; b_yo = [[Buf(), Buf()], [Buf(), Buf()]]
                    psX = [ps(esm, [128, 1024], BF16, "psX") for _ in range(2)]; b_psX = [Buf(), Buf()]
                    psH = [ps(esm, [128, 512], F32, "psH") for _ in range(2)]; b_psH = [Buf(), Buf()]
                    psY = [ps(esm, [128, 512], F32, "psY") for _ in range(4)]; b_psY = [Buf() for _ in range(4)]
                    xcnt = [0]; hcnt = [0]; ycnt = [0]

                    def moe_w(blk):
                        i3 = blk % 3
                        off = bass.IndirectOffsetOnAxis(ap=blk_i[:, blk:blk + 1], axis=0)
                        S.dma_ind(w1b[i3][:, :, :].rearrange("p a b -> p (a b)"), None, w1_d[:, :], off, 0,
                                  reads=[b_blki], writes=[b_w1[i3]])
                        S.dma_ind(w3b[i3][:, :, :].rearrange("p a b -> p (a b)"), None, w3_d[:, :], off, 0,
                                  reads=[b_blki], writes=[b_w3[i3]])
                        S.dma_ind(w2b[i3][:, :, :].rearrange("p a b -> p (a b)"), None, w2_d[:, :], off, 0,
                                  reads=[b_blki], writes=[b_w2[i3]])

                    def moe_x(blk):
                        i2 = blk % 3
                        S.dma("sp", xr[i2][:], xs_d[blk * 256:(blk + 1) * 256, :].rearrange("(a p) d -> p a d", p=128),
                              reads=[b_xsd], writes=[b_xr[i2]])
                        for k in range(8):
                            if k % 4 == 0:
                                pX, bX = psX[(xcnt[0] // 4) % 2], b_psX[(xcnt[0] // 4) % 2]
                            for a in range(2):
                                pe_tr(pX[:, (k % 4) * 256 + a * 128:(k % 4) * 256 + (a + 1) * 128],
                                      xr[i2][:, a, k * 128:(k + 1) * 128], identb[:], [b_xr[i2], b_identb], [bX],
                                      sig=(k % 4 == 3 and a == 1))
                            xcnt[0] += 1
                            if k % 4 == 3:
                                dstx = xT[i2][:, k - 3:k + 1, :].rearrange("p a b -> p (a b)")
                                V("dve", lambda: nc.vector.tensor_copy(out=dstx, in_=pX[:]), [bX], [b_xT[i2][(k // 4) % 2]])

                    def moe_c(blk):
                        i2 = blk % 2
                        i3 = blk % 3
                        ix = blk % 3
                        for c4 in range(4):
                            pH, bH = psH[hcnt[0] % 2], b_psH[hcnt[0] % 2]
                            si = hcnt[0] % 2
                            hcnt[0] += 1
                            for k in range(8):
                                pe_mm(pH[:, 0:256], w1b[i3][:, k, c4 * 128:(c4 + 1) * 128], xT[ix][:, k, :], k == 0, k == 7,
                                      [b_w1[i3], *b_xT[ix]], [bH], sig=False)
                            for k in range(8):
                                pe_mm(pH[:, 256:512], w3b[i3][:, k, c4 * 128:(c4 + 1) * 128], xT[ix][:, k, :], k == 0, k == 7,
                                      [b_w3[i3], *b_xT[ix]], [bH])
                            act(sl[si][:], pH[:, 0:256], AF.Silu, [bH], [b_sl[si]])
                            V("dve", lambda: nc.vector.tensor_tensor(out=hm[i2][:, c4, :], in0=pH[:, 256:512], in1=sl[si][:],
                                                                     op=ALU.mult), [bH, b_sl[si]], [b_hm[i2]])
                        for a in range(2):
                            for hf in range(2):
                                pY, bY = psY[ycnt[0] % 4], b_psY[ycnt[0] % 4]
                                ycnt[0] += 1
                                for k in range(4):
                                    pe_mm(pY[:], hm[i2][:, k, a * 128:(a + 1) * 128], w2b[i3][:, k, hf * 512:(hf + 1) * 512],
                                          k == 0, k == 3, [b_hm[i2], b_w2[i3]], [bY])
                                V("dve", lambda: nc.vector.tensor_copy(out=yo[i2][:, a, hf * 512:(hf + 1) * 512], in_=pY[:]), [bY],
                                  [b_yo[i2][hf]])
                        S.dma("act", ys_d[blk * 256:(blk + 1) * 256, :].rearrange("(a p) d -> p a d", p=128), yo[i2][:],
                              reads=b_yo[i2], writes=[b_ysd], group=True, semof=b_yo[i2][0])

                    moe_w(0)
                    moe_w(1)
                    moe_x(0)
                    moe_x(1)
                    for blk in range(NBLK):
                        if blk + 2 < NBLK:
                            moe_w(blk + 2)
                            moe_x(blk + 2)
                        moe_c(blk)
                    S.barrier()

                with ExitStack() as esf:
                    GA2 = [sb(esf, [128, D], F32, "GA2") for _ in range(2)]; b_GA2 = [Buf(), Buf()]
                    gfb = sb(esf, [128, D], F32, "gfb"); b_gfb = Buf()
                    diag = [sb(esf, [128, 128], F32, "diagf") for _ in range(2)]; b_diag = [Buf(), Buf()]
                    psB = ps(esf, [128, D], F32, "psBf"); b_psB = Buf()
                    S.dma("sp", gfb[:], gfb_d, writes=[b_gfb])
                    for b in range(NB):
                        bcast_row(esf, 5, b, psB, b_psB, diag, b_diag)
                        V("dve", lambda: nc.vector.tensor_copy(out=GA2[b][:], in_=psB[:]), [b_psB], [b_GA2[b]])
                    y0 = [sb(esf, [128, D], BF16, "y0") for _ in range(2)]; b_y0 = [Buf(), Buf()]
                    y1 = [sb(esf, [128, D], BF16, "y1") for _ in range(2)]; b_y1 = [Buf(), Buf()]
                    x1l = [sb(esf, [128, D], F32, "x1l") for _ in range(2)]; b_x1l = [Buf(), Buf()]
                    mo = [sb(esf, [128, D], F32, "mo") for _ in range(2)]; b_mo = [Buf(), Buf()]
                    ot = [sb(esf, [128, D], F32, "ot") for _ in range(2)]; b_ot = [Buf(), Buf()]
                    junkf = sb(esf, [128, D], BF16, "junkf"); b_junkf = Buf()
                    stf = sb(esf, [128, 32, 3], F32, "stf"); b_stf = [Buf() for _ in range(32)]
                    for t in range(32):
                        i2 = t % 2
                        bb = t // 16
                        S.dma_ind(y0[i2][:, :], None, ys_d[:, :], bass.IndirectOffsetOnAxis(ap=dest_i[:, 2 * t:2 * t + 1], axis=0),
                                  NROWS - 1, reads=[b_ysd, b_desti], writes=[b_y0[i2]])
                        S.dma_ind(y1[i2][:, :], None, ys_d[:, :], bass.IndirectOffsetOnAxis(ap=dest_i[:, 2 * t + 1:2 * t + 2], axis=0),
                                  NROWS - 1, reads=[b_ysd, b_desti], writes=[b_y1[i2]])
                        S.dma("sp", x1l[i2][:], x1_d[t * 128:(t + 1) * 128, :], reads=[b_x1d], writes=[b_x1l[i2]])
                        act(mo[i2][:], y0[i2][:], AF.Copy, [b_y0[i2], b_gates], [b_mo[i2]], scale=gates[:, t, 0:1])
                        V("dve", lambda: nc.vector.scalar_tensor_tensor(out=mo[i2][:], in0=y1[i2][:], scalar=gates[:, t, 1:2],
                                                                        in1=mo[i2][:], op0=ALU.mult, op1=ALU.add),
                          [b_y1[i2], b_gates, b_mo[i2]], [b_mo[i2]])
                        if t == 0:
                            dump("moe0", mo[i2][:], [b_mo[i2]])
                        V("pool", lambda: nc.gpsimd.tensor_tensor(out=mo[i2][:], in0=mo[i2][:], in1=GA2[bb][:], op=ALU.mult),
                          [b_mo[i2], b_GA2[bb]], [b_mo[i2]])
                        V("dve", lambda: nc.vector.tensor_tensor(out=mo[i2][:], in0=mo[i2][:], in1=x1l[i2][:], op=ALU.add),
                          [b_mo[i2], b_x1l[i2]], [b_mo[i2]])
                        act(junkf[:], mo[i2][:], AF.Square, [b_mo[i2]], [b_junkf, b_stf[t]], accum_out=stf[:, t, 0:1])
                        act(stf[:, t, 1:2], stf[:, t, 0:1], AF.Sqrt, [b_stf[t], b_eps], [b_stf[t]], scale=1.0 / D, bias=epst[:, 0:1])
                        V("dve", lambda: nc.vector.reciprocal(out=stf[:, t, 2:3], in_=stf[:, t, 1:2]), [b_stf[t]], [b_stf[t]])
                        V("dve", lambda: nc.vector.scalar_tensor_tensor(out=ot[i2][:], in0=mo[i2][:], scalar=stf[:, t, 2:3],
                                                                        in1=gfb[:], op0=ALU.mult, op1=ALU.mult),
                          [b_mo[i2], b_stf[t], b_gfb], [b_ot[i2]])
                        S.dma("act", out_d[bb, (t % 16) * 128:(t % 16 + 1) * 128, :], ot[i2][:], reads=[b_ot[i2]], writes=[b_outd],
                              group=True, semof=b_ot[i2])
        S.barrier()
        build_program.stats = dict(nops=dict(S.nops), nwaits=S.nwaits, nsem=S.nsem)
    return nc


def _fm(v):
    return np.ascontiguousarray(np.asarray(v, np.float32).reshape(-1, 128).T)


def _kp(w):
    K, N = w.shape
    return np.ascontiguousarray(w.reshape(K // 128, 128, N).transpose(1, 0, 2))


def _swap_cols():
    idx = np.arange(64)
    half = idx // 32
    within = idx % 32
    sw = np.where(within < 16, within + 16, within - 16)
    return half * 32 + sw


def _host_consts():
    nf = 16
    inv_freq = (10000.0 ** (-np.arange(nf, dtype=np.float32) / nf)).astype(np.float32)
    t = np.arange(L)
    row = (t // 64).astype(np.float32)
    col = (t % 64).astype(np.float32)
    cos = np.zeros((128, L), np.float32)
    sin = np.zeros((128, L), np.float32)
    for p in range(128):
        d = p % 64
        pos = row if d < 32 else col
        ang = (pos * inv_freq[(d % 32) % 16]).astype(np.float32)
        sign = -1.0 if (d % 32) < 16 else 1.0
        cos[p] = np.cos(ang)
        sin[p] = sign * np.sin(ang)
    cossin = np.concatenate([cos, sin], axis=1)
    ident = np.eye(128, dtype=np.float32)
    iota = np.arange(128, dtype=np.float32).reshape(128, 1)
    utri = np.triu(np.ones((128, 128), np.float32), k=1)
    return cossin, ident, iota, utri


def _bias_table(rpb):
    cq = np.arange(64)
    c_start = np.clip(cq - 8, 0, 48)
    band = (cq[None, :] >= c_start[:, None]) & (cq[None, :] < c_start[:, None] + 16)
    dc = np.clip(cq[None, :] - cq[:, None], -15, 15) + 15
    tab = np.full((64, 8, NTB, 64), -1e30, np.float32)
    for h in range(8):
        for dr in range(15):
            vals = rpb[h, dr][dc]
            tab[:, h, 1 + dr, :] = np.where(band, vals, np.float32(-1e30))
        tab[:, h, 18, :] = tab[:, h, 1 + 3, :]
        tab[:, h, 19, :] = tab[:, h, 1 + 10, :]
    return tab.reshape(64, 8 * NTB * 64)


def _prepare(inputs):
    f = lambda k: np.asarray(inputs[k], np.float32)
    w_in = f("w_in")[0]
    K_OFF, V_OFF, LX_OFF, Q_OFF, LG_OFF, GA_OFF, GB_OFF = 0, 512, 1024, 2048, 2560, 3584, 4608
    sw = _swap_cols()
    wqkv = []
    for hp in range(4):
        cols = []
        for base in (Q_OFF, K_OFF):
            plain = np.concatenate([base + (2 * hp + e) * 64 + np.arange(64) for e in range(2)])
            swp = np.concatenate([base + (2 * hp + e) * 64 + sw for e in range(2)])
            cols += [plain, swp]
        cols.append(V_OFF + hp * 128 + np.arange(128))
        wqkv.append(_kp(w_in[:, np.concatenate(cols)]).reshape(128, 8 * 640))
    wqkv = np.stack(wqkv)
    wlxlg = np.stack([_kp(w_in[:, np.concatenate([LX_OFF + n * 128 + np.arange(128), LG_OFF + n * 128 + np.arange(128)])]
                          ).reshape(128, 8 * 256) for n in range(8)])
    wgagb = np.stack([_kp(w_in[:, np.concatenate([GA_OFF + n * 128 + np.arange(128), GB_OFF + n * 128 + np.arange(128)])]
                          ).reshape(128, 8 * 256) for n in range(8)])
    wua = _kp(f("w_up_attn")[0])
    wul = _kp(f("w_up_lru")[0])
    wup = np.stack([np.concatenate([wua[:, :, n * 128:(n + 1) * 128], wul[:, :, n * 128:(n + 1) * 128]], axis=1
                                   ).reshape(128, 12 * 128) for n in range(8)])
    wout = _kp(f("w_out")[0]).reshape(128, 8 * D)
    wa = f("lru_wa")[0]
    wx = f("lru_wx")[0]
    lruw = np.stack([wa[0], wa[1], wx[0], wx[1]])
    lruw = np.ascontiguousarray(lruw.transpose(2, 0, 1, 3)).reshape(128, 4 * 8 * 128)
    vecs = np.concatenate([
        _fm(f("g_mix")[0]), _fm(f("g_ffn")[0]),
        np.concatenate([_fm(f("conv_w")[0][j]) for j in range(4)], axis=1),
        _fm(f("conv_b")[0]),
        np.concatenate([_fm(f("lru_ba")[0][d_]) for d_ in range(2)], axis=1),
        np.concatenate([_fm(f("lru_bx")[0][d_]) for d_ in range(2)], axis=1),
        np.concatenate([_fm(f("lru_lambda")[0][d_]) for d_ in range(2)], axis=1),
        _fm(f("b_mod")[0]),
    ], axis=1)
    assert vecs.shape == (128, NV)
    wmod = _kp(f("w_mod")[0])
    wr = _kp(np.concatenate([f("router_group_w")[0], f("router_expert_w")[0]], axis=1)).reshape(128, 8 * 36)
    brb = np.ascontiguousarray(np.broadcast_to(
        np.concatenate([f("router_group_b")[0], f("router_expert_b")[0]])[None, :], (128, 36)))
    gfb = np.ascontiguousarray(np.broadcast_to(f("g_final")[None, :], (128, D)))
    gffnb = np.ascontiguousarray(np.broadcast_to(f("g_ffn")[0][None, :], (128, D)))
    w1 = f("expert_w_gate")[0]
    w3 = f("expert_w_up")[0]
    w2 = f("expert_w_down")[0]
    w1h = np.ascontiguousarray(w1.reshape(32, 8, 128, 512).transpose(0, 2, 1, 3)).reshape(32 * 128, 8 * 512)
    w3h = np.ascontiguousarray(w3.reshape(32, 8, 128, 512).transpose(0, 2, 1, 3)).reshape(32 * 128, 8 * 512)
    w2h = np.ascontiguousarray(w2.reshape(32, 4, 128, 1024).transpose(0, 2, 1, 3)).reshape(32 * 128, 4 * 1024)
    cossin, ident, iota, utri = _host_consts()
    btab = _bias_table(f("rpb")[0])
    shared = dict(wmod=wmod, vecs=vecs, wqkv=wqkv, wlxlg=wlxlg, wgagb=wgagb, wup=wup, wout=wout, lruw=lruw,
                  cossin=cossin, btab=btab, wr=wr, brb=brb, gfb=gfb, gffnb=gffnb, w1h=w1h, w3h=w3h, w2h=w2h,
                  ident=ident, iota=iota, utri=utri)
    x = f("x")
    ctx = f("ctx")
    c = f("c")
    c_ctx = f("c_ctx")
    in_maps = []
    for core in range(NCORES):
        b0 = core * NB
        cs = np.stack([c[b0], c[b0 + 1], c_ctx], axis=-1)
        cs = np.ascontiguousarray(cs.reshape(8, 128, 3).transpose(1, 0, 2)).reshape(128, 24)
        m = dict(shared)
        m["xin"] = np.ascontiguousarray(x[b0:b0 + NB])
        m["ctxin"] = np.ascontiguousarray(ctx[b0:b0 + NB])
        m["cs"] = cs
        in_maps.append(m)
    return in_maps


def kernel(**inputs):
    in_maps = _prepare(inputs)
    nc = build_program()
    res = run_bass_kernel_spmd(nc, in_maps, core_ids=list(range(NCORES)))
    out = np.concatenate([np.asarray(r["out"], np.float32) for r in res.results], axis=0)
    return out
```

```python
import numpy as np
import concourse.bass as bass
import concourse.mybir as mybir
from concourse.bass_utils import run_bass_kernel_spmd
from contextlib import ExitStack

F32 = mybir.dt.float32
BF16 = mybir.dt.bfloat16
I32 = mybir.dt.int32
AF = mybir.ActivationFunctionType
ALU = mybir.AluOpType
AX = mybir.AxisListType

D = 1024
L = 2048
C = 256
NB = 2
NCORES = 8
LC = L + C
NTOK = NB * L
NBLK = 64
NROWS = NBLK * 256
EPS = 1e-6

V_GMIX, V_GFFN, V_CONVW, V_CONVB, V_BA, V_BX, V_LAM, V_BMOD, NV = 0, 8, 16, 48, 56, 72, 88, 104, 152
NTB = 21

DEBUG = {}
import os as _os
ATT_NHP = int(_os.environ.get("ATT_NHP", "4"))
ATT_NUNITS = int(_os.environ.get("ATT_NUNITS", "8"))
ATT_NROWS = int(_os.environ.get("ATT_NROWS", "8"))
ATT_NORM = int(_os.environ.get("ATT_NORM", "1"))
ATT_PARTS = int(_os.environ.get("ATT_PARTS", "31"))
STOP_AFTER = None


class Buf:
    __slots__ = ("name", "w", "wx", "r", "dsem", "dcnt", "grp")

    def __init__(self, name=""):
        self.name = name
        self.w = None
        self.wx = {}
        self.r = {}
        self.dsem = {}
        self.dcnt = {}
        self.grp = False


class Sync:
    ROLL = 30000

    def __init__(self, nc, es):
        self.nc = nc
        self.es = es
        self.eng = {"pe": nc.tensor, "act": nc.scalar, "dve": nc.vector, "pool": nc.gpsimd, "sp": nc.sync}
        self.sem = {}
        self.cnt = {}
        self.waited = {k: {} for k in self.eng}
        self.nsem = 0
        self.pe_sems = set()
        for k in self.eng:
            self._newsem(k)
        self.pend = []
        self.dbufs = []
        self.nops = {k: 0 for k in self.eng}
        self.nwaits = 0

    def _alloc(self, name):
        self.nsem += 1
        return self.es.enter_context(self.nc.semaphore(f"{name}{self.nsem}"))

    def _newsem(self, k):
        self.sem[k] = self._alloc("e" + k)
        self.cnt[k] = 0
        if k == "pe":
            self.pe_sems.add(id(self.sem[k]))

    def _wait(self, e, ev):
        semh, val = ev
        assert val is not None, "dependency on an unsignalled PE op"
        key = id(semh)
        if self.waited[e].get(key, 0) < val:
            self.eng[e].wait_ge(semh, val)
            self.waited[e][key] = val
            self.nwaits += 1

    def _dep1(self, e, ev, acc):
        if e == "pe" and (ev[1] is None or id(ev[0]) in self.pe_sems):
            return
        assert ev[1] is not None, "dependency on an unsignalled PE op"
        k = id(ev[0])
        if k not in acc or acc[k][1] < ev[1]:
            acc[k] = ev

    def _deps(self, e, reads, writes, group=False):
        acc = {}
        for b in reads:
            if b.w is not None:
                self._dep1(e, b.w, acc)
            for ev in b.wx.values():
                self._dep1(e, ev, acc)
        for b in writes:
            if not (group and b.grp):
                if b.w is not None:
                    self._dep1(e, b.w, acc)
                for ev in b.wx.values():
                    self._dep1(e, ev, acc)
            for ev in b.r.values():
                self._dep1(e, ev, acc)
        for ev in acc.values():
            self._wait(e, ev)

    def _mark(self, ev, reads, writes, key, group=False):
        for b in reads:
            b.r[key] = ev
        for b in writes:
            if group and b.grp:
                b.wx[key] = ev
            elif group:
                b.w = None
                b.wx = {key: ev}
            else:
                b.w = ev
                b.wx = {}
            b.grp = group
            b.r = {}

    def op(self, e, fn, reads=(), writes=(), sig=True):
        self._deps(e, reads, writes)
        ins = fn()
        self.nops[e] += 1
        if sig:
            if self.cnt[e] >= self.ROLL:
                self._newsem(e)
            self.cnt[e] += 1
            ins.then_inc(self.sem[e], 1)
            ev = [self.sem[e], self.cnt[e]]
            if e == "pe":
                for p in self.pend:
                    p[0] = self.sem[e]
                    p[1] = self.cnt[e]
                self.pend = []
        else:
            assert e == "pe"
            ev = [self.sem[e], None]
            self.pend.append(ev)
        self._mark(ev, reads, writes, e)
        return ins

    def _dma_common(self, q, issue, reads, writes, group, semof):
        d = semof if semof is not None else writes[0]
        c = "sw" if q == "pool" else "hw"
        if c not in d.dsem:
            d.dsem[c] = self._alloc("d")
            d.dcnt[c] = 0
            self.dbufs.append((d, c))
        self._deps(q, reads, writes, group=group)
        ins = issue()
        self.nops[q] += 1
        d.dcnt[c] += 16
        ins.then_inc(d.dsem[c], 16)
        ev = [d.dsem[c], d.dcnt[c]]
        self._mark(ev, reads, writes, id(d.dsem[c]), group=group)
        return ins

    def dma(self, q, out, in_, reads=(), writes=(), group=False, semof=None):
        return self._dma_common(q, lambda: self.eng[q].dma_start(out=out, in_=in_), reads, writes, group, semof)

    def dma_ind(self, out, out_off, in_, in_off, bound, reads=(), writes=(), group=False, semof=None):
        def issue():
            return self.nc.gpsimd.indirect_dma_start(out=out, out_offset=out_off, in_=in_, in_offset=in_off)
        return self._dma_common("pool", issue, reads, writes, group, semof)

    def barrier(self):
        assert not self.pend
        evs = [[self.sem[k], self.cnt[k]] for k in self.eng if k != "sp" and self.cnt[k] > 0]
        evs += [[b.dsem[c], b.dcnt[c]] for (b, c) in self.dbufs if b.dcnt[c] > 0]
        for ev in evs:
            self._wait("sp", ev)
        if self.cnt["sp"] >= self.ROLL:
            self._newsem("sp")
        self.cnt["sp"] += 1
        self.nc.sync.nop().then_inc(self.sem["sp"], 1)
        ev = [self.sem["sp"], self.cnt["sp"]]
        for k in self.eng:
            if k != "sp":
                self._wait(k, ev)


def build_program():
    nc = bass.Bass("TRN2", target_bir_lowering=False)

    def din(name, shape, dt=F32):
        return nc.dram_tensor(name, list(shape), dt, kind="ExternalInput").ap()

    xin = din("xin", [NB, L, D])
    ctxin = din("ctxin", [NB, C, D])
    cs_d = din("cs", [128, 24])
    wmod_d = din("wmod", [128, 8, 6 * D])
    vecs_d = din("vecs", [128, NV])
    wqkv_d = din("wqkv", [4, 128, 8 * 640])
    wlxlg_d = din("wlxlg", [8, 128, 8 * 256])
    wgagb_d = din("wgagb", [8, 128, 8 * 256])
    wup_d = din("wup", [8, 128, 12 * 128])
    wout_d = din("wout", [128, 8 * D])
    lruw_d = din("lruw", [128, 4 * 8 * 128])
    cossin_d = din("cossin", [128, 2 * L])
    btab_d = din("btab", [64, 8 * NTB * 64])
    wr_d = din("wr", [128, 8 * 36])
    brb_d = din("brb", [128, 36])
    gfb_d = din("gfb", [128, D])
    gffnb_d = din("gffnb", [128, D])
    w1_d = din("w1h", [32 * 128, 8 * 512])
    w3_d = din("w3h", [32 * 128, 8 * 512])
    w2_d = din("w2h", [32 * 128, 4 * 1024])
    ident_d = din("ident", [128, 128])
    iota_d = din("iota", [128, 1])
    utri_d = din("utri", [128, 128])
    out_d = nc.dram_tensor("out", [NB, L, D], F32, kind="ExternalOutput").ap()
    x1_d = nc.dram_tensor("x1s", [NTOK, D], F32, kind="Internal").ap()
    h2_d = nc.dram_tensor("h2s", [NTOK, D], BF16, kind="Internal").ap()
    xs_d = nc.dram_tensor("xss", [NROWS, D], BF16, kind="Internal").ap()
    ys_d = nc.dram_tensor("yss", [NROWS, D], BF16, kind="Internal").ap()
    dbg_d = {}
    for name, (shape, dt) in DEBUG.items():
        dbg_d[name] = nc.dram_tensor("dbg_" + name, list(shape), dt, kind="ExternalOutput").ap()

    with ExitStack() as es:
        S = Sync(nc, es)
        uid = [0]

        def sb(es_, shape, dt, name="t"):
            uid[0] += 1
            return es_.enter_context(nc.sbuf_tensor(f"{name}{uid[0]}", list(shape), dt))

        def ps(es_, shape, dt, name="p"):
            uid[0] += 1
            return es_.enter_context(nc.psum_tensor(f"{name}{uid[0]}", list(shape), dt))

        def pe_mm(out, lhsT, rhs, start, stop, reads, writes, sig=None):
            if sig is None:
                sig = stop
            return S.op("pe", lambda: nc.tensor.matmul(out, lhsT, rhs, start=start, stop=stop),
                        reads, writes, sig)

        def pe_tr(out, in_, ident, reads, writes, sig):
            return S.op("pe", lambda: nc.tensor.transpose(out, in_, ident), reads, writes, sig)

        def act(out, in_, func, reads, writes, **kw):
            return S.op("act", lambda: nc.scalar.activation(out=out, in_=in_, func=func, **kw), reads, writes)

        def V(e, fn, reads, writes):
            return S.op(e, fn, reads, writes)

        dbg_buf = Buf("dbg")

        def dump(name, ap, reads):
            if name in dbg_d:
                S.dma("sp", dbg_d[name], ap, reads=reads, writes=[dbg_buf], group=True, semof=Buf("dump_" + name))

        identf = sb(es, [128, 128], F32, "identf"); b_identf = Buf()
        identb = sb(es, [128, 128], BF16, "identb"); b_identb = Buf()
        onesf = sb(es, [128, 128], F32, "onesf"); b_onesf = Buf()
        onesb = sb(es, [128, 128], BF16, "onesb"); b_onesb = Buf()
        utri = sb(es, [128, 128], BF16, "utri"); b_utri = Buf()
        iota = sb(es, [128, 1], F32, "iota"); b_iota = Buf()
        epst = sb(es, [128, 1], F32, "eps"); b_eps = Buf()
        vecs = sb(es, [128, NV], F32, "vecs"); b_vecs = Buf()
        modfm = sb(es, [128, 48, 3], F32, "modfm"); b_modfm = Buf()
        A1 = sb(es, [128, 8, 3], F32, "A1"); b_A1 = Buf()
        lrup = sb(es, [128, 4, 16], F32, "lrup"); b_lrup = Buf()
        S.dma("sp", identf[:], ident_d, writes=[b_identf])
        S.dma("pool", identb[:], ident_d, writes=[b_identb])
        S.dma("pool", utri[:], utri_d, writes=[b_utri])
        S.dma("sp", iota[:], iota_d, writes=[b_iota])
        S.dma("sp", vecs[:], vecs_d, writes=[b_vecs])
        V("dve", lambda: nc.vector.memset(onesf[:], 1.0), [], [b_onesf])
        V("dve", lambda: nc.vector.memset(onesb[:], 1.0), [], [b_onesb])
        V("dve", lambda: nc.vector.memset(epst[:], EPS), [], [b_eps])

        with ExitStack() as es0:
            csb = sb(es0, [128, 24], F32, "cs"); b_cs = Buf()
            scs = sb(es0, [128, 24], BF16, "scs"); b_scs = Buf()
            wm = [sb(es0, [128, 8, 512], BF16, "wm") for _ in range(3)]
            b_wm = [Buf(), Buf(), Buf()]
            psmod = ps(es0, [128, 144], F32, "psmod"); b_psmod = Buf()
            S.dma("sp", csb[:], cs_d, writes=[b_cs])
            act(scs[:], csb[:], AF.Silu, [b_cs], [b_scs])
            for cb in range(12):
                w = wm[cb % 3]; bw = b_wm[cb % 3]
                S.dma("pool", w[:], wmod_d[:, :, cb * 512:(cb + 1) * 512], writes=[bw])
                for cc in range(4):
                    col = cb * 4 + cc
                    for k in range(8):
                        pe_mm(psmod[:, col * 3:(col + 1) * 3], w[:, k, cc * 128:(cc + 1) * 128],
                              scs[:, k * 3:(k + 1) * 3], k == 0, k == 7, [bw, b_scs], [b_psmod],
                              sig=(k == 7 and cc == 3))
            V("dve", lambda: nc.vector.tensor_tensor(
                out=modfm[:], in0=psmod[:, :].rearrange("p (a b) -> p a b", b=3),
                in1=vecs[:, V_BMOD:V_BMOD + 48].unsqueeze(2).to_broadcast([128, 48, 3]), op=ALU.add),
              [b_psmod, b_vecs], [b_modfm])
            V("dve", lambda: nc.vector.scalar_tensor_tensor(
                out=A1[:], in0=modfm[:, 8:16, :], scalar=1.0,
                in1=vecs[:, V_GMIX:V_GMIX + 8].unsqueeze(2).to_broadcast([128, 8, 3]),
                op0=ALU.add, op1=ALU.mult), [b_modfm, b_vecs], [b_A1])
            act(lrup[:, 2, :], vecs[:, V_LAM:V_LAM + 16], AF.Exp, [b_vecs], [b_lrup], scale=-1.0)
            act(lrup[:, 3, :], lrup[:, 2, :], AF.Ln, [b_lrup], [b_lrup], bias=1.0)
            V("dve", lambda: nc.vector.tensor_scalar(out=lrup[:, 0, :], in0=lrup[:, 3, :], scalar1=-8.0, scalar2=None,
                                                     op0=ALU.mult), [b_lrup], [b_lrup])
            V("dve", lambda: nc.vector.tensor_scalar(out=lrup[:, 1, :], in0=lrup[:, 3, :], scalar1=-16.0, scalar2=None,
                                                     op0=ALU.mult), [b_lrup], [b_lrup])
            dump("modfm", modfm[:, :, :].rearrange("p a b -> p (a b)"), [b_modfm])
            S.barrier()

        def bcast_row(es_, v, j, psb, b_psb, diag, b_diag):
            for k in range(8):
                V("dve", lambda: nc.vector.tensor_scalar(out=diag[k % 2][:], in0=identf[:],
                                                         scalar1=modfm[:, v * 8 + k, j:j + 1], scalar2=None,
                                                         op0=ALU.mult), [b_identf, b_modfm], [b_diag[k % 2]])
                pe_mm(psb[:, k * 128:(k + 1) * 128], onesf[:], diag[k % 2][:], True, True,
                      [b_onesf, b_diag[k % 2]], [b_psb], sig=True)

        Mall = sb(es, [128, 32, 32], BF16, "Mall"); b_Mall = Buf()
        oh1all = sb(es, [128, 32, 32], BF16, "oh1"); b_oh1 = Buf()
        gates = sb(es, [128, 32, 2], F32, "gates"); b_gates = Buf()
        dest_f = sb(es, [128, 32, 2], F32, "destf"); b_destf = Buf()
        dest_i = sb(es, [128, 64], I32, "desti"); b_desti = Buf()
        b_x1d = Buf("x1d"); b_h2d = Buf("h2d"); b_xsd = Buf("xsd"); b_ysd = Buf("ysd"); b_outd = Buf("outd")
        zt = sb(es, [128, 2, D], BF16, "zt"); b_zt = Buf()
        V("dve", lambda: nc.vector.memset(zt[:], 0.0), [], [b_zt])
        for blk in range(NBLK):
            S.dma("sp", xs_d[blk * 256:(blk + 1) * 256, :].rearrange("(a p) d -> p a d", p=128), zt[:],
                  reads=[b_zt], writes=[b_xsd], group=True, semof=b_zt)

        for b in range(NB):
            with ExitStack() as esb:
                hT = sb(esb, [128, 8, LC], BF16, "hT")
                b_hT = [[Buf(f"hT{i}a"), Buf(f"hT{i}b")] for i in range(5)]
                oatt = sb(esb, [128, 4, L], BF16, "oatt")
                b_oatt = [[Buf() for _ in range(4)] for _ in range(4)]

                with ExitStack() as es1:
                    xt = [sb(es1, [128, D], F32, "xt") for _ in range(3)]
                    b_xt = [Buf() for _ in range(3)]
                    junk = sb(es1, [128, D], BF16, "junk"); b_junk = Buf()
                    xn = [sb(es1, [128, D], BF16, "xn") for _ in range(8)]
                    b_xn = [Buf() for _ in range(8)]
                    st = sb(es1, [128, 18, 3], F32, "st"); b_st = [Buf() for _ in range(18)]
                    pst = [ps(es1, [128, 512], BF16, "pst") for _ in range(4)]
                    b_pst = [Buf() for _ in range(4)]
                    ti = 0
                    for grp in range(5):
                        ntile = 4 if grp < 4 else 2
                        jmod = b if grp < 4 else 2
                        for i in range(ntile):
                            t = grp * 4 + i
                            xb_, bx_ = xt[ti % 3], b_xt[ti % 3]
                            src = xin[b, t * 128:(t + 1) * 128, :] if grp < 4 else ctxin[b, i * 128:(i + 1) * 128, :]
                            S.dma("sp", xb_[:], src, writes=[bx_])
                            act(junk[:], xb_[:], AF.Square, [bx_], [b_junk, b_st[t]], accum_out=st[:, t, 0:1])
                            act(st[:, t, 1:2], st[:, t, 0:1], AF.Sqrt, [b_st[t], b_eps], [b_st[t]],
                                scale=1.0 / D, bias=epst[:, 0:1])
                            V("dve", lambda: nc.vector.reciprocal(out=st[:, t, 2:3], in_=st[:, t, 1:2]),
                              [b_st[t]], [b_st[t]])
                            xi = (grp % 2) * 4 + i
                            act(xn[xi][:], xb_[:], AF.Copy, [bx_, b_st[t]], [b_xn[xi]], scale=st[:, t, 2:3])
                            ti += 1
                        for k in range(8):
                            pp, bp = pst[k % 4], b_pst[k % 4]
                            for i in range(ntile):
                                xi = (grp % 2) * 4 + i
                                pe_tr(pp[:, i * 128:(i + 1) * 128], xn[xi][:, k * 128:(k + 1) * 128], identb[:],
                                      [b_xn[xi], b_identb], [bp], sig=(i == ntile - 1))
                            n = ntile * 128
                            dst = hT[:, k, grp * 512:grp * 512 + n]
                            if k % 2 == 0:
                                V("dve", lambda: nc.vector.tensor_scalar(
                                    out=dst, in0=pp[:, 0:n], scalar1=A1[:, k, jmod:jmod + 1],
                                    scalar2=modfm[:, k, jmod:jmod + 1], op0=ALU.mult, op1=ALU.add),
                                  [bp, b_A1, b_modfm], [b_hT[grp][0]])
                            else:
                                act(dst, pp[:, 0:n], AF.Identity, [bp, b_A1, b_modfm], [b_hT[grp][1]],
                                    scale=A1[:, k, jmod:jmod + 1], bias=modfm[:, k, jmod:jmod + 1])
                    if b == 0:
                        dump("hT", hT[:, :, :].rearrange("p a b -> p (a b)"), [x for l_ in b_hT for x in l_])
                    S.barrier()
                if STOP_AFTER == "s1":
                    break

                with ExitStack() as es2:
                    cs_t = sb(es2, [128, 2 * L], F32, "cossin"); b_cst = Buf()
                    btab = sb(es2, [128, 8, NTB * 64], BF16, "btab"); b_btab = Buf()
                    S.dma("sp", cs_t[:], cossin_d, writes=[b_cst])
                    S.dma("pool", btab[0:64, :, :].rearrange("p a b -> p (a b)"), btab_d, writes=[b_btab], group=True)
                    S.dma("pool", btab[64:128, :, :].rearrange("p a b -> p (a b)"), btab_d, writes=[b_btab], group=True)
                    wq = [sb(es2, [128, 8, 640], BF16, "wq") for _ in range(1)]; b_wq = [Buf()]
                    Qr = [sb(es2, [128, L], BF16, "Qr") for _ in range(1)]
                    Qp = [sb(es2, [128, L], BF16, "Qp") for _ in range(1)]
                    Kr = [sb(es2, [128, L], BF16, "Kr") for _ in range(1)]
                    Kc = [sb(es2, [128, C], BF16, "Kc") for _ in range(1)]
                    Vx = [sb(es2, [128, 18, 192], BF16, "Vx") for _ in range(1)]
                    b_Q = [[Buf() for _ in range(4)] for _ in range(1)]
                    b_Qp = [[Buf() for _ in range(4)] for _ in range(1)]
                    b_K = [Buf()]
                    b_Kc = [Buf()]
                    b_V = [[Buf() for _ in range(5)]]
                    t1 = [sb(es2, [128, 512], F32, "t1") for _ in range(2)]; b_t1 = [Buf(), Buf()]
                    t2 = [sb(es2, [128, 512], F32, "t2") for _ in range(2)]; b_t2 = [Buf(), Buf()]
                    PTc = [sb(es2, [128, 512], BF16, "PTc") for _ in range(2)]; b_PTc = [Buf(), Buf()]
                    PT = [sb(es2, [128, 320], BF16, "PT") for _ in range(3)]; b_PT = [Buf() for _ in range(3)]
                    rc = [sb(es2, [128, 512], F32, "rc") for _ in range(2)]; b_rc = [Buf(), Buf()]
                    psP = [ps(es2, [128, 512], F32, "psP") for _ in range(2)]; b_psP = [Buf(), Buf()]
                    psSc = [ps(es2, [128, 512], F32, "psSc") for _ in range(2)]; b_psSc = [Buf(), Buf()]
                    psS = [ps(es2, [128, 512], F32, "psS") for _ in range(2)]; b_psS = [Buf(), Buf()]
                    psO = [ps(es2, [128, 512], F32, "psO") for _ in range(2)]; b_psO = [Buf(), Buf()]
                    V("dve", lambda: nc.vector.memset(Vx[0][:, :, 64:128], 1.0), [], b_V[0])
                    pcnt = [0]

                    def nextP():
                        i = pcnt[0] % 2
                        pcnt[0] += 1
                        return psP[i], b_psP[i]

                    cnt_t = [0]
                    for hp in range(ATT_NHP):
                        par = 0
                        w = wq[par]; bw = b_wq[par]
                        S.dma("pool", w[:, :, :].rearrange("p a b -> p (a b)"), wqkv_d[hp], writes=[bw])
                        for blk in range(4 if ATT_PARTS & 1 else 0):
                            tok = slice(blk * 512, (blk + 1) * 512)
                            for which in range(2):
                                c0 = which * 256
                                pa, bpa = nextP()
                                for k in range(8):
                                    pe_mm(pa[:], w[:, k, c0:c0 + 128], hT[:, k, tok], k == 0, k == 7,
                                          [bw, *b_hT[blk]], [bpa])
                                pb, bpb = nextP()
                                for k in range(8):
                                    pe_mm(pb[:], w[:, k, c0 + 128:c0 + 256], hT[:, k, tok], k == 0, k == 7,
                                          [bw, *b_hT[blk]], [bpb])
                                ii = cnt_t[0] % 2
                                cnt_t[0] += 1
                                sc_ = 0.125 if which == 0 else 1.0
                                dstb = b_Q[par][blk] if which == 0 else b_K[par]
                                dst = (Qr if which == 0 else Kr)[par][:, tok]
                                V("dve", lambda: nc.vector.scalar_tensor_tensor(
                                    out=t1[ii][:], in0=pa[:], scalar=sc_, in1=cs_t[:, tok], op0=ALU.mult, op1=ALU.mult),
                                  [bpa, b_cst], [b_t1[ii]])
                                if which == 0 and (ATT_PARTS & 16):
                                    V("dve", lambda: nc.vector.tensor_scalar(out=Qp[par][:, tok], in0=pa[:], scalar1=0.125, scalar2=None,
                                                                             op0=ALU.mult), [bpa], [b_Qp[par][blk]])
                                V("dve", lambda: nc.vector.scalar_tensor_tensor(
                                    out=t2[ii][:], in0=pb[:], scalar=sc_, in1=cs_t[:, L + blk * 512:L + (blk + 1) * 512],
                                    op0=ALU.mult, op1=ALU.mult), [bpb, b_cst], [b_t2[ii]])
                                V("pool" if ATT_PARTS & 8 else "dve", lambda: (nc.gpsimd if ATT_PARTS & 8 else nc.vector).tensor_tensor(out=dst, in0=t1[ii][:], in1=t2[ii][:], op=ALU.add),
                                  [b_t1[ii], b_t2[ii]], [dstb])
                        if ATT_PARTS & 2:
                            pa, bpa = nextP()
                            for k in range(8):
                                pe_mm(pa[:, 0:C], w[:, k, 256:384], hT[:, k, L:LC], k == 0, k == 7, [bw, *b_hT[4]], [bpa])
                            act(Kc[par][:], pa[:, 0:C], AF.Copy, [bpa], [b_Kc[par]])
                        for g4 in range(5 if ATT_PARTS & 4 else 0):
                            nch = 4 if g4 < 4 else 2
                            pa, bpa = nextP()
                            for i in range(nch):
                                ch = g4 * 4 + i
                                for k in range(8):
                                    pe_mm(pa[:, i * 128:(i + 1) * 128], hT[:, k, ch * 128:(ch + 1) * 128],
                                          w[:, k, 512:640], k == 0, k == 7, [bw, *b_hT[g4]], [bpa],
                                          sig=(k == 7 and i == nch - 1))
                            src = pa[:, 0:nch * 128].rearrange("p (c a d) -> p c a d", a=2, d=64)
                            dstv = Vx[par][:, g4 * 4:g4 * 4 + nch, :].rearrange("p c (a d) -> p c a d", d=64)[:, :, 0::2, :]
                            if g4 % 2 == 0:
                                act(dstv, src, AF.Copy, [bpa], [b_V[par][g4]])
                            else:
                                V("dve", lambda: nc.vector.tensor_copy(out=dstv, in_=src), [bpa], [b_V[par][g4]])

                        if b == 0 and hp == 0:
                            dump("Qp", Qp[0][:], b_Qp[0])
                            dump("Qr", Qr[0][:], b_Q[0])
                            dump("Kr", Kr[0][:], b_K)
                            dump("Kc", Kc[0][:], b_Kc)
                            dump("Vx", Vx[0][:, :, :].rearrange("p a b -> p (a b)"), b_V[0])
                        units = []
                        for e in range(2):
                            for qb in range(4):
                                units.append((e, qb))
                        ucnt = [0]
                        for (e, qb) in units[int(_os.environ.get("ATT_USTART", "0")):][:ATT_NUNITS]:
                            h = hp * 2 + e
                            pr = slice(64 * e, 64 * e + 64)
                            qs = slice(qb * 512, (qb + 1) * 512)
                            vcols = slice(0, 128) if e == 0 else slice(64, 192)
                            oi = ucnt[0] % 2
                            ucnt[0] += 1
                            pO, bO = psO[oi], b_psO[oi]
                            for c in range(2):
                                pS, bS = psSc[c], b_psSc[c]
                                pe_mm(pS[:], Kc[par][pr, c * 128:(c + 1) * 128], Qp[par][pr, qs], True, True,
                                      [b_Kc[par], b_Qp[par][qb]], [bS])
                                act(PTc[c][:], pS[:], AF.Exp, [bS], [b_PTc[c]])
                            if b == 0 and hp == 0 and e == 0 and qb == 0:
                                dump("PTc", PTc[0][:], [b_PTc[0]])
                            for c in range(2):
                                pe_mm(pO[:], Vx[par][:, 16 + c, vcols], PTc[c][:], c == 0, False,
                                      [b_V[par][4], b_PTc[c]], [bO], sig=(c == 1))
                            rows = list(range(qb * 8, qb * 8 + ATT_NROWS))
                            plan = []
                            for r in rows:
                                rs = min(max(r - 4, 0), 24)
                                dr0 = rs - r + 7
                                chunks = []
                                if rs % 2 == 0:
                                    for j in range(4):
                                        chunks.append(((rs + 2 * j) // 2, 1 + dr0 + 2 * j))
                                else:
                                    assert dr0 == 3
                                    c0 = (rs - 1) // 2
                                    chunks.append((c0, 17))
                                    for j in range(1, 4):
                                        chunks.append((c0 + j, dr0 + 2 * j))
                                    chunks.append((c0 + 4, 19))
                                plan.append((r, chunks))

                            def emit_qk(idx):
                                r, chunks = plan[idx]
                                si = idx % 2
                                pS, bS = psS[si], b_psS[si]
                                qcol = slice(r * 64, (r + 1) * 64)
                                for j, (kc, blk0) in enumerate(chunks):
                                    o = pS[:, j * 64:(j + 1) * 64]
                                    pe_mm(o, Kr[par][pr, kc * 128:(kc + 1) * 128], Qr[par][pr, qcol], True, False,
                                          [b_K[par], b_Q[par][qb]], [bS], sig=False)
                                    lt = btab[pr, h, blk0 * 64:(blk0 + 2) * 64]
                                    pe_mm(o, lt, identb[pr, pr], False, True, [b_btab, b_identb], [bS],
                                          sig=(j == len(chunks) - 1))
                                n = len(chunks) * 64
                                pi = idx % 3
                                act(PT[pi][:, 0:n], pS[:, 0:n], AF.Exp, [bS], [b_PT[pi]])

                            def emit_pv(idx):
                                r, chunks = plan[idx]
                                pi = idx % 3
                                rr = r - qb * 8
                                for j, (kc, _) in enumerate(chunks):
                                    last = (j == len(chunks) - 1)
                                    pe_mm(pO[:, rr * 64:(rr + 1) * 64], Vx[par][:, kc, vcols], PT[pi][:, j * 64:(j + 1) * 64],
                                          False, last and idx == len(plan) - 1, [b_V[par][kc // 4], b_PT[pi]], [bO], sig=last)

                            if plan:
                                emit_qk(0)
                            for idx in range(len(plan)):
                                if idx + 1 < len(plan):
                                    emit_qk(idx + 1)
                                emit_pv(idx)
                            dn = slice(64, 128) if e == 0 else slice(0, 64)
                            if not ATT_NORM:
                                continue
                            V("dve", lambda: nc.vector.reciprocal(out=rc[oi][pr, :], in_=pO[dn, :]), [bO], [b_rc[oi]])
                            V("dve", lambda: nc.vector.tensor_tensor(out=oatt[pr, hp, qs], in0=pO[pr, :], in1=rc[oi][pr, :],
                                                                     op=ALU.mult), [bO, b_rc[oi]], [b_oatt[hp][qb]])
                    if b == 0:
                        dump("oatt", oatt[:, :, :].rearrange("p a b -> p (a b)"), [x for l_ in b_oatt for x in l_])
                    S.barrier()
                if STOP_AFTER == "s2":
                    break

                olru = sb(esb, [128, 8, L], BF16, "olru")
                b_olru = [Buf() for _ in range(8)]
                with ExitStack() as es3:
                    TL = LC + 3
                    wl = [sb(es3, [128, 8, 256], BF16, "wl") for _ in range(2)]; b_wl = [Buf(), Buf()]
                    lw = sb(es3, [128, 4, 8, 128], BF16, "lw"); b_lw = Buf()
                    S.dma("pool", lw[:, :, :, :].rearrange("p a b c -> p (a b c)"), lruw_d, writes=[b_lw])
                    LXp = sb(es3, [128, TL + 3], F32, "LXp"); b_LXp = Buf()
                    xc = sb(es3, [128, TL], F32, "xc"); b_xc = Buf()
                    xcb = [sb(es3, [128, TL], BF16, "xcb") for _ in range(2)]; b_xcb = [Buf(), Buf()]
                    av = sb(es3, [128, TL], F32, "av"); b_av = Buf()
                    wv = sb(es3, [128, TL], F32, "wv"); b_wv = Buf()
                    iv = sb(es3, [128, TL], F32, "iv"); b_iv = Buf()
                    hv = [sb(es3, [128, TL], F32, "hv") for _ in range(2)]; b_hv = [Buf(), Buf()]
                    gl = sb(es3, [128, L], BF16, "gl"); b_gl = Buf()
                    psA = [ps(es3, [128, 512], F32, "psA") for _ in range(4)]; b_psA = [Buf() for _ in range(4)]
                    pacnt = [0]

                    def nextA():
                        i = pacnt[0] % 4
                        pacnt[0] += 1
                        return psA[i], b_psA[i]

                    V("dve", lambda: nc.vector.memset(LXp[:], 0.0), [], [b_LXp])

                    def load_wl(n):
                        S.dma("pool", wl[n % 2][:, :, :].rearrange("p a b -> p (a b)"), wlxlg_d[n], writes=[b_wl[n % 2]])

                    def lx_conv(n):
                        w = wl[n % 2]; bw = b_wl[n % 2]
                        for blk in range(5):
                            nt = 512 if blk < 4 else C
                            tok = slice(blk * 512, blk * 512 + nt)
                            pa, bpa = nextA()
                            for k in range(8):
                                pe_mm(pa[:, 0:nt], w[:, k, 0:128], hT[:, k, tok], k == 0, k == 7, [bw, *b_hT[blk]], [bpa])
                            d0 = 261 + blk * 512 if blk < 4 else 2
                            act(LXp[:, d0:d0 + nt], pa[:, 0:nt], AF.Copy, [bpa], [b_LXp])
                        cw = lambda j: vecs[:, V_CONVW + j * 8 + n:V_CONVW + j * 8 + n + 1]
                        V("dve", lambda: nc.vector.tensor_scalar(out=xc[:], in0=LXp[:, 0:TL], scalar1=cw(0),
                                                                 scalar2=vecs[:, V_CONVB + n:V_CONVB + n + 1],
                                                                 op0=ALU.mult, op1=ALU.add), [b_LXp, b_vecs], [b_xc])
                        for j in range(1, 3):
                            V("dve", lambda: nc.vector.scalar_tensor_tensor(out=xc[:], in0=LXp[:, j:j + TL], scalar=cw(j),
                                                                            in1=xc[:], op0=ALU.mult, op1=ALU.add),
                              [b_LXp, b_vecs, b_xc], [b_xc])
                        V("dve", lambda: nc.vector.scalar_tensor_tensor(out=xcb[n % 2][:], in0=LXp[:, 3:3 + TL], scalar=cw(3),
                                                                        in1=xc[:], op0=ALU.mult, op1=ALU.add),
                          [b_LXp, b_vecs, b_xc], [b_xcb[n % 2]])

                    load_wl(0)
                    lx_conv(0)
                    for n in range(8):
                        w = wl[n % 2]; bw = b_wl[n % 2]
                        xb_ = xcb[n % 2]; bxb = b_xcb[n % 2]
                        if n + 1 < 8:
                            load_wl(n + 1)
                        if b == 0 and n == 0:
                            dump("xc0", xb_[:], [bxb])
                        for dr in range(2):
                            di = dr * 8 + n
                            for blk in range(5):
                                nt = 512 if blk < 4 else TL - 2048
                                tok = slice(blk * 512, blk * 512 + nt)
                                pr_, bpr = nextA()
                                pe_mm(pr_[:, 0:nt], lw[:, dr, n, :], xb_[:, tok], True, True, [b_lw, bxb], [bpr])
                                pi_, bpi = nextA()
                                pe_mm(pi_[:, 0:nt], lw[:, 2 + dr, n, :], xb_[:, tok], True, True, [b_lw, bxb], [bpi])
                                act(av[:, tok], pr_[:, 0:nt], AF.Sigmoid, [bpr, b_vecs], [b_av],
                                    bias=vecs[:, V_BA + di:V_BA + di + 1])
                                act(iv[:, tok], pi_[:, 0:nt], AF.Sigmoid, [bpi, b_vecs], [b_iv],
                                    bias=vecs[:, V_BX + di:V_BX + di + 1])
                            act(wv[:], av[:], AF.Exp, [b_av, b_lrup], [b_wv], scale=lrup[:, 1, di:di + 1])
                            act(av[:], av[:], AF.Exp, [b_av, b_lrup], [b_av], scale=lrup[:, 0, di:di + 1])
                            act(wv[:], wv[:], AF.Sqrt, [b_wv], [b_wv], scale=-1.0, bias=1.0)
                            V("pool", lambda: nc.gpsimd.tensor_tensor(out=iv[:], in0=iv[:], in1=xb_[:], op=ALU.mult),
                              [b_iv, bxb], [b_iv])
                            V("dve", lambda: nc.vector.tensor_tensor(out=wv[:], in0=iv[:], in1=wv[:], op=ALU.mult),
                              [b_iv, b_wv], [b_wv])
                            hh = hv[dr]; bh = b_hv[dr]
                            if dr == 0:
                                V("dve", lambda: nc.vector.tensor_tensor_scan(
                                    out=hh[:, 0:C], data0=av[:, 0:C], data1=wv[:, 0:C], initial=0.0,
                                    op0=ALU.mult, op1=ALU.add), [b_av, b_wv], [bh])
                                V("dve", lambda: nc.vector.tensor_tensor_scan(
                                    out=hh[:, C + 3:TL], data0=av[:, C + 3:TL], data1=wv[:, C + 3:TL],
                                    initial=hh[:, C - 1:C], op0=ALU.mult, op1=ALU.add), [b_av, b_wv, bh], [bh])
                                if n + 1 < 8:
                                    lx_conv(n + 1)
                            else:
                                V("dve", lambda: nc.vector.tensor_tensor_scan(
                                    out=hh[:, C - 1::-1], data0=av[:, C - 1::-1], data1=wv[:, C - 1::-1], initial=0.0,
                                    op0=ALU.mult, op1=ALU.add), [b_av, b_wv], [bh])
                                V("dve", lambda: nc.vector.tensor_tensor_scan(
                                    out=hh[:, TL - 1:C + 2:-1], data0=av[:, TL - 1:C + 2:-1], data1=wv[:, TL - 1:C + 2:-1],
                                    initial=hh[:, 0:1], op0=ALU.mult, op1=ALU.add), [b_av, b_wv, bh], [bh])
                        for blk in range(4):
                            tok = slice(blk * 512, (blk + 1) * 512)
                            pa, bpa = nextA()
                            for k in range(8):
                                pe_mm(pa[:], w[:, k, 128:256], hT[:, k, tok], k == 0, k == 7, [bw, *b_hT[blk]], [bpa])
                            act(gl[:, tok], pa[:], AF.Gelu_apprx_tanh, [bpa], [b_gl])
                        V("dve", lambda: nc.vector.tensor_tensor(out=hv[0][:, C + 3:TL], in0=hv[0][:, C + 3:TL],
                                                                 in1=hv[1][:, C + 3:TL], op=ALU.add),
                          [b_hv[0], b_hv[1]], [b_hv[0]])
                        if b == 0 and n == 0:
                            dump("hsum0", hv[0][:, C + 3:TL], [b_hv[0]])
                        V("dve", lambda: nc.vector.tensor_tensor(out=olru[:, n, :], in0=hv[0][:, C + 3:TL], in1=gl[:],
                                                                 op=ALU.mult), [b_hv[0], b_gl], [b_olru[n]])
                    if b == 0:
                        dump("olru", olru[:, :, :].rearrange("p a b -> p (a b)"), b_olru)
                    S.barrier()
                if STOP_AFTER == "s3":
                    break

                yT = sb(esb, [128, 8, L], BF16, "yT")
                b_yT = [Buf() for _ in range(4)]
                with ExitStack() as es4:
                    wg_ = [sb(es4, [128, 8, 256], BF16, "wg") for _ in range(2)]; b_wg = [Buf(), Buf()]
                    wu_ = [sb(es4, [128, 12, 128], BF16, "wu") for _ in range(2)]; b_wu = [Buf(), Buf()]
                    ga_ = [sb(es4, [128, 512], F32, "ga") for _ in range(2)]; b_ga = [Buf(), Buf()]
                    gb_ = [sb(es4, [128, 512], F32, "gb") for _ in range(2)]; b_gb = [Buf(), Buf()]
                    ya_ = [sb(es4, [128, 512], F32, "ya") for _ in range(2)]; b_ya = [Buf(), Buf()]
                    yb_ = [sb(es4, [128, 512], F32, "yb") for _ in range(2)]; b_yb = [Buf(), Buf()]
                    psM = [ps(es4, [128, 512], F32, "psM") for _ in range(8)]; b_psM = [Buf() for _ in range(8)]
                    it = 0
                    for f in range(8):
                        wgt, bwg = wg_[f % 2], b_wg[f % 2]
                        wut, bwu = wu_[f % 2], b_wu[f % 2]
                        S.dma("pool", wgt[:, :, :].rearrange("p a b -> p (a b)"), wgagb_d[f], writes=[bwg])
                        S.dma("pool", wut[:, :, :].rearrange("p a b -> p (a b)"), wup_d[f], writes=[bwu])
                        for blk in range(4):
                            tok = slice(blk * 512, (blk + 1) * 512)
                            i2 = it % 2
                            p0, p1, p2, p3 = [psM[(it % 2) * 4 + q] for q in range(4)]
                            q0, q1, q2, q3 = [b_psM[(it % 2) * 4 + q] for q in range(4)]
                            it += 1
                            for k in range(8):
                                pe_mm(p0[:], wgt[:, k, 0:128], hT[:, k, tok], k == 0, k == 7, [bwg, *b_hT[blk]], [q0])
                            act(ga_[i2][:], p0[:], AF.Sigmoid, [q0], [b_ga[i2]])
                            for k in range(4):
                                pe_mm(p1[:], wut[:, k, :], oatt[:, k, tok], k == 0, k == 3, [bwu, b_oatt[k][blk]], [q1])
                            V("dve", lambda: nc.vector.tensor_tensor(out=ya_[i2][:], in0=p1[:], in1=ga_[i2][:], op=ALU.mult),
                              [q1, b_ga[i2]], [b_ya[i2]])
                            for k in range(8):
                                pe_mm(p2[:], wgt[:, k, 128:256], hT[:, k, tok], k == 0, k == 7, [bwg, *b_hT[blk]], [q2])
                            act(gb_[i2][:], p2[:], AF.Sigmoid, [q2], [b_gb[i2]])
                            for k in range(8):
                                pe_mm(p3[:], wut[:, 4 + k, :], olru[:, k, tok], k == 0, k == 7, [bwu, b_olru[k]], [q3])
                            V("dve", lambda: nc.vector.tensor_tensor(out=yb_[i2][:], in0=p3[:], in1=gb_[i2][:], op=ALU.mult),
                              [q3, b_gb[i2]], [b_yb[i2]])
                            V("pool", lambda: nc.gpsimd.tensor_tensor(out=yT[:, f, tok], in0=ya_[i2][:], in1=yb_[i2][:],
                                                                      op=ALU.add), [b_ya[i2], b_yb[i2]], [b_yT[blk]])
                    if b == 0:
                        dump("yT", yT[:, :, :].rearrange("p a b -> p (a b)"), b_yT)
                    S.barrier()
                if STOP_AFTER == "s4":
                    break

                with ExitStack() as es5:
                    wo32 = [sb(es5, [128, D], F32, "wo32") for _ in range(2)]; b_wo32 = [Buf(), Buf()]
                    wob = sb(es5, [128, 8, D], BF16, "wob"); b_wob = Buf()
                    GA1 = sb(es5, [128, D], F32, "GA1"); b_GA1 = Buf()
                    G2 = sb(es5, [128, D], F32, "G2"); b_G2 = Buf()
                    S2 = sb(es5, [128, D], F32, "S2"); b_S2 = Buf()
                    gffnb = sb(es5, [128, D], F32, "gffnb"); b_gffnb = Buf()
                    diag = [sb(es5, [128, 128], F32, "diag") for _ in range(2)]; b_diag = [Buf(), Buf()]
                    wr = sb(es5, [128, 8, 36], F32, "wr"); b_wr = Buf()
                    brb = sb(es5, [128, 36], F32, "brb"); b_brb = Buf()
                    psB = ps(es5, [128, D], F32, "psB"); b_psB = Buf()
                    psO5 = [ps(es5, [128, D], F32, "psO5") for _ in range(2)]; b_psO5 = [Buf(), Buf()]
                    psT = ps(es5, [128, D], F32, "psT"); b_psT = Buf()
                    S.dma("sp", gffnb[:], gffnb_d, writes=[b_gffnb])
                    S.dma("sp", wr[:, :, :].rearrange("p a b -> p (a b)"), wr_d, writes=[b_wr])
                    S.dma("sp", brb[:], brb_d, writes=[b_brb])
                    bcast_row(es5, 2, b, psB, b_psB, diag, b_diag)
                    V("dve", lambda: nc.vector.tensor_copy(out=GA1[:], in_=psB[:]), [b_psB], [b_GA1])
                    bcast_row(es5, 4, b, psB, b_psB, diag, b_diag)
                    V("dve", lambda: nc.vector.scalar_tensor_tensor(out=G2[:], in0=psB[:], scalar=1.0, in1=gffnb[:],
                                                                    op0=ALU.add, op1=ALU.mult), [b_psB, b_gffnb], [b_G2])
                    bcast_row(es5, 3, b, psB, b_psB, diag, b_diag)
                    V("dve", lambda: nc.vector.tensor_copy(out=S2[:], in_=psB[:]), [b_psB], [b_S2])
                    for kk in range(8):
                        S.dma("sp", wo32[kk % 2][:], wout_d[:, kk * D:(kk + 1) * D], writes=[b_wo32[kk % 2]])
                        V("dve", lambda: nc.vector.tensor_tensor(out=wob[:, kk, :], in0=wo32[kk % 2][:], in1=GA1[:],
                                                                 op=ALU.mult), [b_wo32[kk % 2], b_GA1], [b_wob])
                    xt5 = [sb(es5, [128, D], F32, "xt5") for _ in range(2)]; b_xt5 = [Buf(), Buf()]
                    x1t = xt5; b_x1t = b_xt5
                    h2t = [sb(es5, [128, D], F32, "h2t") for _ in range(2)]; b_h2t = [Buf(), Buf()]
                    h2b = [sb(es5, [128, D], BF16, "h2b") for _ in range(2)]; b_h2b = [Buf(), Buf()]
                    h2T = sb(es5, [128, 8, 128], F32, "h2T"); b_h2T = Buf()
                    junk5 = sb(es5, [128, D], BF16, "junk5"); b_junk5 = Buf()
                    st5 = sb(es5, [128, 16, 3], F32, "st5"); b_st5 = [Buf() for _ in range(16)]
                    LGa = sb(es5, [128, 16, 36], F32, "LGa"); b_LGa = Buf()
                    RW = sb(es5, [128, 8, 16], F32, "RW"); b_RW = Buf()
                    RG = sb(es5, [128, 16, 12], F32, "RG"); b_RG = Buf()
                    LEM = sb(es5, [128, 16, 32], F32, "LEM"); b_LEM = Buf()
                    LEM2 = sb(es5, [128, 16, 32], F32, "LEM2"); b_LEM2 = Buf()
                    def wout_mm(j):
                        i2 = j % 2
                        tsl = slice(j * 128, (j + 1) * 128)
                        S.dma("sp", xt5[i2][:], xin[b, tsl, :], writes=[b_xt5[i2]])
                        pO, bO = psO5[i2], b_psO5[i2]
                        for hf in range(2):
                            for k in range(8):
                                pe_mm(pO[:, hf * 512:(hf + 1) * 512], yT[:, k, tsl], wob[:, k, hf * 512:(hf + 1) * 512],
                                      k == 0, k == 7, [b_yT[j // 4], b_wob], [bO], sig=(k == 7 and hf == 1))

                    wout_mm(0)
                    for j in range(16):
                        tg = b * 16 + j
                        i2 = j % 2
                        tsl = slice(j * 128, (j + 1) * 128)
                        pO, bO = psO5[i2], b_psO5[i2]
                        if j + 1 < 16:
                            wout_mm(j + 1)
                        V("dve", lambda: nc.vector.tensor_tensor(out=x1t[i2][:], in0=pO[:], in1=xt5[i2][:], op=ALU.add),
                          [bO, b_xt5[i2]], [b_xt5[i2]])
                        S.dma("pool", x1_d[tg * 128:(tg + 1) * 128, :], x1t[i2][:], reads=[b_x1t[i2]], writes=[b_x1d], group=True, semof=b_x1t[i2])
                        act(junk5[:], x1t[i2][:], AF.Square, [b_x1t[i2]], [b_junk5, b_st5[j]], accum_out=st5[:, j, 0:1])
                        act(st5[:, j, 1:2], st5[:, j, 0:1], AF.Sqrt, [b_st5[j], b_eps], [b_st5[j]], scale=1.0 / D,
                            bias=epst[:, 0:1])
                        V("dve", lambda: nc.vector.reciprocal(out=st5[:, j, 2:3], in_=st5[:, j, 1:2]), [b_st5[j]], [b_st5[j]])
                        V("dve", lambda: nc.vector.scalar_tensor_tensor(out=h2t[i2][:], in0=x1t[i2][:], scalar=st5[:, j, 2:3],
                                                                        in1=G2[:], op0=ALU.mult, op1=ALU.mult),
                          [b_x1t[i2], b_st5[j], b_G2], [b_h2t[i2]])
                        V("dve", lambda: nc.vector.tensor_tensor(out=h2t[i2][:], in0=h2t[i2][:], in1=S2[:], op=ALU.add),
                          [b_h2t[i2], b_S2], [b_h2t[i2]])
                        act(h2b[i2][:], h2t[i2][:], AF.Copy, [b_h2t[i2]], [b_h2b[i2]])
                        S.dma("pool", h2_d[tg * 128:(tg + 1) * 128, :], h2b[i2][:], reads=[b_h2b[i2]], writes=[b_h2d], group=True, semof=b_h2b[i2])
                        for k in range(8):
                            pe_tr(psT[:, k * 128:(k + 1) * 128], h2t[i2][:, k * 128:(k + 1) * 128], identf[:],
                                  [b_h2t[i2], b_identf], [b_psT], sig=(k == 7))
                        act(h2T[:, :, :].rearrange("p a b -> p (a b)"), psT[:], AF.Copy, [b_psT], [b_h2T])
                        for k in range(8):
                            pe_mm(psB[:, 0:36], h2T[:, k, :], wr[:, k, :], k == 0, k == 7, [b_h2T, b_wr], [b_psB])
                        V("dve", lambda: nc.vector.tensor_tensor(out=LGa[:, j, :], in0=psB[:, 0:36], in1=brb[:], op=ALU.add),
                          [b_psB, b_brb], [b_LGa])
                        if tg == 0:
                            dump("logit0", LGa[:, 0, :], [b_LGa])
                    T16 = slice(b * 16, (b + 1) * 16)
                    lg = LGa[:, :, 0:4]
                    le = LGa[:, :, 4:36]
                    rb = [b_LGa, b_RW]
                    V("dve", lambda: nc.vector.tensor_reduce(out=RW[:, 0, 0:16], in_=lg, axis=AX.X, op=ALU.max), [b_LGa], [b_RW])
                    V("dve", lambda: nc.vector.tensor_tensor(out=RG[:, :, 0:4], in0=lg,
                                                             in1=RW[:, 0, 0:16].unsqueeze(2).to_broadcast([128, 16, 4]),
                                                             op=ALU.subtract), rb, [b_RG])
                    act(RG[:, :, 4:8], RG[:, :, 0:4], AF.Exp, [b_RG], [b_RG])
                    V("dve", lambda: nc.vector.tensor_reduce(out=RW[:, 1, 0:16], in_=RG[:, :, 4:8], axis=AX.X, op=ALU.add), [b_RG], [b_RW])
                    V("dve", lambda: nc.vector.reciprocal(out=RW[:, 2, 0:16], in_=RW[:, 1, 0:16]), [b_RW], [b_RW])
                    V("dve", lambda: nc.vector.tensor_scalar(out=RG[:, :, 8:12], in0=RG[:, :, 0:4], scalar1=0.0, scalar2=None,
                                                             op0=ALU.is_equal), [b_RG], [b_RG])
                    V("dve", lambda: nc.vector.tensor_scalar(out=RG[:, :, 8:12], in0=RG[:, :, 8:12], scalar1=-1.0, scalar2=1e30,
                                                             op0=ALU.add, op1=ALU.mult), [b_RG], [b_RG])
                    V("dve", lambda: nc.vector.tensor_tensor(
                        out=LEM[:, :, :].rearrange("p t (g e) -> p t g e", e=8), in0=le.rearrange("p t (g e) -> p t g e", e=8),
                        in1=RG[:, :, 8:12].unsqueeze(3).to_broadcast([128, 16, 4, 8]), op=ALU.add), [b_LGa, b_RG], [b_LEM])
                    V("dve", lambda: nc.vector.tensor_reduce(out=RW[:, 3, 0:16], in_=LEM[:], axis=AX.X, op=ALU.max), [b_LEM], [b_RW])
                    V("dve", lambda: nc.vector.tensor_tensor(out=oh1all[:, T16, :], in0=LEM[:],
                                                             in1=RW[:, 3, 0:16].unsqueeze(2).to_broadcast([128, 16, 32]),
                                                             op=ALU.is_equal), [b_LEM, b_RW], [b_oh1])
                    V("dve", lambda: nc.vector.scalar_tensor_tensor(out=LEM2[:], in0=oh1all[:, T16, :], scalar=-1e30, in1=LEM[:],
                                                                    op0=ALU.mult, op1=ALU.add), [b_oh1, b_LEM], [b_LEM2])
                    V("dve", lambda: nc.vector.tensor_reduce(out=RW[:, 4, 0:16], in_=LEM2[:], axis=AX.X, op=ALU.max), [b_LEM2], [b_RW])
                    V("dve", lambda: nc.vector.tensor_tensor(out=Mall[:, T16, :], in0=LEM[:],
                                                             in1=RW[:, 4, 0:16].unsqueeze(2).to_broadcast([128, 16, 32]),
                                                             op=ALU.is_ge), [b_LEM, b_RW], [b_Mall])
                    V("dve", lambda: nc.vector.tensor_tensor(out=RW[:, 5, 0:16], in0=RW[:, 4, 0:16], in1=RW[:, 3, 0:16], op=ALU.subtract),
                      [b_RW], [b_RW])
                    act(RW[:, 6, 0:16], RW[:, 5, 0:16], AF.Exp, [b_RW], [b_RW])
                    V("dve", lambda: nc.vector.tensor_scalar(out=RW[:, 6, 0:16], in0=RW[:, 6, 0:16], scalar1=1.0, scalar2=None, op0=ALU.add),
                      [b_RW], [b_RW])
                    V("dve", lambda: nc.vector.reciprocal(out=RW[:, 7, 0:16], in_=RW[:, 6, 0:16]), [b_RW], [b_RW])
                    V("dve", lambda: nc.vector.tensor_tensor(out=gates[:, T16, 0], in0=RW[:, 7, 0:16], in1=RW[:, 2, 0:16], op=ALU.mult),
                      [b_RW], [b_gates])
                    V("dve", lambda: nc.vector.tensor_tensor(out=gates[:, T16, 1], in0=RW[:, 2, 0:16], in1=gates[:, T16, 0], op=ALU.subtract),
                      [b_RW, b_gates], [b_gates])
                    S.barrier()
            if STOP_AFTER in ("s1", "s2", "s3", "s4"):
                break

        if STOP_AFTER is None or STOP_AFTER in ("route", "moe"):
            blk_i = sb(es, [128, NBLK], I32, "blki"); b_blki = Buf()
            with ExitStack() as esr:
                psC = ps(esr, [128, 32], F32, "psC"); b_psC = Buf()
                psR = [ps(esr, [128, 32], F32, "psR") for _ in range(2)]; b_psR = [Buf(), Buf()]
                cn = sb(esr, [128, 8, 32], F32, "cn"); b_cn = Buf()
                cni = sb(esr, [128, 32], I32, "cni"); b_cni = Buf()
                cmp_ = sb(esr, [128, NBLK, 32], F32, "cmp"); b_cmp = Buf()
                bst = sb(esr, [128, NBLK], F32, "bst"); b_bst = Buf()
                bs0 = sb(esr, [128, NBLK], F32, "bs0"); b_bs0 = Buf()
                blk_f = sb(esr, [128, NBLK], F32, "blkf"); b_blkf = Buf()
                tmp = sb(esr, [128, 2, 32], F32, "tmpr"); b_tmp = [Buf(), Buf()]
                for t in range(32):
                    pe_mm(psC[:], onesb[:], Mall[:, t, :], t == 0, t == 31, [b_onesb, b_Mall], [b_psC])
                V("dve", lambda: nc.vector.tensor_copy(out=cn[:, 0, :], in_=psC[:]), [b_psC], [b_cn])
                V("dve", lambda: nc.vector.tensor_scalar(out=cn[:, 1, :], in0=cn[:, 0, :], scalar1=255.0, scalar2=None, op0=ALU.add),
                  [b_cn], [b_cn])
                V("dve", lambda: nc.vector.tensor_copy(out=cni[:], in_=cn[:, 1, :]), [b_cn], [b_cni])
                V("dve", lambda: nc.vector.tensor_scalar(out=cni[:], in0=cni[:], scalar1=8, scalar2=8,
                                                         op0=ALU.arith_shift_right, op1=ALU.logical_shift_left), [b_cni], [b_cni])
                V("dve", lambda: nc.vector.tensor_copy(out=cn[:, 2, :], in_=cni[:]), [b_cni], [b_cn])
                V("dve", lambda: nc.vector.tensor_tensor_scan(out=cn[:, 3, :], data0=onesf[:, 0:32], data1=cn[:, 2, :],
                                                              initial=0.0, op0=ALU.mult, op1=ALU.add),
                  [b_cn, b_onesf], [b_cn])
                V("dve", lambda: nc.vector.tensor_tensor(out=cn[:, 4, :], in0=cn[:, 3, :], in1=cn[:, 2, :], op=ALU.subtract),
                  [b_cn], [b_cn])
                V("dve", lambda: nc.vector.tensor_scalar(out=bs0[:], in0=onesf[:, 0:NBLK], scalar1=256.0, scalar2=None, op0=ALU.mult),
                  [b_onesf], [b_bs0])
                V("dve", lambda: nc.vector.tensor_tensor_scan(out=bst[:], data0=onesf[:, 0:NBLK], data1=bs0[:], initial=-256.0,
                                                              op0=ALU.mult, op1=ALU.add), [b_bs0, b_onesf], [b_bst])
                V("dve", lambda: nc.vector.tensor_tensor(
                    out=cmp_[:], in0=cn[:, 3, :].unsqueeze(1).to_broadcast([128, NBLK, 32]),
                    in1=bst[:].unsqueeze(2).to_broadcast([128, NBLK, 32]), op=ALU.is_le), [b_cn, b_bst], [b_cmp])
                V("dve", lambda: nc.vector.tensor_reduce(out=blk_f[:], in_=cmp_[:], axis=AX.X, op=ALU.add), [b_cmp], [b_blkf])
                V("dve", lambda: nc.vector.tensor_scalar(out=blk_f[:], in0=blk_f[:], scalar1=31.0, scalar2=128.0,
                                                         op0=ALU.min, op1=ALU.mult), [b_blkf], [b_blkf])
                V("dve", lambda: nc.vector.tensor_scalar(out=blk_f[:], in0=blk_f[:], scalar1=iota[:, 0:1], scalar2=None,
                                                         op0=ALU.add), [b_blkf, b_iota], [b_blkf])
                V("dve", lambda: nc.vector.tensor_copy(out=blk_i[:], in_=blk_f[:]), [b_blkf], [b_blki])
                dump("blkf", blk_f[:], [b_blkf])
                dump("cn", cn[:, 0:5, :].rearrange("p a b -> p (a b)"), [b_cn])
                for t in range(32):
                    pR, bR = psR[t % 2], b_psR[t % 2]
                    for t2 in range(t):
                        pe_mm(pR[:], onesb[:], Mall[:, t2, :], t2 == 0, False, [b_onesb, b_Mall], [bR], sig=False)
                    pe_mm(pR[:], utri[:], Mall[:, t, :], t == 0, True, [b_utri, b_Mall], [bR])
                    tt = tmp[:, t % 2, :]; bt_ = b_tmp[t % 2]
                    V("dve", lambda: nc.vector.tensor_tensor(out=tt, in0=pR[:], in1=cn[:, 4, :], op=ALU.add), [bR, b_cn], [bt_])
                    V("dve", lambda: nc.vector.tensor_tensor(out=cmp_[:, 0, :], in0=tt, in1=oh1all[:, t, :], op=ALU.mult),
                      [bt_, b_oh1], [b_cmp])
                    V("dve", lambda: nc.vector.tensor_reduce(out=dest_f[:, t, 0:1], in_=cmp_[:, 0, :], axis=AX.X, op=ALU.add),
                      [b_cmp], [b_destf])
                    V("dve", lambda: nc.vector.tensor_tensor(out=cmp_[:, 1, :], in0=Mall[:, t, :], in1=oh1all[:, t, :], op=ALU.subtract),
                      [b_Mall, b_oh1], [b_cmp])
                    V("dve", lambda: nc.vector.tensor_tensor(out=cmp_[:, 1, :], in0=cmp_[:, 1, :], in1=tt, op=ALU.mult),
                      [bt_, b_cmp], [b_cmp])
                    V("dve", lambda: nc.vector.tensor_reduce(out=dest_f[:, t, 1:2], in_=cmp_[:, 1, :], axis=AX.X, op=ALU.add),
                      [b_cmp], [b_destf])
                V("dve", lambda: nc.vector.tensor_copy(out=dest_i[:], in_=dest_f[:, :, :].rearrange("p a b -> p (a b)")), [b_destf], [b_desti])
                dump("destf", dest_f[:, :, :].rearrange("p a b -> p (a b)"), [b_destf])
                dump("gates", gates[:, :, :].rearrange("p a b -> p (a b)"), [b_gates])
                hl = [sb(esr, [128, D], BF16, "hl") for _ in range(2)]; b_hl = [Buf(), Buf()]
                for t in range(32):
                    S.dma("sp", hl[t % 2][:], h2_d[t * 128:(t + 1) * 128, :], reads=[b_h2d], writes=[b_hl[t % 2]])
                    for kk in range(2):
                        S.dma_ind(xs_d[:, :], bass.IndirectOffsetOnAxis(ap=dest_i[:, 2 * t + kk:2 * t + kk + 1], axis=0), hl[t % 2][:, :], None,
                                  NROWS - 1, reads=[b_hl[t % 2], b_desti], writes=[b_xsd], group=True, semof=b_hl[t % 2])
                S.barrier()

            if STOP_AFTER != "route":
                with ExitStack() as esm:
                    w1b = [sb(esm, [128, 8, 512], BF16, "w1b") for _ in range(3)]; b_w1 = [Buf() for _ in range(3)]
                    w3b = [sb(esm, [128, 8, 512], BF16, "w3b") for _ in range(3)]; b_w3 = [Buf() for _ in range(3)]
                    w2b = [sb(esm, [128, 4, 1024], BF16, "w2b") for _ in range(3)]; b_w2 = [Buf() for _ in range(3)]
                    xr = [sb(esm, [128, 2, D], BF16, "xr") for _ in range(3)]; b_xr = [Buf() for _ in range(3)]
                    xT = [sb(esm, [128, 8, 256], BF16, "xT") for _ in range(3)]; b_xT = [[Buf(), Buf()] for _ in range(3)]
                    sl = [sb(esm, [128, 256], F32, "sl") for _ in range(2)]; b_sl = [Buf(), Buf()]
                    hm = [sb(esm, [128, 4, 256], BF16, "hm") for _ in range(2)]; b_hm = [Buf(), Buf()]
                    yo = [sb(esm, [128, 2, D], BF16, "yo") for _ in range(2)]; b_yo = [[Buf(), Buf()], [Buf(), Buf()]]
                    psX = [ps(esm, [128, 1024], BF16, "psX") for _ in range(2)]; b_psX = [Buf(), Buf()]
                    psH = [ps(esm, [128, 512], F32, "psH") for _ in range(2)]; b_psH = [Buf(), Buf()]
                    psY = [ps(esm, [128, 512], F32, "psY") for _ in range(4)]; b_psY = [Buf() for _ in range(4)]
                    xcnt = [0]; hcnt = [0]; ycnt = [0]

                    def moe_w(blk):
                        i3 = blk % 3
                        off = bass.IndirectOffsetOnAxis(ap=blk_i[:, blk:blk + 1], axis=0)
                        S.dma_ind(w1b[i3][:, :, :].rearrange("p a b -> p (a b)"), None, w1_d[:, :], off, 0,
                                  reads=[b_blki], writes=[b_w1[i3]])
                        S.dma_ind(w3b[i3][:, :, :].rearrange("p a b -> p (a b)"), None, w3_d[:, :], off, 0,
                                  reads=[b_blki], writes=[b_w3[i3]])
                        S.dma_ind(w2b[i3][:, :, :].rearrange("p a b -> p (a b)"), None, w2_d[:, :], off, 0,
                                  reads=[b_blki], writes=[b_w2[i3]])

                    def moe_x(blk):
                        i2 = blk % 3
                        S.dma("sp", xr[i2][:], xs_d[blk * 256:(blk + 1) * 256, :].rearrange("(a p) d -> p a d", p=128),
                              reads=[b_xsd], writes=[b_xr[i2]])
                        for k in range(8):
                            if k % 4 == 0:
                                pX, bX = psX[(xcnt[0] // 4) % 2], b_psX[(xcnt[0] // 4) % 2]
                            for a in range(2):
                                pe_tr(pX[:, (k % 4) * 256 + a * 128:(k % 4) * 256 + (a + 1) * 128],
                                      xr[i2][:, a, k * 128:(k + 1) * 128], identb[:], [b_xr[i2], b_identb], [bX],
                                      sig=(k % 4 == 3 and a == 1))
                            xcnt[0] += 1
                            if k % 4 == 3:
                                dstx = xT[i2][:, k - 3:k + 1, :].rearrange("p a b -> p (a b)")
                                V("dve", lambda: nc.vector.tensor_copy(out=dstx, in_=pX[:]), [bX], [b_xT[i2][(k // 4) % 2]])

                    def moe_c(blk):
                        i2 = blk % 2
                        i3 = blk % 3
                        ix = blk % 3
                        for c4 in range(4):
                            pH, bH = psH[hcnt[0] % 2], b_psH[hcnt[0] % 2]
                            si = hcnt[0] % 2
                            hcnt[0] += 1
                            for k in range(8):
                                pe_mm(pH[:, 0:256], w1b[i3][:, k, c4 * 128:(c4 + 1) * 128], xT[ix][:, k, :], k == 0, k == 7,
                                      [b_w1[i3], *b_xT[ix]], [bH], sig=False)
                            for k in range(8):
                                pe_mm(pH[:, 256:512], w3b[i3][:, k, c4 * 128:(c4 + 1) * 128], xT[ix][:, k, :], k == 0, k == 7,
                                      [b_w3[i3], *b_xT[ix]], [bH])
                            act(sl[si][:], pH[:, 0:256], AF.Silu, [bH], [b_sl[si]])
                            V("dve", lambda: nc.vector.tensor_tensor(out=hm[i2][:, c4, :], in0=pH[:, 256:512], in1=sl[si][:],
                                                                     op=ALU.mult), [bH, b_sl[si]], [b_hm[i2]])
                        for a in range(2):
                            for hf in range(2):
                                pY, bY = psY[ycnt[0] % 4], b_psY[ycnt[0] % 4]
                                ycnt[0] += 1
                                for k in range(4):
                                    pe_mm(pY[:], hm[i2][:, k, a * 128:(a + 1) * 128], w2b[i3][:, k, hf * 512:(hf + 1) * 512],
                                          k == 0, k == 3, [b_hm[i2], b_w2[i3]], [bY])
                                V("dve", lambda: nc.vector.tensor_copy(out=yo[i2][:, a, hf * 512:(hf + 1) * 512], in_=pY[:]), [bY],
                                  [b_yo[i2][hf]])
                        S.dma("act", ys_d[blk * 256:(blk + 1) * 256, :].rearrange("(a p) d -> p a d", p=128), yo[i2][:],
                              reads=b_yo[i2], writes=[b_ysd], group=True, semof=b_yo[i2][0])

                    moe_w(0)
                    moe_w(1)
                    moe_x(0)
                    moe_x(1)
                    for blk in range(NBLK):
                        if blk + 2 < NBLK:
                            moe_w(blk + 2)
                            moe_x(blk + 2)
                        moe_c(blk)
                    S.barrier()

                with ExitStack() as esf:
                    GA2 = [sb(esf, [128, D], F32, "GA2") for _ in range(2)]; b_GA2 = [Buf(), Buf()]
                    gfb = sb(esf, [128, D], F32, "gfb"); b_gfb = Buf()
                    diag = [sb(esf, [128, 128], F32, "diagf") for _ in range(2)]; b_diag = [Buf(), Buf()]
                    psB = ps(esf, [128, D], F32, "psBf"); b_psB = Buf()
                    S.dma("sp", gfb[:], gfb_d, writes=[b_gfb])
                    for b in range(NB):
                        bcast_row(esf, 5, b, psB, b_psB, diag, b_diag)
                        V("dve", lambda: nc.vector.tensor_copy(out=GA2[b][:], in_=psB[:]), [b_psB], [b_GA2[b]])
                    y0 = [sb(esf, [128, D], BF16, "y0") for _ in range(3)]; b_y0 = [Buf() for _ in range(3)]
                    y1 = [sb(esf, [128, D], BF16, "y1") for _ in range(3)]; b_y1 = [Buf() for _ in range(3)]
                    x1l = [sb(esf, [128, D], F32, "x1l") for _ in range(3)]; b_x1l = [Buf() for _ in range(3)]
                    mo = [sb(esf, [128, D], F32, "mo") for _ in range(2)]; b_mo = [Buf(), Buf()]
                    ot = [sb(esf, [128, D], F32, "ot") for _ in range(2)]; b_ot = [Buf(), Buf()]
                    junkf = sb(esf, [128, D], BF16, "junkf"); b_junkf = Buf()
                    stf = sb(esf, [128, 32, 3], F32, "stf"); b_stf = [Buf() for _ in range(32)]

                    def fin_load(t):
                        i3 = t % 3
                        S.dma_ind(y0[i3][:, :], None, ys_d[:, :], bass.IndirectOffsetOnAxis(ap=dest_i[:, 2 * t:2 * t + 1], axis=0),
                                  0, reads=[b_ysd, b_desti], writes=[b_y0[i3]])
                        S.dma_ind(y1[i3][:, :], None, ys_d[:, :], bass.IndirectOffsetOnAxis(ap=dest_i[:, 2 * t + 1:2 * t + 2], axis=0),
                                  0, reads=[b_ysd, b_desti], writes=[b_y1[i3]])
                        S.dma("sp", x1l[i3][:], x1_d[t * 128:(t + 1) * 128, :], reads=[b_x1d], writes=[b_x1l[i3]])

                    fin_load(0)
                    fin_load(1)
                    for t in range(32):
                        i2 = t % 2
                        i3 = t % 3
                        bb = t // 16
                        act(mo[i2][:], y0[i3][:], AF.Copy, [b_y0[i3], b_gates], [b_mo[i2]], scale=gates[:, t, 0:1])
                        V("dve", lambda: nc.vector.scalar_tensor_tensor(out=mo[i2][:], in0=y1[i3][:], scalar=gates[:, t, 1:2],
                                                                        in1=mo[i2][:], op0=ALU.mult, op1=ALU.add),
                          [b_y1[i3], b_gates, b_mo[i2]], [b_mo[i2]])
                        if t == 0:
                            dump("moe0", mo[i2][:], [b_mo[i2]])
                        V("pool", lambda: nc.gpsimd.tensor_tensor(out=mo[i2][:], in0=mo[i2][:], in1=GA2[bb][:], op=ALU.mult),
                          [b_mo[i2], b_GA2[bb]], [b_mo[i2]])
                        V("dve", lambda: nc.vector.tensor_tensor(out=mo[i2][:], in0=mo[i2][:], in1=x1l[i3][:], op=ALU.add),
                          [b_mo[i2], b_x1l[i3]], [b_mo[i2]])
                        act(junkf[:], mo[i2][:], AF.Square, [b_mo[i2]], [b_junkf, b_stf[t]], accum_out=stf[:, t, 0:1])
                        act(stf[:, t, 1:2], stf[:, t, 0:1], AF.Sqrt, [b_stf[t], b_eps], [b_stf[t]], scale=1.0 / D, bias=epst[:, 0:1])
                        V("dve", lambda: nc.vector.reciprocal(out=stf[:, t, 2:3], in_=stf[:, t, 1:2]), [b_stf[t]], [b_stf[t]])
                        V("dve", lambda: nc.vector.scalar_tensor_tensor(out=ot[i2][:], in0=mo[i2][:], scalar=stf[:, t, 2:3],
                                                                        in1=gfb[:], op0=ALU.mult, op1=ALU.mult),
                          [b_mo[i2], b_stf[t], b_gfb], [b_ot[i2]])
                        if t + 2 < 32:
                            fin_load(t + 2)
                        S.dma("sp", out_d[bb, (t % 16) * 128:(t % 16 + 1) * 128, :], ot[i2][:], reads=[b_ot[i2]], writes=[b_outd],
                              group=True, semof=b_ot[i2])
        S.barrier()
        build_program.stats = dict(nops=dict(S.nops), nwaits=S.nwaits, nsem=S.nsem)
    return nc


def _fm(v):
    return np.ascontiguousarray(np.asarray(v, np.float32).reshape(-1, 128).T)


def _kp(w):
    K, N = w.shape
    return np.ascontiguousarray(w.reshape(K // 128, 128, N).transpose(1, 0, 2))


def _swap_cols():
    idx = np.arange(64)
    half = idx // 32
    within = idx % 32
    sw = np.where(within < 16, within + 16, within - 16)
    return half * 32 + sw


def _host_consts():
    nf = 16
    inv_freq = (10000.0 ** (-np.arange(nf, dtype=np.float32) / nf)).astype(np.float32)
    t = np.arange(L)
    row = (t // 64).astype(np.float32)
    col = (t % 64).astype(np.float32)
    cos = np.zeros((128, L), np.float32)
    sin = np.zeros((128, L), np.float32)
    for p in range(128):
        d = p % 64
        pos = row if d < 32 else col
        ang = (pos * inv_freq[(d % 32) % 16]).astype(np.float32)
        sign = -1.0 if (d % 32) < 16 else 1.0
        cos[p] = np.cos(ang)
        sin[p] = sign * np.sin(ang)
    cossin = np.concatenate([cos, sin], axis=1)
    ident = np.eye(128, dtype=np.float32)
    iota = np.arange(128, dtype=np.float32).reshape(128, 1)
    utri = np.triu(np.ones((128, 128), np.float32), k=1)
    return cossin, ident, iota, utri


def _bias_table(rpb):
    cq = np.arange(64)
    c_start = np.clip(cq - 8, 0, 48)
    band = (cq[None, :] >= c_start[:, None]) & (cq[None, :] < c_start[:, None] + 16)
    dc = np.clip(cq[None, :] - cq[:, None], -15, 15) + 15
    tab = np.full((64, 8, NTB, 64), -1e30, np.float32)
    for h in range(8):
        for dr in range(15):
            vals = rpb[h, dr][dc]
            tab[:, h, 1 + dr, :] = np.where(band, vals, np.float32(-1e30))
        tab[:, h, 18, :] = tab[:, h, 1 + 3, :]
        tab[:, h, 19, :] = tab[:, h, 1 + 10, :]
    return tab.reshape(64, 8 * NTB * 64)


def _prepare(inputs):
    f = lambda k: np.asarray(inputs[k], np.float32)
    w_in = f("w_in")[0]
    K_OFF, V_OFF, LX_OFF, Q_OFF, LG_OFF, GA_OFF, GB_OFF = 0, 512, 1024, 2048, 2560, 3584, 4608
    sw = _swap_cols()
    wqkv = []
    for hp in range(4):
        cols = []
        for base in (Q_OFF, K_OFF):
            plain = np.concatenate([base + (2 * hp + e) * 64 + np.arange(64) for e in range(2)])
            swp = np.concatenate([base + (2 * hp + e) * 64 + sw for e in range(2)])
            cols += [plain, swp]
        cols.append(V_OFF + hp * 128 + np.arange(128))
        wqkv.append(_kp(w_in[:, np.concatenate(cols)]).reshape(128, 8 * 640))
    wqkv = np.stack(wqkv)
    wlxlg = np.stack([_kp(w_in[:, np.concatenate([LX_OFF + n * 128 + np.arange(128), LG_OFF + n * 128 + np.arange(128)])]
                          ).reshape(128, 8 * 256) for n in range(8)])
    wgagb = np.stack([_kp(w_in[:, np.concatenate([GA_OFF + n * 128 + np.arange(128), GB_OFF + n * 128 + np.arange(128)])]
                          ).reshape(128, 8 * 256) for n in range(8)])
    wua = _kp(f("w_up_attn")[0])
    wul = _kp(f("w_up_lru")[0])
    wup = np.stack([np.concatenate([wua[:, :, n * 128:(n + 1) * 128], wul[:, :, n * 128:(n + 1) * 128]], axis=1
                                   ).reshape(128, 12 * 128) for n in range(8)])
    wout = _kp(f("w_out")[0]).reshape(128, 8 * D)
    wa = f("lru_wa")[0]
    wx = f("lru_wx")[0]
    lruw = np.stack([wa[0], wa[1], wx[0], wx[1]])
    lruw = np.ascontiguousarray(lruw.transpose(2, 0, 1, 3)).reshape(128, 4 * 8 * 128)
    vecs = np.concatenate([
        _fm(f("g_mix")[0]), _fm(f("g_ffn")[0]),
        np.concatenate([_fm(f("conv_w")[0][j]) for j in range(4)], axis=1),
        _fm(f("conv_b")[0]),
        np.concatenate([_fm(f("lru_ba")[0][d_]) for d_ in range(2)], axis=1),
        np.concatenate([_fm(f("lru_bx")[0][d_]) for d_ in range(2)], axis=1),
        np.concatenate([_fm(f("lru_lambda")[0][d_]) for d_ in range(2)], axis=1),
        _fm(f("b_mod")[0]),
    ], axis=1)
    assert vecs.shape == (128, NV)
    wmod = _kp(f("w_mod")[0])
    wr = _kp(np.concatenate([f("router_group_w")[0], f("router_expert_w")[0]], axis=1)).reshape(128, 8 * 36)
    brb = np.ascontiguousarray(np.broadcast_to(
        np.concatenate([f("router_group_b")[0], f("router_expert_b")[0]])[None, :], (128, 36)))
    gfb = np.ascontiguousarray(np.broadcast_to(f("g_final")[None, :], (128, D)))
    gffnb = np.ascontiguousarray(np.broadcast_to(f("g_ffn")[0][None, :], (128, D)))
    w1 = f("expert_w_gate")[0]
    w3 = f("expert_w_up")[0]
    w2 = f("expert_w_down")[0]
    w1h = np.ascontiguousarray(w1.reshape(32, 8, 128, 512).transpose(0, 2, 1, 3)).reshape(32 * 128, 8 * 512)
    w3h = np.ascontiguousarray(w3.reshape(32, 8, 128, 512).transpose(0, 2, 1, 3)).reshape(32 * 128, 8 * 512)
    w2h = np.ascontiguousarray(w2.reshape(32, 4, 128, 1024).transpose(0, 2, 1, 3)).reshape(32 * 128, 4 * 1024)
    cossin, ident, iota, utri = _host_consts()
    btab = _bias_table(f("rpb")[0])
    shared = dict(wmod=wmod, vecs=vecs, wqkv=wqkv, wlxlg=wlxlg, wgagb=wgagb, wup=wup, wout=wout, lruw=lruw,
                  cossin=cossin, btab=btab, wr=wr, brb=brb, gfb=gfb, gffnb=gffnb, w1h=w1h, w3h=w3h, w2h=w2h,
                  ident=ident, iota=iota, utri=utri)
    x = f("x")
    ctx = f("ctx")
    c = f("c")
    c_ctx = f("c_ctx")
    in_maps = []
    for core in range(NCORES):
        b0 = core * NB
        cs = np.stack([c[b0], c[b0 + 1], c_ctx], axis=-1)
        cs = np.ascontiguousarray(cs.reshape(8, 128, 3).transpose(1, 0, 2)).reshape(128, 24)
        m = dict(shared)
        m["xin"] = np.ascontiguousarray(x[b0:b0 + NB])
        m["ctxin"] = np.ascontiguousarray(ctx[b0:b0 + NB])
        m["cs"] = cs
        in_maps.append(m)
    return in_maps


def kernel(**inputs):
    in_maps = _prepare(inputs)
    nc = build_program()
    res = run_bass_kernel_spmd(nc, in_maps, core_ids=list(range(NCORES)))
    out = np.concatenate([np.asarray(r["out"], np.float32) for r in res.results], axis=0)
    return out
```

```python
import numpy as np
import concourse.bass as bass
import concourse.mybir as mybir
from concourse.bass_utils import run_bass_kernel_spmd
from contextlib import ExitStack

F32 = mybir.dt.float32
BF16 = mybir.dt.bfloat16
I32 = mybir.dt.int32
AF = mybir.ActivationFunctionType
ALU = mybir.AluOpType
AX = mybir.AxisListType

D = 1024
L = 2048
C = 256
NB = 2
NCORES = 8
LC = L + C
NTOK = NB * L
NBLK = 64
NROWS = NBLK * 256
EPS = 1e-6

V_GMIX, V_GFFN, V_CONVW, V_CONVB, V_BA, V_BX, V_LAM, V_BMOD, NV = 0, 8, 16, 48, 56, 72, 88, 104, 152
NTB = 21

DEBUG = {}
import os as _os
ATT_NHP = int(_os.environ.get("ATT_NHP", "4"))
ATT_NUNITS = int(_os.environ.get("ATT_NUNITS", "8"))
ATT_NROWS = int(_os.environ.get("ATT_NROWS", "8"))
ATT_NORM = int(_os.environ.get("ATT_NORM", "1"))
ATT_PARTS = int(_os.environ.get("ATT_PARTS", "31"))
STOP_AFTER = None


class Buf:
    __slots__ = ("name", "w", "wx", "r", "dsem", "dcnt", "grp")

    def __init__(self, name=""):
        self.name = name
        self.w = None
        self.wx = {}
        self.r = {}
        self.dsem = {}
        self.dcnt = {}
        self.grp = False


class Sync:
    ROLL = 30000

    def __init__(self, nc, es):
        self.nc = nc
        self.es = es
        self.eng = {"pe": nc.tensor, "act": nc.scalar, "dve": nc.vector, "pool": nc.gpsimd, "sp": nc.sync}
        self.sem = {}
        self.cnt = {}
        self.waited = {k: {} for k in self.eng}
        self.nsem = 0
        self.pe_sems = set()
        for k in self.eng:
            self._newsem(k)
        self.pend = []
        self.dbufs = []
        self.nops = {k: 0 for k in self.eng}
        self.nwaits = 0

    def _alloc(self, name):
        self.nsem += 1
        return self.es.enter_context(self.nc.semaphore(f"{name}{self.nsem}"))

    def _newsem(self, k):
        self.sem[k] = self._alloc("e" + k)
        self.cnt[k] = 0
        if k == "pe":
            self.pe_sems.add(id(self.sem[k]))

    def _wait(self, e, ev):
        semh, val = ev
        assert val is not None, "dependency on an unsignalled PE op"
        key = id(semh)
        if self.waited[e].get(key, 0) < val:
            self.eng[e].wait_ge(semh, val)
            self.waited[e][key] = val
            self.nwaits += 1

    def _dep1(self, e, ev, acc):
        if e == "pe" and (ev[1] is None or id(ev[0]) in self.pe_sems):
            return
        assert ev[1] is not None, "dependency on an unsignalled PE op"
        k = id(ev[0])
        if k not in acc or acc[k][1] < ev[1]:
            acc[k] = ev

    def _deps(self, e, reads, writes, group=False):
        acc = {}
        for b in reads:
            if b.w is not None:
                self._dep1(e, b.w, acc)
            for ev in b.wx.values():
                self._dep1(e, ev, acc)
        for b in writes:
            if not (group and b.grp):
                if b.w is not None:
                    self._dep1(e, b.w, acc)
                for ev in b.wx.values():
                    self._dep1(e, ev, acc)
            for ev in b.r.values():
                self._dep1(e, ev, acc)
        for ev in acc.values():
            self._wait(e, ev)

    def _mark(self, ev, reads, writes, key, group=False):
        for b in reads:
            b.r[key] = ev
        for b in writes:
            if group and b.grp:
                b.wx[key] = ev
            elif group:
                b.w = None
                b.wx = {key: ev}
            else:
                b.w = ev
                b.wx = {}
            b.grp = group
            b.r = {}

    def op(self, e, fn, reads=(), writes=(), sig=True):
        self._deps(e, reads, writes)
        ins = fn()
        self.nops[e] += 1
        if sig:
            if self.cnt[e] >= self.ROLL:
                self._newsem(e)
            self.cnt[e] += 1
            ins.then_inc(self.sem[e], 1)
            ev = [self.sem[e], self.cnt[e]]
            if e == "pe":
                for p in self.pend:
                    p[0] = self.sem[e]
                    p[1] = self.cnt[e]
                self.pend = []
        else:
            assert e == "pe"
            ev = [self.sem[e], None]
            self.pend.append(ev)
        self._mark(ev, reads, writes, e)
        return ins

    def _dma_common(self, q, issue, reads, writes, group, semof):
        d = semof if semof is not None else writes[0]
        c = "sw" if q == "pool" else "hw"
        if c not in d.dsem:
            d.dsem[c] = self._alloc("d")
            d.dcnt[c] = 0
            self.dbufs.append((d, c))
        self._deps(q, reads, writes, group=group)
        ins = issue()
        self.nops[q] += 1
        d.dcnt[c] += 16
        ins.then_inc(d.dsem[c], 16)
        ev = [d.dsem[c], d.dcnt[c]]
        self._mark(ev, reads, writes, id(d.dsem[c]), group=group)
        return ins

    def dma(self, q, out, in_, reads=(), writes=(), group=False, semof=None):
        return self._dma_common(q, lambda: self.eng[q].dma_start(out=out, in_=in_), reads, writes, group, semof)

    def dma_ind(self, out, out_off, in_, in_off, bound, reads=(), writes=(), group=False, semof=None):
        def issue():
            return self.nc.gpsimd.indirect_dma_start(out=out, out_offset=out_off, in_=in_, in_offset=in_off)
        return self._dma_common("pool", issue, reads, writes, group, semof)

    def barrier(self):
        assert not self.pend
        evs = [[self.sem[k], self.cnt[k]] for k in self.eng if k != "sp" and self.cnt[k] > 0]
        evs += [[b.dsem[c], b.dcnt[c]] for (b, c) in self.dbufs if b.dcnt[c] > 0]
        for ev in evs:
            self._wait("sp", ev)
        if self.cnt["sp"] >= self.ROLL:
            self._newsem("sp")
        self.cnt["sp"] += 1
        self.nc.sync.nop().then_inc(self.sem["sp"], 1)
        ev = [self.sem["sp"], self.cnt["sp"]]
        for k in self.eng:
            if k != "sp":
                self._wait(k, ev)


def build_program():
    nc = bass.Bass("TRN2", target_bir_lowering=False)

    def din(name, shape, dt=F32):
        return nc.dram_tensor(name, list(shape), dt, kind="ExternalInput").ap()

    xin = din("xin", [NB, L, D])
    ctxin = din("ctxin", [NB, C, D])
    cs_d = din("cs", [128, 24])
    wmod_d = din("wmod", [128, 8, 6 * D])
    vecs_d = din("vecs", [128, NV])
    wqkv_d = din("wqkv", [4, 128, 8 * 640])
    wlxlg_d = din("wlxlg", [8, 128, 8 * 256])
    wgagb_d = din("wgagb", [8, 128, 8 * 256])
    wup_d = din("wup", [8, 128, 12 * 128])
    wout_d = din("wout", [128, 8 * D])
    lruw_d = din("lruw", [128, 4 * 8 * 128])
    cossin_d = din("cossin", [128, 2 * L])
    btab_d = din("btab", [64, 8 * NTB * 64])
    wr_d = din("wr", [128, 8 * 36])
    brb_d = din("brb", [128, 36])
    gfb_d = din("gfb", [128, D])
    gffnb_d = din("gffnb", [128, D])
    w1_d = din("w1h", [32 * 128, 8 * 512])
    w3_d = din("w3h", [32 * 128, 8 * 512])
    w2_d = din("w2h", [32 * 128, 4 * 1024])
    ident_d = din("ident", [128, 128])
    iota_d = din("iota", [128, 1])
    utri_d = din("utri", [128, 128])
    out_d = nc.dram_tensor("out", [NB, L, D], F32, kind="ExternalOutput").ap()
    x1_d = nc.dram_tensor("x1s", [NTOK, D], F32, kind="Internal").ap()
    h2_d = nc.dram_tensor("h2s", [NTOK, D], BF16, kind="Internal").ap()
    xs_d = nc.dram_tensor("xss", [NROWS, D], BF16, kind="Internal").ap()
    ys_d = nc.dram_tensor("yss", [NROWS, D], BF16, kind="Internal").ap()
    dbg_d = {}
    for name, (shape, dt) in DEBUG.items():
        dbg_d[name] = nc.dram_tensor("dbg_" + name, list(shape), dt, kind="ExternalOutput").ap()

    with ExitStack() as es:
        S = Sync(nc, es)
        uid = [0]

        def sb(es_, shape, dt, name="t"):
            uid[0] += 1
            return es_.enter_context(nc.sbuf_tensor(f"{name}{uid[0]}", list(shape), dt))

        def ps(es_, shape, dt, name="p"):
            uid[0] += 1
            return es_.enter_context(nc.psum_tensor(f"{name}{uid[0]}", list(shape), dt))

        def pe_mm(out, lhsT, rhs, start, stop, reads, writes, sig=None):
            if sig is None:
                sig = stop
            return S.op("pe", lambda: nc.tensor.matmul(out, lhsT, rhs, start=start, stop=stop),
                        reads, writes, sig)

        def pe_tr(out, in_, ident, reads, writes, sig):
            return S.op("pe", lambda: nc.tensor.transpose(out, in_, ident), reads, writes, sig)

        def act(out, in_, func, reads, writes, **kw):
            return S.op("act", lambda: nc.scalar.activation(out=out, in_=in_, func=func, **kw), reads, writes)

        def V(e, fn, reads, writes):
            return S.op(e, fn, reads, writes)

        dbg_buf = Buf("dbg")

        def dump(name, ap, reads):
            if name in dbg_d:
                S.dma("sp", dbg_d[name], ap, reads=reads, writes=[dbg_buf], group=True, semof=Buf("dump_" + name))

        identf = sb(es, [128, 128], F32, "identf"); b_identf = Buf()
        identb = sb(es, [128, 128], BF16, "identb"); b_identb = Buf()
        onesf = sb(es, [128, 128], F32, "onesf"); b_onesf = Buf()
        onesb = sb(es, [128, 128], BF16, "onesb"); b_onesb = Buf()
        utri = sb(es, [128, 128], BF16, "utri"); b_utri = Buf()
        iota = sb(es, [128, 1], F32, "iota"); b_iota = Buf()
        epst = sb(es, [128, 1], F32, "eps"); b_eps = Buf()
        vecs = sb(es, [128, NV], F32, "vecs"); b_vecs = Buf()
        modfm = sb(es, [128, 48, 3], F32, "modfm"); b_modfm = Buf()
        A1 = sb(es, [128, 8, 3], F32, "A1"); b_A1 = Buf()
        lrup = sb(es, [128, 4, 16], F32, "lrup"); b_lrup = Buf()
        S.dma("sp", identf[:], ident_d, writes=[b_identf])
        S.dma("pool", identb[:], ident_d, writes=[b_identb])
        S.dma("pool", utri[:], utri_d, writes=[b_utri])
        S.dma("sp", iota[:], iota_d, writes=[b_iota])
        S.dma("sp", vecs[:], vecs_d, writes=[b_vecs])
        V("dve", lambda: nc.vector.memset(onesf[:], 1.0), [], [b_onesf])
        V("dve", lambda: nc.vector.memset(onesb[:], 1.0), [], [b_onesb])
        V("dve", lambda: nc.vector.memset(epst[:], EPS), [], [b_eps])

        with ExitStack() as es0:
            csb = sb(es0, [128, 24], F32, "cs"); b_cs = Buf()
            scs = sb(es0, [128, 24], BF16, "scs"); b_scs = Buf()
            wm = [sb(es0, [128, 8, 512], BF16, "wm") for _ in range(3)]
            b_wm = [Buf(), Buf(), Buf()]
            psmod = ps(es0, [128, 144], F32, "psmod"); b_psmod = Buf()
            S.dma("sp", csb[:], cs_d, writes=[b_cs])
            act(scs[:], csb[:], AF.Silu, [b_cs], [b_scs])
            for cb in range(12):
                w = wm[cb % 3]; bw = b_wm[cb % 3]
                S.dma("pool", w[:], wmod_d[:, :, cb * 512:(cb + 1) * 512], writes=[bw])
                for cc in range(4):
                    col = cb * 4 + cc
                    for k in range(8):
                        pe_mm(psmod[:, col * 3:(col + 1) * 3], w[:, k, cc * 128:(cc + 1) * 128],
                              scs[:, k * 3:(k + 1) * 3], k == 0, k == 7, [bw, b_scs], [b_psmod],
                              sig=(k == 7 and cc == 3))
            V("dve", lambda: nc.vector.tensor_tensor(
                out=modfm[:], in0=psmod[:, :].rearrange("p (a b) -> p a b", b=3),
                in1=vecs[:, V_BMOD:V_BMOD + 48].unsqueeze(2).to_broadcast([128, 48, 3]), op=ALU.add),
              [b_psmod, b_vecs], [b_modfm])
            V("dve", lambda: nc.vector.scalar_tensor_tensor(
                out=A1[:], in0=modfm[:, 8:16, :], scalar=1.0,
                in1=vecs[:, V_GMIX:V_GMIX + 8].unsqueeze(2).to_broadcast([128, 8, 3]),
                op0=ALU.add, op1=ALU.mult), [b_modfm, b_vecs], [b_A1])
            act(lrup[:, 2, :], vecs[:, V_LAM:V_LAM + 16], AF.Exp, [b_vecs], [b_lrup], scale=-1.0)
            act(lrup[:, 3, :], lrup[:, 2, :], AF.Ln, [b_lrup], [b_lrup], bias=1.0)
            V("dve", lambda: nc.vector.tensor_scalar(out=lrup[:, 0, :], in0=lrup[:, 3, :], scalar1=-8.0, scalar2=None,
                                                     op0=ALU.mult), [b_lrup], [b_lrup])
            V("dve", lambda: nc.vector.tensor_scalar(out=lrup[:, 1, :], in0=lrup[:, 3, :], scalar1=-16.0, scalar2=None,
                                                     op0=ALU.mult), [b_lrup], [b_lrup])
            dump("modfm", modfm[:, :, :].rearrange("p a b -> p (a b)"), [b_modfm])
            S.barrier()

        def bcast_row(es_, v, j, psb, b_psb, diag, b_diag):
            for k in range(8):
                V("dve", lambda: nc.vector.tensor_scalar(out=diag[k % 2][:], in0=identf[:],
                                                         scalar1=modfm[:, v * 8 + k, j:j + 1], scalar2=None,
                                                         op0=ALU.mult), [b_identf, b_modfm], [b_diag[k % 2]])
                pe_mm(psb[:, k * 128:(k + 1) * 128], onesf[:], diag[k % 2][:], True, True,
                      [b_onesf, b_diag[k % 2]], [b_psb], sig=True)

        Mall = sb(es, [128, 32, 32], BF16, "Mall"); b_Mall = Buf()
        oh1all = sb(es, [128, 32, 32], BF16, "oh1"); b_oh1 = Buf()
        gates = sb(es, [128, 32, 2], F32, "gates"); b_gates = Buf()
        dest_f = sb(es, [128, 32, 2], F32, "destf"); b_destf = Buf()
        dest_i = sb(es, [128, 64], I32, "desti"); b_desti = Buf()
        b_x1d = Buf("x1d"); b_h2d = Buf("h2d"); b_xsd = Buf("xsd"); b_ysd = Buf("ysd"); b_outd = Buf("outd")
        zt = sb(es, [128, 2, D], BF16, "zt"); b_zt = Buf()
        V("dve", lambda: nc.vector.memset(zt[:], 0.0), [], [b_zt])
        for blk in range(NBLK):
            S.dma("sp", xs_d[blk * 256:(blk + 1) * 256, :].rearrange("(a p) d -> p a d", p=128), zt[:],
                  reads=[b_zt], writes=[b_xsd], group=True, semof=b_zt)

        for b in range(NB):
            with ExitStack() as esb:
                hT = sb(esb, [128, 8, LC], BF16, "hT")
                b_hT = [[Buf(f"hT{i}a"), Buf(f"hT{i}b")] for i in range(5)]
                oatt = sb(esb, [128, 4, L], BF16, "oatt")
                b_oatt = [[Buf() for _ in range(4)] for _ in range(4)]

                with ExitStack() as es1:
                    xt = [sb(es1, [128, D], F32, "xt") for _ in range(3)]
                    b_xt = [Buf() for _ in range(3)]
                    junk = sb(es1, [128, D], BF16, "junk"); b_junk = Buf()
                    xn = [sb(es1, [128, D], BF16, "xn") for _ in range(8)]
                    b_xn = [Buf() for _ in range(8)]
                    st = sb(es1, [128, 18, 3], F32, "st"); b_st = [Buf() for _ in range(18)]
                    pst = [ps(es1, [128, 512], BF16, "pst") for _ in range(4)]
                    b_pst = [Buf() for _ in range(4)]
                    ti = 0
                    for grp in range(5):
                        ntile = 4 if grp < 4 else 2
                        jmod = b if grp < 4 else 2
                        for i in range(ntile):
                            t = grp * 4 + i
                            xb_, bx_ = xt[ti % 3], b_xt[ti % 3]
                            src = xin[b, t * 128:(t + 1) * 128, :] if grp < 4 else ctxin[b, i * 128:(i + 1) * 128, :]
                            S.dma("sp", xb_[:], src, writes=[bx_])
                            act(junk[:], xb_[:], AF.Square, [bx_], [b_junk, b_st[t]], accum_out=st[:, t, 0:1])
                            act(st[:, t, 1:2], st[:, t, 0:1], AF.Sqrt, [b_st[t], b_eps], [b_st[t]],
                                scale=1.0 / D, bias=epst[:, 0:1])
                            V("dve", lambda: nc.vector.reciprocal(out=st[:, t, 2:3], in_=st[:, t, 1:2]),
                              [b_st[t]], [b_st[t]])
                            xi = (grp % 2) * 4 + i
                            act(xn[xi][:], xb_[:], AF.Copy, [bx_, b_st[t]], [b_xn[xi]], scale=st[:, t, 2:3])
                            ti += 1
                        for k in range(8):
                            pp, bp = pst[k % 4], b_pst[k % 4]
                            for i in range(ntile):
                                xi = (grp % 2) * 4 + i
                                pe_tr(pp[:, i * 128:(i + 1) * 128], xn[xi][:, k * 128:(k + 1) * 128], identb[:],
                                      [b_xn[xi], b_identb], [bp], sig=(i == ntile - 1))
                            n = ntile * 128
                            dst = hT[:, k, grp * 512:grp * 512 + n]
                            if k % 2 == 0:
                                V("dve", lambda: nc.vector.tensor_scalar(
                                    out=dst, in0=pp[:, 0:n], scalar1=A1[:, k, jmod:jmod + 1],
                                    scalar2=modfm[:, k, jmod:jmod + 1], op0=ALU.mult, op1=ALU.add),
                                  [bp, b_A1, b_modfm], [b_hT[grp][0]])
                            else:
                                act(dst, pp[:, 0:n], AF.Identity, [bp, b_A1, b_modfm], [b_hT[grp][1]],
                                    scale=A1[:, k, jmod:jmod + 1], bias=modfm[:, k, jmod:jmod + 1])
                    if b == 0:
                        dump("hT", hT[:, :, :].rearrange("p a b -> p (a b)"), [x for l_ in b_hT for x in l_])
                    S.barrier()
                if STOP_AFTER == "s1":
                    break

                with ExitStack() as es2:
                    cs_t = sb(es2, [128, 2 * L], F32, "cossin"); b_cst = Buf()
                    btab = sb(es2, [128, 8, NTB * 64], BF16, "btab"); b_btab = Buf()
                    S.dma("sp", cs_t[:], cossin_d, writes=[b_cst])
                    S.dma("pool", btab[0:64, :, :].rearrange("p a b -> p (a b)"), btab_d, writes=[b_btab], group=True)
                    S.dma("pool", btab[64:128, :, :].rearrange("p a b -> p (a b)"), btab_d, writes=[b_btab], group=True)
                    wq = [sb(es2, [128, 8, 640], BF16, "wq") for _ in range(1)]; b_wq = [Buf()]
                    Qr = [sb(es2, [128, L], BF16, "Qr") for _ in range(1)]
                    Qp = [sb(es2, [128, L], BF16, "Qp") for _ in range(1)]
                    Kr = [sb(es2, [128, L], BF16, "Kr") for _ in range(1)]
                    Kc = [sb(es2, [128, C], BF16, "Kc") for _ in range(1)]
                    Vx = [sb(es2, [128, 18, 192], BF16, "Vx") for _ in range(1)]
                    b_Q = [[Buf() for _ in range(4)] for _ in range(1)]
                    b_Qp = [[Buf() for _ in range(4)] for _ in range(1)]
                    b_K = [Buf()]
                    b_Kc = [Buf()]
                    b_V = [[Buf() for _ in range(5)]]
                    t1 = [sb(es2, [128, 512], F32, "t1") for _ in range(2)]; b_t1 = [Buf(), Buf()]
                    t2 = [sb(es2, [128, 512], F32, "t2") for _ in range(2)]; b_t2 = [Buf(), Buf()]
                    PTc = [sb(es2, [128, 512], BF16, "PTc") for _ in range(2)]; b_PTc = [Buf(), Buf()]
                    PT = [sb(es2, [128, 320], BF16, "PT") for _ in range(3)]; b_PT = [Buf() for _ in range(3)]
                    rc = [sb(es2, [128, 512], F32, "rc") for _ in range(2)]; b_rc = [Buf(), Buf()]
                    psP = [ps(es2, [128, 512], F32, "psP") for _ in range(2)]; b_psP = [Buf(), Buf()]
                    psSc = [ps(es2, [128, 512], F32, "psSc") for _ in range(2)]; b_psSc = [Buf(), Buf()]
                    psS = [ps(es2, [128, 512], F32, "psS") for _ in range(2)]; b_psS = [Buf(), Buf()]
                    psO = [ps(es2, [128, 512], F32, "psO") for _ in range(2)]; b_psO = [Buf(), Buf()]
                    V("dve", lambda: nc.vector.memset(Vx[0][:, :, 64:128], 1.0), [], b_V[0])
                    pcnt = [0]

                    def nextP():
                        i = pcnt[0] % 2
                        pcnt[0] += 1
                        return psP[i], b_psP[i]

                    cnt_t = [0]
                    for hp in range(ATT_NHP):
                        par = 0
                        w = wq[par]; bw = b_wq[par]
                        S.dma("pool", w[:, :, :].rearrange("p a b -> p (a b)"), wqkv_d[hp], writes=[bw])
                        for blk in range(4 if ATT_PARTS & 1 else 0):
                            tok = slice(blk * 512, (blk + 1) * 512)
                            for which in range(2):
                                c0 = which * 256
                                pa, bpa = nextP()
                                for k in range(8):
                                    pe_mm(pa[:], w[:, k, c0:c0 + 128], hT[:, k, tok], k == 0, k == 7,
                                          [bw, *b_hT[blk]], [bpa])
                                pb, bpb = nextP()
                                for k in range(8):
                                    pe_mm(pb[:], w[:, k, c0 + 128:c0 + 256], hT[:, k, tok], k == 0, k == 7,
                                          [bw, *b_hT[blk]], [bpb])
                                ii = cnt_t[0] % 2
                                cnt_t[0] += 1
                                sc_ = 0.125 if which == 0 else 1.0
                                dstb = b_Q[par][blk] if which == 0 else b_K[par]
                                dst = (Qr if which == 0 else Kr)[par][:, tok]
                                V("dve", lambda: nc.vector.scalar_tensor_tensor(
                                    out=t1[ii][:], in0=pa[:], scalar=sc_, in1=cs_t[:, tok], op0=ALU.mult, op1=ALU.mult),
                                  [bpa, b_cst], [b_t1[ii]])
                                if which == 0 and (ATT_PARTS & 16):
                                    V("dve", lambda: nc.vector.tensor_scalar(out=Qp[par][:, tok], in0=pa[:], scalar1=0.125, scalar2=None,
                                                                             op0=ALU.mult), [bpa], [b_Qp[par][blk]])
                                V("dve", lambda: nc.vector.scalar_tensor_tensor(
                                    out=t2[ii][:], in0=pb[:], scalar=sc_, in1=cs_t[:, L + blk * 512:L + (blk + 1) * 512],
                                    op0=ALU.mult, op1=ALU.mult), [bpb, b_cst], [b_t2[ii]])
                                V("pool" if ATT_PARTS & 8 else "dve", lambda: (nc.gpsimd if ATT_PARTS & 8 else nc.vector).tensor_tensor(out=dst, in0=t1[ii][:], in1=t2[ii][:], op=ALU.add),
                                  [b_t1[ii], b_t2[ii]], [dstb])
                        if ATT_PARTS & 2:
                            pa, bpa = nextP()
                            for k in range(8):
                                pe_mm(pa[:, 0:C], w[:, k, 256:384], hT[:, k, L:LC], k == 0, k == 7, [bw, *b_hT[4]], [bpa])
                            act(Kc[par][:], pa[:, 0:C], AF.Copy, [bpa], [b_Kc[par]])
                        for g4 in range(5 if ATT_PARTS & 4 else 0):
                            nch = 4 if g4 < 4 else 2
                            pa, bpa = nextP()
                            for i in range(nch):
                                ch = g4 * 4 + i
                                for k in range(8):
                                    pe_mm(pa[:, i * 128:(i + 1) * 128], hT[:, k, ch * 128:(ch + 1) * 128],
                                          w[:, k, 512:640], k == 0, k == 7, [bw, *b_hT[g4]], [bpa],
                                          sig=(k == 7 and i == nch - 1))
                            src = pa[:, 0:nch * 128].rearrange("p (c a d) -> p c a d", a=2, d=64)
                            dstv = Vx[par][:, g4 * 4:g4 * 4 + nch, :].rearrange("p c (a d) -> p c a d", d=64)[:, :, 0::2, :]
                            if g4 % 2 == 0:
                                act(dstv, src, AF.Copy, [bpa], [b_V[par][g4]])
                            else:
                                V("dve", lambda: nc.vector.tensor_copy(out=dstv, in_=src), [bpa], [b_V[par][g4]])

                        if b == 0 and hp == 0:
                            dump("Qp", Qp[0][:], b_Qp[0])
                            dump("Qr", Qr[0][:], b_Q[0])
                            dump("Kr", Kr[0][:], b_K)
                            dump("Kc", Kc[0][:], b_Kc)
                            dump("Vx", Vx[0][:, :, :].rearrange("p a b -> p (a b)"), b_V[0])
                        units = []
                        for e in range(2):
                            for qb in range(4):
                                units.append((e, qb))
                        ucnt = [0]
                        for (e, qb) in units[int(_os.environ.get("ATT_USTART", "0")):][:ATT_NUNITS]:
                            h = hp * 2 + e
                            pr = slice(64 * e, 64 * e + 64)
                            qs = slice(qb * 512, (qb + 1) * 512)
                            vcols = slice(0, 128) if e == 0 else slice(64, 192)
                            oi = ucnt[0] % 2
                            ucnt[0] += 1
                            pO, bO = psO[oi], b_psO[oi]
                            for c in range(2):
                                pS, bS = psSc[c], b_psSc[c]
                                pe_mm(pS[:], Kc[par][pr, c * 128:(c + 1) * 128], Qp[par][pr, qs], True, True,
                                      [b_Kc[par], b_Qp[par][qb]], [bS])
                                act(PTc[c][:], pS[:], AF.Exp, [bS], [b_PTc[c]])
                            if b == 0 and hp == 0 and e == 0 and qb == 0:
                                dump("PTc", PTc[0][:], [b_PTc[0]])
                            for c in range(2):
                                pe_mm(pO[:], Vx[par][:, 16 + c, vcols], PTc[c][:], c == 0, False,
                                      [b_V[par][4], b_PTc[c]], [bO], sig=(c == 1))
                            rows = list(range(qb * 8, qb * 8 + ATT_NROWS))
                            plan = []
                            for r in rows:
                                rs = min(max(r - 4, 0), 24)
                                dr0 = rs - r + 7
                                chunks = []
                                if rs % 2 == 0:
                                    for j in range(4):
                                        chunks.append(((rs + 2 * j) // 2, 1 + dr0 + 2 * j))
                                else:
                                    assert dr0 == 3
                                    c0 = (rs - 1) // 2
                                    chunks.append((c0, 17))
                                    for j in range(1, 4):
                                        chunks.append((c0 + j, dr0 + 2 * j))
                                    chunks.append((c0 + 4, 19))
                                plan.append((r, chunks))

                            def emit_qk(idx):
                                r, chunks = plan[idx]
                                si = idx % 2
                                pS, bS = psS[si], b_psS[si]
                                qcol = slice(r * 64, (r + 1) * 64)
                                for j, (kc, blk0) in enumerate(chunks):
                                    o = pS[:, j * 64:(j + 1) * 64]
                                    pe_mm(o, Kr[par][pr, kc * 128:(kc + 1) * 128], Qr[par][pr, qcol], True, False,
                                          [b_K[par], b_Q[par][qb]], [bS], sig=False)
                                    lt = btab[pr, h, blk0 * 64:(blk0 + 2) * 64]
                                    pe_mm(o, lt, identb[pr, pr], False, True, [b_btab, b_identb], [bS],
                                          sig=(j == len(chunks) - 1))
                                n = len(chunks) * 64
                                pi = idx % 3
                                act(PT[pi][:, 0:n], pS[:, 0:n], AF.Exp, [bS], [b_PT[pi]])

                            def emit_pv(idx):
                                r, chunks = plan[idx]
                                pi = idx % 3
                                rr = r - qb * 8
                                for j, (kc, _) in enumerate(chunks):
                                    last = (j == len(chunks) - 1)
                                    pe_mm(pO[:, rr * 64:(rr + 1) * 64], Vx[par][:, kc, vcols], PT[pi][:, j * 64:(j + 1) * 64],
                                          False, last and idx == len(plan) - 1, [b_V[par][kc // 4], b_PT[pi]], [bO], sig=last)

                            if plan:
                                emit_qk(0)
                            for idx in range(len(plan)):
                                if idx + 1 < len(plan):
                                    emit_qk(idx + 1)
                                emit_pv(idx)
                            dn = slice(64, 128) if e == 0 else slice(0, 64)
                            if not ATT_NORM:
                                continue
                            V("dve", lambda: nc.vector.reciprocal(out=rc[oi][pr, :], in_=pO[dn, :]), [bO], [b_rc[oi]])
                            V("dve", lambda: nc.vector.tensor_tensor(out=oatt[pr, hp, qs], in0=pO[pr, :], in1=rc[oi][pr, :],
                                                                     op=ALU.mult), [bO, b_rc[oi]], [b_oatt[hp][qb]])
                    if b == 0:
                        dump("oatt", oatt[:, :, :].rearrange("p a b -> p (a b)"), [x for l_ in b_oatt for x in l_])
                    S.barrier()
                if STOP_AFTER == "s2":
                    break

                olru = sb(esb, [128, 8, L], BF16, "olru")
                b_olru = [Buf() for _ in range(8)]
                with ExitStack() as es3:
                    TL = LC + 3
                    wl = [sb(es3, [128, 8, 256], BF16, "wl") for _ in range(2)]; b_wl = [Buf(), Buf()]
                    lw = sb(es3, [128, 4, 8, 128], BF16, "lw"); b_lw = Buf()
                    S.dma("pool", lw[:, :, :, :].rearrange("p a b c -> p (a b c)"), lruw_d, writes=[b_lw])
                    LXp = sb(es3, [128, TL + 3], F32, "LXp"); b_LXp = Buf()
                    xc = sb(es3, [128, TL], F32, "xc"); b_xc = Buf()
                    xcb = [sb(es3, [128, TL], BF16, "xcb") for _ in range(2)]; b_xcb = [Buf(), Buf()]
                    av = sb(es3, [128, TL], F32, "av"); b_av = Buf()
                    wv = sb(es3, [128, TL], F32, "wv"); b_wv = Buf()
                    iv = sb(es3, [128, TL], F32, "iv"); b_iv = Buf()
                    hv = [sb(es3, [128, TL], F32, "hv") for _ in range(2)]; b_hv = [Buf(), Buf()]
                    gl = sb(es3, [128, L], BF16, "gl"); b_gl = Buf()
                    psA = [ps(es3, [128, 512], F32, "psA") for _ in range(4)]; b_psA = [Buf() for _ in range(4)]
                    pacnt = [0]

                    def nextA():
                        i = pacnt[0] % 4
                        pacnt[0] += 1
                        return psA[i], b_psA[i]

                    V("dve", lambda: nc.vector.memset(LXp[:], 0.0), [], [b_LXp])

                    def load_wl(n):
                        S.dma("pool", wl[n % 2][:, :, :].rearrange("p a b -> p (a b)"), wlxlg_d[n], writes=[b_wl[n % 2]])

                    def lx_conv(n):
                        w = wl[n % 2]; bw = b_wl[n % 2]
                        for blk in range(5):
                            nt = 512 if blk < 4 else C
                            tok = slice(blk * 512, blk * 512 + nt)
                            pa, bpa = nextA()
                            for k in range(8):
                                pe_mm(pa[:, 0:nt], w[:, k, 0:128], hT[:, k, tok], k == 0, k == 7, [bw, *b_hT[blk]], [bpa])
                            d0 = 261 + blk * 512 if blk < 4 else 2
                            act(LXp[:, d0:d0 + nt], pa[:, 0:nt], AF.Copy, [bpa], [b_LXp])
                        cw = lambda j: vecs[:, V_CONVW + j * 8 + n:V_CONVW + j * 8 + n + 1]
                        V("dve", lambda: nc.vector.tensor_scalar(out=xc[:], in0=LXp[:, 0:TL], scalar1=cw(0),
                                                                 scalar2=vecs[:, V_CONVB + n:V_CONVB + n + 1],
                                                                 op0=ALU.mult, op1=ALU.add), [b_LXp, b_vecs], [b_xc])
                        for j in range(1, 3):
                            V("dve", lambda: nc.vector.scalar_tensor_tensor(out=xc[:], in0=LXp[:, j:j + TL], scalar=cw(j),
                                                                            in1=xc[:], op0=ALU.mult, op1=ALU.add),
                              [b_LXp, b_vecs, b_xc], [b_xc])
                        V("dve", lambda: nc.vector.scalar_tensor_tensor(out=xcb[n % 2][:], in0=LXp[:, 3:3 + TL], scalar=cw(3),
                                                                        in1=xc[:], op0=ALU.mult, op1=ALU.add),
                          [b_LXp, b_vecs, b_xc], [b_xcb[n % 2]])

                    load_wl(0)
                    lx_conv(0)
                    for n in range(8):
                        w = wl[n % 2]; bw = b_wl[n % 2]
                        xb_ = xcb[n % 2]; bxb = b_xcb[n % 2]
                        if n + 1 < 8:
                            load_wl(n + 1)
                        if b == 0 and n == 0:
                            dump("xc0", xb_[:], [bxb])
                        for dr in range(2):
                            di = dr * 8 + n
                            for blk in range(5):
                                nt = 512 if blk < 4 else TL - 2048
                                tok = slice(blk * 512, blk * 512 + nt)
                                pr_, bpr = nextA()
                                pe_mm(pr_[:, 0:nt], lw[:, dr, n, :], xb_[:, tok], True, True, [b_lw, bxb], [bpr])
                                pi_, bpi = nextA()
                                pe_mm(pi_[:, 0:nt], lw[:, 2 + dr, n, :], xb_[:, tok], True, True, [b_lw, bxb], [bpi])
                                act(av[:, tok], pr_[:, 0:nt], AF.Sigmoid, [bpr, b_vecs], [b_av],
                                    bias=vecs[:, V_BA + di:V_BA + di + 1])
                                act(iv[:, tok], pi_[:, 0:nt], AF.Sigmoid, [bpi, b_vecs], [b_iv],
                                    bias=vecs[:, V_BX + di:V_BX + di + 1])
                            act(wv[:], av[:], AF.Exp, [b_av, b_lrup], [b_wv], scale=lrup[:, 1, di:di + 1])
                            act(av[:], av[:], AF.Exp, [b_av, b_lrup], [b_av], scale=lrup[:, 0, di:di + 1])
                            act(wv[:], wv[:], AF.Sqrt, [b_wv], [b_wv], scale=-1.0, bias=1.0)
                            V("pool", lambda: nc.gpsimd.tensor_tensor(out=iv[:], in0=iv[:], in1=xb_[:], op=ALU.mult),
                              [b_iv, bxb], [b_iv])
                            V("dve", lambda: nc.vector.tensor_tensor(out=wv[:], in0=iv[:], in1=wv[:], op=ALU.mult),
                              [b_iv, b_wv], [b_wv])
                            hh = hv[dr]; bh = b_hv[dr]
                            if dr == 0:
                                V("dve", lambda: nc.vector.tensor_tensor_scan(
                                    out=hh[:, 0:C], data0=av[:, 0:C], data1=wv[:, 0:C], initial=0.0,
                                    op0=ALU.mult, op1=ALU.add), [b_av, b_wv], [bh])
                                V("dve", lambda: nc.vector.tensor_tensor_scan(
                                    out=hh[:, C + 3:TL], data0=av[:, C + 3:TL], data1=wv[:, C + 3:TL],
                                    initial=hh[:, C - 1:C], op0=ALU.mult, op1=ALU.add), [b_av, b_wv, bh], [bh])
                                if n + 1 < 8:
                                    lx_conv(n + 1)
                            else:
                                V("dve", lambda: nc.vector.tensor_tensor_scan(
                                    out=hh[:, C - 1::-1], data0=av[:, C - 1::-1], data1=wv[:, C - 1::-1], initial=0.0,
                                    op0=ALU.mult, op1=ALU.add), [b_av, b_wv], [bh])
                                V("dve", lambda: nc.vector.tensor_tensor_scan(
                                    out=hh[:, TL - 1:C + 2:-1], data0=av[:, TL - 1:C + 2:-1], data1=wv[:, TL - 1:C + 2:-1],
                                    initial=hh[:, 0:1], op0=ALU.mult, op1=ALU.add), [b_av, b_wv, bh], [bh])
                        for blk in range(4):
                            tok = slice(blk * 512, (blk + 1) * 512)
                            pa, bpa = nextA()
                            for k in range(8):
                                pe_mm(pa[:], w[:, k, 128:256], hT[:, k, tok], k == 0, k == 7, [bw, *b_hT[blk]], [bpa])
                            act(gl[:, tok], pa[:], AF.Gelu_apprx_tanh, [bpa], [b_gl])
                        V("dve", lambda: nc.vector.tensor_tensor(out=hv[0][:, C + 3:TL], in0=hv[0][:, C + 3:TL],
                                                                 in1=hv[1][:, C + 3:TL], op=ALU.add),
                          [b_hv[0], b_hv[1]], [b_hv[0]])
                        if b == 0 and n == 0:
                            dump("hsum0", hv[0][:, C + 3:TL], [b_hv[0]])
                        V("dve", lambda: nc.vector.tensor_tensor(out=olru[:, n, :], in0=hv[0][:, C + 3:TL], in1=gl[:],
                                                                 op=ALU.mult), [b_hv[0], b_gl], [b_olru[n]])
                    if b == 0:
                        dump("olru", olru[:, :, :].rearrange("p a b -> p (a b)"), b_olru)
                    S.barrier()
                if STOP_AFTER == "s3":
                    break

                yT = sb(esb, [128, 8, L], BF16, "yT")
                b_yT = [Buf() for _ in range(4)]
                with ExitStack() as es4:
                    wg_ = [sb(es4, [128, 8, 256], BF16, "wg") for _ in range(2)]; b_wg = [Buf(), Buf()]
                    wu_ = [sb(es4, [128, 12, 128], BF16, "wu") for _ in range(2)]; b_wu = [Buf(), Buf()]
                    ga_ = [sb(es4, [128, 512], F32, "ga") for _ in range(2)]; b_ga = [Buf(), Buf()]
                    gb_ = [sb(es4, [128, 512], F32, "gb") for _ in range(2)]; b_gb = [Buf(), Buf()]
                    ya_ = [sb(es4, [128, 512], F32, "ya") for _ in range(2)]; b_ya = [Buf(), Buf()]
                    yb_ = [sb(es4, [128, 512], F32, "yb") for _ in range(2)]; b_yb = [Buf(), Buf()]
                    psM = [ps(es4, [128, 512], F32, "psM") for _ in range(8)]; b_psM = [Buf() for _ in range(8)]
                    it = 0
                    for f in range(8):
                        wgt, bwg = wg_[f % 2], b_wg[f % 2]
                        wut, bwu = wu_[f % 2], b_wu[f % 2]
                        S.dma("pool", wgt[:, :, :].rearrange("p a b -> p (a b)"), wgagb_d[f], writes=[bwg])
                        S.dma("pool", wut[:, :, :].rearrange("p a b -> p (a b)"), wup_d[f], writes=[bwu])
                        for blk in range(4):
                            tok = slice(blk * 512, (blk + 1) * 512)
                            i2 = it % 2
                            p0, p1, p2, p3 = [psM[(it % 2) * 4 + q] for q in range(4)]
                            q0, q1, q2, q3 = [b_psM[(it % 2) * 4 + q] for q in range(4)]
                            it += 1
                            for k in range(8):
                                pe_mm(p0[:], wgt[:, k, 0:128], hT[:, k, tok], k == 0, k == 7, [bwg, *b_hT[blk]], [q0])
                            act(ga_[i2][:], p0[:], AF.Sigmoid, [q0], [b_ga[i2]])
                            for k in range(4):
                                pe_mm(p1[:], wut[:, k, :], oatt[:, k, tok], k == 0, k == 3, [bwu, b_oatt[k][blk]], [q1])
                            V("dve", lambda: nc.vector.tensor_tensor(out=ya_[i2][:], in0=p1[:], in1=ga_[i2][:], op=ALU.mult),
                              [q1, b_ga[i2]], [b_ya[i2]])
                            for k in range(8):
                                pe_mm(p2[:], wgt[:, k, 128:256], hT[:, k, tok], k == 0, k == 7, [bwg, *b_hT[blk]], [q2])
                            act(gb_[i2][:], p2[:], AF.Sigmoid, [q2], [b_gb[i2]])
                            for k in range(8):
                                pe_mm(p3[:], wut[:, 4 + k, :], olru[:, k, tok], k == 0, k == 7, [bwu, b_olru[k]], [q3])
                            V("dve", lambda: nc.vector.tensor_tensor(out=yb_[i2][:], in0=p3[:], in1=gb_[i2][:], op=ALU.mult),
                              [q3, b_gb[i2]], [b_yb[i2]])
                            V("pool", lambda: nc.gpsimd.tensor_tensor(out=yT[:, f, tok], in0=ya_[i2][:], in1=yb_[i2][:],
                                                                      op=ALU.add), [b_ya[i2], b_yb[i2]], [b_yT[blk]])
                    if b == 0:
                        dump("yT", yT[:, :, :].rearrange("p a b -> p (a b)"), b_yT)
                    S.barrier()
                if STOP_AFTER == "s4":
                    break

                with ExitStack() as es5:
                    wo32 = [sb(es5, [128, D], F32, "wo32") for _ in range(2)]; b_wo32 = [Buf(), Buf()]
                    wob = sb(es5, [128, 8, D], BF16, "wob"); b_wob = Buf()
                    GA1 = sb(es5, [128, D], F32, "GA1"); b_GA1 = Buf()
                    G2 = sb(es5, [128, D], F32, "G2"); b_G2 = Buf()
                    S2 = sb(es5, [128, D], F32, "S2"); b_S2 = Buf()
                    gffnb = sb(es5, [128, D], F32, "gffnb"); b_gffnb = Buf()
                    diag = [sb(es5, [128, 128], F32, "diag") for _ in range(2)]; b_diag = [Buf(), Buf()]
                    wr = sb(es5, [128, 8, 36], F32, "wr"); b_wr = Buf()
                    brb = sb(es5, [128, 36], F32, "brb"); b_brb = Buf()
                    psB = ps(es5, [128, D], F32, "psB"); b_psB = Buf()
                    psO5 = [ps(es5, [128, D], F32, "psO5") for _ in range(2)]; b_psO5 = [Buf(), Buf()]
                    psT = ps(es5, [128, D], F32, "psT"); b_psT = Buf()
                    S.dma("sp", gffnb[:], gffnb_d, writes=[b_gffnb])
                    S.dma("sp", wr[:, :, :].rearrange("p a b -> p (a b)"), wr_d, writes=[b_wr])
                    S.dma("sp", brb[:], brb_d, writes=[b_brb])
                    bcast_row(es5, 2, b, psB, b_psB, diag, b_diag)
                    V("dve", lambda: nc.vector.tensor_copy(out=GA1[:], in_=psB[:]), [b_psB], [b_GA1])
                    bcast_row(es5, 4, b, psB, b_psB, diag, b_diag)
                    V("dve", lambda: nc.vector.scalar_tensor_tensor(out=G2[:], in0=psB[:], scalar=1.0, in1=gffnb[:],
                                                                    op0=ALU.add, op1=ALU.mult), [b_psB, b_gffnb], [b_G2])
                    bcast_row(es5, 3, b, psB, b_psB, diag, b_diag)
                    V("dve", lambda: nc.vector.tensor_copy(out=S2[:], in_=psB[:]), [b_psB], [b_S2])
                    for kk in range(8):
                        S.dma("sp", wo32[kk % 2][:], wout_d[:, kk * D:(kk + 1) * D], writes=[b_wo32[kk % 2]])
                        V("dve", lambda: nc.vector.tensor_tensor(out=wob[:, kk, :], in0=wo32[kk % 2][:], in1=GA1[:],
                                                                 op=ALU.mult), [b_wo32[kk % 2], b_GA1], [b_wob])
                    xt5 = [sb(es5, [128, D], F32, "xt5") for _ in range(2)]; b_xt5 = [Buf(), Buf()]
                    x1t = xt5; b_x1t = b_xt5
                    h2t = [sb(es5, [128, D], F32, "h2t") for _ in range(2)]; b_h2t = [Buf(), Buf()]
                    h2b = [sb(es5, [128, D], BF16, "h2b") for _ in range(2)]; b_h2b = [Buf(), Buf()]
                    h2T = sb(es5, [128, 8, 128], F32, "h2T"); b_h2T = Buf()
                    junk5 = sb(es5, [128, D], BF16, "junk5"); b_junk5 = Buf()
                    st5 = sb(es5, [128, 16, 3], F32, "st5"); b_st5 = [Buf() for _ in range(16)]
                    LGa = sb(es5, [128, 16, 36], F32, "LGa"); b_LGa = Buf()
                    RW = sb(es5, [128, 8, 16], F32, "RW"); b_RW = Buf()
                    RG = sb(es5, [128, 16, 12], F32, "RG"); b_RG = Buf()
                    LEM = sb(es5, [128, 16, 32], F32, "LEM"); b_LEM = Buf()
                    LEM2 = sb(es5, [128, 16, 32], F32, "LEM2"); b_LEM2 = Buf()
                    def wout_mm(j):
                        i2 = j % 2
                        tsl = slice(j * 128, (j + 1) * 128)
                        S.dma("sp", xt5[i2][:], xin[b, tsl, :], writes=[b_xt5[i2]])
                        pO, bO = psO5[i2], b_psO5[i2]
                        for hf in range(2):
                            for k in range(8):
                                pe_mm(pO[:, hf * 512:(hf + 1) * 512], yT[:, k, tsl], wob[:, k, hf * 512:(hf + 1) * 512],
                                      k == 0, k == 7, [b_yT[j // 4], b_wob], [bO], sig=(k == 7 and hf == 1))

                    wout_mm(0)
                    for j in range(16):
                        tg = b * 16 + j
                        i2 = j % 2
                        tsl = slice(j * 128, (j + 1) * 128)
                        pO, bO = psO5[i2], b_psO5[i2]
                        if j + 1 < 16:
                            wout_mm(j + 1)
                        V("dve", lambda: nc.vector.tensor_tensor(out=x1t[i2][:], in0=pO[:], in1=xt5[i2][:], op=ALU.add),
                          [bO, b_xt5[i2]], [b_xt5[i2]])
                        S.dma("pool", x1_d[tg * 128:(tg + 1) * 128, :], x1t[i2][:], reads=[b_x1t[i2]], writes=[b_x1d], group=True, semof=b_x1t[i2])
                        act(junk5[:], x1t[i2][:], AF.Square, [b_x1t[i2]], [b_junk5, b_st5[j]], accum_out=st5[:, j, 0:1])
                        act(st5[:, j, 1:2], st5[:, j, 0:1], AF.Sqrt, [b_st5[j], b_eps], [b_st5[j]], scale=1.0 / D,
                            bias=epst[:, 0:1])
                        V("dve", lambda: nc.vector.reciprocal(out=st5[:, j, 2:3], in_=st5[:, j, 1:2]), [b_st5[j]], [b_st5[j]])
                        V("dve", lambda: nc.vector.scalar_tensor_tensor(out=h2t[i2][:], in0=x1t[i2][:], scalar=st5[:, j, 2:3],
                                                                        in1=G2[:], op0=ALU.mult, op1=ALU.mult),
                          [b_x1t[i2], b_st5[j], b_G2], [b_h2t[i2]])
                        V("dve", lambda: nc.vector.tensor_tensor(out=h2t[i2][:], in0=h2t[i2][:], in1=S2[:], op=ALU.add),
                          [b_h2t[i2], b_S2], [b_h2t[i2]])
                        act(h2b[i2][:], h2t[i2][:], AF.Copy, [b_h2t[i2]], [b_h2b[i2]])
                        S.dma("pool", h2_d[tg * 128:(tg + 1) * 128, :], h2b[i2][:], reads=[b_h2b[i2]], writes=[b_h2d], group=True, semof=b_h2b[i2])
                        for k in range(8):
                            pe_tr(psT[:, k * 128:(k + 1) * 128], h2t[i2][:, k * 128:(k + 1) * 128], identf[:],
                                  [b_h2t[i2], b_identf], [b_psT], sig=(k == 7))
                        act(h2T[:, :, :].rearrange("p a b -> p (a b)"), psT[:], AF.Copy, [b_psT], [b_h2T])
                        for k in range(8):
                            pe_mm(psB[:, 0:36], h2T[:, k, :], wr[:, k, :], k == 0, k == 7, [b_h2T, b_wr], [b_psB])
                        V("dve", lambda: nc.vector.tensor_tensor(out=LGa[:, j, :], in0=psB[:, 0:36], in1=brb[:], op=ALU.add),
                          [b_psB, b_brb], [b_LGa])
                        if tg == 0:
                            dump("logit0", LGa[:, 0, :], [b_LGa])
                    T16 = slice(b * 16, (b + 1) * 16)
                    lg = LGa[:, :, 0:4]
                    le = LGa[:, :, 4:36]
                    rb = [b_LGa, b_RW]
                    V("dve", lambda: nc.vector.tensor_reduce(out=RW[:, 0, 0:16], in_=lg, axis=AX.X, op=ALU.max), [b_LGa], [b_RW])
                    V("dve", lambda: nc.vector.tensor_tensor(out=RG[:, :, 0:4], in0=lg,
                                                             in1=RW[:, 0, 0:16].unsqueeze(2).to_broadcast([128, 16, 4]),
                                                             op=ALU.subtract), rb, [b_RG])
                    act(RG[:, :, 4:8], RG[:, :, 0:4], AF.Exp, [b_RG], [b_RG])
                    V("dve", lambda: nc.vector.tensor_reduce(out=RW[:, 1, 0:16], in_=RG[:, :, 4:8], axis=AX.X, op=ALU.add), [b_RG], [b_RW])
                    V("dve", lambda: nc.vector.reciprocal(out=RW[:, 2, 0:16], in_=RW[:, 1, 0:16]), [b_RW], [b_RW])
                    V("dve", lambda: nc.vector.tensor_scalar(out=RG[:, :, 8:12], in0=RG[:, :, 0:4], scalar1=0.0, scalar2=None,
                                                             op0=ALU.is_equal), [b_RG], [b_RG])
                    V("dve", lambda: nc.vector.tensor_scalar(out=RG[:, :, 8:12], in0=RG[:, :, 8:12], scalar1=-1.0, scalar2=1e30,
                                                             op0=ALU.add, op1=ALU.mult), [b_RG], [b_RG])
                    V("dve", lambda: nc.vector.tensor_tensor(
                        out=LEM[:, :, :].rearrange("p t (g e) -> p t g e", e=8), in0=le.rearrange("p t (g e) -> p t g e", e=8),
                        in1=RG[:, :, 8:12].unsqueeze(3).to_broadcast([128, 16, 4, 8]), op=ALU.add), [b_LGa, b_RG], [b_LEM])
                    V("dve", lambda: nc.vector.tensor_reduce(out=RW[:, 3, 0:16], in_=LEM[:], axis=AX.X, op=ALU.max), [b_LEM], [b_RW])
                    V("dve", lambda: nc.vector.tensor_tensor(out=oh1all[:, T16, :], in0=LEM[:],
                                                             in1=RW[:, 3, 0:16].unsqueeze(2).to_broadcast([128, 16, 32]),
                                                             op=ALU.is_equal), [b_LEM, b_RW], [b_oh1])
                    V("dve", lambda: nc.vector.scalar_tensor_tensor(out=LEM2[:], in0=oh1all[:, T16, :], scalar=-1e30, in1=LEM[:],
                                                                    op0=ALU.mult, op1=ALU.add), [b_oh1, b_LEM], [b_LEM2])
                    V("dve", lambda: nc.vector.tensor_reduce(out=RW[:, 4, 0:16], in_=LEM2[:], axis=AX.X, op=ALU.max), [b_LEM2], [b_RW])
                    V("dve", lambda: nc.vector.tensor_tensor(out=Mall[:, T16, :], in0=LEM[:],
                                                             in1=RW[:, 4, 0:16].unsqueeze(2).to_broadcast([128, 16, 32]),
                                                             op=ALU.is_ge), [b_LEM, b_RW], [b_Mall])
                    V("dve", lambda: nc.vector.tensor_tensor(out=RW[:, 5, 0:16], in0=RW[:, 4, 0:16], in1=RW[:, 3, 0:16], op=ALU.subtract),
                      [b_RW], [b_RW])
                    act(RW[:, 6, 0:16], RW[:, 5, 0:16], AF.Exp, [b_RW], [b_RW])
                    V("dve", lambda: nc.vector.tensor_scalar(out=RW[:, 6, 0:16], in0=RW[:, 6, 0:16], scalar1=1.0, scalar2=None, op0=ALU.add),
                      [b_RW], [b_RW])
                    V("dve", lambda: nc.vector.reciprocal(out=RW[:, 7, 0:16], in_=RW[:, 6, 0:16]), [b_RW], [b_RW])
                    V("dve", lambda: nc.vector.tensor_tensor(out=gates[:, T16, 0], in0=RW[:, 7, 0:16], in1=RW[:, 2, 0:16], op=ALU.mult),
                      [b_RW], [b_gates])
                    V("dve", lambda: nc.vector.tensor_tensor(out=gates[:, T16, 1], in0=RW[:, 2, 0:16], in1=gates[:, T16, 0], op=ALU.subtract),
                      [b_RW, b_gates], [b_gates])
                    S.barrier()
            if STOP_AFTER in ("s1", "s2", "s3", "s4"):
                break

        if STOP_AFTER is None or STOP_AFTER in ("route", "moe"):
            blk_i = sb(es, [128, NBLK], I32, "blki"); b_blki = Buf()
            with ExitStack() as esr:
                psC = ps(esr, [128, 32], F32, "psC"); b_psC = Buf()
                psU = ps(esr, [128, 1024], F32, "psU"); b_psU = Buf()
                psCS = ps(esr, [128, 1024], F32, "psCS"); b_psCS = Buf()
                CSs = sb(esr, [128, 32, 32], F32, "CSs"); b_CSs = Buf()
                INC = sb(esr, [128, 32, 32], F32, "INC"); b_INC = Buf()
                TT = sb(esr, [128, 32, 32], F32, "TT"); b_TT = Buf()
                TM = sb(esr, [128, 32, 32], F32, "TM"); b_TM = Buf()
                cn = sb(esr, [128, 8, 32], F32, "cn"); b_cn = Buf()
                cni = sb(esr, [128, 32], I32, "cni"); b_cni = Buf()
                cmp_ = sb(esr, [128, NBLK, 32], F32, "cmp"); b_cmp = Buf()
                bst = sb(esr, [128, NBLK], F32, "bst"); b_bst = Buf()
                bs0 = sb(esr, [128, NBLK], F32, "bs0"); b_bs0 = Buf()
                blk_f = sb(esr, [128, NBLK], F32, "blkf"); b_blkf = Buf()
                tmp = sb(esr, [128, 2, 32], F32, "tmpr"); b_tmp = [Buf(), Buf()]
                for t in range(32):
                    pe_mm(psC[:], onesb[:], Mall[:, t, :], t == 0, t == 31, [b_onesb, b_Mall], [b_psC])
                V("dve", lambda: nc.vector.tensor_copy(out=cn[:, 0, :], in_=psC[:]), [b_psC], [b_cn])
                V("dve", lambda: nc.vector.tensor_scalar(out=cn[:, 1, :], in0=cn[:, 0, :], scalar1=255.0, scalar2=None, op0=ALU.add),
                  [b_cn], [b_cn])
                V("dve", lambda: nc.vector.tensor_copy(out=cni[:], in_=cn[:, 1, :]), [b_cn], [b_cni])
                V("dve", lambda: nc.vector.tensor_scalar(out=cni[:], in0=cni[:], scalar1=8, scalar2=8,
                                                         op0=ALU.arith_shift_right, op1=ALU.logical_shift_left), [b_cni], [b_cni])
                V("dve", lambda: nc.vector.tensor_copy(out=cn[:, 2, :], in_=cni[:]), [b_cni], [b_cn])
                V("dve", lambda: nc.vector.tensor_tensor_scan(out=cn[:, 3, :], data0=onesf[:, 0:32], data1=cn[:, 2, :],
                                                              initial=0.0, op0=ALU.mult, op1=ALU.add),
                  [b_cn, b_onesf], [b_cn])
                V("dve", lambda: nc.vector.tensor_tensor(out=cn[:, 4, :], in0=cn[:, 3, :], in1=cn[:, 2, :], op=ALU.subtract),
                  [b_cn], [b_cn])
                V("dve", lambda: nc.vector.tensor_scalar(out=bs0[:], in0=onesf[:, 0:NBLK], scalar1=256.0, scalar2=None, op0=ALU.mult),
                  [b_onesf], [b_bs0])
                V("dve", lambda: nc.vector.tensor_tensor_scan(out=bst[:], data0=onesf[:, 0:NBLK], data1=bs0[:], initial=-256.0,
                                                              op0=ALU.mult, op1=ALU.add), [b_bs0, b_onesf], [b_bst])
                V("dve", lambda: nc.vector.tensor_tensor(
                    out=cmp_[:], in0=cn[:, 3, :].unsqueeze(1).to_broadcast([128, NBLK, 32]),
                    in1=bst[:].unsqueeze(2).to_broadcast([128, NBLK, 32]), op=ALU.is_le), [b_cn, b_bst], [b_cmp])
                V("dve", lambda: nc.vector.tensor_reduce(out=blk_f[:], in_=cmp_[:], axis=AX.X, op=ALU.add), [b_cmp], [b_blkf])
                V("dve", lambda: nc.vector.tensor_scalar(out=blk_f[:], in0=blk_f[:], scalar1=31.0, scalar2=128.0,
                                                         op0=ALU.min, op1=ALU.mult), [b_blkf], [b_blkf])
                V("dve", lambda: nc.vector.tensor_scalar(out=blk_f[:], in0=blk_f[:], scalar1=iota[:, 0:1], scalar2=None,
                                                         op0=ALU.add), [b_blkf, b_iota], [b_blkf])
                V("dve", lambda: nc.vector.tensor_copy(out=blk_i[:], in_=blk_f[:]), [b_blkf], [b_blki])
                dump("blkf", blk_f[:], [b_blkf])
                dump("cn", cn[:, 0:5, :].rearrange("p a b -> p (a b)"), [b_cn])
                Mflat = Mall[:, :, :].rearrange("p t e -> p (t e)")
                for hf in range(2):
                    pe_mm(psU[:, hf * 512:(hf + 1) * 512], utri[:], Mflat[:, hf * 512:(hf + 1) * 512], True, True,
                          [b_utri, b_Mall], [b_psU], sig=(hf == 1))
                    pe_mm(psCS[:, hf * 512:(hf + 1) * 512], onesb[:], Mflat[:, hf * 512:(hf + 1) * 512], True, True,
                          [b_onesb, b_Mall], [b_psCS], sig=(hf == 1))
                V("dve", lambda: nc.vector.tensor_copy(out=CSs[:, :, :].rearrange("p t e -> p (t e)"), in_=psCS[:]), [b_psCS], [b_CSs])
                for e in range(32):
                    V("dve", lambda: nc.vector.tensor_tensor_scan(out=INC[:, :, e], data0=onesf[:, 0:32], data1=CSs[:, :, e],
                                                                  initial=0.0, op0=ALU.mult, op1=ALU.add),
                      [b_CSs, b_onesf], [b_INC])
                V("dve", lambda: nc.vector.tensor_tensor(out=TT[:, :, :].rearrange("p t e -> p (t e)"), in0=psU[:],
                                                         in1=INC[:, :, :].rearrange("p t e -> p (t e)"), op=ALU.add),
                  [b_psU, b_INC], [b_TT])
                V("dve", lambda: nc.vector.tensor_tensor(out=TT[:], in0=TT[:], in1=CSs[:], op=ALU.subtract), [b_TT, b_CSs], [b_TT])
                V("dve", lambda: nc.vector.tensor_tensor(out=TT[:], in0=TT[:], in1=cn[:, 4, :].unsqueeze(1).to_broadcast([128, 32, 32]),
                                                         op=ALU.add), [b_TT, b_cn], [b_TT])
                V("dve", lambda: nc.vector.tensor_tensor(out=TM[:], in0=TT[:], in1=oh1all[:], op=ALU.mult), [b_TT, b_oh1], [b_TM])
                V("dve", lambda: nc.vector.tensor_reduce(out=dest_f[:, :, 0], in_=TM[:], axis=AX.X, op=ALU.add), [b_TM], [b_destf])
                V("dve", lambda: nc.vector.tensor_tensor(out=TM[:], in0=Mall[:], in1=oh1all[:], op=ALU.subtract), [b_Mall, b_oh1], [b_TM])
                V("dve", lambda: nc.vector.tensor_tensor(out=TM[:], in0=TM[:], in1=TT[:], op=ALU.mult), [b_TM, b_TT], [b_TM])
                V("dve", lambda: nc.vector.tensor_reduce(out=dest_f[:, :, 1], in_=TM[:], axis=AX.X, op=ALU.add), [b_TM], [b_destf])
                V("dve", lambda: nc.vector.tensor_copy(out=dest_i[:], in_=dest_f[:, :, :].rearrange("p a b -> p (a b)")), [b_destf], [b_desti])
                dump("destf", dest_f[:, :, :].rearrange("p a b -> p (a b)"), [b_destf])
                dump("gates", gates[:, :, :].rearrange("p a b -> p (a b)"), [b_gates])
                hl = [sb(esr, [128, D], BF16, "hl") for _ in range(4)]; b_hl = [Buf() for _ in range(4)]
                for t in range(32):
                    S.dma("sp", hl[t % 4][:], h2_d[t * 128:(t + 1) * 128, :], reads=[b_h2d], writes=[b_hl[t % 4]])
                    for kk in range(2):
                        S.dma_ind(xs_d[:, :], bass.IndirectOffsetOnAxis(ap=dest_i[:, 2 * t + kk:2 * t + kk + 1], axis=0), hl[t % 4][:, :], None,
                                  NROWS - 1, reads=[b_hl[t % 4], b_desti], writes=[b_xsd], group=True, semof=b_hl[t % 4])
                S.barrier()

            if STOP_AFTER != "route":
                with ExitStack() as esm:
                    w1b = [sb(esm, [128, 8, 512], BF16, "w1b") for _ in range(3)]; b_w1 = [Buf() for _ in range(3)]
                    w3b = [sb(esm, [128, 8, 512], BF16, "w3b") for _ in range(3)]; b_w3 = [Buf() for _ in range(3)]
                    w2b = [sb(esm, [128, 4, 1024], BF16, "w2b") for _ in range(3)]; b_w2 = [Buf() for _ in range(3)]
                    xr = [sb(esm, [128, 2, D], BF16, "xr") for _ in range(3)]; b_xr = [Buf() for _ in range(3)]
                    xT = [sb(esm, [128, 8, 256], BF16, "xT") for _ in range(3)]; b_xT = [[Buf(), Buf()] for _ in range(3)]
                    sl = [sb(esm, [128, 256], F32, "sl") for _ in range(2)]; b_sl = [Buf(), Buf()]
                    hm = [sb(esm, [128, 4, 256], BF16, "hm") for _ in range(2)]; b_hm = [Buf(), Buf()]
                    yo = [sb(esm, [128, 2, D], BF16, "yo") for _ in range(2)]; b_yo = [[Buf(), Buf()], [Buf(), Buf()]]
                    psX = [ps(esm, [128, 1024], BF16, "psX") for _ in range(2)]; b_psX = [Buf(), Buf()]
                    psH = [ps(esm, [128, 512], F32, "psH") for _ in range(2)]; b_psH = [Buf(), Buf()]
                    psY = [ps(esm, [128, 512], F32, "psY") for _ in range(4)]; b_psY = [Buf() for _ in range(4)]
                    xcnt = [0]; hcnt = [0]; ycnt = [0]

                    def moe_w(blk):
                        i3 = blk % 3
                        off = bass.IndirectOffsetOnAxis(ap=blk_i[:, blk:blk + 1], axis=0)
                        S.dma_ind(w1b[i3][:, :, :].rearrange("p a b -> p (a b)"), None, w1_d[:, :], off, 0,
                                  reads=[b_blki], writes=[b_w1[i3]])
                        S.dma_ind(w3b[i3][:, :, :].rearrange("p a b -> p (a b)"), None, w3_d[:, :], off, 0,
                                  reads=[b_blki], writes=[b_w3[i3]])
                        S.dma_ind(w2b[i3][:, :, :].rearrange("p a b -> p (a b)"), None, w2_d[:, :], off, 0,
                                  reads=[b_blki], writes=[b_w2[i3]])

                    def moe_x(blk):
                        i2 = blk % 3
                        S.dma("sp", xr[i2][:], xs_d[blk * 256:(blk + 1) * 256, :].rearrange("(a p) d -> p a d", p=128),
                              reads=[b_xsd], writes=[b_xr[i2]])
                        for k in range(8):
                            if k % 4 == 0:
                                pX, bX = psX[(xcnt[0] // 4) % 2], b_psX[(xcnt[0] // 4) % 2]
                            for a in range(2):
                                pe_tr(pX[:, (k % 4) * 256 + a * 128:(k % 4) * 256 + (a + 1) * 128],
                                      xr[i2][:, a, k * 128:(k + 1) * 128], identb[:], [b_xr[i2], b_identb], [bX],
                                      sig=(k % 4 == 3 and a == 1))
                            xcnt[0] += 1
                            if k % 4 == 3:
                                dstx = xT[i2][:, k - 3:k + 1, :].rearrange("p a b -> p (a b)")
                                V("dve", lambda: nc.vector.tensor_copy(out=dstx, in_=pX[:]), [bX], [b_xT[i2][(k // 4) % 2]])

                    def moe_c(blk):
                        i2 = blk % 2
                        i3 = blk % 3
                        ix = blk % 3
                        for c4 in range(4):
                            pH, bH = psH[hcnt[0] % 2], b_psH[hcnt[0] % 2]
                            si = hcnt[0] % 2
                            hcnt[0] += 1
                            for k in range(8):
                                pe_mm(pH[:, 0:256], w1b[i3][:, k, c4 * 128:(c4 + 1) * 128], xT[ix][:, k, :], k == 0, k == 7,
                                      [b_w1[i3], *b_xT[ix]], [bH], sig=False)
                            for k in range(8):
                                pe_mm(pH[:, 256:512], w3b[i3][:, k, c4 * 128:(c4 + 1) * 128], xT[ix][:, k, :], k == 0, k == 7,
                                      [b_w3[i3], *b_xT[ix]], [bH])
                            act(sl[si][:], pH[:, 0:256], AF.Silu, [bH], [b_sl[si]])
                            V("dve", lambda: nc.vector.tensor_tensor(out=hm[i2][:, c4, :], in0=pH[:, 256:512], in1=sl[si][:],
                                                                     op=ALU.mult), [bH, b_sl[si]], [b_hm[i2]])
                        for a in range(2):
                            for hf in range(2):
                                pY, bY = psY[ycnt[0] % 4], b_psY[ycnt[0] % 4]
                                ycnt[0] += 1
                                for k in range(4):
                                    pe_mm(pY[:], hm[i2][:, k, a * 128:(a + 1) * 128], w2b[i3][:, k, hf * 512:(hf + 1) * 512],
                                          k == 0, k == 3, [b_hm[i2], b_w2[i3]], [bY])
                                V("dve", lambda: nc.vector.tensor_copy(out=yo[i2][:, a, hf * 512:(hf + 1) * 512], in_=pY[:]), [bY],
                                  [b_yo[i2][hf]])
                        S.dma("act", ys_d[blk * 256:(blk + 1) * 256, :].rearrange("(a p) d -> p a d", p=128), yo[i2][:],
                              reads=b_yo[i2], writes=[b_ysd], group=True, semof=b_yo[i2][0])

                    moe_w(0)
                    moe_w(1)
                    moe_x(0)
                    moe_x(1)
                    for blk in range(NBLK):
                        if blk + 2 < NBLK:
                            moe_w(blk + 2)
                            moe_x(blk + 2)
                        moe_c(blk)
                    S.barrier()

                with ExitStack() as esf:
                    GA2 = [sb(esf, [128, D], F32, "GA2") for _ in range(2)]; b_GA2 = [Buf(), Buf()]
                    gfb = sb(esf, [128, D], F32, "gfb"); b_gfb = Buf()
                    diag = [sb(esf, [128, 128], F32, "diagf") for _ in range(2)]; b_diag = [Buf(), Buf()]
                    psB = ps(esf, [128, D], F32, "psBf"); b_psB = Buf()
                    S.dma("sp", gfb[:], gfb_d, writes=[b_gfb])
                    for b in range(NB):
                        bcast_row(esf, 5, b, psB, b_psB, diag, b_diag)
                        V("dve", lambda: nc.vector.tensor_copy(out=GA2[b][:], in_=psB[:]), [b_psB], [b_GA2[b]])
                    y0 = [sb(esf, [128, D], BF16, "y0") for _ in range(3)]; b_y0 = [Buf() for _ in range(3)]
                    y1 = [sb(esf, [128, D], BF16, "y1") for _ in range(3)]; b_y1 = [Buf() for _ in range(3)]
                    x1l = [sb(esf, [128, D], F32, "x1l") for _ in range(3)]; b_x1l = [Buf() for _ in range(3)]
                    mo = [sb(esf, [128, D], F32, "mo") for _ in range(2)]; b_mo = [Buf(), Buf()]
                    ot = [sb(esf, [128, D], F32, "ot") for _ in range(2)]; b_ot = [Buf(), Buf()]
                    junkf = sb(esf, [128, D], BF16, "junkf"); b_junkf = Buf()
                    stf = sb(esf, [128, 32, 3], F32, "stf"); b_stf = [Buf() for _ in range(32)]

                    def fin_load(t):
                        i3 = t % 3
                        S.dma_ind(y0[i3][:, :], None, ys_d[:, :], bass.IndirectOffsetOnAxis(ap=dest_i[:, 2 * t:2 * t + 1], axis=0),
                                  0, reads=[b_ysd, b_desti], writes=[b_y0[i3]])
                        S.dma_ind(y1[i3][:, :], None, ys_d[:, :], bass.IndirectOffsetOnAxis(ap=dest_i[:, 2 * t + 1:2 * t + 2], axis=0),
                                  0, reads=[b_ysd, b_desti], writes=[b_y1[i3]])
                        S.dma("sp", x1l[i3][:], x1_d[t * 128:(t + 1) * 128, :], reads=[b_x1d], writes=[b_x1l[i3]])

                    fin_load(0)
                    fin_load(1)
                    for t in range(32):
                        i2 = t % 2
                        i3 = t % 3
                        bb = t // 16
                        act(mo[i2][:], y0[i3][:], AF.Copy, [b_y0[i3], b_gates], [b_mo[i2]], scale=gates[:, t, 0:1])
                        V("dve", lambda: nc.vector.scalar_tensor_tensor(out=mo[i2][:], in0=y1[i3][:], scalar=gates[:, t, 1:2],
                                                                        in1=mo[i2][:], op0=ALU.mult, op1=ALU.add),
                          [b_y1[i3], b_gates, b_mo[i2]], [b_mo[i2]])
                        if t == 0:
                            dump("moe0", mo[i2][:], [b_mo[i2]])
                        V("pool", lambda: nc.gpsimd.tensor_tensor(out=mo[i2][:], in0=mo[i2][:], in1=GA2[bb][:], op=ALU.mult),
                          [b_mo[i2], b_GA2[bb]], [b_mo[i2]])
                        V("dve", lambda: nc.vector.tensor_tensor(out=mo[i2][:], in0=mo[i2][:], in1=x1l[i3][:], op=ALU.add),
                          [b_mo[i2], b_x1l[i3]], [b_mo[i2]])
                        act(junkf[:], mo[i2][:], AF.Square, [b_mo[i2]], [b_junkf, b_stf[t]], accum_out=stf[:, t, 0:1])
                        act(stf[:, t, 1:2], stf[:, t, 0:1], AF.Sqrt, [b_stf[t], b_eps], [b_stf[t]], scale=1.0 / D, bias=epst[:, 0:1])
                        V("dve", lambda: nc.vector.reciprocal(out=stf[:, t, 2:3], in_=stf[:, t, 1:2]), [b_stf[t]], [b_stf[t]])
                        V("dve", lambda: nc.vector.scalar_tensor_tensor(out=ot[i2][:], in0=mo[i2][:], scalar=stf[:, t, 2:3],
                                                                        in1=gfb[:], op0=ALU.mult, op1=ALU.mult),
                          [b_mo[i2], b_stf[t], b_gfb], [b_ot[i2]])
                        if t + 2 < 32:
                            fin_load(t + 2)
                        S.dma("sp", out_d[bb, (t % 16) * 128:(t % 16 + 1) * 128, :], ot[i2][:], reads=[b_ot[i2]], writes=[b_outd],
                              group=True, semof=b_ot[i2])
        S.barrier()
        build_program.stats = dict(nops=dict(S.nops), nwaits=S.nwaits, nsem=S.nsem)
    return nc


def _fm(v):
    return np.ascontiguousarray(np.asarray(v, np.float32).reshape(-1, 128).T)


def _kp(w):
    K, N = w.shape
    return np.ascontiguousarray(w.reshape(K // 128, 128, N).transpose(1, 0, 2))


def _swap_cols():
    idx = np.arange(64)
    half = idx // 32
    within = idx % 32
    sw = np.where(within < 16, within + 16, within - 16)
    return half * 32 + sw


def _host_consts():
    nf = 16
    inv_freq = (10000.0 ** (-np.arange(nf, dtype=np.float32) / nf)).astype(np.float32)
    t = np.arange(L)
    row = (t // 64).astype(np.float32)
    col = (t % 64).astype(np.float32)
    cos = np.zeros((128, L), np.float32)
    sin = np.zeros((128, L), np.float32)
    for p in range(128):
        d = p % 64
        pos = row if d < 32 else col
        ang = (pos * inv_freq[(d % 32) % 16]).astype(np.float32)
        sign = -1.0 if (d % 32) < 16 else 1.0
        cos[p] = np.cos(ang)
        sin[p] = sign * np.sin(ang)
    cossin = np.concatenate([cos, sin], axis=1)
    ident = np.eye(128, dtype=np.float32)
    iota = np.arange(128, dtype=np.float32).reshape(128, 1)
    utri = np.triu(np.ones((128, 128), np.float32), k=1)
    return cossin, ident, iota, utri


def _bias_table(rpb):
    cq = np.arange(64)
    c_start = np.clip(cq - 8, 0, 48)
    band = (cq[None, :] >= c_start[:, None]) & (cq[None, :] < c_start[:, None] + 16)
    dc = np.clip(cq[None, :] - cq[:, None], -15, 15) + 15
    tab = np.full((64, 8, NTB, 64), -1e30, np.float32)
    for h in range(8):
        for dr in range(15):
            vals = rpb[h, dr][dc]
            tab[:, h, 1 + dr, :] = np.where(band, vals, np.float32(-1e30))
        tab[:, h, 18, :] = tab[:, h, 1 + 3, :]
        tab[:, h, 19, :] = tab[:, h, 1 + 10, :]
    return tab.reshape(64, 8 * NTB * 64)


def _prepare(inputs):
    f = lambda k: np.asarray(inputs[k], np.float32)
    w_in = f("w_in")[0]
    K_OFF, V_OFF, LX_OFF, Q_OFF, LG_OFF, GA_OFF, GB_OFF = 0, 512, 1024, 2048, 2560, 3584, 4608
    sw = _swap_cols()
    wqkv = []
    for hp in range(4):
        cols = []
        for base in (Q_OFF, K_OFF):
            plain = np.concatenate([base + (2 * hp + e) * 64 + np.arange(64) for e in range(2)])
            swp = np.concatenate([base + (2 * hp + e) * 64 + sw for e in range(2)])
            cols += [plain, swp]
        cols.append(V_OFF + hp * 128 + np.arange(128))
        wqkv.append(_kp(w_in[:, np.concatenate(cols)]).reshape(128, 8 * 640))
    wqkv = np.stack(wqkv)
    wlxlg = np.stack([_kp(w_in[:, np.concatenate([LX_OFF + n * 128 + np.arange(128), LG_OFF + n * 128 + np.arange(128)])]
                          ).reshape(128, 8 * 256) for n in range(8)])
    wgagb = np.stack([_kp(w_in[:, np.concatenate([GA_OFF + n * 128 + np.arange(128), GB_OFF + n * 128 + np.arange(128)])]
                          ).reshape(128, 8 * 256) for n in range(8)])
    wua = _kp(f("w_up_attn")[0])
    wul = _kp(f("w_up_lru")[0])
    wup = np.stack([np.concatenate([wua[:, :, n * 128:(n + 1) * 128], wul[:, :, n * 128:(n + 1) * 128]], axis=1
                                   ).reshape(128, 12 * 128) for n in range(8)])
    wout = _kp(f("w_out")[0]).reshape(128, 8 * D)
    wa = f("lru_wa")[0]
    wx = f("lru_wx")[0]
    lruw = np.stack([wa[0], wa[1], wx[0], wx[1]])
    lruw = np.ascontiguousarray(lruw.transpose(2, 0, 1, 3)).reshape(128, 4 * 8 * 128)
    vecs = np.concatenate([
        _fm(f("g_mix")[0]), _fm(f("g_ffn")[0]),
        np.concatenate([_fm(f("conv_w")[0][j]) for j in range(4)], axis=1),
        _fm(f("conv_b")[0]),
        np.concatenate([_fm(f("lru_ba")[0][d_]) for d_ in range(2)], axis=1),
        np.concatenate([_fm(f("lru_bx")[0][d_]) for d_ in range(2)], axis=1),
        np.concatenate([_fm(f("lru_lambda")[0][d_]) for d_ in range(2)], axis=1),
        _fm(f("b_mod")[0]),
    ], axis=1)
    assert vecs.shape == (128, NV)
    wmod = _kp(f("w_mod")[0])
    wr = _kp(np.concatenate([f("router_group_w")[0], f("router_expert_w")[0]], axis=1)).reshape(128, 8 * 36)
    brb = np.ascontiguousarray(np.broadcast_to(
        np.concatenate([f("router_group_b")[0], f("router_expert_b")[0]])[None, :], (128, 36)))
    gfb = np.ascontiguousarray(np.broadcast_to(f("g_final")[None, :], (128, D)))
    gffnb = np.ascontiguousarray(np.broadcast_to(f("g_ffn")[0][None, :], (128, D)))
    w1 = f("expert_w_gate")[0]
    w3 = f("expert_w_up")[0]
    w2 = f("expert_w_down")[0]
    w1h = np.ascontiguousarray(w1.reshape(32, 8, 128, 512).transpose(0, 2, 1, 3)).reshape(32 * 128, 8 * 512)
    w3h = np.ascontiguousarray(w3.reshape(32, 8, 128, 512).transpose(0, 2, 1, 3)).reshape(32 * 128, 8 * 512)
    w2h = np.ascontiguousarray(w2.reshape(32, 4, 128, 1024).transpose(0, 2, 1, 3)).reshape(32 * 128, 4 * 1024)
    cossin, ident, iota, utri = _host_consts()
    btab = _bias_table(f("rpb")[0])
    shared = dict(wmod=wmod, vecs=vecs, wqkv=wqkv, wlxlg=wlxlg, wgagb=wgagb, wup=wup, wout=wout, lruw=lruw,
                  cossin=cossin, btab=btab, wr=wr, brb=brb, gfb=gfb, gffnb=gffnb, w1h=w1h, w3h=w3h, w2h=w2h,
                  ident=ident, iota=iota, utri=utri)
    x = f("x")
    ctx = f("ctx")
    c = f("c")
    c_ctx = f("c_ctx")
    in_maps = []
    for core in range(NCORES):
        b0 = core * NB
        cs = np.stack([c[b0], c[b0 + 1], c_ctx], axis=-1)
        cs = np.ascontiguousarray(cs.reshape(8, 128, 3).transpose(1, 0, 2)).reshape(128, 24)
        m = dict(shared)
        m["xin"] = np.ascontiguousarray(x[b0:b0 + NB])
        m["ctxin"] = np.ascontiguousarray(ctx[b0:b0 + NB])
        m["cs"] = cs
        in_maps.append(m)
    return in_maps


def kernel(**inputs):
    in_maps = _prepare(inputs)
    nc = build_program()
    res = run_bass_kernel_spmd(nc, in_maps, core_ids=list(range(NCORES)))
    out = np.concatenate([np.asarray(r["out"], np.float32) for r in res.results], axis=0)
    return out
```

```python
import numpy as np
import concourse.bass as bass
import concourse.mybir as mybir
from concourse.bass_utils import run_bass_kernel_spmd
from contextlib import ExitStack

F32 = mybir.dt.float32
BF16 = mybir.dt.bfloat16
I32 = mybir.dt.int32
AF = mybir.ActivationFunctionType
ALU = mybir.AluOpType
AX = mybir.AxisListType

D = 1024
L = 2048
C = 256
NB = 2
NCORES = 8
LC = L + C
NTOK = NB * L
NBLK = 64
NROWS = NBLK * 256
EPS = 1e-6

V_GMIX, V_GFFN, V_CONVW, V_CONVB, V_BA, V_BX, V_LAM, V_BMOD, NV = 0, 8, 16, 48, 56, 72, 88, 104, 152
NTB = 21

DEBUG = {}
import os as _os
ATT_NHP = int(_os.environ.get("ATT_NHP", "4"))
ATT_NUNITS = int(_os.environ.get("ATT_NUNITS", "8"))
ATT_NROWS = int(_os.environ.get("ATT_NROWS", "8"))
ATT_NORM = int(_os.environ.get("ATT_NORM", "1"))
ATT_PARTS = int(_os.environ.get("ATT_PARTS", "31"))
STOP_AFTER = None


class Buf:
    __slots__ = ("name", "w", "wx", "r", "dsem", "dcnt", "grp")

    def __init__(self, name=""):
        self.name = name
        self.w = None
        self.wx = {}
        self.r = {}
        self.dsem = {}
        self.dcnt = {}
        self.grp = False


class Sync:
    ROLL = 30000

    def __init__(self, nc, es):
        self.nc = nc
        self.es = es
        self.eng = {"pe": nc.tensor, "act": nc.scalar, "dve": nc.vector, "pool": nc.gpsimd, "sp": nc.sync}
        self.sem = {}
        self.cnt = {}
        self.waited = {k: {} for k in self.eng}
        self.nsem = 0
        self.pe_sems = set()
        for k in self.eng:
            self._newsem(k)
        self.pend = []
        self.dbufs = []
        self.nops = {k: 0 for k in self.eng}
        self.nwaits = 0

    def _alloc(self, name):
        self.nsem += 1
        return self.es.enter_context(self.nc.semaphore(f"{name}{self.nsem}"))

    def _newsem(self, k):
        self.sem[k] = self._alloc("e" + k)
        self.cnt[k] = 0
        if k == "pe":
            self.pe_sems.add(id(self.sem[k]))

    def _wait(self, e, ev):
        semh, val = ev
        assert val is not None, "dependency on an unsignalled PE op"
        key = id(semh)
        if self.waited[e].get(key, 0) < val:
            self.eng[e].wait_ge(semh, val)
            self.waited[e][key] = val
            self.nwaits += 1

    def _dep1(self, e, ev, acc):
        if e == "pe" and (ev[1] is None or id(ev[0]) in self.pe_sems):
            return
        assert ev[1] is not None, "dependency on an unsignalled PE op"
        k = id(ev[0])
        if k not in acc or acc[k][1] < ev[1]:
            acc[k] = ev

    def _deps(self, e, reads, writes, group=False):
        acc = {}
        for b in reads:
            if b.w is not None:
                self._dep1(e, b.w, acc)
            for ev in b.wx.values():
                self._dep1(e, ev, acc)
        for b in writes:
            if not (group and b.grp):
                if b.w is not None:
                    self._dep1(e, b.w, acc)
                for ev in b.wx.values():
                    self._dep1(e, ev, acc)
            for ev in b.r.values():
                self._dep1(e, ev, acc)
        for ev in acc.values():
            self._wait(e, ev)

    def _mark(self, ev, reads, writes, key, group=False):
        for b in reads:
            b.r[key] = ev
        for b in writes:
            if group and b.grp:
                b.wx[key] = ev
            elif group:
                b.w = None
                b.wx = {key: ev}
            else:
                b.w = ev
                b.wx = {}
            b.grp = group
            b.r = {}

    def op(self, e, fn, reads=(), writes=(), sig=True):
        self._deps(e, reads, writes)
        ins = fn()
        self.nops[e] += 1
        if sig:
            if self.cnt[e] >= self.ROLL:
                self._newsem(e)
            self.cnt[e] += 1
            ins.then_inc(self.sem[e], 1)
            ev = [self.sem[e], self.cnt[e]]
            if e == "pe":
                for p in self.pend:
                    p[0] = self.sem[e]
                    p[1] = self.cnt[e]
                self.pend = []
        else:
            assert e == "pe"
            ev = [self.sem[e], None]
            self.pend.append(ev)
        self._mark(ev, reads, writes, e)
        return ins

    def _dma_common(self, q, issue, reads, writes, group, semof):
        d = semof if semof is not None else writes[0]
        c = "sw" if q == "pool" else "hw"
        if c not in d.dsem:
            d.dsem[c] = self._alloc("d")
            d.dcnt[c] = 0
            self.dbufs.append((d, c))
        self._deps(q, reads, writes, group=group)
        ins = issue()
        self.nops[q] += 1
        d.dcnt[c] += 16
        ins.then_inc(d.dsem[c], 16)
        ev = [d.dsem[c], d.dcnt[c]]
        self._mark(ev, reads, writes, id(d.dsem[c]), group=group)
        return ins

    def dma(self, q, out, in_, reads=(), writes=(), group=False, semof=None):
        return self._dma_common(q, lambda: self.eng[q].dma_start(out=out, in_=in_), reads, writes, group, semof)

    def dma_ind(self, out, out_off, in_, in_off, bound, reads=(), writes=(), group=False, semof=None, bcheck=None):
        def issue():
            if bcheck is not None:
                return self.nc.gpsimd.indirect_dma_start(out=out, out_offset=out_off, in_=in_, in_offset=in_off,
                                                         bounds_check=bcheck, oob_is_err=False)
            return self.nc.gpsimd.indirect_dma_start(out=out, out_offset=out_off, in_=in_, in_offset=in_off)
        return self._dma_common("pool", issue, reads, writes, group, semof)

    def barrier(self):
        assert not self.pend
        evs = [[self.sem[k], self.cnt[k]] for k in self.eng if k != "sp" and self.cnt[k] > 0]
        evs += [[b.dsem[c], b.dcnt[c]] for (b, c) in self.dbufs if b.dcnt[c] > 0]
        for ev in evs:
            self._wait("sp", ev)
        if self.cnt["sp"] >= self.ROLL:
            self._newsem("sp")
        self.cnt["sp"] += 1
        self.nc.sync.nop().then_inc(self.sem["sp"], 1)
        ev = [self.sem["sp"], self.cnt["sp"]]
        for k in self.eng:
            if k != "sp":
                self._wait(k, ev)


def build_program():
    nc = bass.Bass("TRN2", target_bir_lowering=False)

    def din(name, shape, dt=F32):
        return nc.dram_tensor(name, list(shape), dt, kind="ExternalInput").ap()

    xin = din("xin", [NB, L, D])
    ctxin = din("ctxin", [NB, C, D])
    cs_d = din("cs", [128, 24])
    wmod_d = din("wmod", [128, 8, 6 * D])
    vecs_d = din("vecs", [128, NV])
    wqkv_d = din("wqkv", [4, 128, 8 * 640])
    wlxlg_d = din("wlxlg", [8, 128, 8 * 256])
    wgagb_d = din("wgagb", [8, 128, 8 * 256])
    wup_d = din("wup", [8, 128, 12 * 128])
    wout_d = din("wout", [128, 8 * D])
    lruw_d = din("lruw", [128, 4 * 8 * 128])
    cossin_d = din("cossin", [128, 2 * L])
    btab_d = din("btab", [64, 8 * NTB * 64])
    wr_d = din("wr", [128, 8 * 36])
    brb_d = din("brb", [128, 36])
    gfb_d = din("gfb", [128, D])
    gffnb_d = din("gffnb", [128, D])
    w1_d = din("w1h", [32 * 128, 8 * 512])
    w3_d = din("w3h", [32 * 128, 8 * 512])
    w2_d = din("w2h", [32 * 128, 4 * 1024])
    ident_d = din("ident", [128, 128])
    iota_d = din("iota", [128, 1])
    utri_d = din("utri", [128, 128])
    out_d = nc.dram_tensor("out", [NB, L, D], F32, kind="ExternalOutput").ap()
    x1_d = nc.dram_tensor("x1s", [NTOK, D], F32, kind="Internal").ap()
    h2_d = nc.dram_tensor("h2s", [NTOK, D], BF16, kind="Internal").ap()
    xs_d = nc.dram_tensor("xss", [NROWS, D], BF16, kind="Internal").ap()
    ys_d = nc.dram_tensor("yss", [NROWS, D], BF16, kind="Internal").ap()
    dbg_d = {}
    for name, (shape, dt) in DEBUG.items():
        dbg_d[name] = nc.dram_tensor("dbg_" + name, list(shape), dt, kind="ExternalOutput").ap()

    with ExitStack() as es:
        S = Sync(nc, es)
        uid = [0]

        def sb(es_, shape, dt, name="t"):
            uid[0] += 1
            return es_.enter_context(nc.sbuf_tensor(f"{name}{uid[0]}", list(shape), dt))

        def ps(es_, shape, dt, name="p"):
            uid[0] += 1
            return es_.enter_context(nc.psum_tensor(f"{name}{uid[0]}", list(shape), dt))

        def pe_mm(out, lhsT, rhs, start, stop, reads, writes, sig=None):
            if sig is None:
                sig = stop
            return S.op("pe", lambda: nc.tensor.matmul(out, lhsT, rhs, start=start, stop=stop),
                        reads, writes, sig)

        def pe_tr(out, in_, ident, reads, writes, sig):
            return S.op("pe", lambda: nc.tensor.transpose(out, in_, ident), reads, writes, sig)

        def act(out, in_, func, reads, writes, **kw):
            return S.op("act", lambda: nc.scalar.activation(out=out, in_=in_, func=func, **kw), reads, writes)

        def V(e, fn, reads, writes):
            return S.op(e, fn, reads, writes)

        dbg_buf = Buf("dbg")

        def dump(name, ap, reads):
            if name in dbg_d:
                S.dma("sp", dbg_d[name], ap, reads=reads, writes=[dbg_buf], group=True, semof=Buf("dump_" + name))

        identf = sb(es, [128, 128], F32, "identf"); b_identf = Buf()
        identb = sb(es, [128, 128], BF16, "identb"); b_identb = Buf()
        onesf = sb(es, [128, 128], F32, "onesf"); b_onesf = Buf()
        onesb = sb(es, [128, 128], BF16, "onesb"); b_onesb = Buf()
        utri = sb(es, [128, 128], BF16, "utri"); b_utri = Buf()
        iota = sb(es, [128, 1], F32, "iota"); b_iota = Buf()
        epst = sb(es, [128, 1], F32, "eps"); b_eps = Buf()
        vecs = sb(es, [128, NV], F32, "vecs"); b_vecs = Buf()
        modfm = sb(es, [128, 48, 3], F32, "modfm"); b_modfm = Buf()
        A1 = sb(es, [128, 8, 3], F32, "A1"); b_A1 = Buf()
        lrup = sb(es, [128, 4, 16], F32, "lrup"); b_lrup = Buf()
        S.dma("sp", identf[:], ident_d, writes=[b_identf])
        S.dma("pool", identb[:], ident_d, writes=[b_identb])
        S.dma("pool", utri[:], utri_d, writes=[b_utri])
        S.dma("sp", iota[:], iota_d, writes=[b_iota])
        S.dma("sp", vecs[:], vecs_d, writes=[b_vecs])
        V("dve", lambda: nc.vector.memset(onesf[:], 1.0), [], [b_onesf])
        V("dve", lambda: nc.vector.memset(onesb[:], 1.0), [], [b_onesb])
        V("dve", lambda: nc.vector.memset(epst[:], EPS), [], [b_eps])

        with ExitStack() as es0:
            csb = sb(es0, [128, 24], F32, "cs"); b_cs = Buf()
            scs = sb(es0, [128, 24], BF16, "scs"); b_scs = Buf()
            wm = [sb(es0, [128, 8, 512], BF16, "wm") for _ in range(3)]
            b_wm = [Buf(), Buf(), Buf()]
            psmod = ps(es0, [128, 144], F32, "psmod"); b_psmod = Buf()
            S.dma("sp", csb[:], cs_d, writes=[b_cs])
            act(scs[:], csb[:], AF.Silu, [b_cs], [b_scs])
            for cb in range(12):
                w = wm[cb % 3]; bw = b_wm[cb % 3]
                S.dma("pool", w[:], wmod_d[:, :, cb * 512:(cb + 1) * 512], writes=[bw])
                for cc in range(4):
                    col = cb * 4 + cc
                    for k in range(8):
                        pe_mm(psmod[:, col * 3:(col + 1) * 3], w[:, k, cc * 128:(cc + 1) * 128],
                              scs[:, k * 3:(k + 1) * 3], k == 0, k == 7, [bw, b_scs], [b_psmod],
                              sig=(k == 7 and cc == 3))
            V("dve", lambda: nc.vector.tensor_tensor(
                out=modfm[:], in0=psmod[:, :].rearrange("p (a b) -> p a b", b=3),
                in1=vecs[:, V_BMOD:V_BMOD + 48].unsqueeze(2).to_broadcast([128, 48, 3]), op=ALU.add),
              [b_psmod, b_vecs], [b_modfm])
            V("dve", lambda: nc.vector.scalar_tensor_tensor(
                out=A1[:], in0=modfm[:, 8:16, :], scalar=1.0,
                in1=vecs[:, V_GMIX:V_GMIX + 8].unsqueeze(2).to_broadcast([128, 8, 3]),
                op0=ALU.add, op1=ALU.mult), [b_modfm, b_vecs], [b_A1])
            act(lrup[:, 2, :], vecs[:, V_LAM:V_LAM + 16], AF.Exp, [b_vecs], [b_lrup], scale=-1.0)
            act(lrup[:, 3, :], lrup[:, 2, :], AF.Ln, [b_lrup], [b_lrup], bias=1.0)
            V("dve", lambda: nc.vector.tensor_scalar(out=lrup[:, 0, :], in0=lrup[:, 3, :], scalar1=-8.0, scalar2=None,
                                                     op0=ALU.mult), [b_lrup], [b_lrup])
            V("dve", lambda: nc.vector.tensor_scalar(out=lrup[:, 1, :], in0=lrup[:, 3, :], scalar1=-16.0, scalar2=None,
                                                     op0=ALU.mult), [b_lrup], [b_lrup])
            dump("modfm", modfm[:, :, :].rearrange("p a b -> p (a b)"), [b_modfm])
            S.barrier()

        def bcast_row(es_, v, j, psb, b_psb, diag, b_diag):
            for k in range(8):
                V("dve", lambda: nc.vector.tensor_scalar(out=diag[k % 2][:], in0=identf[:],
                                                         scalar1=modfm[:, v * 8 + k, j:j + 1], scalar2=None,
                                                         op0=ALU.mult), [b_identf, b_modfm], [b_diag[k % 2]])
                pe_mm(psb[:, k * 128:(k + 1) * 128], onesf[:], diag[k % 2][:], True, True,
                      [b_onesf, b_diag[k % 2]], [b_psb], sig=True)

        Mall = sb(es, [128, 32, 32], BF16, "Mall"); b_Mall = Buf()
        oh1all = sb(es, [128, 32, 32], BF16, "oh1"); b_oh1 = Buf()
        gates = sb(es, [128, 32, 2], F32, "gates"); b_gates = Buf()
        dest_f = sb(es, [128, 32, 2], F32, "destf"); b_destf = Buf()
        dest_i = sb(es, [128, 64], I32, "desti"); b_desti = Buf()
        b_x1d = Buf("x1d"); b_h2d = Buf("h2d"); b_xsd = Buf("xsd"); b_ysd = Buf("ysd"); b_outd = Buf("outd")
        zt = sb(es, [128, 2, D], BF16, "zt"); b_zt = Buf()
        V("dve", lambda: nc.vector.memset(zt[:], 0.0), [], [b_zt])
        for blk in range(NBLK):
            S.dma("sp", xs_d[blk * 256:(blk + 1) * 256, :].rearrange("(a p) d -> p a d", p=128), zt[:],
                  reads=[b_zt], writes=[b_xsd], group=True, semof=b_zt)

        for b in range(NB):
            with ExitStack() as esb:
                hT = sb(esb, [128, 8, LC], BF16, "hT")
                b_hT = [[Buf(f"hT{i}a"), Buf(f"hT{i}b")] for i in range(5)]
                oatt = sb(esb, [128, 4, L], BF16, "oatt")
                b_oatt = [[Buf() for _ in range(4)] for _ in range(4)]

                with ExitStack() as es1:
                    xt = [sb(es1, [128, D], F32, "xt") for _ in range(3)]
                    b_xt = [Buf() for _ in range(3)]
                    junk = sb(es1, [128, D], BF16, "junk"); b_junk = Buf()
                    xn = [sb(es1, [128, D], BF16, "xn") for _ in range(8)]
                    b_xn = [Buf() for _ in range(8)]
                    st = sb(es1, [128, 18, 3], F32, "st"); b_st = [Buf() for _ in range(18)]
                    pst = [ps(es1, [128, 512], BF16, "pst") for _ in range(4)]
                    b_pst = [Buf() for _ in range(4)]
                    ti = 0
                    for grp in range(5):
                        ntile = 4 if grp < 4 else 2
                        jmod = b if grp < 4 else 2
                        for i in range(ntile):
                            t = grp * 4 + i
                            xb_, bx_ = xt[ti % 3], b_xt[ti % 3]
                            src = xin[b, t * 128:(t + 1) * 128, :] if grp < 4 else ctxin[b, i * 128:(i + 1) * 128, :]
                            S.dma("sp", xb_[:], src, writes=[bx_])
                            act(junk[:], xb_[:], AF.Square, [bx_], [b_junk, b_st[t]], accum_out=st[:, t, 0:1])
                            act(st[:, t, 1:2], st[:, t, 0:1], AF.Sqrt, [b_st[t], b_eps], [b_st[t]],
                                scale=1.0 / D, bias=epst[:, 0:1])
                            V("dve", lambda: nc.vector.reciprocal(out=st[:, t, 2:3], in_=st[:, t, 1:2]),
                              [b_st[t]], [b_st[t]])
                            xi = (grp % 2) * 4 + i
                            act(xn[xi][:], xb_[:], AF.Copy, [bx_, b_st[t]], [b_xn[xi]], scale=st[:, t, 2:3])
                            ti += 1
                        for k in range(8):
                            pp, bp = pst[k % 4], b_pst[k % 4]
                            for i in range(ntile):
                                xi = (grp % 2) * 4 + i
                                pe_tr(pp[:, i * 128:(i + 1) * 128], xn[xi][:, k * 128:(k + 1) * 128], identb[:],
                                      [b_xn[xi], b_identb], [bp], sig=(i == ntile - 1))
                            n = ntile * 128
                            dst = hT[:, k, grp * 512:grp * 512 + n]
                            if k % 2 == 0:
                                V("dve", lambda: nc.vector.tensor_scalar(
                                    out=dst, in0=pp[:, 0:n], scalar1=A1[:, k, jmod:jmod + 1],
                                    scalar2=modfm[:, k, jmod:jmod + 1], op0=ALU.mult, op1=ALU.add),
                                  [bp, b_A1, b_modfm], [b_hT[grp][0]])
                            else:
                                act(dst, pp[:, 0:n], AF.Identity, [bp, b_A1, b_modfm], [b_hT[grp][1]],
                                    scale=A1[:, k, jmod:jmod + 1], bias=modfm[:, k, jmod:jmod + 1])
                    if b == 0:
                        dump("hT", hT[:, :, :].rearrange("p a b -> p (a b)"), [x for l_ in b_hT for x in l_])
                    S.barrier()
                if STOP_AFTER == "s1":
                    break

                with ExitStack() as es2:
                    cs_t = sb(es2, [128, 2 * L], F32, "cossin"); b_cst = Buf()
                    btab = sb(es2, [128, 8, NTB * 64], BF16, "btab"); b_btab = Buf()
                    S.dma("sp", cs_t[:], cossin_d, writes=[b_cst])
                    S.dma("pool", btab[0:64, :, :].rearrange("p a b -> p (a b)"), btab_d, writes=[b_btab], group=True)
                    S.dma("pool", btab[64:128, :, :].rearrange("p a b -> p (a b)"), btab_d, writes=[b_btab], group=True)
                    wq = [sb(es2, [128, 8, 640], BF16, "wq") for _ in range(1)]; b_wq = [Buf()]
                    Qr = [sb(es2, [128, L], BF16, "Qr") for _ in range(1)]
                    Qp = [sb(es2, [128, L], BF16, "Qp") for _ in range(1)]
                    Kr = [sb(es2, [128, L], BF16, "Kr") for _ in range(1)]
                    Kc = [sb(es2, [128, C], BF16, "Kc") for _ in range(1)]
                    Vx = [sb(es2, [128, 18, 192], BF16, "Vx") for _ in range(1)]
                    b_Q = [[Buf() for _ in range(4)] for _ in range(1)]
                    b_Qp = [[Buf() for _ in range(4)] for _ in range(1)]
                    b_K = [Buf()]
                    b_Kc = [Buf()]
                    b_V = [[Buf() for _ in range(5)]]
                    t1 = [sb(es2, [128, 512], F32, "t1") for _ in range(2)]; b_t1 = [Buf(), Buf()]
                    t2 = [sb(es2, [128, 512], F32, "t2") for _ in range(2)]; b_t2 = [Buf(), Buf()]
                    PTc = [sb(es2, [128, 512], BF16, "PTc") for _ in range(2)]; b_PTc = [Buf(), Buf()]
                    PT = [sb(es2, [128, 320], BF16, "PT") for _ in range(3)]; b_PT = [Buf() for _ in range(3)]
                    rc = [sb(es2, [128, 512], F32, "rc") for _ in range(2)]; b_rc = [Buf(), Buf()]
                    psP = [ps(es2, [128, 512], F32, "psP") for _ in range(2)]; b_psP = [Buf(), Buf()]
                    psSc = [ps(es2, [128, 512], F32, "psSc") for _ in range(2)]; b_psSc = [Buf(), Buf()]
                    psS = [ps(es2, [128, 512], F32, "psS") for _ in range(2)]; b_psS = [Buf(), Buf()]
                    psO = [ps(es2, [128, 512], F32, "psO") for _ in range(2)]; b_psO = [Buf(), Buf()]
                    V("dve", lambda: nc.vector.memset(Vx[0][:, :, 64:128], 1.0), [], b_V[0])
                    pcnt = [0]

                    def nextP():
                        i = pcnt[0] % 2
                        pcnt[0] += 1
                        return psP[i], b_psP[i]

                    cnt_t = [0]
                    for hp in range(ATT_NHP):
                        par = 0
                        w = wq[par]; bw = b_wq[par]
                        S.dma("pool", w[:, :, :].rearrange("p a b -> p (a b)"), wqkv_d[hp], writes=[bw])
                        for blk in range(4 if ATT_PARTS & 1 else 0):
                            tok = slice(blk * 512, (blk + 1) * 512)
                            for which in range(2):
                                c0 = which * 256
                                pa, bpa = nextP()
                                for k in range(8):
                                    pe_mm(pa[:], w[:, k, c0:c0 + 128], hT[:, k, tok], k == 0, k == 7,
                                          [bw, *b_hT[blk]], [bpa])
                                pb, bpb = nextP()
                                for k in range(8):
                                    pe_mm(pb[:], w[:, k, c0 + 128:c0 + 256], hT[:, k, tok], k == 0, k == 7,
                                          [bw, *b_hT[blk]], [bpb])
                                ii = cnt_t[0] % 2
                                cnt_t[0] += 1
                                sc_ = 0.125 if which == 0 else 1.0
                                dstb = b_Q[par][blk] if which == 0 else b_K[par]
                                dst = (Qr if which == 0 else Kr)[par][:, tok]
                                V("dve", lambda: nc.vector.scalar_tensor_tensor(
                                    out=t1[ii][:], in0=pa[:], scalar=sc_, in1=cs_t[:, tok], op0=ALU.mult, op1=ALU.mult),
                                  [bpa, b_cst], [b_t1[ii]])
                                if which == 0 and (ATT_PARTS & 16):
                                    V("dve", lambda: nc.vector.tensor_scalar(out=Qp[par][:, tok], in0=pa[:], scalar1=0.125, scalar2=None,
                                                                             op0=ALU.mult), [bpa], [b_Qp[par][blk]])
                                V("dve", lambda: nc.vector.scalar_tensor_tensor(
                                    out=t2[ii][:], in0=pb[:], scalar=sc_, in1=cs_t[:, L + blk * 512:L + (blk + 1) * 512],
                                    op0=ALU.mult, op1=ALU.mult), [bpb, b_cst], [b_t2[ii]])
                                V("pool" if ATT_PARTS & 8 else "dve", lambda: (nc.gpsimd if ATT_PARTS & 8 else nc.vector).tensor_tensor(out=dst, in0=t1[ii][:], in1=t2[ii][:], op=ALU.add),
                                  [b_t1[ii], b_t2[ii]], [dstb])
                        if ATT_PARTS & 2:
                            pa, bpa = nextP()
                            for k in range(8):
                                pe_mm(pa[:, 0:C], w[:, k, 256:384], hT[:, k, L:LC], k == 0, k == 7, [bw, *b_hT[4]], [bpa])
                            act(Kc[par][:], pa[:, 0:C], AF.Copy, [bpa], [b_Kc[par]])
                        for g4 in range(5 if ATT_PARTS & 4 else 0):
                            nch = 4 if g4 < 4 else 2
                            pa, bpa = nextP()
                            for i in range(nch):
                                ch = g4 * 4 + i
                                for k in range(8):
                                    pe_mm(pa[:, i * 128:(i + 1) * 128], hT[:, k, ch * 128:(ch + 1) * 128],
                                          w[:, k, 512:640], k == 0, k == 7, [bw, *b_hT[g4]], [bpa],
                                          sig=(k == 7 and i == nch - 1))
                            src = pa[:, 0:nch * 128].rearrange("p (c a d) -> p c a d", a=2, d=64)
                            dstv = Vx[par][:, g4 * 4:g4 * 4 + nch, :].rearrange("p c (a d) -> p c a d", d=64)[:, :, 0::2, :]
                            if g4 % 2 == 0:
                                act(dstv, src, AF.Copy, [bpa], [b_V[par][g4]])
                            else:
                                V("dve", lambda: nc.vector.tensor_copy(out=dstv, in_=src), [bpa], [b_V[par][g4]])

                        if b == 0 and hp == 0:
                            dump("Qp", Qp[0][:], b_Qp[0])
                            dump("Qr", Qr[0][:], b_Q[0])
                            dump("Kr", Kr[0][:], b_K)
                            dump("Kc", Kc[0][:], b_Kc)
                            dump("Vx", Vx[0][:, :, :].rearrange("p a b -> p (a b)"), b_V[0])
                        units = []
                        for e in range(2):
                            for qb in range(4):
                                units.append((e, qb))
                        ucnt = [0]
                        for (e, qb) in units[int(_os.environ.get("ATT_USTART", "0")):][:ATT_NUNITS]:
                            h = hp * 2 + e
                            pr = slice(64 * e, 64 * e + 64)
                            qs = slice(qb * 512, (qb + 1) * 512)
                            vcols = slice(0, 128) if e == 0 else slice(64, 192)
                            oi = ucnt[0] % 2
                            ucnt[0] += 1
                            pO, bO = psO[oi], b_psO[oi]
                            for c in range(2):
                                pS, bS = psSc[c], b_psSc[c]
                                pe_mm(pS[:], Kc[par][pr, c * 128:(c + 1) * 128], Qp[par][pr, qs], True, True,
                                      [b_Kc[par], b_Qp[par][qb]], [bS])
                                act(PTc[c][:], pS[:], AF.Exp, [bS], [b_PTc[c]])
                            if b == 0 and hp == 0 and e == 0 and qb == 0:
                                dump("PTc", PTc[0][:], [b_PTc[0]])
                            for c in range(2):
                                pe_mm(pO[:], Vx[par][:, 16 + c, vcols], PTc[c][:], c == 0, False,
                                      [b_V[par][4], b_PTc[c]], [bO], sig=(c == 1))
                            rows = list(range(qb * 8, qb * 8 + ATT_NROWS))
                            plan = []
                            for r in rows:
                                rs = min(max(r - 4, 0), 24)
                                dr0 = rs - r + 7
                                chunks = []
                                if rs % 2 == 0:
                                    for j in range(4):
                                        chunks.append(((rs + 2 * j) // 2, 1 + dr0 + 2 * j))
                                else:
                                    assert dr0 == 3
                                    c0 = (rs - 1) // 2
                                    chunks.append((c0, 17))
                                    for j in range(1, 4):
                                        chunks.append((c0 + j, dr0 + 2 * j))
                                    chunks.append((c0 + 4, 19))
                                plan.append((r, chunks))

                            def emit_qk(idx):
                                r, chunks = plan[idx]
                                si = idx % 2
                                pS, bS = psS[si], b_psS[si]
                                qcol = slice(r * 64, (r + 1) * 64)
                                for j, (kc, blk0) in enumerate(chunks):
                                    o = pS[:, j * 64:(j + 1) * 64]
                                    pe_mm(o, Kr[par][pr, kc * 128:(kc + 1) * 128], Qr[par][pr, qcol], True, False,
                                          [b_K[par], b_Q[par][qb]], [bS], sig=False)
                                    lt = btab[pr, h, blk0 * 64:(blk0 + 2) * 64]
                                    pe_mm(o, lt, identb[pr, pr], False, True, [b_btab, b_identb], [bS],
                                          sig=(j == len(chunks) - 1))
                                n = len(chunks) * 64
                                pi = idx % 3
                                act(PT[pi][:, 0:n], pS[:, 0:n], AF.Exp, [bS], [b_PT[pi]])

                            def emit_pv(idx):
                                r, chunks = plan[idx]
                                pi = idx % 3
                                rr = r - qb * 8
                                for j, (kc, _) in enumerate(chunks):
                                    last = (j == len(chunks) - 1)
                                    pe_mm(pO[:, rr * 64:(rr + 1) * 64], Vx[par][:, kc, vcols], PT[pi][:, j * 64:(j + 1) * 64],
                                          False, last and idx == len(plan) - 1, [b_V[par][kc // 4], b_PT[pi]], [bO], sig=last)

                            if plan:
                                emit_qk(0)
                            for idx in range(len(plan)):
                                if idx + 1 < len(plan):
                                    emit_qk(idx + 1)
                                emit_pv(idx)
                            dn = slice(64, 128) if e == 0 else slice(0, 64)
                            if not ATT_NORM:
                                continue
                            V("dve", lambda: nc.vector.reciprocal(out=rc[oi][pr, :], in_=pO[dn, :]), [bO], [b_rc[oi]])
                            V("dve", lambda: nc.vector.tensor_tensor(out=oatt[pr, hp, qs], in0=pO[pr, :], in1=rc[oi][pr, :],
                                                                     op=ALU.mult), [bO, b_rc[oi]], [b_oatt[hp][qb]])
                    if b == 0:
                        dump("oatt", oatt[:, :, :].rearrange("p a b -> p (a b)"), [x for l_ in b_oatt for x in l_])
                    S.barrier()
                if STOP_AFTER == "s2":
                    break

                olru = sb(esb, [128, 8, L], BF16, "olru")
                b_olru = [Buf() for _ in range(8)]
                with ExitStack() as es3:
                    TL = LC + 3
                    wl = [sb(es3, [128, 8, 256], BF16, "wl") for _ in range(2)]; b_wl = [Buf(), Buf()]
                    lw = sb(es3, [128, 4, 8, 128], BF16, "lw"); b_lw = Buf()
                    S.dma("pool", lw[:, :, :, :].rearrange("p a b c -> p (a b c)"), lruw_d, writes=[b_lw])
                    LXp = sb(es3, [128, TL + 3], F32, "LXp"); b_LXp = Buf()
                    xc = sb(es3, [128, TL], F32, "xc"); b_xc = Buf()
                    xcb = [sb(es3, [128, TL], BF16, "xcb") for _ in range(2)]; b_xcb = [Buf(), Buf()]
                    av = sb(es3, [128, TL], F32, "av"); b_av = Buf()
                    wv = sb(es3, [128, TL], F32, "wv"); b_wv = Buf()
                    iv = sb(es3, [128, TL], F32, "iv"); b_iv = Buf()
                    hv = [sb(es3, [128, TL], F32, "hv") for _ in range(2)]; b_hv = [Buf(), Buf()]
                    gl = sb(es3, [128, L], BF16, "gl"); b_gl = Buf()
                    psA = [ps(es3, [128, 512], F32, "psA") for _ in range(4)]; b_psA = [Buf() for _ in range(4)]
                    pacnt = [0]

                    def nextA():
                        i = pacnt[0] % 4
                        pacnt[0] += 1
                        return psA[i], b_psA[i]

                    V("dve", lambda: nc.vector.memset(LXp[:], 0.0), [], [b_LXp])

                    def load_wl(n):
                        S.dma("pool", wl[n % 2][:, :, :].rearrange("p a b -> p (a b)"), wlxlg_d[n], writes=[b_wl[n % 2]])

                    def lx_conv(n):
                        w = wl[n % 2]; bw = b_wl[n % 2]
                        for blk in range(5):
                            nt = 512 if blk < 4 else C
                            tok = slice(blk * 512, blk * 512 + nt)
                            pa, bpa = nextA()
                            for k in range(8):
                                pe_mm(pa[:, 0:nt], w[:, k, 0:128], hT[:, k, tok], k == 0, k == 7, [bw, *b_hT[blk]], [bpa])
                            d0 = 261 + blk * 512 if blk < 4 else 2
                            act(LXp[:, d0:d0 + nt], pa[:, 0:nt], AF.Copy, [bpa], [b_LXp])
                        cw = lambda j: vecs[:, V_CONVW + j * 8 + n:V_CONVW + j * 8 + n + 1]
                        V("dve", lambda: nc.vector.tensor_scalar(out=xc[:], in0=LXp[:, 0:TL], scalar1=cw(0),
                                                                 scalar2=vecs[:, V_CONVB + n:V_CONVB + n + 1],
                                                                 op0=ALU.mult, op1=ALU.add), [b_LXp, b_vecs], [b_xc])
                        for j in range(1, 3):
                            V("dve", lambda: nc.vector.scalar_tensor_tensor(out=xc[:], in0=LXp[:, j:j + TL], scalar=cw(j),
                                                                            in1=xc[:], op0=ALU.mult, op1=ALU.add),
                              [b_LXp, b_vecs, b_xc], [b_xc])
                        V("dve", lambda: nc.vector.scalar_tensor_tensor(out=xcb[n % 2][:], in0=LXp[:, 3:3 + TL], scalar=cw(3),
                                                                        in1=xc[:], op0=ALU.mult, op1=ALU.add),
                          [b_LXp, b_vecs, b_xc], [b_xcb[n % 2]])

                    load_wl(0)
                    lx_conv(0)
                    for n in range(8):
                        w = wl[n % 2]; bw = b_wl[n % 2]
                        xb_ = xcb[n % 2]; bxb = b_xcb[n % 2]
                        if n + 1 < 8:
                            load_wl(n + 1)
                        if b == 0 and n == 0:
                            dump("xc0", xb_[:], [bxb])
                        for dr in range(2):
                            di = dr * 8 + n
                            for blk in range(5):
                                nt = 512 if blk < 4 else TL - 2048
                                tok = slice(blk * 512, blk * 512 + nt)
                                pr_, bpr = nextA()
                                pe_mm(pr_[:, 0:nt], lw[:, dr, n, :], xb_[:, tok], True, True, [b_lw, bxb], [bpr])
                                pi_, bpi = nextA()
                                pe_mm(pi_[:, 0:nt], lw[:, 2 + dr, n, :], xb_[:, tok], True, True, [b_lw, bxb], [bpi])
                                act(av[:, tok], pr_[:, 0:nt], AF.Sigmoid, [bpr, b_vecs], [b_av],
                                    bias=vecs[:, V_BA + di:V_BA + di + 1])
                                act(iv[:, tok], pi_[:, 0:nt], AF.Sigmoid, [bpi, b_vecs], [b_iv],
                                    bias=vecs[:, V_BX + di:V_BX + di + 1])
                            act(wv[:], av[:], AF.Exp, [b_av, b_lrup], [b_wv], scale=lrup[:, 1, di:di + 1])
                            act(av[:], av[:], AF.Exp, [b_av, b_lrup], [b_av], scale=lrup[:, 0, di:di + 1])
                            act(wv[:], wv[:], AF.Sqrt, [b_wv], [b_wv], scale=-1.0, bias=1.0)
                            V("pool", lambda: nc.gpsimd.tensor_tensor(out=iv[:], in0=iv[:], in1=xb_[:], op=ALU.mult),
                              [b_iv, bxb], [b_iv])
                            V("dve", lambda: nc.vector.tensor_tensor(out=wv[:], in0=iv[:], in1=wv[:], op=ALU.mult),
                              [b_iv, b_wv], [b_wv])
                            hh = hv[dr]; bh = b_hv[dr]
                            if dr == 0:
                                V("dve", lambda: nc.vector.tensor_tensor_scan(
                                    out=hh[:, 0:C], data0=av[:, 0:C], data1=wv[:, 0:C], initial=0.0,
                                    op0=ALU.mult, op1=ALU.add), [b_av, b_wv], [bh])
                                V("dve", lambda: nc.vector.tensor_tensor_scan(
                                    out=hh[:, C + 3:TL], data0=av[:, C + 3:TL], data1=wv[:, C + 3:TL],
                                    initial=hh[:, C - 1:C], op0=ALU.mult, op1=ALU.add), [b_av, b_wv, bh], [bh])
                                if n + 1 < 8:
                                    lx_conv(n + 1)
                            else:
                                V("dve", lambda: nc.vector.tensor_tensor_scan(
                                    out=hh[:, C - 1::-1], data0=av[:, C - 1::-1], data1=wv[:, C - 1::-1], initial=0.0,
                                    op0=ALU.mult, op1=ALU.add), [b_av, b_wv], [bh])
                                V("dve", lambda: nc.vector.tensor_tensor_scan(
                                    out=hh[:, TL - 1:C + 2:-1], data0=av[:, TL - 1:C + 2:-1], data1=wv[:, TL - 1:C + 2:-1],
                                    initial=hh[:, 0:1], op0=ALU.mult, op1=ALU.add), [b_av, b_wv, bh], [bh])
                        for blk in range(4):
                            tok = slice(blk * 512, (blk + 1) * 512)
                            pa, bpa = nextA()
                            for k in range(8):
                                pe_mm(pa[:], w[:, k, 128:256], hT[:, k, tok], k == 0, k == 7, [bw, *b_hT[blk]], [bpa])
                            act(gl[:, tok], pa[:], AF.Gelu_apprx_tanh, [bpa], [b_gl])
                        V("dve", lambda: nc.vector.tensor_tensor(out=hv[0][:, C + 3:TL], in0=hv[0][:, C + 3:TL],
                                                                 in1=hv[1][:, C + 3:TL], op=ALU.add),
                          [b_hv[0], b_hv[1]], [b_hv[0]])
                        if b == 0 and n == 0:
                            dump("hsum0", hv[0][:, C + 3:TL], [b_hv[0]])
                        V("dve", lambda: nc.vector.tensor_tensor(out=olru[:, n, :], in0=hv[0][:, C + 3:TL], in1=gl[:],
                                                                 op=ALU.mult), [b_hv[0], b_gl], [b_olru[n]])
                    if b == 0:
                        dump("olru", olru[:, :, :].rearrange("p a b -> p (a b)"), b_olru)
                    S.barrier()
                if STOP_AFTER == "s3":
                    break

                yT = sb(esb, [128, 8, L], BF16, "yT")
                b_yT = [Buf() for _ in range(4)]
                with ExitStack() as es4:
                    wg_ = [sb(es4, [128, 8, 256], BF16, "wg") for _ in range(2)]; b_wg = [Buf(), Buf()]
                    wu_ = [sb(es4, [128, 12, 128], BF16, "wu") for _ in range(2)]; b_wu = [Buf(), Buf()]
                    ga_ = [sb(es4, [128, 512], F32, "ga") for _ in range(2)]; b_ga = [Buf(), Buf()]
                    gb_ = [sb(es4, [128, 512], F32, "gb") for _ in range(2)]; b_gb = [Buf(), Buf()]
                    ya_ = [sb(es4, [128, 512], F32, "ya") for _ in range(2)]; b_ya = [Buf(), Buf()]
                    yb_ = [sb(es4, [128, 512], F32, "yb") for _ in range(2)]; b_yb = [Buf(), Buf()]
                    psM = [ps(es4, [128, 512], F32, "psM") for _ in range(8)]; b_psM = [Buf() for _ in range(8)]
                    it = 0
                    for f in range(8):
                        wgt, bwg = wg_[f % 2], b_wg[f % 2]
                        wut, bwu = wu_[f % 2], b_wu[f % 2]
                        S.dma("pool", wgt[:, :, :].rearrange("p a b -> p (a b)"), wgagb_d[f], writes=[bwg])
                        S.dma("pool", wut[:, :, :].rearrange("p a b -> p (a b)"), wup_d[f], writes=[bwu])
                        for blk in range(4):
                            tok = slice(blk * 512, (blk + 1) * 512)
                            i2 = it % 2
                            p0, p1, p2, p3 = [psM[(it % 2) * 4 + q] for q in range(4)]
                            q0, q1, q2, q3 = [b_psM[(it % 2) * 4 + q] for q in range(4)]
                            it += 1
                            for k in range(8):
                                pe_mm(p0[:], wgt[:, k, 0:128], hT[:, k, tok], k == 0, k == 7, [bwg, *b_hT[blk]], [q0])
                            act(ga_[i2][:], p0[:], AF.Sigmoid, [q0], [b_ga[i2]])
                            for k in range(4):
                                pe_mm(p1[:], wut[:, k, :], oatt[:, k, tok], k == 0, k == 3, [bwu, b_oatt[k][blk]], [q1])
                            V("dve", lambda: nc.vector.tensor_tensor(out=ya_[i2][:], in0=p1[:], in1=ga_[i2][:], op=ALU.mult),
                              [q1, b_ga[i2]], [b_ya[i2]])
                            for k in range(8):
                                pe_mm(p2[:], wgt[:, k, 128:256], hT[:, k, tok], k == 0, k == 7, [bwg, *b_hT[blk]], [q2])
                            act(gb_[i2][:], p2[:], AF.Sigmoid, [q2], [b_gb[i2]])
                            for k in range(8):
                                pe_mm(p3[:], wut[:, 4 + k, :], olru[:, k, tok], k == 0, k == 7, [bwu, b_olru[k]], [q3])
                            V("dve", lambda: nc.vector.tensor_tensor(out=yb_[i2][:], in0=p3[:], in1=gb_[i2][:], op=ALU.mult),
                              [q3, b_gb[i2]], [b_yb[i2]])
                            V("pool", lambda: nc.gpsimd.tensor_tensor(out=yT[:, f, tok], in0=ya_[i2][:], in1=yb_[i2][:],
                                                                      op=ALU.add), [b_ya[i2], b_yb[i2]], [b_yT[blk]])
                    if b == 0:
                        dump("yT", yT[:, :, :].rearrange("p a b -> p (a b)"), b_yT)
                    S.barrier()
                if STOP_AFTER == "s4":
                    break

                with ExitStack() as es5:
                    wo32 = [sb(es5, [128, D], F32, "wo32") for _ in range(2)]; b_wo32 = [Buf(), Buf()]
                    wob = sb(es5, [128, 8, D], BF16, "wob"); b_wob = Buf()
                    GA1 = sb(es5, [128, D], F32, "GA1"); b_GA1 = Buf()
                    G2 = sb(es5, [128, D], F32, "G2"); b_G2 = Buf()
                    S2 = sb(es5, [128, D], F32, "S2"); b_S2 = Buf()
                    gffnb = sb(es5, [128, D], F32, "gffnb"); b_gffnb = Buf()
                    diag = [sb(es5, [128, 128], F32, "diag") for _ in range(2)]; b_diag = [Buf(), Buf()]
                    wr = sb(es5, [128, 8, 36], F32, "wr"); b_wr = Buf()
                    brb = sb(es5, [128, 36], F32, "brb"); b_brb = Buf()
                    psB = ps(es5, [128, D], F32, "psB"); b_psB = Buf()
                    psO5 = [ps(es5, [128, D], F32, "psO5") for _ in range(2)]; b_psO5 = [Buf(), Buf()]
                    psT = ps(es5, [128, D], F32, "psT"); b_psT = Buf()
                    S.dma("sp", gffnb[:], gffnb_d, writes=[b_gffnb])
                    S.dma("sp", wr[:, :, :].rearrange("p a b -> p (a b)"), wr_d, writes=[b_wr])
                    S.dma("sp", brb[:], brb_d, writes=[b_brb])
                    bcast_row(es5, 2, b, psB, b_psB, diag, b_diag)
                    V("dve", lambda: nc.vector.tensor_copy(out=GA1[:], in_=psB[:]), [b_psB], [b_GA1])
                    bcast_row(es5, 4, b, psB, b_psB, diag, b_diag)
                    V("dve", lambda: nc.vector.scalar_tensor_tensor(out=G2[:], in0=psB[:], scalar=1.0, in1=gffnb[:],
                                                                    op0=ALU.add, op1=ALU.mult), [b_psB, b_gffnb], [b_G2])
                    bcast_row(es5, 3, b, psB, b_psB, diag, b_diag)
                    V("dve", lambda: nc.vector.tensor_copy(out=S2[:], in_=psB[:]), [b_psB], [b_S2])
                    for kk in range(8):
                        S.dma("sp", wo32[kk % 2][:], wout_d[:, kk * D:(kk + 1) * D], writes=[b_wo32[kk % 2]])
                        V("dve", lambda: nc.vector.tensor_tensor(out=wob[:, kk, :], in0=wo32[kk % 2][:], in1=GA1[:],
                                                                 op=ALU.mult), [b_wo32[kk % 2], b_GA1], [b_wob])
                    xt5 = [sb(es5, [128, D], F32, "xt5") for _ in range(2)]; b_xt5 = [Buf(), Buf()]
                    x1t = xt5; b_x1t = b_xt5
                    h2t = [sb(es5, [128, D], F32, "h2t") for _ in range(2)]; b_h2t = [Buf(), Buf()]
                    h2b = [sb(es5, [128, D], BF16, "h2b") for _ in range(2)]; b_h2b = [Buf(), Buf()]
                    h2T = sb(es5, [128, 8, 128], F32, "h2T"); b_h2T = Buf()
                    junk5 = sb(es5, [128, D], BF16, "junk5"); b_junk5 = Buf()
                    st5 = sb(es5, [128, 16, 3], F32, "st5"); b_st5 = [Buf() for _ in range(16)]
                    LGa = sb(es5, [128, 16, 36], F32, "LGa"); b_LGa = Buf()
                    RW = sb(es5, [128, 8, 16], F32, "RW"); b_RW = Buf()
                    RG = sb(es5, [128, 16, 12], F32, "RG"); b_RG = Buf()
                    LEM = sb(es5, [128, 16, 32], F32, "LEM"); b_LEM = Buf()
                    LEM2 = sb(es5, [128, 16, 32], F32, "LEM2"); b_LEM2 = Buf()
                    def wout_mm(j):
                        i2 = j % 2
                        tsl = slice(j * 128, (j + 1) * 128)
                        S.dma("sp", xt5[i2][:], xin[b, tsl, :], writes=[b_xt5[i2]])
                        pO, bO = psO5[i2], b_psO5[i2]
                        for hf in range(2):
                            for k in range(8):
                                pe_mm(pO[:, hf * 512:(hf + 1) * 512], yT[:, k, tsl], wob[:, k, hf * 512:(hf + 1) * 512],
                                      k == 0, k == 7, [b_yT[j // 4], b_wob], [bO], sig=(k == 7 and hf == 1))

                    wout_mm(0)
                    for j in range(16):
                        tg = b * 16 + j
                        i2 = j % 2
                        tsl = slice(j * 128, (j + 1) * 128)
                        pO, bO = psO5[i2], b_psO5[i2]
                        if j + 1 < 16:
                            wout_mm(j + 1)
                        V("dve", lambda: nc.vector.tensor_tensor(out=x1t[i2][:], in0=pO[:], in1=xt5[i2][:], op=ALU.add),
                          [bO, b_xt5[i2]], [b_xt5[i2]])
                        S.dma("pool", x1_d[tg * 128:(tg + 1) * 128, :], x1t[i2][:], reads=[b_x1t[i2]], writes=[b_x1d], group=True, semof=b_x1t[i2])
                        act(junk5[:], x1t[i2][:], AF.Square, [b_x1t[i2]], [b_junk5, b_st5[j]], accum_out=st5[:, j, 0:1])
                        act(st5[:, j, 1:2], st5[:, j, 0:1], AF.Sqrt, [b_st5[j], b_eps], [b_st5[j]], scale=1.0 / D,
                            bias=epst[:, 0:1])
                        V("dve", lambda: nc.vector.reciprocal(out=st5[:, j, 2:3], in_=st5[:, j, 1:2]), [b_st5[j]], [b_st5[j]])
                        V("dve", lambda: nc.vector.scalar_tensor_tensor(out=h2t[i2][:], in0=x1t[i2][:], scalar=st5[:, j, 2:3],
                                                                        in1=G2[:], op0=ALU.mult, op1=ALU.mult),
                          [b_x1t[i2], b_st5[j], b_G2], [b_h2t[i2]])
                        V("dve", lambda: nc.vector.tensor_tensor(out=h2t[i2][:], in0=h2t[i2][:], in1=S2[:], op=ALU.add),
                          [b_h2t[i2], b_S2], [b_h2t[i2]])
                        act(h2b[i2][:], h2t[i2][:], AF.Copy, [b_h2t[i2]], [b_h2b[i2]])
                        S.dma("pool", h2_d[tg * 128:(tg + 1) * 128, :], h2b[i2][:], reads=[b_h2b[i2]], writes=[b_h2d], group=True, semof=b_h2b[i2])
                        for k in range(8):
                            pe_tr(psT[:, k * 128:(k + 1) * 128], h2t[i2][:, k * 128:(k + 1) * 128], identf[:],
                                  [b_h2t[i2], b_identf], [b_psT], sig=(k == 7))
                        act(h2T[:, :, :].rearrange("p a b -> p (a b)"), psT[:], AF.Copy, [b_psT], [b_h2T])
                        for k in range(8):
                            pe_mm(psB[:, 0:36], h2T[:, k, :], wr[:, k, :], k == 0, k == 7, [b_h2T, b_wr], [b_psB])
                        V("dve", lambda: nc.vector.tensor_tensor(out=LGa[:, j, :], in0=psB[:, 0:36], in1=brb[:], op=ALU.add),
                          [b_psB, b_brb], [b_LGa])
                        if tg == 0:
                            dump("logit0", LGa[:, 0, :], [b_LGa])
                    T16 = slice(b * 16, (b + 1) * 16)
                    lg = LGa[:, :, 0:4]
                    le = LGa[:, :, 4:36]
                    rb = [b_LGa, b_RW]
                    V("dve", lambda: nc.vector.tensor_reduce(out=RW[:, 0, 0:16], in_=lg, axis=AX.X, op=ALU.max), [b_LGa], [b_RW])
                    V("dve", lambda: nc.vector.tensor_tensor(out=RG[:, :, 0:4], in0=lg,
                                                             in1=RW[:, 0, 0:16].unsqueeze(2).to_broadcast([128, 16, 4]),
                                                             op=ALU.subtract), rb, [b_RG])
                    act(RG[:, :, 4:8], RG[:, :, 0:4], AF.Exp, [b_RG], [b_RG])
                    V("dve", lambda: nc.vector.tensor_reduce(out=RW[:, 1, 0:16], in_=RG[:, :, 4:8], axis=AX.X, op=ALU.add), [b_RG], [b_RW])
                    V("dve", lambda: nc.vector.reciprocal(out=RW[:, 2, 0:16], in_=RW[:, 1, 0:16]), [b_RW], [b_RW])
                    V("dve", lambda: nc.vector.tensor_scalar(out=RG[:, :, 8:12], in0=RG[:, :, 0:4], scalar1=0.0, scalar2=None,
                                                             op0=ALU.is_equal), [b_RG], [b_RG])
                    V("dve", lambda: nc.vector.tensor_scalar(out=RG[:, :, 8:12], in0=RG[:, :, 8:12], scalar1=-1.0, scalar2=1e30,
                                                             op0=ALU.add, op1=ALU.mult), [b_RG], [b_RG])
                    V("dve", lambda: nc.vector.tensor_tensor(
                        out=LEM[:, :, :].rearrange("p t (g e) -> p t g e", e=8), in0=le.rearrange("p t (g e) -> p t g e", e=8),
                        in1=RG[:, :, 8:12].unsqueeze(3).to_broadcast([128, 16, 4, 8]), op=ALU.add), [b_LGa, b_RG], [b_LEM])
                    V("dve", lambda: nc.vector.tensor_reduce(out=RW[:, 3, 0:16], in_=LEM[:], axis=AX.X, op=ALU.max), [b_LEM], [b_RW])
                    V("dve", lambda: nc.vector.tensor_tensor(out=oh1all[:, T16, :], in0=LEM[:],
                                                             in1=RW[:, 3, 0:16].unsqueeze(2).to_broadcast([128, 16, 32]),
                                                             op=ALU.is_equal), [b_LEM, b_RW], [b_oh1])
                    V("dve", lambda: nc.vector.scalar_tensor_tensor(out=LEM2[:], in0=oh1all[:, T16, :], scalar=-1e30, in1=LEM[:],
                                                                    op0=ALU.mult, op1=ALU.add), [b_oh1, b_LEM], [b_LEM2])
                    V("dve", lambda: nc.vector.tensor_reduce(out=RW[:, 4, 0:16], in_=LEM2[:], axis=AX.X, op=ALU.max), [b_LEM2], [b_RW])
                    V("dve", lambda: nc.vector.tensor_tensor(out=Mall[:, T16, :], in0=LEM[:],
                                                             in1=RW[:, 4, 0:16].unsqueeze(2).to_broadcast([128, 16, 32]),
                                                             op=ALU.is_ge), [b_LEM, b_RW], [b_Mall])
                    V("dve", lambda: nc.vector.tensor_tensor(out=RW[:, 5, 0:16], in0=RW[:, 4, 0:16], in1=RW[:, 3, 0:16], op=ALU.subtract),
                      [b_RW], [b_RW])
                    act(RW[:, 6, 0:16], RW[:, 5, 0:16], AF.Exp, [b_RW], [b_RW])
                    V("dve", lambda: nc.vector.tensor_scalar(out=RW[:, 6, 0:16], in0=RW[:, 6, 0:16], scalar1=1.0, scalar2=None, op0=ALU.add),
                      [b_RW], [b_RW])
                    V("dve", lambda: nc.vector.reciprocal(out=RW[:, 7, 0:16], in_=RW[:, 6, 0:16]), [b_RW], [b_RW])
                    V("dve", lambda: nc.vector.tensor_tensor(out=gates[:, T16, 0], in0=RW[:, 7, 0:16], in1=RW[:, 2, 0:16], op=ALU.mult),
                      [b_RW], [b_gates])
                    V("dve", lambda: nc.vector.tensor_tensor(out=gates[:, T16, 1], in0=RW[:, 2, 0:16], in1=gates[:, T16, 0], op=ALU.subtract),
                      [b_RW, b_gates], [b_gates])
                    S.barrier()
            if STOP_AFTER in ("s1", "s2", "s3", "s4"):
                break

        if STOP_AFTER is None or STOP_AFTER in ("route", "moe"):
            blk_i = sb(es, [128, NBLK], I32, "blki"); b_blki = Buf()
            with ExitStack() as esr:
                psC = ps(esr, [128, 32], F32, "psC"); b_psC = Buf()
                psU = ps(esr, [128, 1024], F32, "psU"); b_psU = Buf()
                psCS = ps(esr, [128, 1024], F32, "psCS"); b_psCS = Buf()
                CSs = sb(esr, [128, 32, 32], F32, "CSs"); b_CSs = Buf()
                INC = sb(esr, [128, 32, 32], F32, "INC"); b_INC = Buf()
                TT = sb(esr, [128, 32, 32], F32, "TT"); b_TT = Buf()
                TM = sb(esr, [128, 32, 32], F32, "TM"); b_TM = Buf()
                cn = sb(esr, [128, 8, 32], F32, "cn"); b_cn = Buf()
                cni = sb(esr, [128, 32], I32, "cni"); b_cni = Buf()
                cmp_ = sb(esr, [128, NBLK, 32], F32, "cmp"); b_cmp = Buf()
                bst = sb(esr, [128, NBLK], F32, "bst"); b_bst = Buf()
                bs0 = sb(esr, [128, NBLK], F32, "bs0"); b_bs0 = Buf()
                blk_f = sb(esr, [128, NBLK], F32, "blkf"); b_blkf = Buf()
                tmp = sb(esr, [128, 2, 32], F32, "tmpr"); b_tmp = [Buf(), Buf()]
                for t in range(32):
                    pe_mm(psC[:], onesb[:], Mall[:, t, :], t == 0, t == 31, [b_onesb, b_Mall], [b_psC])
                V("dve", lambda: nc.vector.tensor_copy(out=cn[:, 0, :], in_=psC[:]), [b_psC], [b_cn])
                V("dve", lambda: nc.vector.tensor_scalar(out=cn[:, 1, :], in0=cn[:, 0, :], scalar1=255.0, scalar2=None, op0=ALU.add),
                  [b_cn], [b_cn])
                V("dve", lambda: nc.vector.tensor_copy(out=cni[:], in_=cn[:, 1, :]), [b_cn], [b_cni])
                V("dve", lambda: nc.vector.tensor_scalar(out=cni[:], in0=cni[:], scalar1=8, scalar2=8,
                                                         op0=ALU.arith_shift_right, op1=ALU.logical_shift_left), [b_cni], [b_cni])
                V("dve", lambda: nc.vector.tensor_copy(out=cn[:, 2, :], in_=cni[:]), [b_cni], [b_cn])
                V("dve", lambda: nc.vector.tensor_tensor_scan(out=cn[:, 3, :], data0=onesf[:, 0:32], data1=cn[:, 2, :],
                                                              initial=0.0, op0=ALU.mult, op1=ALU.add),
                  [b_cn, b_onesf], [b_cn])
                V("dve", lambda: nc.vector.tensor_tensor(out=cn[:, 4, :], in0=cn[:, 3, :], in1=cn[:, 2, :], op=ALU.subtract),
                  [b_cn], [b_cn])
                V("dve", lambda: nc.vector.tensor_scalar(out=bs0[:], in0=onesf[:, 0:NBLK], scalar1=256.0, scalar2=None, op0=ALU.mult),
                  [b_onesf], [b_bs0])
                V("dve", lambda: nc.vector.tensor_tensor_scan(out=bst[:], data0=onesf[:, 0:NBLK], data1=bs0[:], initial=-256.0,
                                                              op0=ALU.mult, op1=ALU.add), [b_bs0, b_onesf], [b_bst])
                V("dve", lambda: nc.vector.tensor_tensor(
                    out=cmp_[:], in0=cn[:, 3, :].unsqueeze(1).to_broadcast([128, NBLK, 32]),
                    in1=bst[:].unsqueeze(2).to_broadcast([128, NBLK, 32]), op=ALU.is_le), [b_cn, b_bst], [b_cmp])
                V("dve", lambda: nc.vector.tensor_reduce(out=blk_f[:], in_=cmp_[:], axis=AX.X, op=ALU.add), [b_cmp], [b_blkf])
                V("dve", lambda: nc.vector.tensor_scalar(out=blk_f[:], in0=blk_f[:], scalar1=128.0, scalar2=None,
                                                         op0=ALU.mult), [b_blkf], [b_blkf])
                V("dve", lambda: nc.vector.tensor_scalar(out=blk_f[:], in0=blk_f[:], scalar1=iota[:, 0:1], scalar2=None,
                                                         op0=ALU.add), [b_blkf, b_iota], [b_blkf])
                V("dve", lambda: nc.vector.tensor_copy(out=blk_i[:], in_=blk_f[:]), [b_blkf], [b_blki])
                dump("blkf", blk_f[:], [b_blkf])
                dump("cn", cn[:, 0:5, :].rearrange("p a b -> p (a b)"), [b_cn])
                Mflat = Mall[:, :, :].rearrange("p t e -> p (t e)")
                for hf in range(2):
                    pe_mm(psU[:, hf * 512:(hf + 1) * 512], utri[:], Mflat[:, hf * 512:(hf + 1) * 512], True, True,
                          [b_utri, b_Mall], [b_psU], sig=(hf == 1))
                    pe_mm(psCS[:, hf * 512:(hf + 1) * 512], onesb[:], Mflat[:, hf * 512:(hf + 1) * 512], True, True,
                          [b_onesb, b_Mall], [b_psCS], sig=(hf == 1))
                V("dve", lambda: nc.vector.tensor_copy(out=CSs[:, :, :].rearrange("p t e -> p (t e)"), in_=psCS[:]), [b_psCS], [b_CSs])
                for e in range(32):
                    V("dve", lambda: nc.vector.tensor_tensor_scan(out=INC[:, :, e], data0=onesf[:, 0:32], data1=CSs[:, :, e],
                                                                  initial=0.0, op0=ALU.mult, op1=ALU.add),
                      [b_CSs, b_onesf], [b_INC])
                V("dve", lambda: nc.vector.tensor_tensor(out=TT[:, :, :].rearrange("p t e -> p (t e)"), in0=psU[:],
                                                         in1=INC[:, :, :].rearrange("p t e -> p (t e)"), op=ALU.add),
                  [b_psU, b_INC], [b_TT])
                V("dve", lambda: nc.vector.tensor_tensor(out=TT[:], in0=TT[:], in1=CSs[:], op=ALU.subtract), [b_TT, b_CSs], [b_TT])
                V("dve", lambda: nc.vector.tensor_tensor(out=TT[:], in0=TT[:], in1=cn[:, 4, :].unsqueeze(1).to_broadcast([128, 32, 32]),
                                                         op=ALU.add), [b_TT, b_cn], [b_TT])
                V("dve", lambda: nc.vector.tensor_tensor(out=TM[:], in0=TT[:], in1=oh1all[:], op=ALU.mult), [b_TT, b_oh1], [b_TM])
                V("dve", lambda: nc.vector.tensor_reduce(out=dest_f[:, :, 0], in_=TM[:], axis=AX.X, op=ALU.add), [b_TM], [b_destf])
                V("dve", lambda: nc.vector.tensor_tensor(out=TM[:], in0=Mall[:], in1=oh1all[:], op=ALU.subtract), [b_Mall, b_oh1], [b_TM])
                V("dve", lambda: nc.vector.tensor_tensor(out=TM[:], in0=TM[:], in1=TT[:], op=ALU.mult), [b_TM, b_TT], [b_TM])
                V("dve", lambda: nc.vector.tensor_reduce(out=dest_f[:, :, 1], in_=TM[:], axis=AX.X, op=ALU.add), [b_TM], [b_destf])
                V("dve", lambda: nc.vector.tensor_copy(out=dest_i[:], in_=dest_f[:, :, :].rearrange("p a b -> p (a b)")), [b_destf], [b_desti])
                dump("destf", dest_f[:, :, :].rearrange("p a b -> p (a b)"), [b_destf])
                dump("gates", gates[:, :, :].rearrange("p a b -> p (a b)"), [b_gates])
                hl = [sb(esr, [128, D], BF16, "hl") for _ in range(4)]; b_hl = [Buf() for _ in range(4)]
                for t in range(32):
                    S.dma("sp", hl[t % 4][:], h2_d[t * 128:(t + 1) * 128, :], reads=[b_h2d], writes=[b_hl[t % 4]])
                    for kk in range(2):
                        S.dma_ind(xs_d[:, :], bass.IndirectOffsetOnAxis(ap=dest_i[:, 2 * t + kk:2 * t + kk + 1], axis=0), hl[t % 4][:, :], None,
                                  NROWS - 1, reads=[b_hl[t % 4], b_desti], writes=[b_xsd], group=True, semof=b_hl[t % 4])
                S.barrier()

            if STOP_AFTER != "route":
                with ExitStack() as esm:
                    w1b = [sb(esm, [128, 8, 512], BF16, "w1b") for _ in range(3)]; b_w1 = [Buf() for _ in range(3)]
                    w3b = [sb(esm, [128, 8, 512], BF16, "w3b") for _ in range(3)]; b_w3 = [Buf() for _ in range(3)]
                    w2b = [sb(esm, [128, 4, 1024], BF16, "w2b") for _ in range(3)]; b_w2 = [Buf() for _ in range(3)]
                    xr = [sb(esm, [128, 2, D], BF16, "xr") for _ in range(3)]; b_xr = [Buf() for _ in range(3)]
                    xT = [sb(esm, [128, 8, 256], BF16, "xT") for _ in range(3)]; b_xT = [[Buf(), Buf()] for _ in range(3)]
                    sl = [sb(esm, [128, 256], F32, "sl") for _ in range(2)]; b_sl = [Buf(), Buf()]
                    hm = [sb(esm, [128, 4, 256], BF16, "hm") for _ in range(2)]; b_hm = [Buf(), Buf()]
                    yo = [sb(esm, [128, 2, D], BF16, "yo") for _ in range(2)]; b_yo = [[Buf(), Buf()], [Buf(), Buf()]]
                    psX = [ps(esm, [128, 1024], BF16, "psX") for _ in range(2)]; b_psX = [Buf(), Buf()]
                    psH = [ps(esm, [128, 512], F32, "psH") for _ in range(2)]; b_psH = [Buf(), Buf()]
                    psY = [ps(esm, [128, 512], F32, "psY") for _ in range(4)]; b_psY = [Buf() for _ in range(4)]
                    xcnt = [0]; hcnt = [0]; ycnt = [0]

                    wreg = nc.gpsimd.to_reg(32 * 128 - 1)

                    def moe_w(blk):
                        i3 = blk % 3
                        off = bass.IndirectOffsetOnAxis(ap=blk_i[:, blk:blk + 1], axis=0)
                        S.dma_ind(w1b[i3][:, :, :].rearrange("p a b -> p (a b)"), None, w1_d[:, :], off, 0,
                                  reads=[b_blki], writes=[b_w1[i3]], bcheck=wreg)
                        S.dma_ind(w3b[i3][:, :, :].rearrange("p a b -> p (a b)"), None, w3_d[:, :], off, 0,
                                  reads=[b_blki], writes=[b_w3[i3]], bcheck=wreg)
                        S.dma_ind(w2b[i3][:, :, :].rearrange("p a b -> p (a b)"), None, w2_d[:, :], off, 0,
                                  reads=[b_blki], writes=[b_w2[i3]], bcheck=wreg)

                    def moe_x(blk):
                        i2 = blk % 3
                        S.dma("sp", xr[i2][:], xs_d[blk * 256:(blk + 1) * 256, :].rearrange("(a p) d -> p a d", p=128),
                              reads=[b_xsd], writes=[b_xr[i2]])
                        for k in range(8):
                            if k % 4 == 0:
                                pX, bX = psX[(xcnt[0] // 4) % 2], b_psX[(xcnt[0] // 4) % 2]
                            for a in range(2):
                                pe_tr(pX[:, (k % 4) * 256 + a * 128:(k % 4) * 256 + (a + 1) * 128],
                                      xr[i2][:, a, k * 128:(k + 1) * 128], identb[:], [b_xr[i2], b_identb], [bX],
                                      sig=(k % 4 == 3 and a == 1))
                            xcnt[0] += 1
                            if k % 4 == 3:
                                dstx = xT[i2][:, k - 3:k + 1, :].rearrange("p a b -> p (a b)")
                                V("dve", lambda: nc.vector.tensor_copy(out=dstx, in_=pX[:]), [bX], [b_xT[i2][(k // 4) % 2]])

                    def moe_c(blk):
                        i2 = blk % 2
                        i3 = blk % 3
                        ix = blk % 3
                        for c4 in range(4):
                            pH, bH = psH[hcnt[0] % 2], b_psH[hcnt[0] % 2]
                            si = hcnt[0] % 2
                            hcnt[0] += 1
                            for k in range(8):
                                pe_mm(pH[:, 0:256], w1b[i3][:, k, c4 * 128:(c4 + 1) * 128], xT[ix][:, k, :], k == 0, k == 7,
                                      [b_w1[i3], *b_xT[ix]], [bH], sig=False)
                            for k in range(8):
                                pe_mm(pH[:, 256:512], w3b[i3][:, k, c4 * 128:(c4 + 1) * 128], xT[ix][:, k, :], k == 0, k == 7,
                                      [b_w3[i3], *b_xT[ix]], [bH])
                            act(sl[si][:], pH[:, 0:256], AF.Silu, [bH], [b_sl[si]])
                            V("dve", lambda: nc.vector.tensor_tensor(out=hm[i2][:, c4, :], in0=pH[:, 256:512], in1=sl[si][:],
                                                                     op=ALU.mult), [bH, b_sl[si]], [b_hm[i2]])
                        for a in range(2):
                            for hf in range(2):
                                pY, bY = psY[ycnt[0] % 4], b_psY[ycnt[0] % 4]
                                ycnt[0] += 1
                                for k in range(4):
                                    pe_mm(pY[:], hm[i2][:, k, a * 128:(a + 1) * 128], w2b[i3][:, k, hf * 512:(hf + 1) * 512],
                                          k == 0, k == 3, [b_hm[i2], b_w2[i3]], [bY])
                                V("dve", lambda: nc.vector.tensor_copy(out=yo[i2][:, a, hf * 512:(hf + 1) * 512], in_=pY[:]), [bY],
                                  [b_yo[i2][hf]])
                        S.dma("act", ys_d[blk * 256:(blk + 1) * 256, :].rearrange("(a p) d -> p a d", p=128), yo[i2][:],
                              reads=b_yo[i2], writes=[b_ysd], group=True, semof=b_yo[i2][0])

                    moe_w(0)
                    moe_w(1)
                    moe_x(0)
                    moe_x(1)
                    for blk in range(NBLK):
                        if blk + 2 < NBLK:
                            moe_w(blk + 2)
                            moe_x(blk + 2)
                        moe_c(blk)
                    S.barrier()

                with ExitStack() as esf:
                    GA2 = [sb(esf, [128, D], F32, "GA2") for _ in range(2)]; b_GA2 = [Buf(), Buf()]
                    gfb = sb(esf, [128, D], F32, "gfb"); b_gfb = Buf()
                    diag = [sb(esf, [128, 128], F32, "diagf") for _ in range(2)]; b_diag = [Buf(), Buf()]
                    psB = ps(esf, [128, D], F32, "psBf"); b_psB = Buf()
                    S.dma("sp", gfb[:], gfb_d, writes=[b_gfb])
                    for b in range(NB):
                        bcast_row(esf, 5, b, psB, b_psB, diag, b_diag)
                        V("dve", lambda: nc.vector.tensor_copy(out=GA2[b][:], in_=psB[:]), [b_psB], [b_GA2[b]])
                    y0 = [sb(esf, [128, D], BF16, "y0") for _ in range(3)]; b_y0 = [Buf() for _ in range(3)]
                    y1 = [sb(esf, [128, D], BF16, "y1") for _ in range(3)]; b_y1 = [Buf() for _ in range(3)]
                    x1l = [sb(esf, [128, D], F32, "x1l") for _ in range(3)]; b_x1l = [Buf() for _ in range(3)]
                    mo = [sb(esf, [128, D], F32, "mo") for _ in range(2)]; b_mo = [Buf(), Buf()]
                    ot = [sb(esf, [128, D], F32, "ot") for _ in range(2)]; b_ot = [Buf(), Buf()]
                    junkf = sb(esf, [128, D], BF16, "junkf"); b_junkf = Buf()
                    stf = sb(esf, [128, 32, 3], F32, "stf"); b_stf = [Buf() for _ in range(32)]

                    def fin_load(t):
                        i3 = t % 3
                        S.dma_ind(y0[i3][:, :], None, ys_d[:, :], bass.IndirectOffsetOnAxis(ap=dest_i[:, 2 * t:2 * t + 1], axis=0),
                                  0, reads=[b_ysd, b_desti], writes=[b_y0[i3]])
                        S.dma_ind(y1[i3][:, :], None, ys_d[:, :], bass.IndirectOffsetOnAxis(ap=dest_i[:, 2 * t + 1:2 * t + 2], axis=0),
                                  0, reads=[b_ysd, b_desti], writes=[b_y1[i3]])
                        S.dma("sp", x1l[i3][:], x1_d[t * 128:(t + 1) * 128, :], reads=[b_x1d], writes=[b_x1l[i3]])

                    fin_load(0)
                    fin_load(1)
                    for t in range(32):
                        i2 = t % 2
                        i3 = t % 3
                        bb = t // 16
                        act(mo[i2][:], y0[i3][:], AF.Copy, [b_y0[i3], b_gates], [b_mo[i2]], scale=gates[:, t, 0:1])
                        V("dve", lambda: nc.vector.scalar_tensor_tensor(out=mo[i2][:], in0=y1[i3][:], scalar=gates[:, t, 1:2],
                                                                        in1=mo[i2][:], op0=ALU.mult, op1=ALU.add),
                          [b_y1[i3], b_gates, b_mo[i2]], [b_mo[i2]])
                        if t == 0:
                            dump("moe0", mo[i2][:], [b_mo[i2]])
                        V("pool", lambda: nc.gpsimd.tensor_tensor(out=mo[i2][:], in0=mo[i2][:], in1=GA2[bb][:], op=ALU.mult),
                          [b_mo[i2], b_GA2[bb]], [b_mo[i2]])
                        V("dve", lambda: nc.vector.tensor_tensor(out=mo[i2][:], in0=mo[i2][:], in1=x1l[i3][:], op=ALU.add),
                          [b_mo[i2], b_x1l[i3]], [b_mo[i2]])
                        act(junkf[:], mo[i2][:], AF.Square, [b_mo[i2]], [b_junkf, b_stf[t]], accum_out=stf[:, t, 0:1])
                        act(stf[:, t, 1:2], stf[:, t, 0:1], AF.Sqrt, [b_stf[t], b_eps], [b_stf[t]], scale=1.0 / D, bias=epst[:, 0:1])
                        V("dve", lambda: nc.vector.reciprocal(out=stf[:, t, 2:3], in_=stf[:, t, 1:2]), [b_stf[t]], [b_stf[t]])
                        V("dve", lambda: nc.vector.scalar_tensor_tensor(out=ot[i2][:], in0=mo[i2][:], scalar=stf[:, t, 2:3],
                                                                        in1=gfb[:], op0=ALU.mult, op1=ALU.mult),
                          [b_mo[i2], b_stf[t], b_gfb], [b_ot[i2]])
                        if t + 2 < 32:
                            fin_load(t + 2)
                        S.dma("sp", out_d[bb, (t % 16) * 128:(t % 16 + 1) * 128, :], ot[i2][:], reads=[b_ot[i2]], writes=[b_outd],
                              group=True, semof=b_ot[i2])
        S.barrier()
        build_program.stats = dict(nops=dict(S.nops), nwaits=S.nwaits, nsem=S.nsem)
    return nc


def _fm(v):
    return np.ascontiguousarray(np.asarray(v, np.float32).reshape(-1, 128).T)


def _kp(w):
    K, N = w.shape
    return np.ascontiguousarray(w.reshape(K // 128, 128, N).transpose(1, 0, 2))


def _swap_cols():
    idx = np.arange(64)
    half = idx // 32
    within = idx % 32
    sw = np.where(within < 16, within + 16, within - 16)
    return half * 32 + sw


def _host_consts():
    nf = 16
    inv_freq = (10000.0 ** (-np.arange(nf, dtype=np.float32) / nf)).astype(np.float32)
    t = np.arange(L)
    row = (t // 64).astype(np.float32)
    col = (t % 64).astype(np.float32)
    cos = np.zeros((128, L), np.float32)
    sin = np.zeros((128, L), np.float32)
    for p in range(128):
        d = p % 64
        pos = row if d < 32 else col
        ang = (pos * inv_freq[(d % 32) % 16]).astype(np.float32)
        sign = -1.0 if (d % 32) < 16 else 1.0
        cos[p] = np.cos(ang)
        sin[p] = sign * np.sin(ang)
    cossin = np.concatenate([cos, sin], axis=1)
    ident = np.eye(128, dtype=np.float32)
    iota = np.arange(128, dtype=np.float32).reshape(128, 1)
    utri = np.triu(np.ones((128, 128), np.float32), k=1)
    return cossin, ident, iota, utri


def _bias_table(rpb):
    cq = np.arange(64)
    c_start = np.clip(cq - 8, 0, 48)
    band = (cq[None, :] >= c_start[:, None]) & (cq[None, :] < c_start[:, None] + 16)
    dc = np.clip(cq[None, :] - cq[:, None], -15, 15) + 15
    tab = np.full((64, 8, NTB, 64), -1e30, np.float32)
    for h in range(8):
        for dr in range(15):
            vals = rpb[h, dr][dc]
            tab[:, h, 1 + dr, :] = np.where(band, vals, np.float32(-1e30))
        tab[:, h, 18, :] = tab[:, h, 1 + 3, :]
        tab[:, h, 19, :] = tab[:, h, 1 + 10, :]
    return tab.reshape(64, 8 * NTB * 64)


def _prepare(inputs):
    f = lambda k: np.asarray(inputs[k], np.float32)
    w_in = f("w_in")[0]
    K_OFF, V_OFF, LX_OFF, Q_OFF, LG_OFF, GA_OFF, GB_OFF = 0, 512, 1024, 2048, 2560, 3584, 4608
    sw = _swap_cols()
    wqkv = []
    for hp in range(4):
        cols = []
        for base in (Q_OFF, K_OFF):
            plain = np.concatenate([base + (2 * hp + e) * 64 + np.arange(64) for e in range(2)])
            swp = np.concatenate([base + (2 * hp + e) * 64 + sw for e in range(2)])
            cols += [plain, swp]
        cols.append(V_OFF + hp * 128 + np.arange(128))
        wqkv.append(_kp(w_in[:, np.concatenate(cols)]).reshape(128, 8 * 640))
    wqkv = np.stack(wqkv)
    wlxlg = np.stack([_kp(w_in[:, np.concatenate([LX_OFF + n * 128 + np.arange(128), LG_OFF + n * 128 + np.arange(128)])]
                          ).reshape(128, 8 * 256) for n in range(8)])
    wgagb = np.stack([_kp(w_in[:, np.concatenate([GA_OFF + n * 128 + np.arange(128), GB_OFF + n * 128 + np.arange(128)])]
                          ).reshape(128, 8 * 256) for n in range(8)])
    wua = _kp(f("w_up_attn")[0])
    wul = _kp(f("w_up_lru")[0])
    wup = np.stack([np.concatenate([wua[:, :, n * 128:(n + 1) * 128], wul[:, :, n * 128:(n + 1) * 128]], axis=1
                                   ).reshape(128, 12 * 128) for n in range(8)])
    wout = _kp(f("w_out")[0]).reshape(128, 8 * D)
    wa = f("lru_wa")[0]
    wx = f("lru_wx")[0]
    lruw = np.stack([wa[0], wa[1], wx[0], wx[1]])
    lruw = np.ascontiguousarray(lruw.transpose(2, 0, 1, 3)).reshape(128, 4 * 8 * 128)
    vecs = np.concatenate([
        _fm(f("g_mix")[0]), _fm(f("g_ffn")[0]),
        np.concatenate([_fm(f("conv_w")[0][j]) for j in range(4)], axis=1),
        _fm(f("conv_b")[0]),
        np.concatenate([_fm(f("lru_ba")[0][d_]) for d_ in range(2)], axis=1),
        np.concatenate([_fm(f("lru_bx")[0][d_]) for d_ in range(2)], axis=1),
        np.concatenate([_fm(f("lru_lambda")[0][d_]) for d_ in range(2)], axis=1),
        _fm(f("b_mod")[0]),
    ], axis=1)
    assert vecs.shape == (128, NV)
    wmod = _kp(f("w_mod")[0])
    wr = _kp(np.concatenate([f("router_group_w")[0], f("router_expert_w")[0]], axis=1)).reshape(128, 8 * 36)
    brb = np.ascontiguousarray(np.broadcast_to(
        np.concatenate([f("router_group_b")[0], f("router_expert_b")[0]])[None, :], (128, 36)))
    gfb = np.ascontiguousarray(np.broadcast_to(f("g_final")[None, :], (128, D)))
    gffnb = np.ascontiguousarray(np.broadcast_to(f("g_ffn")[0][None, :], (128, D)))
    w1 = f("expert_w_gate")[0]
    w3 = f("expert_w_up")[0]
    w2 = f("expert_w_down")[0]
    w1h = np.ascontiguousarray(w1.reshape(32, 8, 128, 512).transpose(0, 2, 1, 3)).reshape(32 * 128, 8 * 512)
    w3h = np.ascontiguousarray(w3.reshape(32, 8, 128, 512).transpose(0, 2, 1, 3)).reshape(32 * 128, 8 * 512)
    w2h = np.ascontiguousarray(w2.reshape(32, 4, 128, 1024).transpose(0, 2, 1, 3)).reshape(32 * 128, 4 * 1024)
    cossin, ident, iota, utri = _host_consts()
    btab = _bias_table(f("rpb")[0])
    shared = dict(wmod=wmod, vecs=vecs, wqkv=wqkv, wlxlg=wlxlg, wgagb=wgagb, wup=wup, wout=wout, lruw=lruw,
                  cossin=cossin, btab=btab, wr=wr, brb=brb, gfb=gfb, gffnb=gffnb, w1h=w1h, w3h=w3h, w2h=w2h,
                  ident=ident, iota=iota, utri=utri)
    x = f("x")
    ctx = f("ctx")
    c = f("c")
    c_ctx = f("c_ctx")
    in_maps = []
    for core in range(NCORES):
        b0 = core * NB
        cs = np.stack([c[b0], c[b0 + 1], c_ctx], axis=-1)
        cs = np.ascontiguousarray(cs.reshape(8, 128, 3).transpose(1, 0, 2)).reshape(128, 24)
        m = dict(shared)
        m["xin"] = np.ascontiguousarray(x[b0:b0 + NB])
        m["ctxin"] = np.ascontiguousarray(ctx[b0:b0 + NB])
        m["cs"] = cs
        in_maps.append(m)
    return in_maps


def kernel(**inputs):
    in_maps = _prepare(inputs)
    nc = build_program()
    res = run_bass_kernel_spmd(nc, in_maps, core_ids=list(range(NCORES)))
    out = np.concatenate([np.asarray(r["out"], np.float32) for r in res.results], axis=0)
    return out
```

```python
import numpy as np
import concourse.bass as bass
import concourse.mybir as mybir
from concourse.bass_utils import run_bass_kernel_spmd
from contextlib import ExitStack

F32 = mybir.dt.float32
BF16 = mybir.dt.bfloat16
I32 = mybir.dt.int32
AF = mybir.ActivationFunctionType
ALU = mybir.AluOpType
AX = mybir.AxisListType

D = 1024
L = 2048
C = 256
NB = 2
NCORES = 8
LC = L + C
NTOK = NB * L
NBLK = 64
NROWS = NBLK * 256
EPS = 1e-6

V_GMIX, V_GFFN, V_CONVW, V_CONVB, V_BA, V_BX, V_LAM, V_BMOD, NV = 0, 8, 16, 48, 56, 72, 88, 104, 152
NTB = 21

DEBUG = {}
import os as _os
ATT_NHP = int(_os.environ.get("ATT_NHP", "4"))
ATT_NUNITS = int(_os.environ.get("ATT_NUNITS", "8"))
ATT_NROWS = int(_os.environ.get("ATT_NROWS", "8"))
ATT_NORM = int(_os.environ.get("ATT_NORM", "1"))
ATT_PARTS = int(_os.environ.get("ATT_PARTS", "31"))
STOP_AFTER = None


class Buf:
    __slots__ = ("name", "w", "wx", "r", "dsem", "dcnt", "grp")

    def __init__(self, name=""):
        self.name = name
        self.w = None
        self.wx = {}
        self.r = {}
        self.dsem = {}
        self.dcnt = {}
        self.grp = False


class Sync:
    ROLL = 30000

    def __init__(self, nc, es):
        self.nc = nc
        self.es = es
        self.eng = {"pe": nc.tensor, "act": nc.scalar, "dve": nc.vector, "pool": nc.gpsimd, "sp": nc.sync}
        self.sem = {}
        self.cnt = {}
        self.waited = {k: {} for k in self.eng}
        self.nsem = 0
        self.pe_sems = set()
        for k in self.eng:
            self._newsem(k)
        self.pend = []
        self.dbufs = []
        self.nops = {k: 0 for k in self.eng}
        self.nwaits = 0

    def _alloc(self, name):
        self.nsem += 1
        return self.es.enter_context(self.nc.semaphore(f"{name}{self.nsem}"))

    def _newsem(self, k):
        self.sem[k] = self._alloc("e" + k)
        self.cnt[k] = 0
        if k == "pe":
            self.pe_sems.add(id(self.sem[k]))

    def _wait(self, e, ev):
        semh, val = ev
        assert val is not None, "dependency on an unsignalled PE op"
        key = id(semh)
        if self.waited[e].get(key, 0) < val:
            self.eng[e].wait_ge(semh, val)
            self.waited[e][key] = val
            self.nwaits += 1

    def _dep1(self, e, ev, acc):
        if e == "pe" and (ev[1] is None or id(ev[0]) in self.pe_sems):
            return
        assert ev[1] is not None, "dependency on an unsignalled PE op"
        k = id(ev[0])
        if k not in acc or acc[k][1] < ev[1]:
            acc[k] = ev

    def _deps(self, e, reads, writes, group=False):
        acc = {}
        for b in reads:
            if b.w is not None:
                self._dep1(e, b.w, acc)
            for ev in b.wx.values():
                self._dep1(e, ev, acc)
        for b in writes:
            if not (group and b.grp):
                if b.w is not None:
                    self._dep1(e, b.w, acc)
                for ev in b.wx.values():
                    self._dep1(e, ev, acc)
            for ev in b.r.values():
                self._dep1(e, ev, acc)
        for ev in acc.values():
            self._wait(e, ev)

    def _mark(self, ev, reads, writes, key, group=False):
        for b in reads:
            b.r[key] = ev
        for b in writes:
            if group and b.grp:
                b.wx[key] = ev
            elif group:
                b.w = None
                b.wx = {key: ev}
            else:
                b.w = ev
                b.wx = {}
            b.grp = group
            b.r = {}

    def op(self, e, fn, reads=(), writes=(), sig=True):
        self._deps(e, reads, writes)
        ins = fn()
        self.nops[e] += 1
        if sig:
            if self.cnt[e] >= self.ROLL:
                self._newsem(e)
            self.cnt[e] += 1
            ins.then_inc(self.sem[e], 1)
            ev = [self.sem[e], self.cnt[e]]
            if e == "pe":
                for p in self.pend:
                    p[0] = self.sem[e]
                    p[1] = self.cnt[e]
                self.pend = []
        else:
            assert e == "pe"
            ev = [self.sem[e], None]
            self.pend.append(ev)
        self._mark(ev, reads, writes, e)
        return ins

    def _dma_common(self, q, issue, reads, writes, group, semof):
        d = semof if semof is not None else writes[0]
        c = "sw" if q == "pool" else "hw"
        if c not in d.dsem:
            d.dsem[c] = self._alloc("d")
            d.dcnt[c] = 0
            self.dbufs.append((d, c))
        self._deps(q, reads, writes, group=group)
        ins = issue()
        self.nops[q] += 1
        d.dcnt[c] += 16
        ins.then_inc(d.dsem[c], 16)
        ev = [d.dsem[c], d.dcnt[c]]
        self._mark(ev, reads, writes, id(d.dsem[c]), group=group)
        return ins

    def dma(self, q, out, in_, reads=(), writes=(), group=False, semof=None):
        return self._dma_common(q, lambda: self.eng[q].dma_start(out=out, in_=in_), reads, writes, group, semof)

    def dma_ind(self, out, out_off, in_, in_off, bound, reads=(), writes=(), group=False, semof=None, bcheck=None):
        def issue():
            if bcheck is not None:
                return self.nc.gpsimd.indirect_dma_start(out=out, out_offset=out_off, in_=in_, in_offset=in_off,
                                                         bounds_check=bcheck, oob_is_err=False)
            return self.nc.gpsimd.indirect_dma_start(out=out, out_offset=out_off, in_=in_, in_offset=in_off)
        return self._dma_common("pool", issue, reads, writes, group, semof)

    def barrier(self):
        assert not self.pend
        evs = [[self.sem[k], self.cnt[k]] for k in self.eng if k != "sp" and self.cnt[k] > 0]
        evs += [[b.dsem[c], b.dcnt[c]] for (b, c) in self.dbufs if b.dcnt[c] > 0]
        for ev in evs:
            self._wait("sp", ev)
        if self.cnt["sp"] >= self.ROLL:
            self._newsem("sp")
        self.cnt["sp"] += 1
        self.nc.sync.nop().then_inc(self.sem["sp"], 1)
        ev = [self.sem["sp"], self.cnt["sp"]]
        for k in self.eng:
            if k != "sp":
                self._wait(k, ev)


def build_program():
    nc = bass.Bass("TRN2", target_bir_lowering=False)

    def din(name, shape, dt=F32):
        return nc.dram_tensor(name, list(shape), dt, kind="ExternalInput").ap()

    xin = din("xin", [NB, L, D])
    ctxin = din("ctxin", [NB, C, D])
    cs_d = din("cs", [128, 24])
    wmod_d = din("wmod", [128, 8, 6 * D])
    vecs_d = din("vecs", [128, NV])
    wqkv_d = din("wqkv", [4, 128, 8 * 640])
    wlxlg_d = din("wlxlg", [8, 128, 8 * 256])
    wgagb_d = din("wgagb", [8, 128, 8 * 256])
    wup_d = din("wup", [8, 128, 12 * 128])
    wout_d = din("wout", [128, 8 * D])
    lruw_d = din("lruw", [128, 4 * 8 * 128])
    cossin_d = din("cossin", [128, 2 * L])
    btab_d = din("btab", [64, 8 * NTB * 64])
    wr_d = din("wr", [128, 8 * 36])
    brb_d = din("brb", [128, 36])
    gfb_d = din("gfb", [128, D])
    gffnb_d = din("gffnb", [128, D])
    w1_d = din("w1h", [32 * 128, 8 * 512])
    w3_d = din("w3h", [32 * 128, 8 * 512])
    w2_d = din("w2h", [32 * 128, 4 * 1024])
    ident_d = din("ident", [128, 128])
    iota_d = din("iota", [128, 1])
    utri_d = din("utri", [128, 128])
    out_d = nc.dram_tensor("out", [NB, L, D], F32, kind="ExternalOutput").ap()
    x1_d = nc.dram_tensor("x1s", [NTOK, D], F32, kind="Internal").ap()
    h2_d = nc.dram_tensor("h2s", [NTOK, D], BF16, kind="Internal").ap()
    xs_d = nc.dram_tensor("xss", [NROWS, D], BF16, kind="Internal").ap()
    ys_d = nc.dram_tensor("yss", [NROWS, D], BF16, kind="Internal").ap()
    dbg_d = {}
    for name, (shape, dt) in DEBUG.items():
        dbg_d[name] = nc.dram_tensor("dbg_" + name, list(shape), dt, kind="ExternalOutput").ap()

    with ExitStack() as es:
        S = Sync(nc, es)
        uid = [0]

        def sb(es_, shape, dt, name="t"):
            uid[0] += 1
            return es_.enter_context(nc.sbuf_tensor(f"{name}{uid[0]}", list(shape), dt))

        def ps(es_, shape, dt, name="p"):
            uid[0] += 1
            return es_.enter_context(nc.psum_tensor(f"{name}{uid[0]}", list(shape), dt))

        def pe_mm(out, lhsT, rhs, start, stop, reads, writes, sig=None):
            if sig is None:
                sig = stop
            return S.op("pe", lambda: nc.tensor.matmul(out, lhsT, rhs, start=start, stop=stop),
                        reads, writes, sig)

        def pe_tr(out, in_, ident, reads, writes, sig):
            return S.op("pe", lambda: nc.tensor.transpose(out, in_, ident), reads, writes, sig)

        def act(out, in_, func, reads, writes, **kw):
            return S.op("act", lambda: nc.scalar.activation(out=out, in_=in_, func=func, **kw), reads, writes)

        def V(e, fn, reads, writes):
            return S.op(e, fn, reads, writes)

        dbg_buf = Buf("dbg")

        def dump(name, ap, reads):
            if name in dbg_d:
                S.dma("sp", dbg_d[name], ap, reads=reads, writes=[dbg_buf], group=True, semof=Buf("dump_" + name))

        identf = sb(es, [128, 128], F32, "identf"); b_identf = Buf()
        identb = sb(es, [128, 128], BF16, "identb"); b_identb = Buf()
        onesf = sb(es, [128, 128], F32, "onesf"); b_onesf = Buf()
        onesb = sb(es, [128, 128], BF16, "onesb"); b_onesb = Buf()
        utri = sb(es, [128, 128], BF16, "utri"); b_utri = Buf()
        iota = sb(es, [128, 1], F32, "iota"); b_iota = Buf()
        epst = sb(es, [128, 1], F32, "eps"); b_eps = Buf()
        vecs = sb(es, [128, NV], F32, "vecs"); b_vecs = Buf()
        modfm = sb(es, [128, 48, 3], F32, "modfm"); b_modfm = Buf()
        A1 = sb(es, [128, 8, 3], F32, "A1"); b_A1 = Buf()
        lrup = sb(es, [128, 4, 16], F32, "lrup"); b_lrup = Buf()
        S.dma("sp", identf[:], ident_d, writes=[b_identf])
        S.dma("pool", identb[:], ident_d, writes=[b_identb])
        S.dma("pool", utri[:], utri_d, writes=[b_utri])
        S.dma("sp", iota[:], iota_d, writes=[b_iota])
        S.dma("sp", vecs[:], vecs_d, writes=[b_vecs])
        V("dve", lambda: nc.vector.memset(onesf[:], 1.0), [], [b_onesf])
        V("dve", lambda: nc.vector.memset(onesb[:], 1.0), [], [b_onesb])
        V("dve", lambda: nc.vector.memset(epst[:], EPS), [], [b_eps])

        with ExitStack() as es0:
            csb = sb(es0, [128, 24], F32, "cs"); b_cs = Buf()
            scs = sb(es0, [128, 24], BF16, "scs"); b_scs = Buf()
            wm = [sb(es0, [128, 8, 512], BF16, "wm") for _ in range(3)]
            b_wm = [Buf(), Buf(), Buf()]
            psmod = ps(es0, [128, 144], F32, "psmod"); b_psmod = Buf()
            S.dma("sp", csb[:], cs_d, writes=[b_cs])
            act(scs[:], csb[:], AF.Silu, [b_cs], [b_scs])
            for cb in range(12):
                w = wm[cb % 3]; bw = b_wm[cb % 3]
                S.dma("pool", w[:], wmod_d[:, :, cb * 512:(cb + 1) * 512], writes=[bw])
                for cc in range(4):
                    col = cb * 4 + cc
                    for k in range(8):
                        pe_mm(psmod[:, col * 3:(col + 1) * 3], w[:, k, cc * 128:(cc + 1) * 128],
                              scs[:, k * 3:(k + 1) * 3], k == 0, k == 7, [bw, b_scs], [b_psmod],
                              sig=(k == 7 and cc == 3))
            V("dve", lambda: nc.vector.tensor_tensor(
                out=modfm[:], in0=psmod[:, :].rearrange("p (a b) -> p a b", b=3),
                in1=vecs[:, V_BMOD:V_BMOD + 48].unsqueeze(2).to_broadcast([128, 48, 3]), op=ALU.add),
              [b_psmod, b_vecs], [b_modfm])
            V("dve", lambda: nc.vector.scalar_tensor_tensor(
                out=A1[:], in0=modfm[:, 8:16, :], scalar=1.0,
                in1=vecs[:, V_GMIX:V_GMIX + 8].unsqueeze(2).to_broadcast([128, 8, 3]),
                op0=ALU.add, op1=ALU.mult), [b_modfm, b_vecs], [b_A1])
            act(lrup[:, 2, :], vecs[:, V_LAM:V_LAM + 16], AF.Exp, [b_vecs], [b_lrup], scale=-1.0)
            act(lrup[:, 3, :], lrup[:, 2, :], AF.Ln, [b_lrup], [b_lrup], bias=1.0)
            V("dve", lambda: nc.vector.tensor_scalar(out=lrup[:, 0, :], in0=lrup[:, 3, :], scalar1=-8.0, scalar2=None,
                                                     op0=ALU.mult), [b_lrup], [b_lrup])
            V("dve", lambda: nc.vector.tensor_scalar(out=lrup[:, 1, :], in0=lrup[:, 3, :], scalar1=-16.0, scalar2=None,
                                                     op0=ALU.mult), [b_lrup], [b_lrup])
            dump("modfm", modfm[:, :, :].rearrange("p a b -> p (a b)"), [b_modfm])
            S.barrier()

        def bcast_row(es_, v, j, psb, b_psb, diag, b_diag):
            for k in range(8):
                V("dve", lambda: nc.vector.tensor_scalar(out=diag[k % 2][:], in0=identf[:],
                                                         scalar1=modfm[:, v * 8 + k, j:j + 1], scalar2=None,
                                                         op0=ALU.mult), [b_identf, b_modfm], [b_diag[k % 2]])
                pe_mm(psb[:, k * 128:(k + 1) * 128], onesf[:], diag[k % 2][:], True, True,
                      [b_onesf, b_diag[k % 2]], [b_psb], sig=True)

        Mall = sb(es, [128, 32, 32], BF16, "Mall"); b_Mall = Buf()
        oh1all = sb(es, [128, 32, 32], BF16, "oh1"); b_oh1 = Buf()
        gates = sb(es, [128, 32, 2], F32, "gates"); b_gates = Buf()
        dest_f = sb(es, [128, 32, 2], F32, "destf"); b_destf = Buf()
        dest_i = sb(es, [128, 64], I32, "desti"); b_desti = Buf()
        b_x1d = Buf("x1d"); b_h2d = Buf("h2d"); b_xsd = Buf("xsd"); b_ysd = Buf("ysd"); b_outd = Buf("outd")
        zt = sb(es, [128, 2, D], BF16, "zt"); b_zt = Buf()
        V("dve", lambda: nc.vector.memset(zt[:], 0.0), [], [b_zt])

        for b in range(NB):
            with ExitStack() as esb:
                hT = sb(esb, [128, 8, LC], BF16, "hT")
                b_hT = [[Buf(f"hT{i}a"), Buf(f"hT{i}b")] for i in range(5)]
                oatt = sb(esb, [128, 4, L], BF16, "oatt")
                b_oatt = [[Buf() for _ in range(4)] for _ in range(4)]

                with ExitStack() as es1:
                    xt = [sb(es1, [128, D], F32, "xt") for _ in range(3)]
                    b_xt = [Buf() for _ in range(3)]
                    junk = sb(es1, [128, D], BF16, "junk"); b_junk = Buf()
                    xn = [sb(es1, [128, D], BF16, "xn") for _ in range(8)]
                    b_xn = [Buf() for _ in range(8)]
                    st = sb(es1, [128, 18, 3], F32, "st"); b_st = [Buf() for _ in range(18)]
                    pst = [ps(es1, [128, 512], BF16, "pst") for _ in range(4)]
                    b_pst = [Buf() for _ in range(4)]
                    ti = 0
                    for grp in range(5):
                        ntile = 4 if grp < 4 else 2
                        jmod = b if grp < 4 else 2
                        for i in range(ntile):
                            t = grp * 4 + i
                            xb_, bx_ = xt[ti % 3], b_xt[ti % 3]
                            src = xin[b, t * 128:(t + 1) * 128, :] if grp < 4 else ctxin[b, i * 128:(i + 1) * 128, :]
                            S.dma("sp", xb_[:], src, writes=[bx_])
                            act(junk[:], xb_[:], AF.Square, [bx_], [b_junk, b_st[t]], accum_out=st[:, t, 0:1])
                            act(st[:, t, 1:2], st[:, t, 0:1], AF.Sqrt, [b_st[t], b_eps], [b_st[t]],
                                scale=1.0 / D, bias=epst[:, 0:1])
                            V("dve", lambda: nc.vector.reciprocal(out=st[:, t, 2:3], in_=st[:, t, 1:2]),
                              [b_st[t]], [b_st[t]])
                            xi = (grp % 2) * 4 + i
                            act(xn[xi][:], xb_[:], AF.Copy, [bx_, b_st[t]], [b_xn[xi]], scale=st[:, t, 2:3])
                            ti += 1
                        for k in range(8):
                            pp, bp = pst[k % 4], b_pst[k % 4]
                            for i in range(ntile):
                                xi = (grp % 2) * 4 + i
                                pe_tr(pp[:, i * 128:(i + 1) * 128], xn[xi][:, k * 128:(k + 1) * 128], identb[:],
                                      [b_xn[xi], b_identb], [bp], sig=(i == ntile - 1))
                            n = ntile * 128
                            dst = hT[:, k, grp * 512:grp * 512 + n]
                            if k % 2 == 0:
                                V("dve", lambda: nc.vector.tensor_scalar(
                                    out=dst, in0=pp[:, 0:n], scalar1=A1[:, k, jmod:jmod + 1],
                                    scalar2=modfm[:, k, jmod:jmod + 1], op0=ALU.mult, op1=ALU.add),
                                  [bp, b_A1, b_modfm], [b_hT[grp][0]])
                            else:
                                act(dst, pp[:, 0:n], AF.Identity, [bp, b_A1, b_modfm], [b_hT[grp][1]],
                                    scale=A1[:, k, jmod:jmod + 1], bias=modfm[:, k, jmod:jmod + 1])
                    if b == 0:
                        dump("hT", hT[:, :, :].rearrange("p a b -> p (a b)"), [x for l_ in b_hT for x in l_])
                    S.barrier()
                if STOP_AFTER == "s1":
                    break

                with ExitStack() as es2:
                    cs_t = sb(es2, [128, 2 * L], F32, "cossin"); b_cst = Buf()
                    btab = sb(es2, [128, 8, NTB * 64], BF16, "btab"); b_btab = Buf()
                    S.dma("sp", cs_t[:], cossin_d, writes=[b_cst])
                    S.dma("pool", btab[0:64, :, :].rearrange("p a b -> p (a b)"), btab_d, writes=[b_btab], group=True)
                    S.dma("pool", btab[64:128, :, :].rearrange("p a b -> p (a b)"), btab_d, writes=[b_btab], group=True)
                    wq = [sb(es2, [128, 8, 640], BF16, "wq") for _ in range(1)]; b_wq = [Buf()]
                    Qr = [sb(es2, [128, L], BF16, "Qr") for _ in range(1)]
                    Qp = [sb(es2, [128, L], BF16, "Qp") for _ in range(1)]
                    Kr = [sb(es2, [128, L], BF16, "Kr") for _ in range(1)]
                    Kc = [sb(es2, [128, C], BF16, "Kc") for _ in range(1)]
                    Vx = [sb(es2, [128, 18, 192], BF16, "Vx") for _ in range(1)]
                    b_Q = [[Buf() for _ in range(4)] for _ in range(1)]
                    b_Qp = [[Buf() for _ in range(4)] for _ in range(1)]
                    b_K = [Buf()]
                    b_Kc = [Buf()]
                    b_V = [[Buf() for _ in range(5)]]
                    t1 = [sb(es2, [128, 512], F32, "t1") for _ in range(2)]; b_t1 = [Buf(), Buf()]
                    t2 = [sb(es2, [128, 512], F32, "t2") for _ in range(2)]; b_t2 = [Buf(), Buf()]
                    PTc = [sb(es2, [128, 512], BF16, "PTc") for _ in range(2)]; b_PTc = [Buf(), Buf()]
                    PT = [sb(es2, [128, 320], BF16, "PT") for _ in range(3)]; b_PT = [Buf() for _ in range(3)]
                    rc = [sb(es2, [128, 512], F32, "rc") for _ in range(2)]; b_rc = [Buf(), Buf()]
                    psP = [ps(es2, [128, 512], F32, "psP") for _ in range(2)]; b_psP = [Buf(), Buf()]
                    psSc = [ps(es2, [128, 512], F32, "psSc") for _ in range(2)]; b_psSc = [Buf(), Buf()]
                    psS = [ps(es2, [128, 512], F32, "psS") for _ in range(2)]; b_psS = [Buf(), Buf()]
                    psO = [ps(es2, [128, 512], F32, "psO") for _ in range(2)]; b_psO = [Buf(), Buf()]
                    V("dve", lambda: nc.vector.memset(Vx[0][:, :, 64:128], 1.0), [], b_V[0])
                    pcnt = [0]

                    def nextP():
                        i = pcnt[0] % 2
                        pcnt[0] += 1
                        return psP[i], b_psP[i]

                    cnt_t = [0]
                    for hp in range(ATT_NHP):
                        par = 0
                        w = wq[par]; bw = b_wq[par]
                        S.dma("pool", w[:, :, :].rearrange("p a b -> p (a b)"), wqkv_d[hp], writes=[bw])
                        for blk in range(4 if ATT_PARTS & 1 else 0):
                            tok = slice(blk * 512, (blk + 1) * 512)
                            for which in range(2):
                                c0 = which * 256
                                pa, bpa = nextP()
                                for k in range(8):
                                    pe_mm(pa[:], w[:, k, c0:c0 + 128], hT[:, k, tok], k == 0, k == 7,
                                          [bw, *b_hT[blk]], [bpa])
                                pb, bpb = nextP()
                                for k in range(8):
                                    pe_mm(pb[:], w[:, k, c0 + 128:c0 + 256], hT[:, k, tok], k == 0, k == 7,
                                          [bw, *b_hT[blk]], [bpb])
                                ii = cnt_t[0] % 2
                                cnt_t[0] += 1
                                sc_ = 0.125 if which == 0 else 1.0
                                dstb = b_Q[par][blk] if which == 0 else b_K[par]
                                dst = (Qr if which == 0 else Kr)[par][:, tok]
                                V("dve", lambda: nc.vector.scalar_tensor_tensor(
                                    out=t1[ii][:], in0=pa[:], scalar=sc_, in1=cs_t[:, tok], op0=ALU.mult, op1=ALU.mult),
                                  [bpa, b_cst], [b_t1[ii]])
                                if which == 0 and (ATT_PARTS & 16):
                                    V("dve", lambda: nc.vector.tensor_scalar(out=Qp[par][:, tok], in0=pa[:], scalar1=0.125, scalar2=None,
                                                                             op0=ALU.mult), [bpa], [b_Qp[par][blk]])
                                V("dve", lambda: nc.vector.scalar_tensor_tensor(
                                    out=t2[ii][:], in0=pb[:], scalar=sc_, in1=cs_t[:, L + blk * 512:L + (blk + 1) * 512],
                                    op0=ALU.mult, op1=ALU.mult), [bpb, b_cst], [b_t2[ii]])
                                V("pool" if ATT_PARTS & 8 else "dve", lambda: (nc.gpsimd if ATT_PARTS & 8 else nc.vector).tensor_tensor(out=dst, in0=t1[ii][:], in1=t2[ii][:], op=ALU.add),
                                  [b_t1[ii], b_t2[ii]], [dstb])
                        if ATT_PARTS & 2:
                            pa, bpa = nextP()
                            for k in range(8):
                                pe_mm(pa[:, 0:C], w[:, k, 256:384], hT[:, k, L:LC], k == 0, k == 7, [bw, *b_hT[4]], [bpa])
                            act(Kc[par][:], pa[:, 0:C], AF.Copy, [bpa], [b_Kc[par]])
                        for g4 in range(5 if ATT_PARTS & 4 else 0):
                            nch = 4 if g4 < 4 else 2
                            pa, bpa = nextP()
                            for i in range(nch):
                                ch = g4 * 4 + i
                                for k in range(8):
                                    pe_mm(pa[:, i * 128:(i + 1) * 128], hT[:, k, ch * 128:(ch + 1) * 128],
                                          w[:, k, 512:640], k == 0, k == 7, [bw, *b_hT[g4]], [bpa],
                                          sig=(k == 7 and i == nch - 1))
                            src = pa[:, 0:nch * 128].rearrange("p (c a d) -> p c a d", a=2, d=64)
                            dstv = Vx[par][:, g4 * 4:g4 * 4 + nch, :].rearrange("p c (a d) -> p c a d", d=64)[:, :, 0::2, :]
                            if g4 % 2 == 0:
                                act(dstv, src, AF.Copy, [bpa], [b_V[par][g4]])
                            else:
                                V("dve", lambda: nc.vector.tensor_copy(out=dstv, in_=src), [bpa], [b_V[par][g4]])

                        if b == 0 and hp == 0:
                            dump("Qp", Qp[0][:], b_Qp[0])
                            dump("Qr", Qr[0][:], b_Q[0])
                            dump("Kr", Kr[0][:], b_K)
                            dump("Kc", Kc[0][:], b_Kc)
                            dump("Vx", Vx[0][:, :, :].rearrange("p a b -> p (a b)"), b_V[0])
                        units = []
                        for e in range(2):
                            for qb in range(4):
                                units.append((e, qb))
                        ucnt = [0]
                        for (e, qb) in units[int(_os.environ.get("ATT_USTART", "0")):][:ATT_NUNITS]:
                            h = hp * 2 + e
                            pr = slice(64 * e, 64 * e + 64)
                            qs = slice(qb * 512, (qb + 1) * 512)
                            vcols = slice(0, 128) if e == 0 else slice(64, 192)
                            oi = ucnt[0] % 2
                            ucnt[0] += 1
                            pO, bO = psO[oi], b_psO[oi]
                            for c in range(2):
                                pS, bS = psSc[c], b_psSc[c]
                                pe_mm(pS[:], Kc[par][pr, c * 128:(c + 1) * 128], Qp[par][pr, qs], True, True,
                                      [b_Kc[par], b_Qp[par][qb]], [bS])
                                act(PTc[c][:], pS[:], AF.Exp, [bS], [b_PTc[c]])
                            if b == 0 and hp == 0 and e == 0 and qb == 0:
                                dump("PTc", PTc[0][:], [b_PTc[0]])
                            for c in range(2):
                                pe_mm(pO[:], Vx[par][:, 16 + c, vcols], PTc[c][:], c == 0, False,
                                      [b_V[par][4], b_PTc[c]], [bO], sig=(c == 1))
                            rows = list(range(qb * 8, qb * 8 + ATT_NROWS))
                            plan = []
                            for r in rows:
                                rs = min(max(r - 4, 0), 24)
                                dr0 = rs - r + 7
                                chunks = []
                                if rs % 2 == 0:
                                    for j in range(4):
                                        chunks.append(((rs + 2 * j) // 2, 1 + dr0 + 2 * j))
                                else:
                                    assert dr0 == 3
                                    c0 = (rs - 1) // 2
                                    chunks.append((c0, 17))
                                    for j in range(1, 4):
                                        chunks.append((c0 + j, dr0 + 2 * j))
                                    chunks.append((c0 + 4, 19))
                                plan.append((r, chunks))

                            def emit_qk(idx):
                                r, chunks = plan[idx]
                                si = idx % 2
                                pS, bS = psS[si], b_psS[si]
                                qcol = slice(r * 64, (r + 1) * 64)
                                for j, (kc, blk0) in enumerate(chunks):
                                    o = pS[:, j * 64:(j + 1) * 64]
                                    pe_mm(o, Kr[par][pr, kc * 128:(kc + 1) * 128], Qr[par][pr, qcol], True, False,
                                          [b_K[par], b_Q[par][qb]], [bS], sig=False)
                                    lt = btab[pr, h, blk0 * 64:(blk0 + 2) * 64]
                                    pe_mm(o, lt, identb[pr, pr], False, True, [b_btab, b_identb], [bS],
                                          sig=(j == len(chunks) - 1))
                                n = len(chunks) * 64
                                pi = idx % 3
                                act(PT[pi][:, 0:n], pS[:, 0:n], AF.Exp, [bS], [b_PT[pi]])

                            def emit_pv(idx):
                                r, chunks = plan[idx]
                                pi = idx % 3
                                rr = r - qb * 8
                                for j, (kc, _) in enumerate(chunks):
                                    last = (j == len(chunks) - 1)
                                    pe_mm(pO[:, rr * 64:(rr + 1) * 64], Vx[par][:, kc, vcols], PT[pi][:, j * 64:(j + 1) * 64],
                                          False, last and idx == len(plan) - 1, [b_V[par][kc // 4], b_PT[pi]], [bO], sig=last)

                            if plan:
                                emit_qk(0)
                            for idx in range(len(plan)):
                                if idx + 1 < len(plan):
                                    emit_qk(idx + 1)
                                emit_pv(idx)
                            dn = slice(64, 128) if e == 0 else slice(0, 64)
                            if not ATT_NORM:
                                continue
                            V("dve", lambda: nc.vector.reciprocal(out=rc[oi][pr, :], in_=pO[dn, :]), [bO], [b_rc[oi]])
                            V("dve", lambda: nc.vector.tensor_tensor(out=oatt[pr, hp, qs], in0=pO[pr, :], in1=rc[oi][pr, :],
                                                                     op=ALU.mult), [bO, b_rc[oi]], [b_oatt[hp][qb]])
                    if b == 0:
                        dump("oatt", oatt[:, :, :].rearrange("p a b -> p (a b)"), [x for l_ in b_oatt for x in l_])
                    S.barrier()
                if STOP_AFTER == "s2":
                    break

                olru = sb(esb, [128, 8, L], BF16, "olru")
                b_olru = [Buf() for _ in range(8)]
                with ExitStack() as es3:
                    TL = LC + 3
                    wl = [sb(es3, [128, 8, 256], BF16, "wl") for _ in range(2)]; b_wl = [Buf(), Buf()]
                    lw = sb(es3, [128, 4, 8, 128], BF16, "lw"); b_lw = Buf()
                    S.dma("pool", lw[:, :, :, :].rearrange("p a b c -> p (a b c)"), lruw_d, writes=[b_lw])
                    LXp = sb(es3, [128, TL + 3], F32, "LXp"); b_LXp = Buf()
                    xc = sb(es3, [128, TL], F32, "xc"); b_xc = Buf()
                    xcb = [sb(es3, [128, TL], BF16, "xcb") for _ in range(2)]; b_xcb = [Buf(), Buf()]
                    av = sb(es3, [128, TL], F32, "av"); b_av = Buf()
                    wv = sb(es3, [128, TL], F32, "wv"); b_wv = Buf()
                    iv = sb(es3, [128, TL], F32, "iv"); b_iv = Buf()
                    hv = [sb(es3, [128, TL], F32, "hv") for _ in range(2)]; b_hv = [Buf(), Buf()]
                    gl = sb(es3, [128, L], BF16, "gl"); b_gl = Buf()
                    psA = [ps(es3, [128, 512], F32, "psA") for _ in range(4)]; b_psA = [Buf() for _ in range(4)]
                    pacnt = [0]

                    def nextA():
                        i = pacnt[0] % 4
                        pacnt[0] += 1
                        return psA[i], b_psA[i]

                    V("dve", lambda: nc.vector.memset(LXp[:], 0.0), [], [b_LXp])
                    if b == 0:
                        for blk in range(NBLK):
                            S.dma("sp", xs_d[blk * 256:(blk + 1) * 256, :].rearrange("(a p) d -> p a d", p=128), zt[:],
                                  reads=[b_zt], writes=[b_xsd], group=True, semof=b_zt)

                    def load_wl(n):
                        S.dma("pool", wl[n % 2][:, :, :].rearrange("p a b -> p (a b)"), wlxlg_d[n], writes=[b_wl[n % 2]])

                    def lx_conv(n):
                        w = wl[n % 2]; bw = b_wl[n % 2]
                        for blk in range(5):
                            nt = 512 if blk < 4 else C
                            tok = slice(blk * 512, blk * 512 + nt)
                            pa, bpa = nextA()
                            for k in range(8):
                                pe_mm(pa[:, 0:nt], w[:, k, 0:128], hT[:, k, tok], k == 0, k == 7, [bw, *b_hT[blk]], [bpa])
                            d0 = 261 + blk * 512 if blk < 4 else 2
                            act(LXp[:, d0:d0 + nt], pa[:, 0:nt], AF.Copy, [bpa], [b_LXp])
                        cw = lambda j: vecs[:, V_CONVW + j * 8 + n:V_CONVW + j * 8 + n + 1]
                        V("dve", lambda: nc.vector.tensor_scalar(out=xc[:], in0=LXp[:, 0:TL], scalar1=cw(0),
                                                                 scalar2=vecs[:, V_CONVB + n:V_CONVB + n + 1],
                                                                 op0=ALU.mult, op1=ALU.add), [b_LXp, b_vecs], [b_xc])
                        for j in range(1, 3):
                            V("dve", lambda: nc.vector.scalar_tensor_tensor(out=xc[:], in0=LXp[:, j:j + TL], scalar=cw(j),
                                                                            in1=xc[:], op0=ALU.mult, op1=ALU.add),
                              [b_LXp, b_vecs, b_xc], [b_xc])
                        V("dve", lambda: nc.vector.scalar_tensor_tensor(out=xcb[n % 2][:], in0=LXp[:, 3:3 + TL], scalar=cw(3),
                                                                        in1=xc[:], op0=ALU.mult, op1=ALU.add),
                          [b_LXp, b_vecs, b_xc], [b_xcb[n % 2]])

                    load_wl(0)
                    lx_conv(0)
                    for n in range(8):
                        w = wl[n % 2]; bw = b_wl[n % 2]
                        xb_ = xcb[n % 2]; bxb = b_xcb[n % 2]
                        if n + 1 < 8:
                            load_wl(n + 1)
                        if b == 0 and n == 0:
                            dump("xc0", xb_[:], [bxb])
                        for dr in range(2):
                            di = dr * 8 + n
                            for blk in range(5):
                                nt = 512 if blk < 4 else TL - 2048
                                tok = slice(blk * 512, blk * 512 + nt)
                                pr_, bpr = nextA()
                                pe_mm(pr_[:, 0:nt], lw[:, dr, n, :], xb_[:, tok], True, True, [b_lw, bxb], [bpr])
                                pi_, bpi = nextA()
                                pe_mm(pi_[:, 0:nt], lw[:, 2 + dr, n, :], xb_[:, tok], True, True, [b_lw, bxb], [bpi])
                                act(av[:, tok], pr_[:, 0:nt], AF.Sigmoid, [bpr, b_vecs], [b_av],
                                    bias=vecs[:, V_BA + di:V_BA + di + 1])
                                act(iv[:, tok], pi_[:, 0:nt], AF.Sigmoid, [bpi, b_vecs], [b_iv],
                                    bias=vecs[:, V_BX + di:V_BX + di + 1])
                            act(wv[:], av[:], AF.Exp, [b_av, b_lrup], [b_wv], scale=lrup[:, 1, di:di + 1])
                            act(av[:], av[:], AF.Exp, [b_av, b_lrup], [b_av], scale=lrup[:, 0, di:di + 1])
                            act(wv[:], wv[:], AF.Sqrt, [b_wv], [b_wv], scale=-1.0, bias=1.0)
                            V("pool", lambda: nc.gpsimd.tensor_tensor(out=iv[:], in0=iv[:], in1=xb_[:], op=ALU.mult),
                              [b_iv, bxb], [b_iv])
                            V("dve", lambda: nc.vector.tensor_tensor(out=wv[:], in0=iv[:], in1=wv[:], op=ALU.mult),
                              [b_iv, b_wv], [b_wv])
                            hh = hv[dr]; bh = b_hv[dr]
                            if dr == 0:
                                V("dve", lambda: nc.vector.tensor_tensor_scan(
                                    out=hh[:, 0:C], data0=av[:, 0:C], data1=wv[:, 0:C], initial=0.0,
                                    op0=ALU.mult, op1=ALU.add), [b_av, b_wv], [bh])
                                V("dve", lambda: nc.vector.tensor_tensor_scan(
                                    out=hh[:, C + 3:TL], data0=av[:, C + 3:TL], data1=wv[:, C + 3:TL],
                                    initial=hh[:, C - 1:C], op0=ALU.mult, op1=ALU.add), [b_av, b_wv, bh], [bh])
                                if n + 1 < 8:
                                    lx_conv(n + 1)
                            else:
                                V("dve", lambda: nc.vector.tensor_tensor_scan(
                                    out=hh[:, C - 1::-1], data0=av[:, C - 1::-1], data1=wv[:, C - 1::-1], initial=0.0,
                                    op0=ALU.mult, op1=ALU.add), [b_av, b_wv], [bh])
                                V("dve", lambda: nc.vector.tensor_tensor_scan(
                                    out=hh[:, TL - 1:C + 2:-1], data0=av[:, TL - 1:C + 2:-1], data1=wv[:, TL - 1:C + 2:-1],
                                    initial=hh[:, 0:1], op0=ALU.mult, op1=ALU.add), [b_av, b_wv, bh], [bh])
                        for blk in range(4):
                            tok = slice(blk * 512, (blk + 1) * 512)
                            pa, bpa = nextA()
                            for k in range(8):
                                pe_mm(pa[:], w[:, k, 128:256], hT[:, k, tok], k == 0, k == 7, [bw, *b_hT[blk]], [bpa])
                            act(gl[:, tok], pa[:], AF.Gelu_apprx_tanh, [bpa], [b_gl])
                        V("dve", lambda: nc.vector.tensor_tensor(out=hv[0][:, C + 3:TL], in0=hv[0][:, C + 3:TL],
                                                                 in1=hv[1][:, C + 3:TL], op=ALU.add),
                          [b_hv[0], b_hv[1]], [b_hv[0]])
                        if b == 0 and n == 0:
                            dump("hsum0", hv[0][:, C + 3:TL], [b_hv[0]])
                        V("dve", lambda: nc.vector.tensor_tensor(out=olru[:, n, :], in0=hv[0][:, C + 3:TL], in1=gl[:],
                                                                 op=ALU.mult), [b_hv[0], b_gl], [b_olru[n]])
                    if b == 0:
                        dump("olru", olru[:, :, :].rearrange("p a b -> p (a b)"), b_olru)
                    S.barrier()
                if STOP_AFTER == "s3":
                    break

                yT = sb(esb, [128, 8, L], BF16, "yT")
                b_yT = [Buf() for _ in range(4)]
                with ExitStack() as es4:
                    wg_ = [sb(es4, [128, 8, 256], BF16, "wg") for _ in range(2)]; b_wg = [Buf(), Buf()]
                    wu_ = [sb(es4, [128, 12, 128], BF16, "wu") for _ in range(2)]; b_wu = [Buf(), Buf()]
                    ga_ = [sb(es4, [128, 512], F32, "ga") for _ in range(2)]; b_ga = [Buf(), Buf()]
                    gb_ = [sb(es4, [128, 512], F32, "gb") for _ in range(2)]; b_gb = [Buf(), Buf()]
                    ya_ = [sb(es4, [128, 512], F32, "ya") for _ in range(2)]; b_ya = [Buf(), Buf()]
                    yb_ = [sb(es4, [128, 512], F32, "yb") for _ in range(2)]; b_yb = [Buf(), Buf()]
                    psM = [ps(es4, [128, 512], F32, "psM") for _ in range(8)]; b_psM = [Buf() for _ in range(8)]
                    it = 0
                    for f in range(8):
                        wgt, bwg = wg_[f % 2], b_wg[f % 2]
                        wut, bwu = wu_[f % 2], b_wu[f % 2]
                        S.dma("pool", wgt[:, :, :].rearrange("p a b -> p (a b)"), wgagb_d[f], writes=[bwg])
                        S.dma("pool", wut[:, :, :].rearrange("p a b -> p (a b)"), wup_d[f], writes=[bwu])
                        for blk in range(4):
                            tok = slice(blk * 512, (blk + 1) * 512)
                            i2 = it % 2
                            p0, p1, p2, p3 = [psM[(it % 2) * 4 + q] for q in range(4)]
                            q0, q1, q2, q3 = [b_psM[(it % 2) * 4 + q] for q in range(4)]
                            it += 1
                            for k in range(8):
                                pe_mm(p0[:], wgt[:, k, 0:128], hT[:, k, tok], k == 0, k == 7, [bwg, *b_hT[blk]], [q0])
                            act(ga_[i2][:], p0[:], AF.Sigmoid, [q0], [b_ga[i2]])
                            for k in range(4):
                                pe_mm(p1[:], wut[:, k, :], oatt[:, k, tok], k == 0, k == 3, [bwu, b_oatt[k][blk]], [q1])
                            V("dve", lambda: nc.vector.tensor_tensor(out=ya_[i2][:], in0=p1[:], in1=ga_[i2][:], op=ALU.mult),
                              [q1, b_ga[i2]], [b_ya[i2]])
                            for k in range(8):
                                pe_mm(p2[:], wgt[:, k, 128:256], hT[:, k, tok], k == 0, k == 7, [bwg, *b_hT[blk]], [q2])
                            act(gb_[i2][:], p2[:], AF.Sigmoid, [q2], [b_gb[i2]])
                            for k in range(8):
                                pe_mm(p3[:], wut[:, 4 + k, :], olru[:, k, tok], k == 0, k == 7, [bwu, b_olru[k]], [q3])
                            V("dve", lambda: nc.vector.tensor_tensor(out=yb_[i2][:], in0=p3[:], in1=gb_[i2][:], op=ALU.mult),
                              [q3, b_gb[i2]], [b_yb[i2]])
                            V("pool", lambda: nc.gpsimd.tensor_tensor(out=yT[:, f, tok], in0=ya_[i2][:], in1=yb_[i2][:],
                                                                      op=ALU.add), [b_ya[i2], b_yb[i2]], [b_yT[blk]])
                    if b == 0:
                        dump("yT", yT[:, :, :].rearrange("p a b -> p (a b)"), b_yT)
                    S.barrier()
                if STOP_AFTER == "s4":
                    break

                with ExitStack() as es5:
                    wo32 = [sb(es5, [128, D], F32, "wo32") for _ in range(2)]; b_wo32 = [Buf(), Buf()]
                    wob = sb(es5, [128, 8, D], BF16, "wob"); b_wob = Buf()
                    GA1 = sb(es5, [128, D], F32, "GA1"); b_GA1 = Buf()
                    G2 = sb(es5, [128, D], F32, "G2"); b_G2 = Buf()
                    S2 = sb(es5, [128, D], F32, "S2"); b_S2 = Buf()
                    gffnb = sb(es5, [128, D], F32, "gffnb"); b_gffnb = Buf()
                    diag = [sb(es5, [128, 128], F32, "diag") for _ in range(2)]; b_diag = [Buf(), Buf()]
                    wr = sb(es5, [128, 8, 36], F32, "wr"); b_wr = Buf()
                    brb = sb(es5, [128, 36], F32, "brb"); b_brb = Buf()
                    psB = ps(es5, [128, D], F32, "psB"); b_psB = Buf()
                    psO5 = [ps(es5, [128, D], F32, "psO5") for _ in range(2)]; b_psO5 = [Buf(), Buf()]
                    psT = ps(es5, [128, D], F32, "psT"); b_psT = Buf()
                    S.dma("sp", gffnb[:], gffnb_d, writes=[b_gffnb])
                    S.dma("sp", wr[:, :, :].rearrange("p a b -> p (a b)"), wr_d, writes=[b_wr])
                    S.dma("sp", brb[:], brb_d, writes=[b_brb])
                    bcast_row(es5, 2, b, psB, b_psB, diag, b_diag)
                    V("dve", lambda: nc.vector.tensor_copy(out=GA1[:], in_=psB[:]), [b_psB], [b_GA1])
                    bcast_row(es5, 4, b, psB, b_psB, diag, b_diag)
                    V("dve", lambda: nc.vector.scalar_tensor_tensor(out=G2[:], in0=psB[:], scalar=1.0, in1=gffnb[:],
                                                                    op0=ALU.add, op1=ALU.mult), [b_psB, b_gffnb], [b_G2])
                    bcast_row(es5, 3, b, psB, b_psB, diag, b_diag)
                    V("dve", lambda: nc.vector.tensor_copy(out=S2[:], in_=psB[:]), [b_psB], [b_S2])
                    for kk in range(8):
                        S.dma("sp", wo32[kk % 2][:], wout_d[:, kk * D:(kk + 1) * D], writes=[b_wo32[kk % 2]])
                        V("dve", lambda: nc.vector.tensor_tensor(out=wob[:, kk, :], in0=wo32[kk % 2][:], in1=GA1[:],
                                                                 op=ALU.mult), [b_wo32[kk % 2], b_GA1], [b_wob])
                    xt5 = [sb(es5, [128, D], F32, "xt5") for _ in range(2)]; b_xt5 = [Buf(), Buf()]
                    x1t = xt5; b_x1t = b_xt5
                    h2t = [sb(es5, [128, D], F32, "h2t") for _ in range(2)]; b_h2t = [Buf(), Buf()]
                    h2b = [sb(es5, [128, D], BF16, "h2b") for _ in range(2)]; b_h2b = [Buf(), Buf()]
                    h2T = sb(es5, [128, 8, 128], F32, "h2T"); b_h2T = Buf()
                    junk5 = sb(es5, [128, D], BF16, "junk5"); b_junk5 = Buf()
                    st5 = sb(es5, [128, 16, 3], F32, "st5"); b_st5 = [Buf() for _ in range(16)]
                    LGa = sb(es5, [128, 16, 36], F32, "LGa"); b_LGa = Buf()
                    RW = sb(es5, [128, 8, 16], F32, "RW"); b_RW = Buf()
                    RG = sb(es5, [128, 16, 12], F32, "RG"); b_RG = Buf()
                    LEM = sb(es5, [128, 16, 32], F32, "LEM"); b_LEM = Buf()
                    LEM2 = sb(es5, [128, 16, 32], F32, "LEM2"); b_LEM2 = Buf()
                    def wout_mm(j):
                        i2 = j % 2
                        tsl = slice(j * 128, (j + 1) * 128)
                        S.dma("sp", xt5[i2][:], xin[b, tsl, :], writes=[b_xt5[i2]])
                        pO, bO = psO5[i2], b_psO5[i2]
                        for hf in range(2):
                            for k in range(8):
                                pe_mm(pO[:, hf * 512:(hf + 1) * 512], yT[:, k, tsl], wob[:, k, hf * 512:(hf + 1) * 512],
                                      k == 0, k == 7, [b_yT[j // 4], b_wob], [bO], sig=(k == 7 and hf == 1))

                    wout_mm(0)
                    for j in range(16):
                        tg = b * 16 + j
                        i2 = j % 2
                        tsl = slice(j * 128, (j + 1) * 128)
                        pO, bO = psO5[i2], b_psO5[i2]
                        if j + 1 < 16:
                            wout_mm(j + 1)
                        V("dve", lambda: nc.vector.tensor_tensor(out=x1t[i2][:], in0=pO[:], in1=xt5[i2][:], op=ALU.add),
                          [bO, b_xt5[i2]], [b_xt5[i2]])
                        S.dma("pool", x1_d[tg * 128:(tg + 1) * 128, :], x1t[i2][:], reads=[b_x1t[i2]], writes=[b_x1d], group=True, semof=b_x1t[i2])
                        act(junk5[:], x1t[i2][:], AF.Square, [b_x1t[i2]], [b_junk5, b_st5[j]], accum_out=st5[:, j, 0:1])
                        act(st5[:, j, 1:2], st5[:, j, 0:1], AF.Sqrt, [b_st5[j], b_eps], [b_st5[j]], scale=1.0 / D,
                            bias=epst[:, 0:1])
                        V("dve", lambda: nc.vector.reciprocal(out=st5[:, j, 2:3], in_=st5[:, j, 1:2]), [b_st5[j]], [b_st5[j]])
                        V("dve", lambda: nc.vector.scalar_tensor_tensor(out=h2t[i2][:], in0=x1t[i2][:], scalar=st5[:, j, 2:3],
                                                                        in1=G2[:], op0=ALU.mult, op1=ALU.mult),
                          [b_x1t[i2], b_st5[j], b_G2], [b_h2t[i2]])
                        V("dve", lambda: nc.vector.tensor_tensor(out=h2t[i2][:], in0=h2t[i2][:], in1=S2[:], op=ALU.add),
                          [b_h2t[i2], b_S2], [b_h2t[i2]])
                        act(h2b[i2][:], h2t[i2][:], AF.Copy, [b_h2t[i2]], [b_h2b[i2]])
                        S.dma("pool", h2_d[tg * 128:(tg + 1) * 128, :], h2b[i2][:], reads=[b_h2b[i2]], writes=[b_h2d], group=True, semof=b_h2b[i2])
                        for k in range(8):
                            pe_tr(psT[:, k * 128:(k + 1) * 128], h2t[i2][:, k * 128:(k + 1) * 128], identf[:],
                                  [b_h2t[i2], b_identf], [b_psT], sig=(k == 7))
                        act(h2T[:, :, :].rearrange("p a b -> p (a b)"), psT[:], AF.Copy, [b_psT], [b_h2T])
                        for k in range(8):
                            pe_mm(psB[:, 0:36], h2T[:, k, :], wr[:, k, :], k == 0, k == 7, [b_h2T, b_wr], [b_psB])
                        V("dve", lambda: nc.vector.tensor_tensor(out=LGa[:, j, :], in0=psB[:, 0:36], in1=brb[:], op=ALU.add),
                          [b_psB, b_brb], [b_LGa])
                        if tg == 0:
                            dump("logit0", LGa[:, 0, :], [b_LGa])
                    T16 = slice(b * 16, (b + 1) * 16)
                    lg = LGa[:, :, 0:4]
                    le = LGa[:, :, 4:36]
                    rb = [b_LGa, b_RW]
                    V("dve", lambda: nc.vector.tensor_reduce(out=RW[:, 0, 0:16], in_=lg, axis=AX.X, op=ALU.max), [b_LGa], [b_RW])
                    V("dve", lambda: nc.vector.tensor_tensor(out=RG[:, :, 0:4], in0=lg,
                                                             in1=RW[:, 0, 0:16].unsqueeze(2).to_broadcast([128, 16, 4]),
                                                             op=ALU.subtract), rb, [b_RG])
                    act(RG[:, :, 4:8], RG[:, :, 0:4], AF.Exp, [b_RG], [b_RG])
                    V("dve", lambda: nc.vector.tensor_reduce(out=RW[:, 1, 0:16], in_=RG[:, :, 4:8], axis=AX.X, op=ALU.add), [b_RG], [b_RW])
                    V("dve", lambda: nc.vector.reciprocal(out=RW[:, 2, 0:16], in_=RW[:, 1, 0:16]), [b_RW], [b_RW])
                    V("dve", lambda: nc.vector.tensor_scalar(out=RG[:, :, 8:12], in0=RG[:, :, 0:4], scalar1=0.0, scalar2=None,
                                                             op0=ALU.is_equal), [b_RG], [b_RG])
                    V("dve", lambda: nc.vector.tensor_scalar(out=RG[:, :, 8:12], in0=RG[:, :, 8:12], scalar1=-1.0, scalar2=1e30,
                                                             op0=ALU.add, op1=ALU.mult), [b_RG], [b_RG])
                    V("dve", lambda: nc.vector.tensor_tensor(
                        out=LEM[:, :, :].rearrange("p t (g e) -> p t g e", e=8), in0=le.rearrange("p t (g e) -> p t g e", e=8),
                        in1=RG[:, :, 8:12].unsqueeze(3).to_broadcast([128, 16, 4, 8]), op=ALU.add), [b_LGa, b_RG], [b_LEM])
                    V("dve", lambda: nc.vector.tensor_reduce(out=RW[:, 3, 0:16], in_=LEM[:], axis=AX.X, op=ALU.max), [b_LEM], [b_RW])
                    V("dve", lambda: nc.vector.tensor_tensor(out=oh1all[:, T16, :], in0=LEM[:],
                                                             in1=RW[:, 3, 0:16].unsqueeze(2).to_broadcast([128, 16, 32]),
                                                             op=ALU.is_equal), [b_LEM, b_RW], [b_oh1])
                    V("dve", lambda: nc.vector.scalar_tensor_tensor(out=LEM2[:], in0=oh1all[:, T16, :], scalar=-1e30, in1=LEM[:],
                                                                    op0=ALU.mult, op1=ALU.add), [b_oh1, b_LEM], [b_LEM2])
                    V("dve", lambda: nc.vector.tensor_reduce(out=RW[:, 4, 0:16], in_=LEM2[:], axis=AX.X, op=ALU.max), [b_LEM2], [b_RW])
                    V("dve", lambda: nc.vector.tensor_tensor(out=Mall[:, T16, :], in0=LEM[:],
                                                             in1=RW[:, 4, 0:16].unsqueeze(2).to_broadcast([128, 16, 32]),
                                                             op=ALU.is_ge), [b_LEM, b_RW], [b_Mall])
                    V("dve", lambda: nc.vector.tensor_tensor(out=RW[:, 5, 0:16], in0=RW[:, 4, 0:16], in1=RW[:, 3, 0:16], op=ALU.subtract),
                      [b_RW], [b_RW])
                    act(RW[:, 6, 0:16], RW[:, 5, 0:16], AF.Exp, [b_RW], [b_RW])
                    V("dve", lambda: nc.vector.tensor_scalar(out=RW[:, 6, 0:16], in0=RW[:, 6, 0:16], scalar1=1.0, scalar2=None, op0=ALU.add),
                      [b_RW], [b_RW])
                    V("dve", lambda: nc.vector.reciprocal(out=RW[:, 7, 0:16], in_=RW[:, 6, 0:16]), [b_RW], [b_RW])
                    V("dve", lambda: nc.vector.tensor_tensor(out=gates[:, T16, 0], in0=RW[:, 7, 0:16], in1=RW[:, 2, 0:16], op=ALU.mult),
                      [b_RW], [b_gates])
                    V("dve", lambda: nc.vector.tensor_tensor(out=gates[:, T16, 1], in0=RW[:, 2, 0:16], in1=gates[:, T16, 0], op=ALU.subtract),
                      [b_RW, b_gates], [b_gates])
                    S.barrier()
            if STOP_AFTER in ("s1", "s2", "s3", "s4"):
                break

        if STOP_AFTER is None or STOP_AFTER in ("route", "moe"):
            blk_i = sb(es, [128, NBLK], I32, "blki"); b_blki = Buf()
            with ExitStack() as esr:
                psC = ps(esr, [128, 32], F32, "psC"); b_psC = Buf()
                psU = ps(esr, [128, 1024], F32, "psU"); b_psU = Buf()
                psCS = ps(esr, [128, 1024], F32, "psCS"); b_psCS = Buf()
                CSs = sb(esr, [128, 32, 32], F32, "CSs"); b_CSs = Buf()
                INC = sb(esr, [128, 32, 32], F32, "INC"); b_INC = Buf()
                TT = sb(esr, [128, 32, 32], F32, "TT"); b_TT = Buf()
                TM = sb(esr, [128, 32, 32], F32, "TM"); b_TM = Buf()
                cn = sb(esr, [128, 8, 32], F32, "cn"); b_cn = Buf()
                cni = sb(esr, [128, 32], I32, "cni"); b_cni = Buf()
                cmp_ = sb(esr, [128, NBLK, 32], F32, "cmp"); b_cmp = Buf()
                bst = sb(esr, [128, NBLK], F32, "bst"); b_bst = Buf()
                bs0 = sb(esr, [128, NBLK], F32, "bs0"); b_bs0 = Buf()
                blk_f = sb(esr, [128, NBLK], F32, "blkf"); b_blkf = Buf()
                tmp = sb(esr, [128, 2, 32], F32, "tmpr"); b_tmp = [Buf(), Buf()]
                for t in range(32):
                    pe_mm(psC[:], onesb[:], Mall[:, t, :], t == 0, t == 31, [b_onesb, b_Mall], [b_psC])
                V("dve", lambda: nc.vector.tensor_copy(out=cn[:, 0, :], in_=psC[:]), [b_psC], [b_cn])
                V("dve", lambda: nc.vector.tensor_scalar(out=cn[:, 1, :], in0=cn[:, 0, :], scalar1=255.0, scalar2=None, op0=ALU.add),
                  [b_cn], [b_cn])
                V("dve", lambda: nc.vector.tensor_copy(out=cni[:], in_=cn[:, 1, :]), [b_cn], [b_cni])
                V("dve", lambda: nc.vector.tensor_scalar(out=cni[:], in0=cni[:], scalar1=8, scalar2=8,
                                                         op0=ALU.arith_shift_right, op1=ALU.logical_shift_left), [b_cni], [b_cni])
                V("dve", lambda: nc.vector.tensor_copy(out=cn[:, 2, :], in_=cni[:]), [b_cni], [b_cn])
                V("dve", lambda: nc.vector.tensor_tensor_scan(out=cn[:, 3, :], data0=onesf[:, 0:32], data1=cn[:, 2, :],
                                                              initial=0.0, op0=ALU.mult, op1=ALU.add),
                  [b_cn, b_onesf], [b_cn])
                V("dve", lambda: nc.vector.tensor_tensor(out=cn[:, 4, :], in0=cn[:, 3, :], in1=cn[:, 2, :], op=ALU.subtract),
                  [b_cn], [b_cn])
                V("dve", lambda: nc.vector.tensor_scalar(out=bs0[:], in0=onesf[:, 0:NBLK], scalar1=256.0, scalar2=None, op0=ALU.mult),
                  [b_onesf], [b_bs0])
                V("dve", lambda: nc.vector.tensor_tensor_scan(out=bst[:], data0=onesf[:, 0:NBLK], data1=bs0[:], initial=-256.0,
                                                              op0=ALU.mult, op1=ALU.add), [b_bs0, b_onesf], [b_bst])
                V("dve", lambda: nc.vector.tensor_tensor(
                    out=cmp_[:], in0=cn[:, 3, :].unsqueeze(1).to_broadcast([128, NBLK, 32]),
                    in1=bst[:].unsqueeze(2).to_broadcast([128, NBLK, 32]), op=ALU.is_le), [b_cn, b_bst], [b_cmp])
                V("dve", lambda: nc.vector.tensor_reduce(out=blk_f[:], in_=cmp_[:], axis=AX.X, op=ALU.add), [b_cmp], [b_blkf])
                V("dve", lambda: nc.vector.tensor_scalar(out=blk_f[:], in0=blk_f[:], scalar1=128.0, scalar2=None,
                                                         op0=ALU.mult), [b_blkf], [b_blkf])
                V("dve", lambda: nc.vector.tensor_scalar(out=blk_f[:], in0=blk_f[:], scalar1=iota[:, 0:1], scalar2=None,
                                                         op0=ALU.add), [b_blkf, b_iota], [b_blkf])
                V("dve", lambda: nc.vector.tensor_copy(out=blk_i[:], in_=blk_f[:]), [b_blkf], [b_blki])
                dump("blkf", blk_f[:], [b_blkf])
                dump("cn", cn[:, 0:5, :].rearrange("p a b -> p (a b)"), [b_cn])
                Mflat = Mall[:, :, :].rearrange("p t e -> p (t e)")
                for hf in range(2):
                    pe_mm(psU[:, hf * 512:(hf + 1) * 512], utri[:], Mflat[:, hf * 512:(hf + 1) * 512], True, True,
                          [b_utri, b_Mall], [b_psU], sig=(hf == 1))
                    pe_mm(psCS[:, hf * 512:(hf + 1) * 512], onesb[:], Mflat[:, hf * 512:(hf + 1) * 512], True, True,
                          [b_onesb, b_Mall], [b_psCS], sig=(hf == 1))
                V("dve", lambda: nc.vector.tensor_copy(out=CSs[:, :, :].rearrange("p t e -> p (t e)"), in_=psCS[:]), [b_psCS], [b_CSs])
                for e in range(32):
                    V("dve", lambda: nc.vector.tensor_tensor_scan(out=INC[:, :, e], data0=onesf[:, 0:32], data1=CSs[:, :, e],
                                                                  initial=0.0, op0=ALU.mult, op1=ALU.add),
                      [b_CSs, b_onesf], [b_INC])
                V("dve", lambda: nc.vector.tensor_tensor(out=TT[:, :, :].rearrange("p t e -> p (t e)"), in0=psU[:],
                                                         in1=INC[:, :, :].rearrange("p t e -> p (t e)"), op=ALU.add),
                  [b_psU, b_INC], [b_TT])
                V("dve", lambda: nc.vector.tensor_tensor(out=TT[:], in0=TT[:], in1=CSs[:], op=ALU.subtract), [b_TT, b_CSs], [b_TT])
                V("dve", lambda: nc.vector.tensor_tensor(out=TT[:], in0=TT[:], in1=cn[:, 4, :].unsqueeze(1).to_broadcast([128, 32, 32]),
                                                         op=ALU.add), [b_TT, b_cn], [b_TT])
                V("dve", lambda: nc.vector.tensor_tensor(out=TM[:], in0=TT[:], in1=oh1all[:], op=ALU.mult), [b_TT, b_oh1], [b_TM])
                V("dve", lambda: nc.vector.tensor_reduce(out=dest_f[:, :, 0], in_=TM[:], axis=AX.X, op=ALU.add), [b_TM], [b_destf])
                V("dve", lambda: nc.vector.tensor_tensor(out=TM[:], in0=Mall[:], in1=oh1all[:], op=ALU.subtract), [b_Mall, b_oh1], [b_TM])
                V("dve", lambda: nc.vector.tensor_tensor(out=TM[:], in0=TM[:], in1=TT[:], op=ALU.mult), [b_TM, b_TT], [b_TM])
                V("dve", lambda: nc.vector.tensor_reduce(out=dest_f[:, :, 1], in_=TM[:], axis=AX.X, op=ALU.add), [b_TM], [b_destf])
                V("dve", lambda: nc.vector.tensor_copy(out=dest_i[:], in_=dest_f[:, :, :].rearrange("p a b -> p (a b)")), [b_destf], [b_desti])
                dump("destf", dest_f[:, :, :].rearrange("p a b -> p (a b)"), [b_destf])
                dump("gates", gates[:, :, :].rearrange("p a b -> p (a b)"), [b_gates])
                hl = [sb(esr, [128, D], BF16, "hl") for _ in range(4)]; b_hl = [Buf() for _ in range(4)]
                for t in range(32):
                    S.dma("sp", hl[t % 4][:], h2_d[t * 128:(t + 1) * 128, :], reads=[b_h2d], writes=[b_hl[t % 4]])
                    for kk in range(2):
                        S.dma_ind(xs_d[:, :], bass.IndirectOffsetOnAxis(ap=dest_i[:, 2 * t + kk:2 * t + kk + 1], axis=0), hl[t % 4][:, :], None,
                                  NROWS - 1, reads=[b_hl[t % 4], b_desti], writes=[b_xsd], group=True, semof=b_hl[t % 4])
                S.barrier()

            if STOP_AFTER != "route":
                with ExitStack() as esm:
                    w1b = [sb(esm, [128, 8, 512], BF16, "w1b") for _ in range(3)]; b_w1 = [Buf() for _ in range(3)]
                    w3b = [sb(esm, [128, 8, 512], BF16, "w3b") for _ in range(3)]; b_w3 = [Buf() for _ in range(3)]
                    w2b = [sb(esm, [128, 4, 1024], BF16, "w2b") for _ in range(3)]; b_w2 = [Buf() for _ in range(3)]
                    xr = [sb(esm, [128, 2, D], BF16, "xr") for _ in range(3)]; b_xr = [Buf() for _ in range(3)]
                    xT = [sb(esm, [128, 8, 256], BF16, "xT") for _ in range(3)]; b_xT = [[Buf(), Buf()] for _ in range(3)]
                    sl = [sb(esm, [128, 256], F32, "sl") for _ in range(2)]; b_sl = [Buf(), Buf()]
                    hm = [sb(esm, [128, 4, 256], BF16, "hm") for _ in range(2)]; b_hm = [Buf(), Buf()]
                    yo = [sb(esm, [128, 2, D], BF16, "yo") for _ in range(2)]; b_yo = [[Buf(), Buf()], [Buf(), Buf()]]
                    psX = [ps(esm, [128, 1024], BF16, "psX") for _ in range(2)]; b_psX = [Buf(), Buf()]
                    psH = [ps(esm, [128, 512], F32, "psH") for _ in range(2)]; b_psH = [Buf(), Buf()]
                    psY = [ps(esm, [128, 512], F32, "psY") for _ in range(4)]; b_psY = [Buf() for _ in range(4)]
                    xcnt = [0]; hcnt = [0]; ycnt = [0]

                    wreg = nc.gpsimd.to_reg(32 * 128 - 1)

                    def moe_w(blk):
                        i3 = blk % 3
                        off = bass.IndirectOffsetOnAxis(ap=blk_i[:, blk:blk + 1], axis=0)
                        S.dma_ind(w1b[i3][:, :, :].rearrange("p a b -> p (a b)"), None, w1_d[:, :], off, 0,
                                  reads=[b_blki], writes=[b_w1[i3]], bcheck=wreg)
                        S.dma_ind(w3b[i3][:, :, :].rearrange("p a b -> p (a b)"), None, w3_d[:, :], off, 0,
                                  reads=[b_blki], writes=[b_w3[i3]], bcheck=wreg)
                        S.dma_ind(w2b[i3][:, :, :].rearrange("p a b -> p (a b)"), None, w2_d[:, :], off, 0,
                                  reads=[b_blki], writes=[b_w2[i3]], bcheck=wreg)

                    def moe_x(blk):
                        i2 = blk % 3
                        S.dma("sp", xr[i2][:], xs_d[blk * 256:(blk + 1) * 256, :].rearrange("(a p) d -> p a d", p=128),
                              reads=[b_xsd], writes=[b_xr[i2]])
                        for k in range(8):
                            if k % 4 == 0:
                                pX, bX = psX[(xcnt[0] // 4) % 2], b_psX[(xcnt[0] // 4) % 2]
                            for a in range(2):
                                pe_tr(pX[:, (k % 4) * 256 + a * 128:(k % 4) * 256 + (a + 1) * 128],
                                      xr[i2][:, a, k * 128:(k + 1) * 128], identb[:], [b_xr[i2], b_identb], [bX],
                                      sig=(k % 4 == 3 and a == 1))
                            xcnt[0] += 1
                            if k % 4 == 3:
                                dstx = xT[i2][:, k - 3:k + 1, :].rearrange("p a b -> p (a b)")
                                V("dve", lambda: nc.vector.tensor_copy(out=dstx, in_=pX[:]), [bX], [b_xT[i2][(k // 4) % 2]])

                    def moe_c(blk):
                        i2 = blk % 2
                        i3 = blk % 3
                        ix = blk % 3
                        for c4 in range(4):
                            pH, bH = psH[hcnt[0] % 2], b_psH[hcnt[0] % 2]
                            si = hcnt[0] % 2
                            hcnt[0] += 1
                            for k in range(8):
                                pe_mm(pH[:, 0:256], w1b[i3][:, k, c4 * 128:(c4 + 1) * 128], xT[ix][:, k, :], k == 0, k == 7,
                                      [b_w1[i3], *b_xT[ix]], [bH], sig=False)
                            for k in range(8):
                                pe_mm(pH[:, 256:512], w3b[i3][:, k, c4 * 128:(c4 + 1) * 128], xT[ix][:, k, :], k == 0, k == 7,
                                      [b_w3[i3], *b_xT[ix]], [bH])
                            act(sl[si][:], pH[:, 0:256], AF.Silu, [bH], [b_sl[si]])
                            V("dve", lambda: nc.vector.tensor_tensor(out=hm[i2][:, c4, :], in0=pH[:, 256:512], in1=sl[si][:],
                                                                     op=ALU.mult), [bH, b_sl[si]], [b_hm[i2]])
                        for a in range(2):
                            for hf in range(2):
                                pY, bY = psY[ycnt[0] % 4], b_psY[ycnt[0] % 4]
                                ycnt[0] += 1
                                for k in range(4):
                                    pe_mm(pY[:], hm[i2][:, k, a * 128:(a + 1) * 128], w2b[i3][:, k, hf * 512:(hf + 1) * 512],
                                          k == 0, k == 3, [b_hm[i2], b_w2[i3]], [bY])
                                V("dve", lambda: nc.vector.tensor_copy(out=yo[i2][:, a, hf * 512:(hf + 1) * 512], in_=pY[:]), [bY],
                                  [b_yo[i2][hf]])
                        S.dma("act", ys_d[blk * 256:(blk + 1) * 256, :].rearrange("(a p) d -> p a d", p=128), yo[i2][:],
                              reads=b_yo[i2], writes=[b_ysd], group=True, semof=b_yo[i2][0])

                    moe_w(0)
                    moe_w(1)
                    moe_x(0)
                    moe_x(1)
                    for blk in range(NBLK):
                        if blk + 2 < NBLK:
                            moe_w(blk + 2)
                            moe_x(blk + 2)
                        moe_c(blk)
                    S.barrier()

                with ExitStack() as esf:
                    GA2 = [sb(esf, [128, D], F32, "GA2") for _ in range(2)]; b_GA2 = [Buf(), Buf()]
                    gfb = sb(esf, [128, D], F32, "gfb"); b_gfb = Buf()
                    diag = [sb(esf, [128, 128], F32, "diagf") for _ in range(2)]; b_diag = [Buf(), Buf()]
                    psB = ps(esf, [128, D], F32, "psBf"); b_psB = Buf()
                    S.dma("sp", gfb[:], gfb_d, writes=[b_gfb])
                    for b in range(NB):
                        bcast_row(esf, 5, b, psB, b_psB, diag, b_diag)
                        V("dve", lambda: nc.vector.tensor_copy(out=GA2[b][:], in_=psB[:]), [b_psB], [b_GA2[b]])
                    y0 = [sb(esf, [128, D], BF16, "y0") for _ in range(3)]; b_y0 = [Buf() for _ in range(3)]
                    y1 = [sb(esf, [128, D], BF16, "y1") for _ in range(3)]; b_y1 = [Buf() for _ in range(3)]
                    x1l = [sb(esf, [128, D], F32, "x1l") for _ in range(3)]; b_x1l = [Buf() for _ in range(3)]
                    mo = [sb(esf, [128, D], F32, "mo") for _ in range(2)]; b_mo = [Buf(), Buf()]
                    ot = [sb(esf, [128, D], F32, "ot") for _ in range(2)]; b_ot = [Buf(), Buf()]
                    junkf = sb(esf, [128, D], BF16, "junkf"); b_junkf = Buf()
                    stf = sb(esf, [128, 32, 3], F32, "stf"); b_stf = [Buf() for _ in range(32)]

                    def fin_load(t):
                        i3 = t % 3
                        S.dma_ind(y0[i3][:, :], None, ys_d[:, :], bass.IndirectOffsetOnAxis(ap=dest_i[:, 2 * t:2 * t + 1], axis=0),
                                  0, reads=[b_ysd, b_desti], writes=[b_y0[i3]])
                        S.dma_ind(y1[i3][:, :], None, ys_d[:, :], bass.IndirectOffsetOnAxis(ap=dest_i[:, 2 * t + 1:2 * t + 2], axis=0),
                                  0, reads=[b_ysd, b_desti], writes=[b_y1[i3]])
                        S.dma("sp", x1l[i3][:], x1_d[t * 128:(t + 1) * 128, :], reads=[b_x1d], writes=[b_x1l[i3]])

                    fin_load(0)
                    fin_load(1)
                    for t in range(32):
                        i2 = t % 2
                        i3 = t % 3
                        bb = t // 16
                        act(mo[i2][:], y0[i3][:], AF.Copy, [b_y0[i3], b_gates], [b_mo[i2]], scale=gates[:, t, 0:1])
                        V("dve", lambda: nc.vector.scalar_tensor_tensor(out=mo[i2][:], in0=y1[i3][:], scalar=gates[:, t, 1:2],
                                                                        in1=mo[i2][:], op0=ALU.mult, op1=ALU.add),
                          [b_y1[i3], b_gates, b_mo[i2]], [b_mo[i2]])
                        if t == 0:
                            dump("moe0", mo[i2][:], [b_mo[i2]])
                        V("pool", lambda: nc.gpsimd.tensor_tensor(out=mo[i2][:], in0=mo[i2][:], in1=GA2[bb][:], op=ALU.mult),
                          [b_mo[i2], b_GA2[bb]], [b_mo[i2]])
                        V("dve", lambda: nc.vector.tensor_tensor(out=mo[i2][:], in0=mo[i2][:], in1=x1l[i3][:], op=ALU.add),
                          [b_mo[i2], b_x1l[i3]], [b_mo[i2]])
                        act(junkf[:], mo[i2][:], AF.Square, [b_mo[i2]], [b_junkf, b_stf[t]], accum_out=stf[:, t, 0:1])
                        act(stf[:, t, 1:2], stf[:, t, 0:1], AF.Sqrt, [b_stf[t], b_eps], [b_stf[t]], scale=1.0 / D, bias=epst[:, 0:1])
                        V("dve", lambda: nc.vector.reciprocal(out=stf[:, t, 2:3], in_=stf[:, t, 1:2]), [b_stf[t]], [b_stf[t]])
                        V("dve", lambda: nc.vector.scalar_tensor_tensor(out=ot[i2][:], in0=mo[i2][:], scalar=stf[:, t, 2:3],
                                                                        in1=gfb[:], op0=ALU.mult, op1=ALU.mult),
                          [b_mo[i2], b_stf[t], b_gfb], [b_ot[i2]])
                        if t + 2 < 32:
                            fin_load(t + 2)
                        S.dma("sp", out_d[bb, (t % 16) * 128:(t % 16 + 1) * 128, :], ot[i2][:], reads=[b_ot[i2]], writes=[b_outd],
                              group=True, semof=b_ot[i2])
        S.barrier()
        build_program.stats = dict(nops=dict(S.nops), nwaits=S.nwaits, nsem=S.nsem)
    return nc


def _fm(v):
    return np.ascontiguousarray(np.asarray(v, np.float32).reshape(-1, 128).T)


def _kp(w):
    K, N = w.shape
    return np.ascontiguousarray(w.reshape(K // 128, 128, N).transpose(1, 0, 2))


def _swap_cols():
    idx = np.arange(64)
    half = idx // 32
    within = idx % 32
    sw = np.where(within < 16, within + 16, within - 16)
    return half * 32 + sw


def _host_consts():
    nf = 16
    inv_freq = (10000.0 ** (-np.arange(nf, dtype=np.float32) / nf)).astype(np.float32)
    t = np.arange(L)
    row = (t // 64).astype(np.float32)
    col = (t % 64).astype(np.float32)
    cos = np.zeros((128, L), np.float32)
    sin = np.zeros((128, L), np.float32)
    for p in range(128):
        d = p % 64
        pos = row if d < 32 else col
        ang = (pos * inv_freq[(d % 32) % 16]).astype(np.float32)
        sign = -1.0 if (d % 32) < 16 else 1.0
        cos[p] = np.cos(ang)
        sin[p] = sign * np.sin(ang)
    cossin = np.concatenate([cos, sin], axis=1)
    ident = np.eye(128, dtype=np.float32)
    iota = np.arange(128, dtype=np.float32).reshape(128, 1)
    utri = np.triu(np.ones((128, 128), np.float32), k=1)
    return cossin, ident, iota, utri


def _bias_table(rpb):
    cq = np.arange(64)
    c_start = np.clip(cq - 8, 0, 48)
    band = (cq[None, :] >= c_start[:, None]) & (cq[None, :] < c_start[:, None] + 16)
    dc = np.clip(cq[None, :] - cq[:, None], -15, 15) + 15
    tab = np.full((64, 8, NTB, 64), -1e30, np.float32)
    for h in range(8):
        for dr in range(15):
            vals = rpb[h, dr][dc]
            tab[:, h, 1 + dr, :] = np.where(band, vals, np.float32(-1e30))
        tab[:, h, 18, :] = tab[:, h, 1 + 3, :]
        tab[:, h, 19, :] = tab[:, h, 1 + 10, :]
    return tab.reshape(64, 8 * NTB * 64)


def _prepare(inputs):
    f = lambda k: np.asarray(inputs[k], np.float32)
    w_in = f("w_in")[0]
    K_OFF, V_OFF, LX_OFF, Q_OFF, LG_OFF, GA_OFF, GB_OFF = 0, 512, 1024, 2048, 2560, 3584, 4608
    sw = _swap_cols()
    wqkv = []
    for hp in range(4):
        cols = []
        for base in (Q_OFF, K_OFF):
            plain = np.concatenate([base + (2 * hp + e) * 64 + np.arange(64) for e in range(2)])
            swp = np.concatenate([base + (2 * hp + e) * 64 + sw for e in range(2)])
            cols += [plain, swp]
        cols.append(V_OFF + hp * 128 + np.arange(128))
        wqkv.append(_kp(w_in[:, np.concatenate(cols)]).reshape(128, 8 * 640))
    wqkv = np.stack(wqkv)
    wlxlg = np.stack([_kp(w_in[:, np.concatenate([LX_OFF + n * 128 + np.arange(128), LG_OFF + n * 128 + np.arange(128)])]
                          ).reshape(128, 8 * 256) for n in range(8)])
    wgagb = np.stack([_kp(w_in[:, np.concatenate([GA_OFF + n * 128 + np.arange(128), GB_OFF + n * 128 + np.arange(128)])]
                          ).reshape(128, 8 * 256) for n in range(8)])
    wua = _kp(f("w_up_attn")[0])
    wul = _kp(f("w_up_lru")[0])
    wup = np.stack([np.concatenate([wua[:, :, n * 128:(n + 1) * 128], wul[:, :, n * 128:(n + 1) * 128]], axis=1
                                   ).reshape(128, 12 * 128) for n in range(8)])
    wout = _kp(f("w_out")[0]).reshape(128, 8 * D)
    wa = f("lru_wa")[0]
    wx = f("lru_wx")[0]
    lruw = np.stack([wa[0], wa[1], wx[0], wx[1]])
    lruw = np.ascontiguousarray(lruw.transpose(2, 0, 1, 3)).reshape(128, 4 * 8 * 128)
    vecs = np.concatenate([
        _fm(f("g_mix")[0]), _fm(f("g_ffn")[0]),
        np.concatenate([_fm(f("conv_w")[0][j]) for j in range(4)], axis=1),
        _fm(f("conv_b")[0]),
        np.concatenate([_fm(f("lru_ba")[0][d_]) for d_ in range(2)], axis=1),
        np.concatenate([_fm(f("lru_bx")[0][d_]) for d_ in range(2)], axis=1),
        np.concatenate([_fm(f("lru_lambda")[0][d_]) for d_ in range(2)], axis=1),
        _fm(f("b_mod")[0]),
    ], axis=1)
    assert vecs.shape == (128, NV)
    wmod = _kp(f("w_mod")[0])
    wr = _kp(np.concatenate([f("router_group_w")[0], f("router_expert_w")[0]], axis=1)).reshape(128, 8 * 36)
    brb = np.ascontiguousarray(np.broadcast_to(
        np.concatenate([f("router_group_b")[0], f("router_expert_b")[0]])[None, :], (128, 36)))
    gfb = np.ascontiguousarray(np.broadcast_to(f("g_final")[None, :], (128, D)))
    gffnb = np.ascontiguousarray(np.broadcast_to(f("g_ffn")[0][None, :], (128, D)))
    w1 = f("expert_w_gate")[0]
    w3 = f("expert_w_up")[0]
    w2 = f("expert_w_down")[0]
    w1h = np.ascontiguousarray(w1.reshape(32, 8, 128, 512).transpose(0, 2, 1, 3)).reshape(32 * 128, 8 * 512)
    w3h = np.ascontiguousarray(w3.reshape(32, 8, 128, 512).transpose(0, 2, 1, 3)).reshape(32 * 128, 8 * 512)
    w2h = np.ascontiguousarray(w2.reshape(32, 4, 128, 1024).transpose(0, 2, 1, 3)).reshape(32 * 128, 4 * 1024)
    cossin, ident, iota, utri = _host_consts()
    btab = _bias_table(f("rpb")[0])
    shared = dict(wmod=wmod, vecs=vecs, wqkv=wqkv, wlxlg=wlxlg, wgagb=wgagb, wup=wup, wout=wout, lruw=lruw,
                  cossin=cossin, btab=btab, wr=wr, brb=brb, gfb=gfb, gffnb=gffnb, w1h=w1h, w3h=w3h, w2h=w2h,
                  ident=ident, iota=iota, utri=utri)
    x = f("x")
    ctx = f("ctx")
    c = f("c")
    c_ctx = f("c_ctx")
    in_maps = []
    for core in range(NCORES):
        b0 = core * NB
        cs = np.stack([c[b0], c[b0 + 1], c_ctx], axis=-1)
        cs = np.ascontiguousarray(cs.reshape(8, 128, 3).transpose(1, 0, 2)).reshape(128, 24)
        m = dict(shared)
        m["xin"] = np.ascontiguousarray(x[b0:b0 + NB])
        m["ctxin"] = np.ascontiguousarray(ctx[b0:b0 + NB])
        m["cs"] = cs
        in_maps.append(m)
    return in_maps


def kernel(**inputs):
    in_maps = _prepare(inputs)
    nc = build_program()
    res = run_bass_kernel_spmd(nc, in_maps, core_ids=list(range(NCORES)))
    out = np.concatenate([np.asarray(r["out"], np.float32) for r in res.results], axis=0)
    return out
```
